# Optimizing a Trainium2 kernel written in Bass

```python
import math
import jax, jax.numpy as jnp
from jax import lax
import numpy as np

D_MODEL = 1024
BATCH = 2
SEQ = 16384
DEPTH = 1

N_HEADS = 8
HEAD_DIM = D_MODEL // N_HEADS
ATTN_WIDTH = N_HEADS * HEAD_DIM
CONV_WIDTH = D_MODEL
CONV_KERNEL = 31
MOBA_BLOCK = 256
MOBA_TOPK = 3
Q_CHUNK = 64
D_FF = -(-8 * D_MODEL // (3 * 256)) * 256
EPS = 1e-6
IN_SPLITS = (CONV_WIDTH, CONV_WIDTH, ATTN_WIDTH, ATTN_WIDTH, ATTN_WIDTH, D_MODEL, D_MODEL)
IN_WIDTH = sum(IN_SPLITS)

kernel_name = 'hybrid_conformer_conv_moba_swiglu'


def rmsnorm(x, g):
    xf = x.astype(jnp.float32)
    y = xf * lax.rsqrt(jnp.mean(xf * xf, axis=-1, keepdims=True) + EPS)
    return (y * g.astype(jnp.float32)).astype(x.dtype)


def layernorm(x, g, b):
    xf = x.astype(jnp.float32)
    mu = jnp.mean(xf, axis=-1, keepdims=True)
    xc = xf - mu
    y = xc * lax.rsqrt(jnp.mean(xc * xc, axis=-1, keepdims=True) + EPS)
    return (y * g.astype(jnp.float32) + b.astype(jnp.float32)).astype(x.dtype)


def alibi_slopes(n_heads):
    return jnp.exp2(-8.0 * jnp.arange(1, n_heads + 1, dtype=jnp.float32) / n_heads)


def conformer_conv(u, gate, dw_w, dw_b, ln_g, ln_b, w_pw2):
    a = u * jax.nn.sigmoid(gate)
    c = a.shape[-1]
    y = lax.conv_general_dilated(
        a, dw_w.astype(a.dtype)[:, None, :], window_strides=(1,),
        padding=[(CONV_KERNEL - 1, 0)], dimension_numbers=('NWC', 'WIO', 'NWC'),
        feature_group_count=c) + dw_b
    y = jax.nn.silu(layernorm(y, ln_g, ln_b))
    return y @ w_pw2


def moba_attention(q, k, v, qn_g, kn_g):
    B, S, H, Dh = q.shape
    L = MOBA_BLOCK
    nb = S // L
    nc = S // Q_CHUNK
    ksel = min(MOBA_TOPK, nb)
    q = rmsnorm(q, qn_g).transpose(0, 2, 1, 3)
    k = rmsnorm(k, kn_g).transpose(0, 2, 1, 3)
    v = v.transpose(0, 2, 1, 3)
    kb = k.reshape(B, H, nb, L, Dh)
    vb = v.reshape(B, H, nb, L, Dh)
    kmean = jnp.mean(kb.astype(jnp.float32), axis=3)
    slopes = alibi_slopes(H)
    sl4 = slopes.reshape(1, H, 1, 1)
    sl5 = slopes.reshape(1, H, 1, 1, 1)
    scale = Dh ** -0.5
    bi = jnp.arange(B)[:, None, None, None]
    hi = jnp.arange(H)[None, :, None, None]
    blk_ids = jnp.arange(nb)
    offs = jnp.arange(L)
    q_chunks = q.reshape(B, H, nc, Q_CHUNK, Dh).transpose(2, 0, 1, 3, 4)

    def chunk(args):
        ci, qc = args
        t = ci * Q_CHUNK + jnp.arange(Q_CHUNK)
        cur = (ci * Q_CHUNK) // L
        gate = jnp.einsum('bhqd,bhnd->bhqn', qc.astype(jnp.float32), kmean)
        gate = jnp.where(blk_ids < cur, gate, -jnp.inf)
        _, idx = lax.top_k(gate, ksel)
        sel_ok = idx < cur
        k_sel = kb[bi, hi, idx]
        v_sel = vb[bi, hi, idx]
        s_past = jnp.einsum('bhqd,bhqkld->bhqkl', qc, k_sel).astype(jnp.float32) * scale
        pos_past = idx[..., None] * L + offs
        dist_past = (t[:, None, None] - pos_past).astype(jnp.float32)
        s_past = jnp.where(sel_ok[..., None], s_past - sl5 * dist_past, -jnp.inf)
        s_past = s_past.reshape(B, H, Q_CHUNK, ksel * L)
        k_own = lax.dynamic_index_in_dim(kb, cur, axis=2, keepdims=False)
        v_own = lax.dynamic_index_in_dim(vb, cur, axis=2, keepdims=False)
        s_own = jnp.einsum('bhqd,bhld->bhql', qc, k_own).astype(jnp.float32) * scale
        dist_own = t[:, None] - (cur * L + offs)[None, :]
        s_own = jnp.where(dist_own >= 0, s_own - sl4 * dist_own.astype(jnp.float32), -jnp.inf)
        p = jax.nn.softmax(jnp.concatenate([s_past, s_own], axis=-1), axis=-1)
        p_past = p[..., :ksel * L].reshape(B, H, Q_CHUNK, ksel, L).astype(v.dtype)
        p_own = p[..., ksel * L:].astype(v.dtype)
        return (jnp.einsum('bhqkl,bhqkld->bhqd', p_past, v_sel)
                + jnp.einsum('bhql,bhld->bhqd', p_own, v_own))

    o = lax.map(chunk, (jnp.arange(nc), q_chunks))
    return o.transpose(1, 0, 3, 2, 4).reshape(B, S, H * Dh)


def swiglu(x, w_gate, w_up, w_down):
    return (jax.nn.silu(x @ w_gate) * (x @ w_up)) @ w_down


def setup_inputs(seed: int = 0) -> dict:
    key = jax.random.key(seed)
    ks = jax.random.split(key, 20)
    f32 = jnp.float32

    def nrm(k, shape, fan_in):
        return jax.random.normal(k, shape, f32) * (fan_in ** -0.5)

    def gain(k, shape):
        return 1.0 + 0.01 * jax.random.normal(k, shape, f32)

    def bias(k, shape):
        return 0.01 * jax.random.normal(k, shape, f32)

    return {
        'x': jax.random.normal(ks[0], (BATCH, SEQ, D_MODEL), f32),
        'norm1_g': gain(ks[1], (DEPTH, D_MODEL)),
        'w_in': nrm(ks[2], (DEPTH, D_MODEL, IN_WIDTH), D_MODEL),
        'dw_w': nrm(ks[3], (DEPTH, CONV_KERNEL, CONV_WIDTH), CONV_KERNEL),
        'dw_b': bias(ks[4], (DEPTH, CONV_WIDTH)),
        'conv_ln_g': gain(ks[5], (DEPTH, CONV_WIDTH)),
        'conv_ln_b': bias(ks[6], (DEPTH, CONV_WIDTH)),
        'w_conv_out': nrm(ks[7], (DEPTH, CONV_WIDTH, D_MODEL), CONV_WIDTH),
        'q_norm_g': gain(ks[8], (DEPTH, HEAD_DIM)),
        'k_norm_g': gain(ks[9], (DEPTH, HEAD_DIM)),
        'w_attn_out': nrm(ks[10], (DEPTH, ATTN_WIDTH, D_MODEL), ATTN_WIDTH),
        'w_out': nrm(ks[11], (DEPTH, D_MODEL, D_MODEL), D_MODEL),
        'norm2_g': gain(ks[12], (DEPTH, D_MODEL)),
        'w_ffn_gate': nrm(ks[13], (DEPTH, D_MODEL, D_FF), D_MODEL),
        'w_ffn_up': nrm(ks[14], (DEPTH, D_MODEL, D_FF), D_MODEL),
        'w_ffn_down': nrm(ks[15], (DEPTH, D_FF, D_MODEL), D_FF),
    }


def reference(x, norm1_g, w_in, dw_w, dw_b, conv_ln_g, conv_ln_b, w_conv_out,
              q_norm_g, k_norm_g, w_attn_out, w_out, norm2_g,
              w_ffn_gate, w_ffn_up, w_ffn_down):
    B, S, _ = x.shape
    cuts = [int(c) for c in np.cumsum(IN_SPLITS)[:-1]]
    h = x
    for l in range(DEPTH):
        n = rmsnorm(h, norm1_g[l])
        z = n @ w_in[l]
        cu, cg, q, k, v, gc, ga = jnp.split(z, cuts, axis=-1)
        y_conv = conformer_conv(cu, cg, dw_w[l], dw_b[l], conv_ln_g[l], conv_ln_b[l], w_conv_out[l])
        att = moba_attention(q.reshape(B, S, N_HEADS, HEAD_DIM),
                             k.reshape(B, S, N_HEADS, HEAD_DIM),
                             v.reshape(B, S, N_HEADS, HEAD_DIM),
                             q_norm_g[l], k_norm_g[l])
        y_attn = att @ w_attn_out[l]
        merged = jax.nn.sigmoid(gc) * y_conv + jax.nn.sigmoid(ga) * y_attn
        h = h + merged @ w_out[l]
        h = h + swiglu(rmsnorm(h, norm2_g[l]), w_ffn_gate[l], w_ffn_up[l], w_ffn_down[l])
    return h
```

```python
import numpy as np
from contextlib import ExitStack
import concourse.bass as bass
import concourse.mybir as mybir
from concourse.bass_utils import run_bass_kernel_spmd

F32 = mybir.dt.float32
BF16 = mybir.dt.bfloat16
AF = mybir.ActivationFunctionType
ALU = mybir.AluOpType
AX = mybir.AxisListType

D = 1024
KC = 8
NH = 8
DH = 128
FF = 2816
FC = 22
S = 16384
L = 256
NBK = 64
EPS = 1e-6
NEG = -30000.0


class T:
    __slots__ = ("name", "w", "r")

    def __init__(self, name):
        self.name = name
        self.w = None
        self.r = []


class Q:
    def __init__(self, name, sem, scale=1):
        self.name = name
        self.sem = sem
        self.scale = scale
        self.count = 0
        self.ops = []
        self.seen = {}


class Prog:
    def __init__(self, nc, es):
        self.nc = nc
        self.es = es
        self.pe = Q("pe", self.newsem("s_pe"))
        self.act = Q("act", self.newsem("s_act"))
        self.dve = Q("dve", self.newsem("s_dve"))
        self.pool = Q("pool", self.newsem("s_pool"))
        self.sp = Q("sp", self.newsem("s_sp"))
        self.engines = [self.pe, self.act, self.dve, self.pool, self.sp]

    def newsem(self, name):
        return self.es.enter_context(self.nc.semaphore(name))

    def dmaq(self, name):
        return Q(name, self.newsem("d_" + name), 16)

    def _wait(self, q, tok):
        sq, cnt = tok
        if q.seen.get(sq, 0) >= cnt:
            return
        q.seen[sq] = cnt
        q.ops.append(lambda e, s=sq.sem, v=cnt * sq.scale: e.wait_ge(s, v))

    def emit(self, q, fn, reads=(), writes=(), ms=True, sig=None):
        sig = sig or q
        for t in reads:
            if t.w is not None:
                self._wait(q, t.w)
        for t in writes:
            if t.w is not None and t.w[0] is not q:
                self._wait(q, t.w)
            for tok in t.r:
                if tok[0] is not q:
                    self._wait(q, tok)
        if ms:
            sig.count += 1
            tok = (sig, sig.count)
            q.ops.append(lambda e, s=sig.sem, v=sig.scale: fn(e).then_inc(s, v))
        else:
            tok = (sig, sig.count + 1)
            q.ops.append(lambda e: fn(e))
        for t in writes:
            t.w = tok
            t.r = []
        for t in reads:
            t.r.append(tok)
            if len(t.r) > 16:
                best = {}
                for sq, c in t.r:
                    if sq not in best or best[sq] < c:
                        best[sq] = c
                t.r = list(best.items())
        return tok

    def dma(self, q, out, in_, reads, writes, sig, **kw):
        return self.emit(q, lambda e: e.dma_start(out=out, in_=in_, **kw), reads, writes, sig=sig)

    def wait_all(self, q, ts):
        for t in ts:
            if t.w is not None:
                self._wait(q, t.w)
            for tok in t.r:
                self._wait(q, tok)

    def barrier(self, ts):
        for q in self.engines:
            self.wait_all(q, ts)

    def run(self):
        nc = self.nc
        with nc.Block() as block:
            @block.tensor
            def _(e):
                for op in self.pe.ops:
                    op(e)

            @block.scalar
            def _(e):
                for op in self.act.ops:
                    op(e)

            @block.vector
            def _(e):
                for op in self.dve.ops:
                    op(e)

            @block.gpsimd
            def _(e):
                for op in self.pool.ops:
                    op(e)

            @block.sync
            def _(e):
                for op in self.sp.ops:
                    op(e)


class Arena:
    def __init__(self, ap, nwords):
        self.ap = ap
        self.n = nwords
        self.off = 0

    def alloc(self, shape, dtype):
        shape = list(shape)
        np_ = shape[0]
        free = 1
        for s_ in shape[1:]:
            free *= s_
        esz = 4 if dtype == F32 else 2
        words = (free * esz + 3) // 4
        words = (words + 7) // 8 * 8
        assert self.off + words <= self.n, ("arena overflow", self.off, words, self.n)
        v = self.ap[0:np_, self.off:self.off + words]
        self.off += words
        if dtype != F32:
            v = v.bitcast(dtype)
        v = v[:, 0:free]
        if len(shape) == 2:
            return v
        names = " ".join("d%d" % i for i in range(len(shape) - 1))
        kw = {"d%d" % i: shape[i + 1] for i in range(len(shape) - 1)}
        return v.rearrange("p (%s) -> p %s" % (names, names), **kw)


def unit_table():
    u = {}
    for i in range(4):
        u["cv%d" % i] = [("w_in", 0, 8, (2 * i) * 128, 128, 0, "g1"), ("w_in", 0, 8, (2 * i + 1) * 128, 128, 128, "g1"),
                         ("w_in", 0, 8, 1024 + (2 * i) * 128, 128, 256, "g1"),
                         ("w_in", 0, 8, 1024 + (2 * i + 1) * 128, 128, 384, "g1")]
    for nm, c0 in (("q", 2048), ("k", 3072), ("v", 4096), ("gc", 5120), ("ga", 6144)):
        for i in range(2):
            u["%s%d" % (nm, i)] = [("w_in", 0, 8, c0 + i * 512, 512, 0, "g1")]
    for nm, src in (("co", "w_conv_out"), ("ao", "w_attn_out"), ("wo", "w_out")):
        for i in range(2):
            u["%s%d" % (nm, i)] = [(src, 0, 8, i * 512, 512, 0, None)]
    for i in range(11):
        u["gu%d" % i] = [("w_ffn_gate", 0, 8, (2 * i) * 128, 128, 0, "g2"),
                         ("w_ffn_gate", 0, 8, (2 * i + 1) * 128, 128, 128, "g2"),
                         ("w_ffn_up", 0, 8, (2 * i) * 128, 128, 256, "g2"),
                         ("w_ffn_up", 0, 8, (2 * i + 1) * 128, 128, 384, "g2")]
    for hf in range(2):
        for gi, (f0, nf) in enumerate(((0, 8), (8, 8), (16, 6))):
            u["dn%d_%d" % (hf, gi)] = [("w_ffn_down", f0 * 128, nf, hf * 512, 512, 0, None)]
    return u


UNITS = unit_table()
UNAMES = list(UNITS.keys())
UIDX = {n: i for i, n in enumerate(UNAMES)}
NU = len(UNAMES)
CHUNK_SEQ = (["cv%d" % i for i in range(4)] + ["q0", "q1", "gc0", "gc1", "ga0", "ga1", "co0", "co1", "ao0", "ao1",
                                               "wo0", "wo1"] + ["gu%d" % i for i in range(11)] +
             ["dn0_0", "dn0_1", "dn0_2", "dn1_0", "dn1_1", "dn1_2"])


def build_nc(nch=8, nkv=32):
    nc = bass.Bass("TRN2", target_bir_lowering=False)

    def din(name, shape):
        return nc.dram_tensor(name, list(shape), F32, kind="ExternalInput").ap()

    x_all = din("x_all", [S, D])
    x_own = din("x_own", [4096, D])
    x_halo = din("x_halo", [512, D])
    gate_bias = din("gate_bias", [16, 64])
    a2_d = din("a2", [16, 8, 64])
    b2_d = din("b2", [16, 64])
    cms_d = din("cmsel", [128, 4 * 2 * 256])
    lo_d = din("lo", [1, 8 * 512])
    kb_d = din("kbias", [128, 16])
    id_d = din("ident", [128, 128])
    wsrc = {
        "w_in": din("w_in", [D, 7168]), "w_conv_out": din("w_conv_out", [D, D]), "w_attn_out": din("w_attn_out", [D, D]),
        "w_out": din("w_out", [D, D]), "w_ffn_gate": din("w_ffn_gate", [D, FF]), "w_ffn_up": din("w_ffn_up", [D, FF]),
        "w_ffn_down": din("w_ffn_down", [FF, D]),
    }
    norm1_g = din("norm1_g", [1, D])
    norm2_g = din("norm2_g", [1, D])
    dw_w = din("dw_w", [31, D])
    dw_b = din("dw_b", [1, D])
    ln_g = din("conv_ln_g", [1, D])
    ln_b = din("conv_ln_b", [1, D])
    qg_d = din("q_norm_g", [1, DH])
    kg_d = din("k_norm_g", [1, DH])
    out_d = nc.dram_tensor("out_own", [4096, D], F32, kind="ExternalOutput").ap()
    wsc = nc.dram_tensor("wsc", [NU, 128, 4096], BF16).ap()
    kt_d = nc.dram_tensor("kt_s", [NH, 128, S], BF16).ap()
    vv_d = nc.dram_tensor("vv_s", [128, NBK, NH, 2, 129], BF16).ap()

    with ExitStack() as es:
        P = Prog(nc, es)
        NW = 53000
        arena_t = es.enter_context(nc.sbuf_tensor("arena", [128, NW], F32))
        AR = Arena(arena_t[:, :], NW)
        banks = [es.enter_context(nc.psum_tensor("pb%d" % i, [128, 512], F32)) for i in range(8)]
        tb = [T("pb%d" % i) for i in range(8)]
        rot = [0]

        def nbank():
            i = rot[0]
            rot[0] = (rot[0] + 1) % 4
            return i

        def bf_view(i):
            return banks[i][:, :].bitcast(BF16)

        identf = AR.alloc([128, 128], F32)
        identb = AR.alloc([128, 128], BF16)
        onesb = AR.alloc([128, 128], BF16)
        esel = AR.alloc([65, 64], BF16)
        cms = AR.alloc([128, 4, 2, 256], BF16)
        kb = AR.alloc([128, 16], F32)
        kmean = AR.alloc([128, 8, 64], F32)
        VT = AR.alloc([65, 8, 512], BF16)
        g1 = AR.alloc([128, 8], F32)
        g2 = AR.alloc([128, 8], F32)
        cw = AR.alloc([128, 8, 31], F32)
        cb = AR.alloc([128, 8], F32)
        lg = AR.alloc([128, 8], F32)
        lb = AR.alloc([128, 8], F32)
        qg = AR.alloc([128, 1], F32)
        kg = AR.alloc([128, 1], F32)
        epsc = AR.alloc([128, 1], F32)
        t_const = T("const")
        t_kmean = T("kmean")
        t_vt64 = T("vt64")
        dq_c = P.dmaq("const")
        const_mark = AR.off

        cms_f = AR.alloc([128, 2048], F32)
        lo_f = AR.alloc([65, 4096], F32)
        t_cst = T("cst")
        pq = P.pool
        P.dma(pq, identf, id_d, [], [t_cst], dq_c)
        P.dma(pq, cms_f, cms_d, [], [t_cst], dq_c)
        P.dma(pq, lo_f[64:65, :], lo_d, [], [t_cst], dq_c)
        P.dma(pq, kb, kb_d, [], [t_cst], dq_c)
        P.dma(pq, g1, norm1_g[0].rearrange("(kc p) -> p kc", p=128), [], [t_cst], dq_c, allow_slow_non_contiguous=True)
        P.dma(pq, g2, norm2_g[0].rearrange("(kc p) -> p kc", p=128), [], [t_cst], dq_c, allow_slow_non_contiguous=True)
        P.dma(pq, cb, dw_b[0].rearrange("(kc p) -> p kc", p=128), [], [t_cst], dq_c, allow_slow_non_contiguous=True)
        P.dma(pq, lg, ln_g[0].rearrange("(kc p) -> p kc", p=128), [], [t_cst], dq_c, allow_slow_non_contiguous=True)
        P.dma(pq, lb, ln_b[0].rearrange("(kc p) -> p kc", p=128), [], [t_cst], dq_c, allow_slow_non_contiguous=True)
        for kc_ in range(8):
            P.dma(pq, cw[:, kc_, :], dw_w[:, kc_ * 128:(kc_ + 1) * 128].rearrange("j p -> p j"), [], [t_cst], dq_c,
                  allow_slow_non_contiguous=True)
        P.dma(pq, qg, qg_d.rearrange("o p -> p o"), [], [t_cst], dq_c, allow_slow_non_contiguous=True)
        P.dma(pq, kg, kg_d.rearrange("o p -> p o"), [], [t_cst], dq_c, allow_slow_non_contiguous=True)
        dv = P.dve
        P.emit(dv, lambda e: e.tensor_copy(out=identb, in_=identf), [t_cst], [t_const])
        P.emit(dv, lambda e: e.memset(onesb, 1.0), [], [t_const])
        P.emit(dv, lambda e: e.memset(kmean.rearrange("p a b -> p (a b)"), 0.0), [], [t_const])
        P.emit(dv, lambda e: e.memset(epsc, EPS), [], [t_const])
        P.emit(dv, lambda e: e.tensor_copy(out=esel[0:64, :], in_=identf[0:64, 0:64]), [t_cst], [t_const])
        P.emit(dv, lambda e: e.memset(esel[64:65, :], 1.0), [], [t_const])
        P.emit(dv, lambda e: e.tensor_copy(out=cms.rearrange("p a b c -> p (a b c)"), in_=cms_f), [t_cst], [t_const])
        P.emit(dv, lambda e: e.tensor_copy(out=VT[64:65, :, :].rearrange("p a b -> p (a b)"), in_=lo_f[64:65, :]),
               [t_cst], [t_const])
        P.emit(dv, lambda e: e.tensor_scalar(out=qg, in0=qg, scalar1=float(DH) ** -0.5, scalar2=None, op0=ALU.mult),
               [t_cst], [t_const])
        P.barrier([t_cst, t_const])
        AR.off = const_mark
        phase_mark = AR.off

        def esel_lhsT(n):
            a = esel[0:65, n:n + 1]
            return bass.AP(a.tensor, a.offset, [[a.ap[0][0], 65], [0, 128]])

        stg = [AR.alloc([128, 8, 512], F32) for _ in range(2)]
        wbf = [AR.alloc([128, 8, 512], BF16) for _ in range(2)]
        t_stg = [T("stg0"), T("stg1")]
        t_wbf = [T("wbf0"), T("wbf1")]
        dq_stg = [P.dmaq("stg0"), P.dmaq("stg1")]
        dq_wbf = [P.dmaq("wbf0"), P.dmaq("wbf1")]
        t_unit = [T("unit%d" % i) for i in range(NU)]
        gains = {"g1": g1, "g2": g2}
        kv_units = ["k0", "k1", "v0", "v1"]
        conv_order = kv_units + [n_ for n_ in UNAMES if n_ not in kv_units]

        def conv_load(oi):
            nm = conv_order[oi]
            b = oi % 2
            for (src, row0, nk_, c0, ncol, dst, _g) in UNITS[nm]:
                sap = wsrc[src][row0:row0 + nk_ * 128, c0:c0 + ncol].rearrange("(kc p) n -> p kc n", p=128)
                P.dma(P.sp, stg[b][:, 0:nk_, dst:dst + ncol], sap, [], [t_stg[b]], dq_stg[b])

        def conv_cast_store(oi, act_only):
            nm = conv_order[oi]
            ui = UIDX[nm]
            b = oi % 2
            segs = UNITS[nm]
            nk = segs[0][2]
            gname = segs[0][6]
            for kc in range(nk):
                use_dve = (kc % 2 == 0) and not act_only
                if gname is None:
                    if use_dve:
                        P.emit(P.dve, lambda e, b=b, kc=kc: e.tensor_copy(out=wbf[b][:, kc, :], in_=stg[b][:, kc, :]),
                               [t_stg[b]], [t_wbf[b]])
                    else:
                        P.emit(P.act, lambda e, b=b, kc=kc: e.copy(out=wbf[b][:, kc, :], in_=stg[b][:, kc, :]),
                               [t_stg[b]], [t_wbf[b]])
                else:
                    gt = gains[gname]
                    if use_dve:
                        P.emit(P.dve, lambda e, b=b, kc=kc, gt=gt: e.tensor_scalar(
                            out=wbf[b][:, kc, :], in0=stg[b][:, kc, :], scalar1=gt[:, kc:kc + 1], scalar2=None,
                            op0=ALU.mult), [t_stg[b]], [t_wbf[b]])
                    else:
                        P.emit(P.act, lambda e, b=b, kc=kc, gt=gt: e.activation(
                            out=wbf[b][:, kc, :], in_=stg[b][:, kc, :], func=AF.Copy, scale=gt[:, kc:kc + 1]),
                            [t_stg[b]], [t_wbf[b]])
            P.dma(P.sp, wsc[ui, :, 0:nk * 512], wbf[b][:, 0:nk, :].rearrange("p a b -> p (a b)"), [t_wbf[b]],
                  [t_unit[ui]], dq_wbf[b])

        conv_load(0)
        conv_load(1)
        for oi in range(4):
            conv_cast_store(oi, act_only=False)
            conv_load(oi + 2)
        conv_next = [4]

        wk = AR.alloc([128, 8, 1024], BF16)
        wv = AR.alloc([128, 8, 1024], BF16)
        t_wk = T("wk")
        t_wv = T("wv")
        dq_wk = P.dmaq("wk")
        dq_wv = P.dmaq("wv")
        for i in range(2):
            P.dma(P.pool, wk[:, :, i * 512:(i + 1) * 512], wsc[UIDX["k%d" % i]].rearrange("p (a b) -> p a b", b=512),
                  [t_unit[UIDX["k%d" % i]]], [t_wk], dq_wk)
            P.dma(P.pool, wv[:, :, i * 512:(i + 1) * 512], wsc[UIDX["v%d" % i]].rearrange("p (a b) -> p a b", b=512),
                  [t_unit[UIDX["v%d" % i]]], [t_wv], dq_wv)
        xin1 = [AR.alloc([128, 4, 1024], F32) for _ in range(2)]
        t_xin1 = [T("xin1_0"), T("xin1_1")]
        dq_xin1 = [P.dmaq("xin1_0"), P.dmaq("xin1_1")]
        junk_cur = [AR.alloc([128, 1024], BF16)]
        t_junk = T("junk")
        ssq = [AR.alloc([128, 4], F32) for _ in range(2)]
        t_ssq = [T("ssq0"), T("ssq1")]
        rinv = [AR.alloc([128, 4], F32) for _ in range(2)]
        t_rinv = [T("rinv0"), T("rinv1")]
        xn = [AR.alloc([128, 1024], BF16) for _ in range(2)]
        t_xn = [T("xn0"), T("xn1")]
        xnT1 = [AR.alloc([128, 8, 512], BF16) for _ in range(2)]
        t_xnT1 = [T("xnT1_0"), T("xnT1_1")]
        sqb = [AR.alloc([128, 512], BF16) for _ in range(2)]
        t_sqb = [T("sqb0"), T("sqb1")]
        rkf = [AR.alloc([128, 512], F32) for _ in range(2)]
        t_rkf = [T("rkf0"), T("rkf1")]
        kst = [AR.alloc([128, 8, 512], BF16) for _ in range(2)]
        t_kst = [T("kst0"), T("kst1")]
        dq_kst = [P.dmaq("kst0"), P.dmaq("kst1")]
        vst = [AR.alloc([128, 2, 8, 2, 129], BF16) for _ in range(2)]
        t_vst = [T("vst0"), T("vst1")]
        dq_vst = [P.dmaq("vst0"), P.dmaq("vst1")]
        t_kt = T("kt_dram")
        t_vv = T("vv_dram")
        for b in range(2):
            P.emit(P.dve, lambda e, b=b: e.memset(vst[b].rearrange("p a b c d -> p (a b c d)"), 1.0), [], [t_vst[b]])

        def norm_tile(src, np_, t_src, ssq_ap, rinv_ap, t_s, t_r, xn_ap, t_x):
            jv = junk_cur[0][0:np_, :]
            P.emit(P.dve, lambda e: e.memset(ssq_ap, 0.0), [], [t_s])
            P.emit(P.act, lambda e: e.activation(out=jv, in_=src, func=AF.Square, accum_out=ssq_ap),
                   [t_src], [t_junk, t_s])
            P.emit(P.act, lambda e: e.activation(out=rinv_ap, in_=ssq_ap, func=AF.Ln, scale=1.0 / D,
                                                 bias=epsc[0:np_, :]), [t_s], [t_r])
            P.emit(P.act, lambda e: e.activation(out=rinv_ap, in_=rinv_ap, func=AF.Exp, scale=-0.5), [t_r], [t_r])
            P.emit(P.dve, lambda e: e.tensor_scalar(out=xn_ap, in0=src, scalar1=rinv_ap, scalar2=None, op0=ALU.mult),
                   [t_src, t_r], [t_x])

        def transpose_tile(xn_ap, np_, t_x, dst, t_dst, use_act):
            bi = nbank()
            pv = bf_view(bi)[:, 0:8 * np_].rearrange("p (a b) -> p a b", b=np_)
            for kc in range(8):
                P.emit(P.pe, lambda e, kc=kc: e.transpose(out=pv[:, kc, :], in_=xn_ap[:, kc * 128:(kc + 1) * 128],
                                                          identity=identb[0:np_, 0:np_]),
                       [t_x], [tb[bi]], ms=(kc == 7))
            if use_act:
                P.emit(P.act, lambda e: e.copy(out=dst, in_=pv), [tb[bi]], [t_dst])
            else:
                P.emit(P.dve, lambda e: e.tensor_copy(out=dst, in_=pv), [tb[bi]], [t_dst])

        def x1_load(c):
            b = c % 2
            P.dma(P.pool, xin1[b], x_all[c * 512:(c + 1) * 512, :].rearrange("(t p) d -> p t d", p=128), [],
                  [t_xin1[b]], dq_xin1[b])

        def p1_norm_transpose(c):
            b = c % 2
            for t in range(4):
                xb = t % 2
                norm_tile(xin1[b][:, t, :], 128, t_xin1[b], ssq[b][:, t:t + 1], rinv[b][:, t:t + 1], t_ssq[b], t_rinv[b],
                          xn[xb], t_xn[xb])
                transpose_tile(xn[xb], 128, t_xn[xb], xnT1[b][:, :, t * 128:(t + 1) * 128], t_xnT1[b],
                               use_act=(t % 2 == 0))

        x1_load(0)
        if nkv > 1:
            x1_load(1)
        p1_norm_transpose(0)
        for c in range(nkv):
            b = c % 2
            if conv_next[0] < len(conv_order):
                oi = conv_next[0]
                conv_cast_store(oi, act_only=True)
                if oi + 2 < len(conv_order):
                    conv_load(oi + 2)
                conv_next[0] += 1
            pend = None

            def k_tail(h, bi, pk, b=b, c=c):
                sb = h % 2
                b2i = nbank()
                p2 = banks[b2i][:, :]
                P.emit(P.pe, lambda e, p2=p2, sb=sb: e.matmul(out=p2, lhsT=onesb, rhs=sqb[sb], start=True, stop=True),
                       [t_sqb[sb]], [tb[b2i]])
                P.emit(P.act, lambda e, p2=p2, sb=sb: e.activation(out=rkf[sb], in_=p2, func=AF.Ln, scale=1.0 / DH,
                                                                   bias=epsc), [tb[b2i]], [t_rkf[sb]])
                P.emit(P.act, lambda e, sb=sb: e.activation(out=rkf[sb], in_=rkf[sb], func=AF.Exp, scale=-0.5),
                       [t_rkf[sb]], [t_rkf[sb]])
                P.emit(P.dve, lambda e, pk=pk, sb=sb, h=h, b=b: e.scalar_tensor_tensor(
                    out=kst[b][:, h, :], in0=pk, scalar=kg[:, 0:1], in1=rkf[sb], op0=ALU.mult, op1=ALU.mult),
                    [tb[bi], t_rkf[sb]], [t_kst[b]])
                P.emit(P.dve, lambda e, h=h, b=b, c=c: e.tensor_reduce(
                    out=kmean[:, h, 2 * c:2 * c + 2], in_=kst[b][:, h, :].rearrange("p (a l) -> p a l", l=256),
                    axis=AX.X, op=ALU.add), [t_kst[b]], [t_kmean])

            for h in range(NH):
                bi = nbank()
                pk = banks[bi][:, :]
                for kc in range(8):
                    P.emit(P.pe, lambda e, kc=kc, h=h, pk=pk, b=b: e.matmul(out=pk, lhsT=wk[:, kc, h * 128:(h + 1) * 128],
                                                                           rhs=xnT1[b][:, kc, :], start=(kc == 0),
                                                                           stop=(kc == 7)),
                           [t_wk, t_xnT1[b]], [tb[bi]], ms=(kc == 7))
                sb = h % 2
                P.emit(P.act, lambda e, pk=pk, sb=sb: e.activation(out=sqb[sb], in_=pk, func=AF.Square),
                       [tb[bi]], [t_sqb[sb]])
                if pend is not None:
                    k_tail(*pend)
                pend = (h, bi, pk)
            k_tail(*pend)
            P.dma(P.pool, kt_d[:, :, c * 512:(c + 1) * 512].rearrange("h p t -> p h t"), kst[b], [t_kst[b]], [t_kt],
                  dq_kst[b])
            if c + 2 < nkv:
                x1_load(c + 2)
            for blk in range(2):
                for kh in range(2):
                    for hf in range(2):
                        bi = nbank()
                        pv_ = banks[bi][:, :]
                        for kc in range(8):
                            P.emit(P.pe, lambda e, kc=kc, blk=blk, kh=kh, hf=hf, pv_=pv_, b=b: e.matmul(
                                out=pv_, lhsT=xnT1[b][:, kc, blk * 256 + kh:blk * 256 + 256:2],
                                rhs=wv[:, kc, hf * 512:(hf + 1) * 512], start=(kc == 0), stop=(kc == 7)),
                                [t_wv, t_xnT1[b]], [tb[bi]], ms=(kc == 7))
                        dst = vst[b][:, blk, hf * 4:(hf + 1) * 4, kh, 0:128]
                        src = pv_.rearrange("p (a d) -> p a d", d=128)
                        if hf == 0:
                            P.emit(P.act, lambda e, dst=dst, src=src: e.copy(out=dst, in_=src), [tb[bi]], [t_vst[b]])
                        else:
                            P.emit(P.dve, lambda e, dst=dst, src=src: e.tensor_copy(out=dst, in_=src), [tb[bi]],
                                   [t_vst[b]])
                if blk == 0 and c + 1 < nkv:
                    p1_norm_transpose(c + 1)
            P.dma(P.pool, vv_d[:, 2 * c:2 * c + 2].rearrange("p a b c d -> p (a b c d)"),
                  vst[b].rearrange("p a b c d -> p (a b c d)"), [t_vst[b]], [t_vv], dq_vst[b])
        while conv_next[0] < len(conv_order):
            oi = conv_next[0]
            conv_cast_store(oi, act_only=False)
            if oi + 2 < len(conv_order):
                conv_load(oi + 2)
            conv_next[0] += 1
        ph1 = t_xin1 + t_ssq + t_rinv + t_xn + t_sqb + t_rkf + t_kst + t_vst + t_xnT1 + [t_junk, t_wk, t_wv, t_kt, t_vv,
                                                                                         t_kmean] + tb + t_stg + t_wbf
        P.barrier(ph1)
        AR.off = phase_mark

        NSLOT = 3
        wr = [AR.alloc([128, 8, 512], BF16) for _ in range(NSLOT)]
        t_wr = [T("wr%d" % i) for i in range(NSLOT)]
        dq_wr = [P.dmaq("wr%d" % i) for i in range(NSLOT)]
        seq_all = []
        for g in range(nch):
            seq_all += CHUNK_SEQ
        wstate = {"issued": 0, "used": 0}

        def w_issue():
            i = wstate["issued"]
            if i >= len(seq_all):
                return
            nm = seq_all[i]
            s_ = i % NSLOT
            ui = UIDX[nm]
            nk = UNITS[nm][0][2]
            P.dma(P.sp, wr[s_][:, 0:nk, :].rearrange("p a b -> p (a b)"), wsc[ui, :, 0:nk * 512], [t_unit[ui]],
                  [t_wr[s_]], dq_wr[s_])
            wstate["issued"] += 1

        def w_next(expect):
            i = wstate["used"]
            assert seq_all[i] == expect, (seq_all[i], expect)
            while wstate["issued"] < min(i + NSLOT, len(seq_all)):
                w_issue()
            wstate["used"] += 1
            s_ = i % NSLOT
            return wr[s_], t_wr[s_]

        xin = AR.alloc([128, 4, 1024], F32)
        t_xin = T("xin")
        dq_xin = P.dmaq("xin")
        xh = AR.alloc([32, 2, 1024], F32)
        t_xh = T("xh")
        dq_xh = P.dmaq("xh")
        a2t = AR.alloc([128, 2, 8, 64], F32)
        b2t = AR.alloc([128, 2, 64], F32)
        gbt = AR.alloc([128, 2, 64], F32)
        t_tab = T("tab")
        dq_tab = P.dmaq("tab")
        junk_cur[0] = AR.alloc([128, 1024], BF16)
        ssq2 = AR.alloc([128, 8], F32)
        rinv2 = AR.alloc([128, 8], F32)
        t_ssq2 = T("ssq2")
        t_rinv2 = T("rinv2")
        xn = [AR.alloc([128, 1024], BF16) for _ in range(2)]
        xnT_raw = AR.alloc([128, 8 * 576], BF16)
        xnT = xnT_raw.rearrange("p (a b) -> p a b", b=576)
        t_xnT = T("xnT")
        R1 = AR.alloc([128, 8, 576 + 512], F32)
        a_t = R1[:, :, 0:576]
        y_t = R1[:, :, 576:1088]
        act_t = R1.rearrange("p a b -> p (a b)").bitcast(BF16)[:, 0:FC * 512].rearrange("p (a b) -> p a b", b=512)
        t_a = T("a")
        t_y = T("y")
        t_actt = T("act")
        sig_t = [AR.alloc([128, 1, 288], F32) for _ in range(2)]
        t_sig = [T("sig0"), T("sig1")]
        ybf = [AR.alloc([128, 512], BF16) for _ in range(2)]
        t_ybf = [T("ybf0"), T("ybf1")]
        ysq = [AR.alloc([128, 512], BF16) for _ in range(2)]
        t_ysq = [T("ysq0"), T("ysq1")]
        mean_t = AR.alloc([128, 512], F32)
        rstd_t = AR.alloc([128, 512], F32)
        t_mean = T("mean")
        t_rstd = T("rstd")
        xc = [AR.alloc([128, 512], F32) for _ in range(2)]
        t_xc = [T("xc0"), T("xc1")]
        ysl = AR.alloc([128, 8, 512], BF16)
        t_ysl = T("ysl")
        mT = ysl
        t_mT = t_ysl
        t1 = AR.alloc([128, 8, 512], BF16)
        t_t1 = T("t1")
        qT = AR.alloc([128, 8, 512], BF16)
        t_qT = T("qT")
        qf = [AR.alloc([128, 512], F32) for _ in range(2)]
        t_qf = [T("qf0"), T("qf1")]
        gcs = AR.alloc([128, 8, 512], BF16)
        gas = AR.alloc([128, 8, 512], BF16)
        t_gcs = T("gcs")
        t_gas = T("gas")
        attT = AR.alloc([128, 8, 512], BF16)
        t_attT = T("attT")
        hnT = xnT_raw[:, 0:8 * 512].rearrange("p (a b) -> p a b", b=512)
        t_hnT = t_xnT
        NKV = 2
        ktp = [AR.alloc([128, 2048], BF16) for _ in range(NKV)]
        vp = [AR.alloc([128, 8, 2, 129], BF16) for _ in range(NKV)]
        t_ktp = [T("ktp%d" % i) for i in range(NKV)]
        t_vp = [T("vp%d" % i) for i in range(NKV)]
        dq_ktp = [P.dmaq("ktp%d" % i) for i in range(NKV)]
        dq_vp = [P.dmaq("vp%d" % i) for i in range(NKV)]
        NPT = 4
        PT = [AR.alloc([128, 512], BF16) for _ in range(NPT)]
        t_PT = [T("PT%d" % i) for i in range(NPT)]
        gs_t = [AR.alloc([128, 64], F32) for _ in range(2)]
        m8_t = [AR.alloc([128, 8], F32) for _ in range(2)]
        tmp_t = [AR.alloc([128, 64], F32) for _ in range(2)]
        hiv_t = [AR.alloc([128, 64], BF16) for _ in range(2)]
        t_gs = [T("gs0"), T("gs1")]
        t_m8 = [T("m80"), T("m81")]
        t_tmp = [T("tmp0"), T("tmp1")]
        t_hiv = [T("hiv0"), T("hiv1")]
        rl_t = [AR.alloc([128, 1], F32) for _ in range(2)]
        t_rl = [T("rl0"), T("rl1")]
        atok = [AR.alloc([128, 128], BF16) for _ in range(2)]
        t_atok = [T("atok0"), T("atok1")]
        sg_t = xc
        t_sg = t_xc
        tm2 = xc
        t_tm2 = t_xc
        dq_out = P.dmaq("out")
        t_outd = T("out_dram")
        kvstate = {"n": 0}
        ptstate = {"n": 0}

        def mm_group(out_ap, bank_i, pairs, extra_reads, first=True, last=True):
            n_ = len(pairs)
            for i_, (l_, r_) in enumerate(pairs):
                P.emit(P.pe, lambda e, l_=l_, r_=r_, i_=i_: e.matmul(out=out_ap, lhsT=l_, rhs=r_,
                                                                     start=(first and i_ == 0),
                                                                     stop=(last and i_ == n_ - 1)),
                       extra_reads, [tb[bank_i]], ms=(i_ == n_ - 1))

        for g in range(nch):
            P.dma(P.pool, xin, x_own[g * 512:(g + 1) * 512, :].rearrange("(t p) d -> p t d", p=128), [], [t_xin], dq_xin)
            P.dma(P.pool, xh, x_halo[g * 64:(g + 1) * 64, :].rearrange("(o p) d -> p o d", p=32), [], [t_xh], dq_xh)
            P.dma(P.pool, a2t, a2_d[2 * g:2 * g + 2].partition_broadcast(128), [], [t_tab], dq_tab)
            P.dma(P.pool, b2t, b2_d[2 * g:2 * g + 2].partition_broadcast(128), [], [t_tab], dq_tab)
            P.dma(P.pool, gbt, gate_bias[2 * g:2 * g + 2].partition_broadcast(128), [], [t_tab], dq_tab)
            for o in range(2):
                xb = o % 2
                norm_tile(xh[0:32, o, :], 32, t_xh, ssq2[0:32, 4 + o:5 + o], rinv2[0:32, 4 + o:5 + o], t_ssq2, t_rinv2,
                          xn[xb][0:32, :], t_xn[xb])
                transpose_tile(xn[xb][0:32, :], 32, t_xn[xb], xnT[:, :, o * 288:o * 288 + 32], t_xnT, use_act=(o == 0))
            for t in range(4):
                xb = t % 2
                c0 = (t // 2) * 288 + 32 + (t % 2) * 128
                norm_tile(xin[:, t, :], 128, t_xin, ssq2[:, t:t + 1], rinv2[:, t:t + 1], t_ssq2, t_rinv2, xn[xb], t_xn[xb])
                transpose_tile(xn[xb], 128, t_xn[xb], xnT[:, :, c0:c0 + 128], t_xnT, use_act=(t % 2 == 0))
            for u in range(4):
                w_, tw = w_next("cv%d" % u)
                for ci in range(2):
                    cc = 2 * u + ci
                    bu = nbank()
                    bg = nbank()
                    bu2 = nbank()
                    bg2 = nbank()
                    for ob, (bcu, bcg) in enumerate(((bu, bg), (bu2, bg2))):
                        mm_group(banks[bcu][:, 0:288],
                                 bcu, [(w_[:, kc, ci * 128:(ci + 1) * 128], xnT[:, kc, ob * 288:(ob + 1) * 288])
                                       for kc in range(8)], [tw, t_xnT])
                        mm_group(banks[bcg][:, 0:288],
                                 bcg, [(w_[:, kc, 256 + ci * 128:256 + (ci + 1) * 128], xnT[:, kc, ob * 288:(ob + 1) * 288])
                                       for kc in range(8)], [tw, t_xnT])
                        sb = ob
                        P.emit(P.act, lambda e, bcg=bcg, sb=sb, ob=ob: e.activation(out=sig_t[sb][:, 0, :],
                                                                                   in_=banks[bcg][:, 0:288],
                                                                                   func=AF.Sigmoid),
                               [tb[bcg]], [t_sig[sb]])
                        P.emit(P.dve, lambda e, bcu=bcu, sb=sb, ob=ob, cc=cc: e.tensor_tensor(
                            out=a_t[:, cc, ob * 288:(ob + 1) * 288], in0=banks[bcu][:, 0:288], in1=sig_t[sb][:, 0, :],
                            op=ALU.mult), [tb[bcu], t_sig[sb]], [t_a])
            for u in range(2):
                w_, tw = w_next("q%d" % u)
                for hi_ in range(4):
                    h = 4 * u + hi_
                    bi = nbank()
                    pq_ = banks[bi][:, :]
                    for ob in range(2):
                        mm_group(pq_[:, ob * 256:(ob + 1) * 256], bi,
                                 [(w_[:, kc, hi_ * 128:(hi_ + 1) * 128], xnT[:, kc, ob * 288 + 32:ob * 288 + 288])
                                  for kc in range(8)], [tw, t_xnT])
                    sb = h % 2
                    P.emit(P.act, lambda e, pq_=pq_, sb=sb: e.activation(out=ysq[sb], in_=pq_, func=AF.Square),
                           [tb[bi]], [t_ysq[sb]])
                    b2i = nbank()
                    p2 = banks[b2i][:, :]
                    mm_group(p2, b2i, [(onesb, ysq[sb])], [t_ysq[sb]])
                    P.emit(P.act, lambda e, p2=p2, sb=sb: e.activation(out=xc[sb], in_=p2, func=AF.Ln, scale=1.0 / DH,
                                                                       bias=epsc), [tb[b2i]], [t_xc[sb]])
                    P.emit(P.act, lambda e, sb=sb: e.activation(out=xc[sb], in_=xc[sb], func=AF.Exp, scale=-0.5),
                           [t_xc[sb]], [t_xc[sb]])
                    P.emit(P.dve, lambda e, pq_=pq_, sb=sb: e.scalar_tensor_tensor(
                        out=qf[sb], in0=pq_, scalar=qg[:, 0:1], in1=xc[sb], op0=ALU.mult, op1=ALU.mult),
                        [tb[bi], t_xc[sb]], [t_qf[sb]])
                    P.emit(P.act, lambda e, sb=sb, h=h: e.copy(out=qT[:, h, :], in_=qf[sb]), [t_qf[sb]], [t_qT])
                    for t in range(4):
                        ob = t // 2
                        mb = t % 2
                        bgi = nbank()
                        pg = banks[bgi][:, 0:64]
                        mm_group(pg, bgi, [(qf[sb][:, t * 128:(t + 1) * 128], kmean[:, h, :])], [t_qf[sb], t_kmean])
                        P.emit(P.dve, lambda e, pg=pg, mb=mb, ob=ob: e.tensor_tensor(out=gs_t[mb], in0=pg,
                                                                                    in1=gbt[:, ob, :], op=ALU.add),
                               [tb[bgi], t_tab], [t_gs[mb]])
                        P.emit(P.dve, lambda e, mb=mb: e.max(out=m8_t[mb], in_=gs_t[mb]), [t_gs[mb]], [t_m8[mb]])
                        P.emit(P.dve, lambda e, mb=mb, ob=ob, h=h: e.scalar_tensor_tensor(
                            out=tmp_t[mb], in0=gs_t[mb], scalar=m8_t[mb][:, 2:3], in1=a2t[:, ob, h, :], op0=ALU.is_ge,
                            op1=ALU.mult), [t_gs[mb], t_m8[mb], t_tab], [t_tmp[mb]])
                        P.emit(P.dve, lambda e, mb=mb, ob=ob: e.tensor_tensor(out=hiv_t[mb], in0=tmp_t[mb],
                                                                             in1=b2t[:, ob, :], op=ALU.add),
                               [t_tmp[mb], t_tab], [t_hiv[mb]])
                        bti = nbank()
                        ptv = bf_view(bti)[0:64, 0:128]
                        P.emit(P.pe, lambda e, ptv=ptv, mb=mb: e.transpose(out=ptv, in_=hiv_t[mb], identity=identb),
                               [t_hiv[mb]], [tb[bti]])
                        P.emit(P.act, lambda e, ptv=ptv, h=h, t=t: e.copy(out=VT[0:64, h, t * 128:(t + 1) * 128], in_=ptv),
                               [tb[bti]], [t_vt64])
            for nm, dst, tdst in (("gc", gcs, t_gcs), ("ga", gas, t_gas)):
                for u in range(2):
                    w_, tw = w_next("%s%d" % (nm, u))
                    for ci in range(4):
                        cc = 4 * u + ci
                        bi = nbank()
                        for ob in range(2):
                            mm_group(banks[bi][:, ob * 256:(ob + 1) * 256], bi,
                                     [(w_[:, kc, ci * 128:(ci + 1) * 128], xnT[:, kc, ob * 288 + 32:ob * 288 + 288])
                                      for kc in range(8)], [tw, t_xnT])
                        P.emit(P.act, lambda e, bi=bi, dst=dst, cc=cc: e.activation(out=dst[:, cc, :], in_=banks[bi][:, :],
                                                                                   func=AF.Sigmoid), [tb[bi]], [tdst])
            def conv_cc(cc):
                yv = y_t[:, cc, :].rearrange("p (o l) -> p o l", l=256)

                def av(j, cc=cc):
                    return a_t[:, cc, :].rearrange("p (o l) -> p o l", l=288)[:, :, 2 + j:2 + j + 256]
                P.emit(P.dve, lambda e, yv=yv, av=av, cc=cc: e.tensor_scalar(out=yv, in0=av(0), scalar1=cw[:, cc, 0:1],
                                                                            scalar2=cb[:, cc:cc + 1], op0=ALU.mult,
                                                                            op1=ALU.add), [t_a], [t_y])
                for j in range(1, 31):
                    P.emit(P.dve, lambda e, yv=yv, av=av, cc=cc, j=j: e.scalar_tensor_tensor(
                        out=yv, in0=av(j), scalar=cw[:, cc, j:j + 1], in1=yv, op0=ALU.mult, op1=ALU.add), [t_a, t_y], [t_y])

            def normalize_head(h):
                for t in range(4):
                    ob_i = 4 + t
                    o_ap = banks[ob_i][:, 0:129]
                    mb = t % 2
                    P.emit(P.dve, lambda e, o_ap=o_ap, mb=mb: e.reciprocal(out=rl_t[mb], in_=o_ap[:, 128:129]),
                           [tb[ob_i]], [t_rl[mb]])
                    P.emit(P.dve, lambda e, o_ap=o_ap, mb=mb: e.tensor_scalar(out=atok[mb], in0=o_ap[:, 0:128],
                                                                              scalar1=rl_t[mb][:, 0:1], scalar2=None,
                                                                              op0=ALU.mult),
                           [tb[ob_i], t_rl[mb]], [t_atok[mb]])
                    bti = (srot[0] - 3) % 4 if t % 2 == 0 else srot[0] % 4
                    ptv = bf_view(bti)[:, 0:128]
                    P.emit(P.pe, lambda e, ptv=ptv, mb=mb: e.transpose(out=ptv, in_=atok[mb], identity=identb),
                           [t_atok[mb]], [tb[bti]])
                    P.emit(P.act, lambda e, ptv=ptv, h=h, t=t: e.copy(out=attT[:, h, t * 128:(t + 1) * 128], in_=ptv),
                           [tb[bti]], [t_attT])

            nblk = 8 * g + 8
            npp = nblk // 8
            units = [(h, n, kh) for h in range(NH) for n in range(nblk) for kh in range(2)]
            piece_list = [(h, n0) for h in range(NH) for n0 in range(0, nblk, 8)]
            piece_slot = {}
            piece_issued = [0]

            def ensure_piece(k):
                while piece_issued[0] <= k and piece_issued[0] < len(piece_list):
                    h_, n0_ = piece_list[piece_issued[0]]
                    s_ = kvstate["n"] % NKV
                    kvstate["n"] += 1
                    P.dma(P.pool, ktp[s_], kt_d[h_, :, n0_ * 256:(n0_ + 8) * 256], [t_kt], [t_ktp[s_]], dq_ktp[s_])
                    P.dma(P.pool, vp[s_], vv_d[:, n0_:n0_ + 8, h_, :, :], [t_vv], [t_vp[s_]], dq_vp[s_])
                    piece_slot[piece_issued[0]] = s_
                    piece_issued[0] += 1

            LA = 2
            srot = [0]
            ensure_piece(1)
            st = {}
            for idx in range(len(units) + LA):
                if idx < len(units):
                    h, n, kh = units[idx]
                    s_ = piece_slot[h * npp + n // 8]
                    nl = n % 8
                    bi = srot[0]
                    srot[0] = (srot[0] + 1) % 4
                    S_ = banks[bi][:, :]
                    cand = n >= 8 * g
                    P.emit(P.pe, lambda e, S_=S_, s_=s_, nl=nl, kh=kh, h=h: e.matmul(
                        out=S_, lhsT=ktp[s_][:, nl * 256 + kh:nl * 256 + 256:2], rhs=qT[:, h, :], start=True,
                        stop=False), [t_ktp[s_], t_qT], [tb[bi]], ms=False)
                    P.emit(P.pe, lambda e, S_=S_, n=n, h=h, cand=cand: e.matmul(
                        out=S_, lhsT=esel_lhsT(n), rhs=VT[0:65, h, :], start=False, stop=(not cand)),
                        [t_vt64], [tb[bi]], ms=(not cand))
                    if cand:
                        ob = (n - 8 * g) // 4
                        cnd = (n - 8 * g) % 4
                        P.emit(P.pe, lambda e, S_=S_, ob=ob, cnd=cnd, kh=kh: e.matmul(
                            out=S_[:, ob * 256:(ob + 1) * 256], lhsT=identb, rhs=cms[:, cnd, kh, :], start=False,
                            stop=True), [], [tb[bi]], ms=True)
                    st[idx] = (bi, s_)
                j = idx - LA
                if j >= 0:
                    h, n, kh = units[j]
                    bi, s_ = st.pop(j)
                    S_ = banks[bi][:, :]
                    nl = n % 8
                    if n % 8 == 0 and kh == 0:
                        ensure_piece(h * npp + n // 8 + 1)
                    first = (n == 0 and kh == 0)
                    lastblk = (n == nblk - 1 and kh == 1)
                    r_ = ptstate["n"] % NPT
                    ptstate["n"] += 1
                    P.emit(P.act, lambda e, S_=S_, r_=r_, h=h, kh=kh: e.activation(
                        out=PT[r_], in_=S_, func=AF.Exp, bias=kb[:, 2 * h + kh:2 * h + kh + 1]),
                        [tb[bi]], [t_PT[r_]])
                    for t in range(4):
                        ob_i = 4 + t
                        o_ap = banks[ob_i][:, 0:129]
                        P.emit(P.pe, lambda e, o_ap=o_ap, r_=r_, t=t, s_=s_, nl=nl, kh=kh, first=first,
                               lastblk=lastblk: e.matmul(out=o_ap, lhsT=PT[r_][:, t * 128:(t + 1) * 128],
                                                         rhs=vp[s_][:, nl, kh, :], start=first, stop=lastblk),
                               [t_PT[r_], t_vp[s_]], [tb[ob_i]], ms=(t == 3))
                    if lastblk:
                        normalize_head(h)
                        conv_cc(h)
            bs1 = nbank()
            bs2 = nbank()
            for cc in range(8):
                sb = cc % 2
                P.emit(P.act, lambda e, sb=sb, cc=cc: e.copy(out=ybf[sb], in_=y_t[:, cc, :]), [t_y], [t_ybf[sb]])
                P.emit(P.act, lambda e, sb=sb, cc=cc: e.activation(out=ysq[sb], in_=y_t[:, cc, :], func=AF.Square),
                       [t_y], [t_ysq[sb]])
                P.emit(P.pe, lambda e, sb=sb, cc=cc, bs1=bs1: e.matmul(out=banks[bs1][:, :], lhsT=onesb, rhs=ybf[sb],
                                                              start=(cc == 0), stop=(cc == 7)),
                       [t_ybf[sb]], [tb[bs1]], ms=True)
                P.emit(P.pe, lambda e, sb=sb, cc=cc, bs2=bs2: e.matmul(out=banks[bs2][:, :], lhsT=onesb, rhs=ysq[sb],
                                                              start=(cc == 0), stop=(cc == 7)),
                       [t_ysq[sb]], [tb[bs2]], ms=True)
            P.emit(P.dve, lambda e, bs1=bs1: e.tensor_scalar(out=mean_t, in0=banks[bs1][:, :], scalar1=1.0 / D, scalar2=None,
                                                    op0=ALU.mult), [tb[bs1]], [t_mean])
            P.emit(P.dve, lambda e: e.tensor_tensor(out=rstd_t, in0=mean_t, in1=mean_t, op=ALU.mult), [t_mean], [t_rstd])
            P.emit(P.dve, lambda e, bs2=bs2: e.scalar_tensor_tensor(out=rstd_t, in0=banks[bs2][:, :], scalar=1.0 / D, in1=rstd_t,
                                                           op0=ALU.mult, op1=ALU.subtract), [tb[bs2], t_rstd], [t_rstd])
            P.emit(P.act, lambda e: e.activation(out=rstd_t, in_=rstd_t, func=AF.Ln, bias=epsc), [t_rstd], [t_rstd])
            P.emit(P.act, lambda e: e.activation(out=rstd_t, in_=rstd_t, func=AF.Exp, scale=-0.5), [t_rstd], [t_rstd])
            for cc in range(8):
                sb = cc % 2
                P.emit(P.dve, lambda e, sb=sb, cc=cc: e.tensor_tensor(out=xc[sb], in0=y_t[:, cc, :], in1=mean_t,
                                                                      op=ALU.subtract), [t_y, t_mean], [t_xc[sb]])
                P.emit(P.dve, lambda e, sb=sb: e.tensor_tensor(out=xc[sb], in0=xc[sb], in1=rstd_t, op=ALU.mult),
                       [t_xc[sb], t_rstd], [t_xc[sb]])
                P.emit(P.act, lambda e, sb=sb, cc=cc: e.activation(out=ysl[:, cc, :], in_=xc[sb], func=AF.Silu,
                                                                   scale=lg[:, cc:cc + 1], bias=lb[:, cc:cc + 1]),
                       [t_xc[sb]], [t_ysl])
            for u in range(2):
                w_, tw = w_next("co%d" % u)
                for ci in range(4):
                    cc = 4 * u + ci
                    bi = nbank()
                    mm_group(banks[bi][:, :], bi, [(w_[:, kc, ci * 128:(ci + 1) * 128], ysl[:, kc, :]) for kc in range(8)],
                             [tw, t_ysl])
                    P.emit(P.dve, lambda e, bi=bi, cc=cc: e.tensor_tensor(out=t1[:, cc, :], in0=banks[bi][:, :],
                                                                          in1=gcs[:, cc, :], op=ALU.mult),
                           [tb[bi], t_gcs], [t_t1])
            for u in range(2):
                w_, tw = w_next("ao%d" % u)
                for ci in range(4):
                    cc = 4 * u + ci
                    bi = nbank()
                    sb = cc % 2
                    mm_group(banks[bi][:, :], bi, [(w_[:, kc, ci * 128:(ci + 1) * 128], attT[:, kc, :]) for kc in range(8)],
                             [tw, t_attT])
                    P.emit(P.dve, lambda e, bi=bi, cc=cc, sb=sb: e.tensor_tensor(out=tm2[sb], in0=banks[bi][:, :],
                                                                                 in1=gas[:, cc, :], op=ALU.mult),
                           [tb[bi], t_gas], [t_tm2[sb]])
                    P.emit(P.dve, lambda e, cc=cc, sb=sb: e.tensor_tensor(out=mT[:, cc, :], in0=tm2[sb], in1=t1[:, cc, :],
                                                                          op=ALU.add), [t_tm2[sb], t_t1], [t_mT])
            for hf in range(2):
                w_, tw = w_next("wo%d" % hf)
                for t in range(4):
                    bi = nbank()
                    mm_group(banks[bi][:, :], bi, [(mT[:, kc, t * 128:(t + 1) * 128], w_[:, kc, :]) for kc in range(8)],
                             [tw, t_mT])
                    P.emit(P.dve, lambda e, bi=bi, t=t, hf=hf: e.tensor_tensor(
                        out=xin[:, t, hf * 512:(hf + 1) * 512], in0=banks[bi][:, :], in1=xin[:, t, hf * 512:(hf + 1) * 512],
                        op=ALU.add), [tb[bi], t_xin], [t_xin])
            for t in range(4):
                xb = t % 2
                norm_tile(xin[:, t, :], 128, t_xin, ssq2[:, t:t + 1], rinv2[:, t:t + 1], t_ssq2, t_rinv2, xn[xb], t_xn[xb])
                transpose_tile(xn[xb], 128, t_xn[xb], hnT[:, :, t * 128:(t + 1) * 128], t_hnT, use_act=(t % 2 == 0))
            for u in range(11):
                w_, tw = w_next("gu%d" % u)
                for ci in range(2):
                    fc = 2 * u + ci
                    sb = fc % 2
                    bgt = nbank()
                    mm_group(banks[bgt][:, :], bgt, [(w_[:, kc, ci * 128:(ci + 1) * 128], hnT[:, kc, :]) for kc in range(8)],
                             [tw, t_hnT])
                    but = nbank()
                    mm_group(banks[but][:, :], but,
                             [(w_[:, kc, 256 + ci * 128:256 + (ci + 1) * 128], hnT[:, kc, :]) for kc in range(8)],
                             [tw, t_hnT])
                    P.emit(P.act, lambda e, bgt=bgt, sb=sb: e.activation(out=sg_t[sb], in_=banks[bgt][:, :], func=AF.Silu),
                           [tb[bgt]], [t_sg[sb]])
                    P.emit(P.dve, lambda e, but=but, sb=sb, fc=fc: e.tensor_tensor(out=act_t[:, fc, :], in0=banks[but][:, :],
                                                                                   in1=sg_t[sb], op=ALU.mult),
                           [tb[but], t_sg[sb]], [t_actt, t_a, t_y])
            for hf in range(2):
                for gi, (f0, nf) in enumerate(((0, 8), (8, 8), (16, 6))):
                    w_, tw = w_next("dn%d_%d" % (hf, gi))
                    for t in range(4):
                        bi = 4 + t
                        for fl in range(nf):
                            fc = f0 + fl
                            P.emit(P.pe, lambda e, bi=bi, t=t, fc=fc, fl=fl, w_=w_: e.matmul(
                                out=banks[bi][:, :], lhsT=act_t[:, fc, t * 128:(t + 1) * 128], rhs=w_[:, fl, :],
                                start=(fc == 0), stop=(fc == FC - 1)), [tw, t_actt], [tb[bi]], ms=(fl == nf - 1))
                for t in range(4):
                    bi = 4 + t
                    P.emit(P.dve, lambda e, bi=bi, t=t, hf=hf: e.tensor_tensor(
                        out=xin[:, t, hf * 512:(hf + 1) * 512], in0=banks[bi][:, :], in1=xin[:, t, hf * 512:(hf + 1) * 512],
                        op=ALU.add), [tb[bi], t_xin], [t_xin])
            P.dma(P.pool, out_d[g * 512:(g + 1) * 512, :].rearrange("(t p) d -> p t d", p=128), xin, [t_xin], [t_outd],
                  dq_out)
            t_a.r += t_actt.r
            t_y.r += t_actt.r
            if t_actt.w is not None:
                t_a.r.append(t_actt.w)
                t_y.r.append(t_actt.w)
        P.wait_all(P.pool, [t_outd])
        P.wait_all(P.sp, [t_outd])
        P.run()
    return nc


def host_tables(j):
    slopes = 2.0 ** (-8.0 * np.arange(1, NH + 1) / NH)
    gate_bias = np.zeros((16, 64), np.float32)
    a2 = np.zeros((16, 8, 64), np.float32)
    b2 = np.full((16, 64), NEG, np.float32)
    for i in range(16):
        cur = 4 * i + j
        gate_bias[i, cur:] = -1e30
        b2[i, cur] = 0.0
        for n in range(cur):
            a2[i, :, n] = -NEG - slopes * 256.0 * (cur - n)
    cmsel = np.zeros((128, 4, 2, 256), np.float32)
    p = np.arange(128)[:, None]
    rq = np.arange(256)[None, :]
    for kh in range(2):
        rk = 2 * p + kh
        cmsel[:, j, kh, :] = np.where(rq >= rk, 0.0, NEG)
    lo = np.zeros((1, 8, 512), np.float32)
    for h in range(8):
        lo[0, h, :] = -slopes[h] * (np.arange(512) % 256)
    kbias = np.zeros((128, 16), np.float32)
    for h in range(8):
        for kh in range(2):
            kbias[:, 2 * h + kh] = slopes[h] * (2 * np.arange(128) + kh)
    return {"gate_bias": gate_bias, "a2": a2, "b2": b2, "cmsel": cmsel.reshape(128, -1),
            "lo": lo.reshape(1, -1), "kbias": kbias, "ident": np.eye(128, dtype=np.float32)}


def make_in_maps(inputs):
    x = np.asarray(inputs["x"], np.float32)
    shared = {
        "w_in": np.ascontiguousarray(inputs["w_in"][0]), "w_conv_out": np.ascontiguousarray(inputs["w_conv_out"][0]),
        "w_attn_out": np.ascontiguousarray(inputs["w_attn_out"][0]), "w_out": np.ascontiguousarray(inputs["w_out"][0]),
        "w_ffn_gate": np.ascontiguousarray(inputs["w_ffn_gate"][0]), "w_ffn_up": np.ascontiguousarray(inputs["w_ffn_up"][0]),
        "w_ffn_down": np.ascontiguousarray(inputs["w_ffn_down"][0]),
        "norm1_g": np.asarray(inputs["norm1_g"], np.float32).reshape(1, D),
        "norm2_g": np.asarray(inputs["norm2_g"], np.float32).reshape(1, D),
        "dw_w": np.ascontiguousarray(inputs["dw_w"][0]), "dw_b": np.asarray(inputs["dw_b"], np.float32).reshape(1, D),
        "conv_ln_g": np.asarray(inputs["conv_ln_g"], np.float32).reshape(1, D),
        "conv_ln_b": np.asarray(inputs["conv_ln_b"], np.float32).reshape(1, D),
        "q_norm_g": np.asarray(inputs["q_norm_g"], np.float32).reshape(1, DH),
        "k_norm_g": np.asarray(inputs["k_norm_g"], np.float32).reshape(1, DH),
    }
    shared = {k: np.asarray(v, np.float32) for k, v in shared.items()}
    maps = []
    for c in range(8):
        b, j = c // 4, c % 4
        xb = x[b]
        xblk = xb.reshape(NBK, L, D)
        own = [4 * i + j for i in range(16)]
        x_own = np.ascontiguousarray(xblk[own].reshape(4096, D))
        halo = np.zeros((16, 32, D), np.float32)
        for i, n in enumerate(own):
            if n > 0:
                halo[i] = xb[n * L - 32:n * L]
        m = dict(shared)
        m.update(host_tables(j))
        m["x_all"] = np.ascontiguousarray(xb)
        m["x_own"] = x_own
        m["x_halo"] = halo.reshape(512, D)
        maps.append(m)
    return maps


_NC_CACHE = {}


def kernel(**inputs):
    if "nc" not in _NC_CACHE:
        _NC_CACHE["nc"] = build_nc()
    nc = _NC_CACHE["nc"]
    maps = make_in_maps(inputs)
    res = run_bass_kernel_spmd(nc, maps, core_ids=list(range(8)))
    out = np.zeros((2, S, D), np.float32)
    ov = out.reshape(2, NBK, L, D)
    for c in range(8):
        b, j = c // 4, c % 4
        o = np.asarray(res.results[c]["out_own"], np.float32).reshape(16, L, D)
        for i in range(16):
            ov[b, 4 * i + j] = o[i]
    return out
```

```python
import numpy as np
from contextlib import ExitStack
import concourse.bass as bass
import concourse.mybir as mybir
from concourse.bass_utils import run_bass_kernel_spmd

F32 = mybir.dt.float32
BF16 = mybir.dt.bfloat16
AF = mybir.ActivationFunctionType
ALU = mybir.AluOpType
AX = mybir.AxisListType

D = 1024
KC = 8
NH = 8
DH = 128
FF = 2816
FC = 22
S = 16384
L = 256
NBK = 64
EPS = 1e-6
NEG = -30000.0


class T:
    __slots__ = ("name", "w", "r")

    def __init__(self, name):
        self.name = name
        self.w = None
        self.r = []


class Q:
    def __init__(self, name, sem, scale=1):
        self.name = name
        self.sem = sem
        self.scale = scale
        self.count = 0
        self.ops = []
        self.seen = {}


class Prog:
    def __init__(self, nc, es):
        self.nc = nc
        self.es = es
        self.pe = Q("pe", self.newsem("s_pe"))
        self.act = Q("act", self.newsem("s_act"))
        self.dve = Q("dve", self.newsem("s_dve"))
        self.pool = Q("pool", self.newsem("s_pool"))
        self.sp = Q("sp", self.newsem("s_sp"))
        self.engines = [self.pe, self.act, self.dve, self.pool, self.sp]

    def newsem(self, name):
        return self.es.enter_context(self.nc.semaphore(name))

    def dmaq(self, name):
        return Q(name, self.newsem("d_" + name), 16)

    def _wait(self, q, tok):
        sq, cnt = tok
        if q.seen.get(sq, 0) >= cnt:
            return
        q.seen[sq] = cnt
        q.ops.append(lambda e, s=sq.sem, v=cnt * sq.scale: e.wait_ge(s, v))

    def emit(self, q, fn, reads=(), writes=(), ms=True, sig=None):
        sig = sig or q
        for t in reads:
            if t.w is not None:
                self._wait(q, t.w)
        for t in writes:
            if t.w is not None and t.w[0] is not q:
                self._wait(q, t.w)
            for tok in t.r:
                if tok[0] is not q:
                    self._wait(q, tok)
        if ms:
            sig.count += 1
            tok = (sig, sig.count)
            q.ops.append(lambda e, s=sig.sem, v=sig.scale: fn(e).then_inc(s, v))
        else:
            tok = (sig, sig.count + 1)
            q.ops.append(lambda e: fn(e))
        for t in writes:
            t.w = tok
            t.r = []
        for t in reads:
            t.r.append(tok)
            if len(t.r) > 16:
                best = {}
                for sq, c in t.r:
                    if sq not in best or best[sq] < c:
                        best[sq] = c
                t.r = list(best.items())
        return tok

    def dma(self, q, out, in_, reads, writes, sig, **kw):
        return self.emit(q, lambda e: e.dma_start(out=out, in_=in_, **kw), reads, writes, sig=sig)

    def wait_all(self, q, ts):
        for t in ts:
            if t.w is not None:
                self._wait(q, t.w)
            for tok in t.r:
                self._wait(q, tok)

    def barrier(self, ts):
        for q in self.engines:
            self.wait_all(q, ts)

    def run(self):
        nc = self.nc
        with nc.Block() as block:
            @block.tensor
            def _(e):
                for op in self.pe.ops:
                    op(e)

            @block.scalar
            def _(e):
                for op in self.act.ops:
                    op(e)

            @block.vector
            def _(e):
                for op in self.dve.ops:
                    op(e)

            @block.gpsimd
            def _(e):
                for op in self.pool.ops:
                    op(e)

            @block.sync
            def _(e):
                for op in self.sp.ops:
                    op(e)


class Arena:
    def __init__(self, ap, nwords):
        self.ap = ap
        self.n = nwords
        self.off = 0

    def alloc(self, shape, dtype):
        shape = list(shape)
        np_ = shape[0]
        free = 1
        for s_ in shape[1:]:
            free *= s_
        esz = 4 if dtype == F32 else 2
        words = (free * esz + 3) // 4
        words = (words + 7) // 8 * 8
        assert self.off + words <= self.n, ("arena overflow", self.off, words, self.n)
        v = self.ap[0:np_, self.off:self.off + words]
        self.off += words
        if dtype != F32:
            v = v.bitcast(dtype)
        v = v[:, 0:free]
        if len(shape) == 2:
            return v
        names = " ".join("d%d" % i for i in range(len(shape) - 1))
        kw = {"d%d" % i: shape[i + 1] for i in range(len(shape) - 1)}
        return v.rearrange("p (%s) -> p %s" % (names, names), **kw)


def unit_table():
    u = {}
    for i in range(4):
        u["cv%d" % i] = [("w_in", 0, 8, (2 * i) * 128, 128, 0, "g1"), ("w_in", 0, 8, (2 * i + 1) * 128, 128, 128, "g1"),
                         ("w_in", 0, 8, 1024 + (2 * i) * 128, 128, 256, "g1"),
                         ("w_in", 0, 8, 1024 + (2 * i + 1) * 128, 128, 384, "g1")]
    for nm, c0 in (("q", 2048), ("k", 3072), ("v", 4096), ("gc", 5120), ("ga", 6144)):
        for i in range(2):
            u["%s%d" % (nm, i)] = [("w_in", 0, 8, c0 + i * 512, 512, 0, "g1")]
    for nm, src in (("co", "w_conv_out"), ("ao", "w_attn_out"), ("wo", "w_out")):
        for i in range(2):
            u["%s%d" % (nm, i)] = [(src, 0, 8, i * 512, 512, 0, None)]
    for i in range(11):
        u["gu%d" % i] = [("w_ffn_gate", 0, 8, (2 * i) * 128, 128, 0, "g2"),
                         ("w_ffn_gate", 0, 8, (2 * i + 1) * 128, 128, 128, "g2"),
                         ("w_ffn_up", 0, 8, (2 * i) * 128, 128, 256, "g2"),
                         ("w_ffn_up", 0, 8, (2 * i + 1) * 128, 128, 384, "g2")]
    for hf in range(2):
        for gi, (f0, nf) in enumerate(((0, 8), (8, 8), (16, 6))):
            u["dn%d_%d" % (hf, gi)] = [("w_ffn_down", f0 * 128, nf, hf * 512, 512, 0, None)]
    return u


UNITS = unit_table()
UNAMES = list(UNITS.keys())
UIDX = {n: i for i, n in enumerate(UNAMES)}
NU = len(UNAMES)
CHUNK_SEQ = (["cv%d" % i for i in range(4)] + ["q0", "q1", "gc0", "gc1", "ga0", "ga1", "co0", "co1", "ao0", "ao1",
                                               "wo0", "wo1"] + ["gu%d" % i for i in range(11)] +
             ["dn0_0", "dn0_1", "dn0_2", "dn1_0", "dn1_1", "dn1_2"])


def build_nc(nch=8, nkv=32):
    nc = bass.Bass("TRN2", target_bir_lowering=False)

    def din(name, shape):
        return nc.dram_tensor(name, list(shape), F32, kind="ExternalInput").ap()

    x_all = din("x_all", [S, D])
    x_own = din("x_own", [4096, D])
    x_halo = din("x_halo", [512, D])
    gate_bias = din("gate_bias", [16, 64])
    a2_d = din("a2", [16, 8, 64])
    b2_d = din("b2", [16, 64])
    cms_d = din("cmsel", [128, 4 * 2 * 256])
    lo_d = din("lo", [1, 8 * 512])
    kb_d = din("kbias", [128, 16])
    id_d = din("ident", [128, 128])
    wsrc = {
        "w_in": din("w_in", [D, 7168]), "w_conv_out": din("w_conv_out", [D, D]), "w_attn_out": din("w_attn_out", [D, D]),
        "w_out": din("w_out", [D, D]), "w_ffn_gate": din("w_ffn_gate", [D, FF]), "w_ffn_up": din("w_ffn_up", [D, FF]),
        "w_ffn_down": din("w_ffn_down", [FF, D]),
    }
    norm1_g = din("norm1_g", [1, D])
    norm2_g = din("norm2_g", [1, D])
    dw_w = din("dw_w", [31, D])
    dw_b = din("dw_b", [1, D])
    ln_g = din("conv_ln_g", [1, D])
    ln_b = din("conv_ln_b", [1, D])
    qg_d = din("q_norm_g", [1, DH])
    kg_d = din("k_norm_g", [1, DH])
    out_d = nc.dram_tensor("out_own", [4096, D], F32, kind="ExternalOutput").ap()
    wsc = nc.dram_tensor("wsc", [NU, 128, 4096], BF16).ap()
    kt_d = nc.dram_tensor("kt_s", [NH, 128, S], BF16).ap()
    vv_d = nc.dram_tensor("vv_s", [128, NBK, NH, 2, 129], BF16).ap()

    with ExitStack() as es:
        P = Prog(nc, es)
        NW = 53000
        arena_t = es.enter_context(nc.sbuf_tensor("arena", [128, NW], F32))
        AR = Arena(arena_t[:, :], NW)
        banks = [es.enter_context(nc.psum_tensor("pb%d" % i, [128, 512], F32)) for i in range(8)]
        tb = [T("pb%d" % i) for i in range(8)]
        rot = [0]

        def nbank():
            i = rot[0]
            rot[0] = (rot[0] + 1) % 8
            return i

        def bf_view(i):
            return banks[i][:, :].bitcast(BF16)

        identf = AR.alloc([128, 128], F32)
        identb = AR.alloc([128, 128], BF16)
        onesb = AR.alloc([128, 128], BF16)
        esel = AR.alloc([65, 64], BF16)
        cms = AR.alloc([128, 4, 2, 256], BF16)
        kb = AR.alloc([128, 16], F32)
        kmean = AR.alloc([128, 8, 64], F32)
        VT = AR.alloc([65, 8, 512], BF16)
        g1 = AR.alloc([128, 8], F32)
        g2 = AR.alloc([128, 8], F32)
        cw = AR.alloc([128, 8, 31], F32)
        cb = AR.alloc([128, 8], F32)
        lg = AR.alloc([128, 8], F32)
        lb = AR.alloc([128, 8], F32)
        qg = AR.alloc([128, 1], F32)
        kg = AR.alloc([128, 1], F32)
        epsc = AR.alloc([128, 1], F32)
        t_const = T("const")
        t_kmean = T("kmean")
        t_vt64 = T("vt64")
        dq_c = P.dmaq("const")
        const_mark = AR.off

        cms_f = AR.alloc([128, 2048], F32)
        lo_f = AR.alloc([65, 4096], F32)
        t_cst = T("cst")
        pq = P.pool
        P.dma(pq, identf, id_d, [], [t_cst], dq_c)
        P.dma(pq, cms_f, cms_d, [], [t_cst], dq_c)
        P.dma(pq, lo_f[64:65, :], lo_d, [], [t_cst], dq_c)
        P.dma(pq, kb, kb_d, [], [t_cst], dq_c)
        P.dma(pq, g1, norm1_g[0].rearrange("(kc p) -> p kc", p=128), [], [t_cst], dq_c, allow_slow_non_contiguous=True)
        P.dma(pq, g2, norm2_g[0].rearrange("(kc p) -> p kc", p=128), [], [t_cst], dq_c, allow_slow_non_contiguous=True)
        P.dma(pq, cb, dw_b[0].rearrange("(kc p) -> p kc", p=128), [], [t_cst], dq_c, allow_slow_non_contiguous=True)
        P.dma(pq, lg, ln_g[0].rearrange("(kc p) -> p kc", p=128), [], [t_cst], dq_c, allow_slow_non_contiguous=True)
        P.dma(pq, lb, ln_b[0].rearrange("(kc p) -> p kc", p=128), [], [t_cst], dq_c, allow_slow_non_contiguous=True)
        for kc_ in range(8):
            P.dma(pq, cw[:, kc_, :], dw_w[:, kc_ * 128:(kc_ + 1) * 128].rearrange("j p -> p j"), [], [t_cst], dq_c,
                  allow_slow_non_contiguous=True)
        P.dma(pq, qg, qg_d.rearrange("o p -> p o"), [], [t_cst], dq_c, allow_slow_non_contiguous=True)
        P.dma(pq, kg, kg_d.rearrange("o p -> p o"), [], [t_cst], dq_c, allow_slow_non_contiguous=True)
        dv = P.dve
        P.emit(dv, lambda e: e.tensor_copy(out=identb, in_=identf), [t_cst], [t_const])
        P.emit(dv, lambda e: e.memset(onesb, 1.0), [], [t_const])
        P.emit(dv, lambda e: e.memset(kmean.rearrange("p a b -> p (a b)"), 0.0), [], [t_const])
        P.emit(dv, lambda e: e.memset(epsc, EPS), [], [t_const])
        P.emit(dv, lambda e: e.tensor_copy(out=esel[0:64, :], in_=identf[0:64, 0:64]), [t_cst], [t_const])
        P.emit(dv, lambda e: e.memset(esel[64:65, :], 1.0), [], [t_const])
        P.emit(dv, lambda e: e.tensor_copy(out=cms.rearrange("p a b c -> p (a b c)"), in_=cms_f), [t_cst], [t_const])
        P.emit(dv, lambda e: e.tensor_copy(out=VT[64:65, :, :].rearrange("p a b -> p (a b)"), in_=lo_f[64:65, :]),
               [t_cst], [t_const])
        P.emit(dv, lambda e: e.tensor_scalar(out=qg, in0=qg, scalar1=float(DH) ** -0.5, scalar2=None, op0=ALU.mult),
               [t_cst], [t_const])
        P.barrier([t_cst, t_const])
        AR.off = const_mark
        phase_mark = AR.off

        def esel_lhsT(n):
            a = esel[0:65, n:n + 1]
            return bass.AP(a.tensor, a.offset, [[a.ap[0][0], 65], [0, 128]])

        stg = [AR.alloc([128, 8, 512], F32) for _ in range(2)]
        wbf = [AR.alloc([128, 8, 512], BF16) for _ in range(2)]
        t_stg = [T("stg0"), T("stg1")]
        t_wbf = [T("wbf0"), T("wbf1")]
        dq_stg = [P.dmaq("stg0"), P.dmaq("stg1")]
        dq_wbf = [P.dmaq("wbf0"), P.dmaq("wbf1")]
        t_unit = [T("unit%d" % i) for i in range(NU)]
        gains = {"g1": g1, "g2": g2}
        kv_units = ["k0", "k1", "v0", "v1"]
        conv_order = kv_units + [n_ for n_ in UNAMES if n_ not in kv_units]

        def conv_load(oi):
            nm = conv_order[oi]
            b = oi % 2
            for (src, row0, nk_, c0, ncol, dst, _g) in UNITS[nm]:
                sap = wsrc[src][row0:row0 + nk_ * 128, c0:c0 + ncol].rearrange("(kc p) n -> p kc n", p=128)
                P.dma(P.sp, stg[b][:, 0:nk_, dst:dst + ncol], sap, [], [t_stg[b]], dq_stg[b])

        def conv_cast_store(oi, act_only):
            nm = conv_order[oi]
            ui = UIDX[nm]
            b = oi % 2
            segs = UNITS[nm]
            nk = segs[0][2]
            gname = segs[0][6]
            for kc in range(nk):
                use_dve = (kc % 2 == 0) and not act_only
                if gname is None:
                    if use_dve:
                        P.emit(P.dve, lambda e, b=b, kc=kc: e.tensor_copy(out=wbf[b][:, kc, :], in_=stg[b][:, kc, :]),
                               [t_stg[b]], [t_wbf[b]])
                    else:
                        P.emit(P.act, lambda e, b=b, kc=kc: e.copy(out=wbf[b][:, kc, :], in_=stg[b][:, kc, :]),
                               [t_stg[b]], [t_wbf[b]])
                else:
                    gt = gains[gname]
                    if use_dve:
                        P.emit(P.dve, lambda e, b=b, kc=kc, gt=gt: e.tensor_scalar(
                            out=wbf[b][:, kc, :], in0=stg[b][:, kc, :], scalar1=gt[:, kc:kc + 1], scalar2=None,
                            op0=ALU.mult), [t_stg[b]], [t_wbf[b]])
                    else:
                        P.emit(P.act, lambda e, b=b, kc=kc, gt=gt: e.activation(
                            out=wbf[b][:, kc, :], in_=stg[b][:, kc, :], func=AF.Copy, scale=gt[:, kc:kc + 1]),
                            [t_stg[b]], [t_wbf[b]])
            P.dma(P.sp, wsc[ui, :, 0:nk * 512], wbf[b][:, 0:nk, :].rearrange("p a b -> p (a b)"), [t_wbf[b]],
                  [t_unit[ui]], dq_wbf[b])

        conv_load(0)
        conv_load(1)
        for oi in range(4):
            conv_cast_store(oi, act_only=False)
            conv_load(oi + 2)
        conv_next = [4]

        wk = AR.alloc([128, 8, 1024], BF16)
        wv = AR.alloc([128, 8, 1024], BF16)
        t_wk = T("wk")
        t_wv = T("wv")
        dq_wk = P.dmaq("wk")
        dq_wv = P.dmaq("wv")
        for i in range(2):
            P.dma(P.pool, wk[:, :, i * 512:(i + 1) * 512], wsc[UIDX["k%d" % i]].rearrange("p (a b) -> p a b", b=512),
                  [t_unit[UIDX["k%d" % i]]], [t_wk], dq_wk)
            P.dma(P.pool, wv[:, :, i * 512:(i + 1) * 512], wsc[UIDX["v%d" % i]].rearrange("p (a b) -> p a b", b=512),
                  [t_unit[UIDX["v%d" % i]]], [t_wv], dq_wv)
        xin1 = [AR.alloc([128, 4, 1024], F32) for _ in range(2)]
        t_xin1 = [T("xin1_0"), T("xin1_1")]
        dq_xin1 = [P.dmaq("xin1_0"), P.dmaq("xin1_1")]
        junk_cur = [AR.alloc([128, 1024], BF16)]
        t_junk = T("junk")
        ssq = [AR.alloc([128, 4], F32) for _ in range(2)]
        t_ssq = [T("ssq0"), T("ssq1")]
        rinv = [AR.alloc([128, 4], F32) for _ in range(2)]
        t_rinv = [T("rinv0"), T("rinv1")]
        xn = [AR.alloc([128, 1024], BF16) for _ in range(2)]
        t_xn = [T("xn0"), T("xn1")]
        xnT1 = [AR.alloc([128, 8, 512], BF16) for _ in range(2)]
        t_xnT1 = [T("xnT1_0"), T("xnT1_1")]
        sqb = [AR.alloc([128, 512], BF16) for _ in range(2)]
        t_sqb = [T("sqb0"), T("sqb1")]
        rkf = [AR.alloc([128, 512], F32) for _ in range(2)]
        t_rkf = [T("rkf0"), T("rkf1")]
        kst = [AR.alloc([128, 8, 512], BF16) for _ in range(2)]
        t_kst = [T("kst0"), T("kst1")]
        dq_kst = [P.dmaq("kst0"), P.dmaq("kst1")]
        vst = [AR.alloc([128, 2, 8, 2, 129], BF16) for _ in range(2)]
        t_vst = [T("vst0"), T("vst1")]
        dq_vst = [P.dmaq("vst0"), P.dmaq("vst1")]
        t_kt = T("kt_dram")
        t_vv = T("vv_dram")
        for b in range(2):
            P.emit(P.dve, lambda e, b=b: e.memset(vst[b].rearrange("p a b c d -> p (a b c d)"), 1.0), [], [t_vst[b]])

        def norm_tile(src, np_, t_src, ssq_ap, rinv_ap, t_s, t_r, xn_ap, t_x):
            jv = junk_cur[0][0:np_, :]
            P.emit(P.dve, lambda e: e.memset(ssq_ap, 0.0), [], [t_s])
            P.emit(P.act, lambda e: e.activation(out=jv, in_=src, func=AF.Square, accum_out=ssq_ap),
                   [t_src], [t_junk, t_s])
            P.emit(P.act, lambda e: e.activation(out=rinv_ap, in_=ssq_ap, func=AF.Ln, scale=1.0 / D,
                                                 bias=epsc[0:np_, :]), [t_s], [t_r])
            P.emit(P.act, lambda e: e.activation(out=rinv_ap, in_=rinv_ap, func=AF.Exp, scale=-0.5), [t_r], [t_r])
            P.emit(P.dve, lambda e: e.tensor_scalar(out=xn_ap, in0=src, scalar1=rinv_ap, scalar2=None, op0=ALU.mult),
                   [t_src, t_r], [t_x])

        def transpose_tile(xn_ap, np_, t_x, dst, t_dst, use_act):
            bi = nbank()
            pv = bf_view(bi)[:, 0:8 * np_].rearrange("p (a b) -> p a b", b=np_)
            for kc in range(8):
                P.emit(P.pe, lambda e, kc=kc: e.transpose(out=pv[:, kc, :], in_=xn_ap[:, kc * 128:(kc + 1) * 128],
                                                          identity=identb[0:np_, 0:np_]),
                       [t_x], [tb[bi]], ms=(kc == 7))
            if use_act:
                P.emit(P.act, lambda e: e.copy(out=dst, in_=pv), [tb[bi]], [t_dst])
            else:
                P.emit(P.dve, lambda e: e.tensor_copy(out=dst, in_=pv), [tb[bi]], [t_dst])

        def x1_load(c):
            b = c % 2
            P.dma(P.pool, xin1[b], x_all[c * 512:(c + 1) * 512, :].rearrange("(t p) d -> p t d", p=128), [],
                  [t_xin1[b]], dq_xin1[b])

        def p1_norm_transpose(c):
            b = c % 2
            for t in range(4):
                xb = t % 2
                norm_tile(xin1[b][:, t, :], 128, t_xin1[b], ssq[b][:, t:t + 1], rinv[b][:, t:t + 1], t_ssq[b], t_rinv[b],
                          xn[xb], t_xn[xb])
                transpose_tile(xn[xb], 128, t_xn[xb], xnT1[b][:, :, t * 128:(t + 1) * 128], t_xnT1[b],
                               use_act=(t % 2 == 0))

        x1_load(0)
        if nkv > 1:
            x1_load(1)
        p1_norm_transpose(0)
        for c in range(nkv):
            b = c % 2
            if conv_next[0] < len(conv_order):
                oi = conv_next[0]
                conv_cast_store(oi, act_only=True)
                if oi + 2 < len(conv_order):
                    conv_load(oi + 2)
                conv_next[0] += 1
            pend = None

            def k_tail(h, bi, pk, b=b, c=c):
                sb = h % 2
                b2i = nbank()
                p2 = banks[b2i][:, :]
                P.emit(P.pe, lambda e, p2=p2, sb=sb: e.matmul(out=p2, lhsT=onesb, rhs=sqb[sb], start=True, stop=True),
                       [t_sqb[sb]], [tb[b2i]])
                P.emit(P.act, lambda e, p2=p2, sb=sb: e.activation(out=rkf[sb], in_=p2, func=AF.Ln, scale=1.0 / DH,
                                                                   bias=epsc), [tb[b2i]], [t_rkf[sb]])
                P.emit(P.act, lambda e, sb=sb: e.activation(out=rkf[sb], in_=rkf[sb], func=AF.Exp, scale=-0.5),
                       [t_rkf[sb]], [t_rkf[sb]])
                P.emit(P.dve, lambda e, pk=pk, sb=sb, h=h, b=b: e.scalar_tensor_tensor(
                    out=kst[b][:, h, :], in0=pk, scalar=kg[:, 0:1], in1=rkf[sb], op0=ALU.mult, op1=ALU.mult),
                    [tb[bi], t_rkf[sb]], [t_kst[b]])
                P.emit(P.dve, lambda e, h=h, b=b, c=c: e.tensor_reduce(
                    out=kmean[:, h, 2 * c:2 * c + 2], in_=kst[b][:, h, :].rearrange("p (a l) -> p a l", l=256),
                    axis=AX.X, op=ALU.add), [t_kst[b]], [t_kmean])

            for h in range(NH):
                bi = nbank()
                pk = banks[bi][:, :]
                for kc in range(8):
                    P.emit(P.pe, lambda e, kc=kc, h=h, pk=pk, b=b: e.matmul(out=pk, lhsT=wk[:, kc, h * 128:(h + 1) * 128],
                                                                           rhs=xnT1[b][:, kc, :], start=(kc == 0),
                                                                           stop=(kc == 7)),
                           [t_wk, t_xnT1[b]], [tb[bi]], ms=(kc == 7))
                sb = h % 2
                P.emit(P.act, lambda e, pk=pk, sb=sb: e.activation(out=sqb[sb], in_=pk, func=AF.Square),
                       [tb[bi]], [t_sqb[sb]])
                if pend is not None:
                    k_tail(*pend)
                pend = (h, bi, pk)
            k_tail(*pend)
            P.dma(P.pool, kt_d[:, :, c * 512:(c + 1) * 512].rearrange("h p t -> p h t"), kst[b], [t_kst[b]], [t_kt],
                  dq_kst[b])
            if c + 2 < nkv:
                x1_load(c + 2)
            for blk in range(2):
                for kh in range(2):
                    for hf in range(2):
                        bi = nbank()
                        pv_ = banks[bi][:, :]
                        for kc in range(8):
                            P.emit(P.pe, lambda e, kc=kc, blk=blk, kh=kh, hf=hf, pv_=pv_, b=b: e.matmul(
                                out=pv_, lhsT=xnT1[b][:, kc, blk * 256 + kh:blk * 256 + 256:2],
                                rhs=wv[:, kc, hf * 512:(hf + 1) * 512], start=(kc == 0), stop=(kc == 7)),
                                [t_wv, t_xnT1[b]], [tb[bi]], ms=(kc == 7))
                        dst = vst[b][:, blk, hf * 4:(hf + 1) * 4, kh, 0:128]
                        src = pv_.rearrange("p (a d) -> p a d", d=128)
                        if hf == 0:
                            P.emit(P.act, lambda e, dst=dst, src=src: e.copy(out=dst, in_=src), [tb[bi]], [t_vst[b]])
                        else:
                            P.emit(P.dve, lambda e, dst=dst, src=src: e.tensor_copy(out=dst, in_=src), [tb[bi]],
                                   [t_vst[b]])
                if blk == 0 and c + 1 < nkv:
                    p1_norm_transpose(c + 1)
            P.dma(P.pool, vv_d[:, 2 * c:2 * c + 2].rearrange("p a b c d -> p (a b c d)"),
                  vst[b].rearrange("p a b c d -> p (a b c d)"), [t_vst[b]], [t_vv], dq_vst[b])
        while conv_next[0] < len(conv_order):
            oi = conv_next[0]
            conv_cast_store(oi, act_only=False)
            if oi + 2 < len(conv_order):
                conv_load(oi + 2)
            conv_next[0] += 1
        ph1 = t_xin1 + t_ssq + t_rinv + t_xn + t_sqb + t_rkf + t_kst + t_vst + t_xnT1 + [t_junk, t_wk, t_wv, t_kt, t_vv,
                                                                                         t_kmean] + tb + t_stg + t_wbf
        P.barrier(ph1)
        AR.off = phase_mark

        NSLOT = 3
        wr = [AR.alloc([128, 8, 512], BF16) for _ in range(NSLOT)]
        t_wr = [T("wr%d" % i) for i in range(NSLOT)]
        dq_wr = [P.dmaq("wr%d" % i) for i in range(NSLOT)]
        seq_all = []
        for g in range(nch):
            seq_all += CHUNK_SEQ
        wstate = {"issued": 0, "used": 0}

        def w_issue():
            i = wstate["issued"]
            if i >= len(seq_all):
                return
            nm = seq_all[i]
            s_ = i % NSLOT
            ui = UIDX[nm]
            nk = UNITS[nm][0][2]
            P.dma(P.sp, wr[s_][:, 0:nk, :].rearrange("p a b -> p (a b)"), wsc[ui, :, 0:nk * 512], [t_unit[ui]],
                  [t_wr[s_]], dq_wr[s_])
            wstate["issued"] += 1

        def w_next(expect):
            i = wstate["used"]
            assert seq_all[i] == expect, (seq_all[i], expect)
            while wstate["issued"] < min(i + NSLOT, len(seq_all)):
                w_issue()
            wstate["used"] += 1
            s_ = i % NSLOT
            return wr[s_], t_wr[s_]

        xin = AR.alloc([128, 4, 1024], F32)
        t_xin = T("xin")
        dq_xin = P.dmaq("xin")
        xh = AR.alloc([32, 2, 1024], F32)
        t_xh = T("xh")
        dq_xh = P.dmaq("xh")
        a2t = AR.alloc([128, 2, 8, 64], F32)
        b2t = AR.alloc([128, 2, 64], F32)
        gbt = AR.alloc([128, 2, 64], F32)
        t_tab = T("tab")
        dq_tab = P.dmaq("tab")
        junk_cur[0] = AR.alloc([128, 1024], BF16)
        ssq2 = AR.alloc([128, 8], F32)
        rinv2 = AR.alloc([128, 8], F32)
        t_ssq2 = T("ssq2")
        t_rinv2 = T("rinv2")
        xn = [AR.alloc([128, 1024], BF16) for _ in range(2)]
        xnT_raw = AR.alloc([128, 8 * 576], BF16)
        xnT = xnT_raw.rearrange("p (a b) -> p a b", b=576)
        t_xnT = T("xnT")
        R1 = AR.alloc([128, 8, 576 + 512], F32)
        a_t = R1[:, :, 0:576]
        y_t = R1[:, :, 576:1088]
        act_t = R1.rearrange("p a b -> p (a b)").bitcast(BF16)[:, 0:FC * 512].rearrange("p (a b) -> p a b", b=512)
        t_a = T("a")
        t_y = T("y")
        t_actt = T("act")
        sig_t = [AR.alloc([128, 1, 288], F32) for _ in range(2)]
        t_sig = [T("sig0"), T("sig1")]
        ybf = [AR.alloc([128, 512], BF16) for _ in range(2)]
        t_ybf = [T("ybf0"), T("ybf1")]
        ysq = [AR.alloc([128, 512], BF16) for _ in range(2)]
        t_ysq = [T("ysq0"), T("ysq1")]
        mean_t = AR.alloc([128, 512], F32)
        rstd_t = AR.alloc([128, 512], F32)
        t_mean = T("mean")
        t_rstd = T("rstd")
        xc = [AR.alloc([128, 512], F32) for _ in range(2)]
        t_xc = [T("xc0"), T("xc1")]
        ysl = AR.alloc([128, 8, 512], BF16)
        t_ysl = T("ysl")
        mT = ysl
        t_mT = t_ysl
        t1 = AR.alloc([128, 8, 512], BF16)
        t_t1 = T("t1")
        qT = AR.alloc([128, 8, 512], BF16)
        t_qT = T("qT")
        qf = [AR.alloc([128, 512], F32) for _ in range(2)]
        t_qf = [T("qf0"), T("qf1")]
        gcs = AR.alloc([128, 8, 512], BF16)
        gas = AR.alloc([128, 8, 512], BF16)
        t_gcs = T("gcs")
        t_gas = T("gas")
        attT = AR.alloc([128, 8, 512], BF16)
        t_attT = T("attT")
        hnT = xnT_raw[:, 0:8 * 512].rearrange("p (a b) -> p a b", b=512)
        t_hnT = t_xnT
        NKV = 2
        ktp = [AR.alloc([128, 2048], BF16) for _ in range(NKV)]
        vp = [AR.alloc([128, 8, 2, 129], BF16) for _ in range(NKV)]
        t_ktp = [T("ktp%d" % i) for i in range(NKV)]
        t_vp = [T("vp%d" % i) for i in range(NKV)]
        dq_ktp = [P.dmaq("ktp%d" % i) for i in range(NKV)]
        dq_vp = [P.dmaq("vp%d" % i) for i in range(NKV)]
        NPT = 4
        PT = [AR.alloc([128, 512], BF16) for _ in range(NPT)]
        t_PT = [T("PT%d" % i) for i in range(NPT)]
        gs_t = [AR.alloc([128, 64], F32) for _ in range(2)]
        m8_t = [AR.alloc([128, 8], F32) for _ in range(2)]
        tmp_t = [AR.alloc([128, 64], F32) for _ in range(2)]
        hiv_t = [AR.alloc([128, 64], BF16) for _ in range(2)]
        t_gs = [T("gs0"), T("gs1")]
        t_m8 = [T("m80"), T("m81")]
        t_tmp = [T("tmp0"), T("tmp1")]
        t_hiv = [T("hiv0"), T("hiv1")]
        rl_t = [AR.alloc([128, 1], F32) for _ in range(2)]
        t_rl = [T("rl0"), T("rl1")]
        atok = [AR.alloc([128, 128], BF16) for _ in range(2)]
        t_atok = [T("atok0"), T("atok1")]
        sg_t = xc
        t_sg = t_xc
        tm2 = xc
        t_tm2 = t_xc
        dq_out = P.dmaq("out")
        t_outd = T("out_dram")
        kvstate = {"n": 0}
        ptstate = {"n": 0}

        def mm_group(out_ap, bank_i, pairs, extra_reads, first=True, last=True):
            n_ = len(pairs)
            for i_, (l_, r_) in enumerate(pairs):
                P.emit(P.pe, lambda e, l_=l_, r_=r_, i_=i_: e.matmul(out=out_ap, lhsT=l_, rhs=r_,
                                                                     start=(first and i_ == 0),
                                                                     stop=(last and i_ == n_ - 1)),
                       extra_reads, [tb[bank_i]], ms=(i_ == n_ - 1))

        for g in range(nch):
            P.dma(P.pool, xin, x_own[g * 512:(g + 1) * 512, :].rearrange("(t p) d -> p t d", p=128), [], [t_xin], dq_xin)
            P.dma(P.pool, xh, x_halo[g * 64:(g + 1) * 64, :].rearrange("(o p) d -> p o d", p=32), [], [t_xh], dq_xh)
            P.dma(P.pool, a2t, a2_d[2 * g:2 * g + 2].partition_broadcast(128), [], [t_tab], dq_tab)
            P.dma(P.pool, b2t, b2_d[2 * g:2 * g + 2].partition_broadcast(128), [], [t_tab], dq_tab)
            P.dma(P.pool, gbt, gate_bias[2 * g:2 * g + 2].partition_broadcast(128), [], [t_tab], dq_tab)
            for o in range(2):
                xb = o % 2
                norm_tile(xh[0:32, o, :], 32, t_xh, ssq2[0:32, 4 + o:5 + o], rinv2[0:32, 4 + o:5 + o], t_ssq2, t_rinv2,
                          xn[xb][0:32, :], t_xn[xb])
                transpose_tile(xn[xb][0:32, :], 32, t_xn[xb], xnT[:, :, o * 288:o * 288 + 32], t_xnT, use_act=(o == 0))
            for t in range(4):
                xb = t % 2
                c0 = (t // 2) * 288 + 32 + (t % 2) * 128
                norm_tile(xin[:, t, :], 128, t_xin, ssq2[:, t:t + 1], rinv2[:, t:t + 1], t_ssq2, t_rinv2, xn[xb], t_xn[xb])
                transpose_tile(xn[xb], 128, t_xn[xb], xnT[:, :, c0:c0 + 128], t_xnT, use_act=(t % 2 == 0))
            for u in range(4):
                w_, tw = w_next("cv%d" % u)
                for ci in range(2):
                    cc = 2 * u + ci
                    bu = nbank()
                    bg = nbank()
                    bu2 = nbank()
                    bg2 = nbank()
                    for ob, (bcu, bcg) in enumerate(((bu, bg), (bu2, bg2))):
                        mm_group(banks[bcu][:, 0:288],
                                 bcu, [(w_[:, kc, ci * 128:(ci + 1) * 128], xnT[:, kc, ob * 288:(ob + 1) * 288])
                                       for kc in range(8)], [tw, t_xnT])
                        mm_group(banks[bcg][:, 0:288],
                                 bcg, [(w_[:, kc, 256 + ci * 128:256 + (ci + 1) * 128], xnT[:, kc, ob * 288:(ob + 1) * 288])
                                       for kc in range(8)], [tw, t_xnT])
                        sb = ob
                        P.emit(P.act, lambda e, bcg=bcg, sb=sb, ob=ob: e.activation(out=sig_t[sb][:, 0, :],
                                                                                   in_=banks[bcg][:, 0:288],
                                                                                   func=AF.Sigmoid),
                               [tb[bcg]], [t_sig[sb]])
                        P.emit(P.dve, lambda e, bcu=bcu, sb=sb, ob=ob, cc=cc: e.tensor_tensor(
                            out=a_t[:, cc, ob * 288:(ob + 1) * 288], in0=banks[bcu][:, 0:288], in1=sig_t[sb][:, 0, :],
                            op=ALU.mult), [tb[bcu], t_sig[sb]], [t_a])
            for u in range(2):
                w_, tw = w_next("q%d" % u)
                for hi_ in range(4):
                    h = 4 * u + hi_
                    bi = nbank()
                    pq_ = banks[bi][:, :]
                    for ob in range(2):
                        mm_group(pq_[:, ob * 256:(ob + 1) * 256], bi,
                                 [(w_[:, kc, hi_ * 128:(hi_ + 1) * 128], xnT[:, kc, ob * 288 + 32:ob * 288 + 288])
                                  for kc in range(8)], [tw, t_xnT])
                    sb = h % 2
                    P.emit(P.act, lambda e, pq_=pq_, sb=sb: e.activation(out=ysq[sb], in_=pq_, func=AF.Square),
                           [tb[bi]], [t_ysq[sb]])
                    b2i = nbank()
                    p2 = banks[b2i][:, :]
                    mm_group(p2, b2i, [(onesb, ysq[sb])], [t_ysq[sb]])
                    P.emit(P.act, lambda e, p2=p2, sb=sb: e.activation(out=xc[sb], in_=p2, func=AF.Ln, scale=1.0 / DH,
                                                                       bias=epsc), [tb[b2i]], [t_xc[sb]])
                    P.emit(P.act, lambda e, sb=sb: e.activation(out=xc[sb], in_=xc[sb], func=AF.Exp, scale=-0.5),
                           [t_xc[sb]], [t_xc[sb]])
                    P.emit(P.dve, lambda e, pq_=pq_, sb=sb: e.scalar_tensor_tensor(
                        out=qf[sb], in0=pq_, scalar=qg[:, 0:1], in1=xc[sb], op0=ALU.mult, op1=ALU.mult),
                        [tb[bi], t_xc[sb]], [t_qf[sb]])
                    P.emit(P.act, lambda e, sb=sb, h=h: e.copy(out=qT[:, h, :], in_=qf[sb]), [t_qf[sb]], [t_qT])
                    for t in range(4):
                        ob = t // 2
                        mb = t % 2
                        bgi = nbank()
                        pg = banks[bgi][:, 0:64]
                        mm_group(pg, bgi, [(qf[sb][:, t * 128:(t + 1) * 128], kmean[:, h, :])], [t_qf[sb], t_kmean])
                        P.emit(P.dve, lambda e, pg=pg, mb=mb, ob=ob: e.tensor_tensor(out=gs_t[mb], in0=pg,
                                                                                    in1=gbt[:, ob, :], op=ALU.add),
                               [tb[bgi], t_tab], [t_gs[mb]])
                        P.emit(P.dve, lambda e, mb=mb: e.max(out=m8_t[mb], in_=gs_t[mb]), [t_gs[mb]], [t_m8[mb]])
                        P.emit(P.dve, lambda e, mb=mb, ob=ob, h=h: e.scalar_tensor_tensor(
                            out=tmp_t[mb], in0=gs_t[mb], scalar=m8_t[mb][:, 2:3], in1=a2t[:, ob, h, :], op0=ALU.is_ge,
                            op1=ALU.mult), [t_gs[mb], t_m8[mb], t_tab], [t_tmp[mb]])
                        P.emit(P.dve, lambda e, mb=mb, ob=ob: e.tensor_tensor(out=hiv_t[mb], in0=tmp_t[mb],
                                                                             in1=b2t[:, ob, :], op=ALU.add),
                               [t_tmp[mb], t_tab], [t_hiv[mb]])
                        bti = nbank()
                        ptv = bf_view(bti)[0:64, 0:128]
                        P.emit(P.pe, lambda e, ptv=ptv, mb=mb: e.transpose(out=ptv, in_=hiv_t[mb], identity=identb),
                               [t_hiv[mb]], [tb[bti]])
                        P.emit(P.act, lambda e, ptv=ptv, h=h, t=t: e.copy(out=VT[0:64, h, t * 128:(t + 1) * 128], in_=ptv),
                               [tb[bti]], [t_vt64])
            for nm, dst, tdst in (("gc", gcs, t_gcs), ("ga", gas, t_gas)):
                for u in range(2):
                    w_, tw = w_next("%s%d" % (nm, u))
                    for ci in range(4):
                        cc = 4 * u + ci
                        bi = nbank()
                        for ob in range(2):
                            mm_group(banks[bi][:, ob * 256:(ob + 1) * 256], bi,
                                     [(w_[:, kc, ci * 128:(ci + 1) * 128], xnT[:, kc, ob * 288 + 32:ob * 288 + 288])
                                      for kc in range(8)], [tw, t_xnT])
                        P.emit(P.act, lambda e, bi=bi, dst=dst, cc=cc: e.activation(out=dst[:, cc, :], in_=banks[bi][:, :],
                                                                                   func=AF.Sigmoid), [tb[bi]], [tdst])
            def conv_cc(cc):
                yv = y_t[:, cc, :].rearrange("p (o l) -> p o l", l=256)

                def av(j, cc=cc):
                    return a_t[:, cc, :].rearrange("p (o l) -> p o l", l=288)[:, :, 2 + j:2 + j + 256]
                P.emit(P.dve, lambda e, yv=yv, av=av, cc=cc: e.tensor_scalar(out=yv, in0=av(0), scalar1=cw[:, cc, 0:1],
                                                                            scalar2=cb[:, cc:cc + 1], op0=ALU.mult,
                                                                            op1=ALU.add), [t_a], [t_y])
                for j in range(1, 31):
                    P.emit(P.dve, lambda e, yv=yv, av=av, cc=cc, j=j: e.scalar_tensor_tensor(
                        out=yv, in0=av(j), scalar=cw[:, cc, j:j + 1], in1=yv, op0=ALU.mult, op1=ALU.add), [t_a, t_y], [t_y])

            def normalize_head(h):
                for t in range(4):
                    ob_i = 4 + t
                    o_ap = banks[ob_i][:, 0:129]
                    mb = t % 2
                    P.emit(P.dve, lambda e, o_ap=o_ap, mb=mb: e.reciprocal(out=rl_t[mb], in_=o_ap[:, 128:129]),
                           [tb[ob_i]], [t_rl[mb]])
                    P.emit(P.dve, lambda e, o_ap=o_ap, mb=mb: e.tensor_scalar(out=atok[mb], in0=o_ap[:, 0:128],
                                                                              scalar1=rl_t[mb][:, 0:1], scalar2=None,
                                                                              op0=ALU.mult),
                           [tb[ob_i], t_rl[mb]], [t_atok[mb]])
                    bti = (srot[0] - 3) % 4 if t % 2 == 0 else srot[0] % 4
                    ptv = bf_view(bti)[:, 0:128]
                    P.emit(P.pe, lambda e, ptv=ptv, mb=mb: e.transpose(out=ptv, in_=atok[mb], identity=identb),
                           [t_atok[mb]], [tb[bti]])
                    P.emit(P.act, lambda e, ptv=ptv, h=h, t=t: e.copy(out=attT[:, h, t * 128:(t + 1) * 128], in_=ptv),
                           [tb[bti]], [t_attT])

            nblk = 8 * g + 8
            npp = nblk // 8
            units = [(h, n, kh) for h in range(NH) for n in range(nblk) for kh in range(2)]
            piece_list = [(h, n0) for h in range(NH) for n0 in range(0, nblk, 8)]
            piece_slot = {}
            piece_issued = [0]

            def ensure_piece(k):
                while piece_issued[0] <= k and piece_issued[0] < len(piece_list):
                    h_, n0_ = piece_list[piece_issued[0]]
                    s_ = kvstate["n"] % NKV
                    kvstate["n"] += 1
                    P.dma(P.pool, ktp[s_], kt_d[h_, :, n0_ * 256:(n0_ + 8) * 256], [t_kt], [t_ktp[s_]], dq_ktp[s_])
                    P.dma(P.pool, vp[s_], vv_d[:, n0_:n0_ + 8, h_, :, :], [t_vv], [t_vp[s_]], dq_vp[s_])
                    piece_slot[piece_issued[0]] = s_
                    piece_issued[0] += 1

            LA = 2
            srot = [0]
            ensure_piece(1)
            st = {}
            for idx in range(len(units) + LA):
                if idx < len(units):
                    h, n, kh = units[idx]
                    s_ = piece_slot[h * npp + n // 8]
                    nl = n % 8
                    bi = srot[0]
                    srot[0] = (srot[0] + 1) % 4
                    S_ = banks[bi][:, :]
                    cand = n >= 8 * g
                    P.emit(P.pe, lambda e, S_=S_, s_=s_, nl=nl, kh=kh, h=h: e.matmul(
                        out=S_, lhsT=ktp[s_][:, nl * 256 + kh:nl * 256 + 256:2], rhs=qT[:, h, :], start=True,
                        stop=False), [t_ktp[s_], t_qT], [tb[bi]], ms=False)
                    P.emit(P.pe, lambda e, S_=S_, n=n, h=h, cand=cand: e.matmul(
                        out=S_, lhsT=esel_lhsT(n), rhs=VT[0:65, h, :], start=False, stop=(not cand)),
                        [t_vt64], [tb[bi]], ms=(not cand))
                    if cand:
                        ob = (n - 8 * g) // 4
                        cnd = (n - 8 * g) % 4
                        P.emit(P.pe, lambda e, S_=S_, ob=ob, cnd=cnd, kh=kh: e.matmul(
                            out=S_[:, ob * 256:(ob + 1) * 256], lhsT=identb, rhs=cms[:, cnd, kh, :], start=False,
                            stop=True), [], [tb[bi]], ms=True)
                    st[idx] = (bi, s_)
                j = idx - LA
                if j >= 0:
                    h, n, kh = units[j]
                    bi, s_ = st.pop(j)
                    S_ = banks[bi][:, :]
                    nl = n % 8
                    if n % 8 == 0 and kh == 0:
                        ensure_piece(h * npp + n // 8 + 1)
                    first = (n == 0 and kh == 0)
                    lastblk = (n == nblk - 1 and kh == 1)
                    r_ = ptstate["n"] % NPT
                    ptstate["n"] += 1
                    P.emit(P.act, lambda e, S_=S_, r_=r_, h=h, kh=kh: e.activation(
                        out=PT[r_], in_=S_, func=AF.Exp, bias=kb[:, 2 * h + kh:2 * h + kh + 1]),
                        [tb[bi]], [t_PT[r_]])
                    for t in range(4):
                        ob_i = 4 + t
                        o_ap = banks[ob_i][:, 0:129]
                        P.emit(P.pe, lambda e, o_ap=o_ap, r_=r_, t=t, s_=s_, nl=nl, kh=kh, first=first,
                               lastblk=lastblk: e.matmul(out=o_ap, lhsT=PT[r_][:, t * 128:(t + 1) * 128],
                                                         rhs=vp[s_][:, nl, kh, :], start=first, stop=lastblk),
                               [t_PT[r_], t_vp[s_]], [tb[ob_i]], ms=(t == 3))
                    if lastblk:
                        normalize_head(h)
                        conv_cc(h)
            bs1 = nbank()
            bs2 = nbank()
            for cc in range(8):
                sb = cc % 2
                P.emit(P.act, lambda e, sb=sb, cc=cc: e.copy(out=ybf[sb], in_=y_t[:, cc, :]), [t_y], [t_ybf[sb]])
                P.emit(P.act, lambda e, sb=sb, cc=cc: e.activation(out=ysq[sb], in_=y_t[:, cc, :], func=AF.Square),
                       [t_y], [t_ysq[sb]])
                P.emit(P.pe, lambda e, sb=sb, cc=cc, bs1=bs1: e.matmul(out=banks[bs1][:, :], lhsT=onesb, rhs=ybf[sb],
                                                              start=(cc == 0), stop=(cc == 7)),
                       [t_ybf[sb]], [tb[bs1]], ms=True)
                P.emit(P.pe, lambda e, sb=sb, cc=cc, bs2=bs2: e.matmul(out=banks[bs2][:, :], lhsT=onesb, rhs=ysq[sb],
                                                              start=(cc == 0), stop=(cc == 7)),
                       [t_ysq[sb]], [tb[bs2]], ms=True)
            P.emit(P.dve, lambda e, bs1=bs1: e.tensor_scalar(out=mean_t, in0=banks[bs1][:, :], scalar1=1.0 / D, scalar2=None,
                                                    op0=ALU.mult), [tb[bs1]], [t_mean])
            P.emit(P.dve, lambda e: e.tensor_tensor(out=rstd_t, in0=mean_t, in1=mean_t, op=ALU.mult), [t_mean], [t_rstd])
            P.emit(P.dve, lambda e, bs2=bs2: e.scalar_tensor_tensor(out=rstd_t, in0=banks[bs2][:, :], scalar=1.0 / D, in1=rstd_t,
                                                           op0=ALU.mult, op1=ALU.subtract), [tb[bs2], t_rstd], [t_rstd])
            P.emit(P.act, lambda e: e.activation(out=rstd_t, in_=rstd_t, func=AF.Ln, bias=epsc), [t_rstd], [t_rstd])
            P.emit(P.act, lambda e: e.activation(out=rstd_t, in_=rstd_t, func=AF.Exp, scale=-0.5), [t_rstd], [t_rstd])
            for cc in range(8):
                sb = cc % 2
                P.emit(P.dve, lambda e, sb=sb, cc=cc: e.tensor_tensor(out=xc[sb], in0=y_t[:, cc, :], in1=mean_t,
                                                                      op=ALU.subtract), [t_y, t_mean], [t_xc[sb]])
                P.emit(P.dve, lambda e, sb=sb: e.tensor_tensor(out=xc[sb], in0=xc[sb], in1=rstd_t, op=ALU.mult),
                       [t_xc[sb], t_rstd], [t_xc[sb]])
                P.emit(P.act, lambda e, sb=sb, cc=cc: e.activation(out=ysl[:, cc, :], in_=xc[sb], func=AF.Silu,
                                                                   scale=lg[:, cc:cc + 1], bias=lb[:, cc:cc + 1]),
                       [t_xc[sb]], [t_ysl])
            for u in range(2):
                w_, tw = w_next("co%d" % u)
                for ci in range(4):
                    cc = 4 * u + ci
                    bi = nbank()
                    mm_group(banks[bi][:, :], bi, [(w_[:, kc, ci * 128:(ci + 1) * 128], ysl[:, kc, :]) for kc in range(8)],
                             [tw, t_ysl])
                    P.emit(P.dve, lambda e, bi=bi, cc=cc: e.tensor_tensor(out=t1[:, cc, :], in0=banks[bi][:, :],
                                                                          in1=gcs[:, cc, :], op=ALU.mult),
                           [tb[bi], t_gcs], [t_t1])
            for u in range(2):
                w_, tw = w_next("ao%d" % u)
                for ci in range(4):
                    cc = 4 * u + ci
                    bi = nbank()
                    sb = cc % 2
                    mm_group(banks[bi][:, :], bi, [(w_[:, kc, ci * 128:(ci + 1) * 128], attT[:, kc, :]) for kc in range(8)],
                             [tw, t_attT])
                    P.emit(P.dve, lambda e, bi=bi, cc=cc, sb=sb: e.tensor_tensor(out=tm2[sb], in0=banks[bi][:, :],
                                                                                 in1=gas[:, cc, :], op=ALU.mult),
                           [tb[bi], t_gas], [t_tm2[sb]])
                    P.emit(P.dve, lambda e, cc=cc, sb=sb: e.tensor_tensor(out=mT[:, cc, :], in0=tm2[sb], in1=t1[:, cc, :],
                                                                          op=ALU.add), [t_tm2[sb], t_t1], [t_mT])
            for hf in range(2):
                w_, tw = w_next("wo%d" % hf)
                for t in range(4):
                    bi = nbank()
                    mm_group(banks[bi][:, :], bi, [(mT[:, kc, t * 128:(t + 1) * 128], w_[:, kc, :]) for kc in range(8)],
                             [tw, t_mT])
                    P.emit(P.dve, lambda e, bi=bi, t=t, hf=hf: e.tensor_tensor(
                        out=xin[:, t, hf * 512:(hf + 1) * 512], in0=banks[bi][:, :], in1=xin[:, t, hf * 512:(hf + 1) * 512],
                        op=ALU.add), [tb[bi], t_xin], [t_xin])
            for t in range(4):
                xb = t % 2
                norm_tile(xin[:, t, :], 128, t_xin, ssq2[:, t:t + 1], rinv2[:, t:t + 1], t_ssq2, t_rinv2, xn[xb], t_xn[xb])
                transpose_tile(xn[xb], 128, t_xn[xb], hnT[:, :, t * 128:(t + 1) * 128], t_hnT, use_act=(t % 2 == 0))
            for u in range(11):
                w_, tw = w_next("gu%d" % u)
                for ci in range(2):
                    fc = 2 * u + ci
                    sb = fc % 2
                    bgt = nbank()
                    mm_group(banks[bgt][:, :], bgt, [(w_[:, kc, ci * 128:(ci + 1) * 128], hnT[:, kc, :]) for kc in range(8)],
                             [tw, t_hnT])
                    but = nbank()
                    mm_group(banks[but][:, :], but,
                             [(w_[:, kc, 256 + ci * 128:256 + (ci + 1) * 128], hnT[:, kc, :]) for kc in range(8)],
                             [tw, t_hnT])
                    P.emit(P.act, lambda e, bgt=bgt, sb=sb: e.activation(out=sg_t[sb], in_=banks[bgt][:, :], func=AF.Silu),
                           [tb[bgt]], [t_sg[sb]])
                    P.emit(P.dve, lambda e, but=but, sb=sb, fc=fc: e.tensor_tensor(out=act_t[:, fc, :], in0=banks[but][:, :],
                                                                                   in1=sg_t[sb], op=ALU.mult),
                           [tb[but], t_sg[sb]], [t_actt, t_a, t_y])
            for hf in range(2):
                for gi, (f0, nf) in enumerate(((0, 8), (8, 8), (16, 6))):
                    w_, tw = w_next("dn%d_%d" % (hf, gi))
                    for t in range(4):
                        bi = 4 + t
                        for fl in range(nf):
                            fc = f0 + fl
                            P.emit(P.pe, lambda e, bi=bi, t=t, fc=fc, fl=fl, w_=w_: e.matmul(
                                out=banks[bi][:, :], lhsT=act_t[:, fc, t * 128:(t + 1) * 128], rhs=w_[:, fl, :],
                                start=(fc == 0), stop=(fc == FC - 1)), [tw, t_actt], [tb[bi]], ms=(fl == nf - 1))
                for t in range(4):
                    bi = 4 + t
                    P.emit(P.dve, lambda e, bi=bi, t=t, hf=hf: e.tensor_tensor(
                        out=xin[:, t, hf * 512:(hf + 1) * 512], in0=banks[bi][:, :], in1=xin[:, t, hf * 512:(hf + 1) * 512],
                        op=ALU.add), [tb[bi], t_xin], [t_xin])
            P.dma(P.pool, out_d[g * 512:(g + 1) * 512, :].rearrange("(t p) d -> p t d", p=128), xin, [t_xin], [t_outd],
                  dq_out)
            t_a.r += t_actt.r
            t_y.r += t_actt.r
            if t_actt.w is not None:
                t_a.r.append(t_actt.w)
                t_y.r.append(t_actt.w)
        P.wait_all(P.pool, [t_outd])
        P.wait_all(P.sp, [t_outd])
        P.run()
    return nc


def host_tables(j):
    slopes = 2.0 ** (-8.0 * np.arange(1, NH + 1) / NH)
    gate_bias = np.zeros((16, 64), np.float32)
    a2 = np.zeros((16, 8, 64), np.float32)
    b2 = np.full((16, 64), NEG, np.float32)
    for i in range(16):
        cur = 4 * i + j
        gate_bias[i, cur:] = -1e30
        b2[i, cur] = 0.0
        for n in range(cur):
            a2[i, :, n] = -NEG - slopes * 256.0 * (cur - n)
    cmsel = np.zeros((128, 4, 2, 256), np.float32)
    p = np.arange(128)[:, None]
    rq = np.arange(256)[None, :]
    for kh in range(2):
        rk = 2 * p + kh
        cmsel[:, j, kh, :] = np.where(rq >= rk, 0.0, NEG)
    lo = np.zeros((1, 8, 512), np.float32)
    for h in range(8):
        lo[0, h, :] = -slopes[h] * (np.arange(512) % 256)
    kbias = np.zeros((128, 16), np.float32)
    for h in range(8):
        for kh in range(2):
            kbias[:, 2 * h + kh] = slopes[h] * (2 * np.arange(128) + kh)
    return {"gate_bias": gate_bias, "a2": a2, "b2": b2, "cmsel": cmsel.reshape(128, -1),
            "lo": lo.reshape(1, -1), "kbias": kbias, "ident": np.eye(128, dtype=np.float32)}


def make_in_maps(inputs):
    x = np.asarray(inputs["x"], np.float32)
    shared = {
        "w_in": np.ascontiguousarray(inputs["w_in"][0]), "w_conv_out": np.ascontiguousarray(inputs["w_conv_out"][0]),
        "w_attn_out": np.ascontiguousarray(inputs["w_attn_out"][0]), "w_out": np.ascontiguousarray(inputs["w_out"][0]),
        "w_ffn_gate": np.ascontiguousarray(inputs["w_ffn_gate"][0]), "w_ffn_up": np.ascontiguousarray(inputs["w_ffn_up"][0]),
        "w_ffn_down": np.ascontiguousarray(inputs["w_ffn_down"][0]),
        "norm1_g": np.asarray(inputs["norm1_g"], np.float32).reshape(1, D),
        "norm2_g": np.asarray(inputs["norm2_g"], np.float32).reshape(1, D),
        "dw_w": np.ascontiguousarray(inputs["dw_w"][0]), "dw_b": np.asarray(inputs["dw_b"], np.float32).reshape(1, D),
        "conv_ln_g": np.asarray(inputs["conv_ln_g"], np.float32).reshape(1, D),
        "conv_ln_b": np.asarray(inputs["conv_ln_b"], np.float32).reshape(1, D),
        "q_norm_g": np.asarray(inputs["q_norm_g"], np.float32).reshape(1, DH),
        "k_norm_g": np.asarray(inputs["k_norm_g"], np.float32).reshape(1, DH),
    }
    shared = {k: np.asarray(v, np.float32) for k, v in shared.items()}
    maps = []
    for c in range(8):
        b, j = c // 4, c % 4
        xb = x[b]
        xblk = xb.reshape(NBK, L, D)
        own = [4 * i + j for i in range(16)]
        x_own = np.ascontiguousarray(xblk[own].reshape(4096, D))
        halo = np.zeros((16, 32, D), np.float32)
        for i, n in enumerate(own):
            if n > 0:
                halo[i] = xb[n * L - 32:n * L]
        m = dict(shared)
        m.update(host_tables(j))
        m["x_all"] = np.ascontiguousarray(xb)
        m["x_own"] = x_own
        m["x_halo"] = halo.reshape(512, D)
        maps.append(m)
    return maps


_NC_CACHE = {}


def kernel(**inputs):
    if "nc" not in _NC_CACHE:
        _NC_CACHE["nc"] = build_nc()
    nc = _NC_CACHE["nc"]
    maps = make_in_maps(inputs)
    res = run_bass_kernel_spmd(nc, maps, core_ids=list(range(8)))
    out = np.zeros((2, S, D), np.float32)
    ov = out.reshape(2, NBK, L, D)
    for c in range(8):
        b, j = c // 4, c % 4
        o = np.asarray(res.results[c]["out_own"], np.float32).reshape(16, L, D)
        for i in range(16):
            ov[b, 4 * i + j] = o[i]
    return out
```

```python
import numpy as np
from contextlib import ExitStack
import concourse.bass as bass
import concourse.mybir as mybir
from concourse.bass_utils import run_bass_kernel_spmd

F32 = mybir.dt.float32
BF16 = mybir.dt.bfloat16
AF = mybir.ActivationFunctionType
ALU = mybir.AluOpType
AX = mybir.AxisListType

D = 1024
KC = 8
NH = 8
DH = 128
FF = 2816
FC = 22
S = 16384
L = 256
NBK = 64
EPS = 1e-6
NEG = -30000.0


class T:
    __slots__ = ("name", "w", "r")

    def __init__(self, name):
        self.name = name
        self.w = None
        self.r = []


class Q:
    def __init__(self, name, sem, scale=1):
        self.name = name
        self.sem = sem
        self.scale = scale
        self.count = 0
        self.ops = []
        self.seen = {}


class Prog:
    def __init__(self, nc, es):
        self.nc = nc
        self.es = es
        self.pe = Q("pe", self.newsem("s_pe"))
        self.act = Q("act", self.newsem("s_act"))
        self.dve = Q("dve", self.newsem("s_dve"))
        self.pool = Q("pool", self.newsem("s_pool"))
        self.sp = Q("sp", self.newsem("s_sp"))
        self.engines = [self.pe, self.act, self.dve, self.pool, self.sp]

    def newsem(self, name):
        return self.es.enter_context(self.nc.semaphore(name))

    def dmaq(self, name):
        return Q(name, self.newsem("d_" + name), 16)

    def _wait(self, q, tok):
        sq, cnt = tok
        if q.seen.get(sq, 0) >= cnt:
            return
        q.seen[sq] = cnt
        q.ops.append(lambda e, s=sq.sem, v=cnt * sq.scale: e.wait_ge(s, v))

    def emit(self, q, fn, reads=(), writes=(), ms=True, sig=None):
        sig = sig or q
        for t in reads:
            if t.w is not None:
                self._wait(q, t.w)
        for t in writes:
            if t.w is not None and t.w[0] is not q:
                self._wait(q, t.w)
            for tok in t.r:
                if tok[0] is not q:
                    self._wait(q, tok)
        if ms:
            sig.count += 1
            tok = (sig, sig.count)
            q.ops.append(lambda e, s=sig.sem, v=sig.scale: fn(e).then_inc(s, v))
        else:
            tok = (sig, sig.count + 1)
            q.ops.append(lambda e: fn(e))
        for t in writes:
            t.w = tok
            t.r = []
        for t in reads:
            t.r.append(tok)
            if len(t.r) > 16:
                best = {}
                for sq, c in t.r:
                    if sq not in best or best[sq] < c:
                        best[sq] = c
                t.r = list(best.items())
        return tok

    def dma(self, q, out, in_, reads, writes, sig, **kw):
        return self.emit(q, lambda e: e.dma_start(out=out, in_=in_, **kw), reads, writes, sig=sig)

    def wait_all(self, q, ts):
        for t in ts:
            if t.w is not None:
                self._wait(q, t.w)
            for tok in t.r:
                self._wait(q, tok)

    def barrier(self, ts):
        for q in self.engines:
            self.wait_all(q, ts)

    def run(self):
        nc = self.nc
        with nc.Block() as block:
            @block.tensor
            def _(e):
                for op in self.pe.ops:
                    op(e)

            @block.scalar
            def _(e):
                for op in self.act.ops:
                    op(e)

            @block.vector
            def _(e):
                for op in self.dve.ops:
                    op(e)

            @block.gpsimd
            def _(e):
                for op in self.pool.ops:
                    op(e)

            @block.sync
            def _(e):
                for op in self.sp.ops:
                    op(e)


class Arena:
    def __init__(self, ap, nwords):
        self.ap = ap
        self.n = nwords
        self.off = 0

    def alloc(self, shape, dtype):
        shape = list(shape)
        np_ = shape[0]
        free = 1
        for s_ in shape[1:]:
            free *= s_
        esz = 4 if dtype == F32 else 2
        words = (free * esz + 3) // 4
        words = (words + 7) // 8 * 8
        assert self.off + words <= self.n, ("arena overflow", self.off, words, self.n)
        v = self.ap[0:np_, self.off:self.off + words]
        self.off += words
        if dtype != F32:
            v = v.bitcast(dtype)
        v = v[:, 0:free]
        if len(shape) == 2:
            return v
        names = " ".join("d%d" % i for i in range(len(shape) - 1))
        kw = {"d%d" % i: shape[i + 1] for i in range(len(shape) - 1)}
        return v.rearrange("p (%s) -> p %s" % (names, names), **kw)


def unit_table():
    u = {}
    for i in range(4):
        u["cv%d" % i] = [("w_in", 0, 8, (2 * i) * 128, 128, 0, "g1"), ("w_in", 0, 8, (2 * i + 1) * 128, 128, 128, "g1"),
                         ("w_in", 0, 8, 1024 + (2 * i) * 128, 128, 256, "g1"),
                         ("w_in", 0, 8, 1024 + (2 * i + 1) * 128, 128, 384, "g1")]
    for nm, c0 in (("q", 2048), ("k", 3072), ("v", 4096), ("gc", 5120), ("ga", 6144)):
        for i in range(2):
            u["%s%d" % (nm, i)] = [("w_in", 0, 8, c0 + i * 512, 512, 0, "g1")]
    for nm, src in (("co", "w_conv_out"), ("ao", "w_attn_out"), ("wo", "w_out")):
        for i in range(2):
            u["%s%d" % (nm, i)] = [(src, 0, 8, i * 512, 512, 0, None)]
    for i in range(11):
        u["gu%d" % i] = [("w_ffn_gate", 0, 8, (2 * i) * 128, 128, 0, "g2"),
                         ("w_ffn_gate", 0, 8, (2 * i + 1) * 128, 128, 128, "g2"),
                         ("w_ffn_up", 0, 8, (2 * i) * 128, 128, 256, "g2"),
                         ("w_ffn_up", 0, 8, (2 * i + 1) * 128, 128, 384, "g2")]
    for hf in range(2):
        for gi, (f0, nf) in enumerate(((0, 8), (8, 8), (16, 6))):
            u["dn%d_%d" % (hf, gi)] = [("w_ffn_down", f0 * 128, nf, hf * 512, 512, 0, None)]
    return u


UNITS = unit_table()
UNAMES = list(UNITS.keys())
UIDX = {n: i for i, n in enumerate(UNAMES)}
NU = len(UNAMES)
CHUNK_SEQ = (["cv%d" % i for i in range(4)] + ["q0", "gc0", "q1", "gc1", "ga0", "ga1", "ao0", "ao1", "co0", "co1",
                                               "wo0", "wo1"] + ["gu%d" % i for i in range(11)] +
             ["dn0_0", "dn0_1", "dn0_2", "dn1_0", "dn1_1", "dn1_2"])


def build_nc(nch=8, nkv=32):
    nc = bass.Bass("TRN2", target_bir_lowering=False)

    def din(name, shape):
        return nc.dram_tensor(name, list(shape), F32, kind="ExternalInput").ap()

    x_all = din("x_all", [S, D])
    x_own = din("x_own", [4096, D])
    x_halo = din("x_halo", [512, D])
    gate_bias = din("gate_bias", [16, 64])
    a2_d = din("a2", [16, 8, 64])
    b2_d = din("b2", [16, 64])
    cms_d = din("cmsel", [128, 4 * 2 * 256])
    lo_d = din("lo", [1, 8 * 512])
    kb_d = din("kbias", [128, 16])
    id_d = din("ident", [128, 128])
    wsrc = {
        "w_in": din("w_in", [D, 7168]), "w_conv_out": din("w_conv_out", [D, D]), "w_attn_out": din("w_attn_out", [D, D]),
        "w_out": din("w_out", [D, D]), "w_ffn_gate": din("w_ffn_gate", [D, FF]), "w_ffn_up": din("w_ffn_up", [D, FF]),
        "w_ffn_down": din("w_ffn_down", [FF, D]),
    }
    norm1_g = din("norm1_g", [1, D])
    norm2_g = din("norm2_g", [1, D])
    dw_w = din("dw_w", [31, D])
    dw_b = din("dw_b", [1, D])
    ln_g = din("conv_ln_g", [1, D])
    ln_b = din("conv_ln_b", [1, D])
    qg_d = din("q_norm_g", [1, DH])
    kg_d = din("k_norm_g", [1, DH])
    out_d = nc.dram_tensor("out_own", [4096, D], F32, kind="ExternalOutput").ap()
    wsc = nc.dram_tensor("wsc", [NU, 128, 4096], BF16).ap()
    kt_d = nc.dram_tensor("kt_s", [NH, 128, S], BF16).ap()
    vv_d = nc.dram_tensor("vv_s", [128, NBK, NH, 2, 129], BF16).ap()

    with ExitStack() as es:
        P = Prog(nc, es)
        NW = 53200
        arena_t = es.enter_context(nc.sbuf_tensor("arena", [128, NW], F32))
        AR = Arena(arena_t[:, :], NW)
        banks = [es.enter_context(nc.psum_tensor("pb%d" % i, [128, 512], F32)) for i in range(8)]
        tb = [T("pb%d" % i) for i in range(8)]
        rot = [0]

        held = set()

        def nbank():
            while rot[0] in held:
                rot[0] = (rot[0] + 1) % 8
            i = rot[0]
            rot[0] = (rot[0] + 1) % 8
            return i

        def bf_view(i):
            return banks[i][:, :].bitcast(BF16)

        identf = AR.alloc([128, 128], F32)
        identb = AR.alloc([128, 128], BF16)
        onesb = AR.alloc([128, 128], BF16)
        esel = AR.alloc([65, 64], BF16)
        cms = AR.alloc([128, 4, 2, 256], BF16)
        kb = AR.alloc([128, 16], F32)
        kmean = AR.alloc([128, 8, 64], F32)
        kmean_b = AR.alloc([128, 8, 64], BF16)
        VT = AR.alloc([65, 8, 512], BF16)
        g1 = AR.alloc([128, 8], F32)
        g2 = AR.alloc([128, 8], F32)
        cw = AR.alloc([128, 8, 31], F32)
        cb = AR.alloc([128, 8], F32)
        lg = AR.alloc([128, 8], F32)
        lb = AR.alloc([128, 8], F32)
        qg = AR.alloc([128, 1], F32)
        kg = AR.alloc([128, 1], F32)
        epsc = AR.alloc([128, 1], F32)
        t_const = T("const")
        t_kmean = T("kmean")
        t_vt64 = T("vt64")
        dq_c = P.dmaq("const")
        const_mark = AR.off

        cms_f = AR.alloc([128, 2048], F32)
        lo_f = AR.alloc([65, 4096], F32)
        t_cst = T("cst")
        pq = P.pool
        P.dma(pq, identf, id_d, [], [t_cst], dq_c)
        P.dma(pq, cms_f, cms_d, [], [t_cst], dq_c)
        P.dma(pq, lo_f[64:65, :], lo_d, [], [t_cst], dq_c)
        P.dma(pq, kb, kb_d, [], [t_cst], dq_c)
        P.dma(pq, g1, norm1_g[0].rearrange("(kc p) -> p kc", p=128), [], [t_cst], dq_c, allow_slow_non_contiguous=True)
        P.dma(pq, g2, norm2_g[0].rearrange("(kc p) -> p kc", p=128), [], [t_cst], dq_c, allow_slow_non_contiguous=True)
        P.dma(pq, cb, dw_b[0].rearrange("(kc p) -> p kc", p=128), [], [t_cst], dq_c, allow_slow_non_contiguous=True)
        P.dma(pq, lg, ln_g[0].rearrange("(kc p) -> p kc", p=128), [], [t_cst], dq_c, allow_slow_non_contiguous=True)
        P.dma(pq, lb, ln_b[0].rearrange("(kc p) -> p kc", p=128), [], [t_cst], dq_c, allow_slow_non_contiguous=True)
        for kc_ in range(8):
            P.dma(pq, cw[:, kc_, :], dw_w[:, kc_ * 128:(kc_ + 1) * 128].rearrange("j p -> p j"), [], [t_cst], dq_c,
                  allow_slow_non_contiguous=True)
        P.dma(pq, qg, qg_d.rearrange("o p -> p o"), [], [t_cst], dq_c, allow_slow_non_contiguous=True)
        P.dma(pq, kg, kg_d.rearrange("o p -> p o"), [], [t_cst], dq_c, allow_slow_non_contiguous=True)
        dv = P.dve
        P.emit(dv, lambda e: e.tensor_copy(out=identb, in_=identf), [t_cst], [t_const])
        P.emit(dv, lambda e: e.memset(onesb, 1.0), [], [t_const])
        P.emit(dv, lambda e: e.memset(kmean.rearrange("p a b -> p (a b)"), 0.0), [], [t_const])
        P.emit(dv, lambda e: e.memset(epsc, EPS), [], [t_const])
        P.emit(dv, lambda e: e.tensor_copy(out=esel[0:64, :], in_=identf[0:64, 0:64]), [t_cst], [t_const])
        P.emit(dv, lambda e: e.memset(esel[64:65, :], 1.0), [], [t_const])
        P.emit(dv, lambda e: e.tensor_copy(out=cms.rearrange("p a b c -> p (a b c)"), in_=cms_f), [t_cst], [t_const])
        P.emit(dv, lambda e: e.tensor_copy(out=VT[64:65, :, :].rearrange("p a b -> p (a b)"), in_=lo_f[64:65, :]),
               [t_cst], [t_const])
        P.emit(dv, lambda e: e.tensor_scalar(out=qg, in0=qg, scalar1=float(DH) ** -0.5, scalar2=None, op0=ALU.mult),
               [t_cst], [t_const])
        P.barrier([t_cst, t_const])
        AR.off = const_mark
        phase_mark = AR.off

        def esel_lhsT(n):
            a = esel[0:65, n:n + 1]
            return bass.AP(a.tensor, a.offset, [[a.ap[0][0], 65], [0, 128]])

        stg = [AR.alloc([128, 8, 512], F32) for _ in range(2)]
        wbf = [AR.alloc([128, 8, 512], BF16) for _ in range(2)]
        t_stg = [T("stg0"), T("stg1")]
        t_wbf = [T("wbf0"), T("wbf1")]
        dq_stg = [P.dmaq("stg0"), P.dmaq("stg1")]
        dq_wbf = [P.dmaq("wbf0"), P.dmaq("wbf1")]
        t_unit = [T("unit%d" % i) for i in range(NU)]
        gains = {"g1": g1, "g2": g2}
        kv_units = ["k0", "k1", "v0", "v1"]
        conv_order = kv_units + [n_ for n_ in UNAMES if n_ not in kv_units]

        def conv_load(oi):
            nm = conv_order[oi]
            b = oi % 2
            for (src, row0, nk_, c0, ncol, dst, _g) in UNITS[nm]:
                sap = wsrc[src][row0:row0 + nk_ * 128, c0:c0 + ncol].rearrange("(kc p) n -> p kc n", p=128)
                P.dma(P.sp, stg[b][:, 0:nk_, dst:dst + ncol], sap, [], [t_stg[b]], dq_stg[b])

        def conv_cast_store(oi, act_only):
            nm = conv_order[oi]
            ui = UIDX[nm]
            b = oi % 2
            segs = UNITS[nm]
            nk = segs[0][2]
            gname = segs[0][6]
            for kc in range(nk):
                use_dve = (kc % 2 == 0) and not act_only
                if gname is None:
                    if use_dve:
                        P.emit(P.dve, lambda e, b=b, kc=kc: e.tensor_copy(out=wbf[b][:, kc, :], in_=stg[b][:, kc, :]),
                               [t_stg[b]], [t_wbf[b]])
                    else:
                        P.emit(P.act, lambda e, b=b, kc=kc: e.copy(out=wbf[b][:, kc, :], in_=stg[b][:, kc, :]),
                               [t_stg[b]], [t_wbf[b]])
                else:
                    gt = gains[gname]
                    if use_dve:
                        P.emit(P.dve, lambda e, b=b, kc=kc, gt=gt: e.tensor_scalar(
                            out=wbf[b][:, kc, :], in0=stg[b][:, kc, :], scalar1=gt[:, kc:kc + 1], scalar2=None,
                            op0=ALU.mult), [t_stg[b]], [t_wbf[b]])
                    else:
                        P.emit(P.act, lambda e, b=b, kc=kc, gt=gt: e.activation(
                            out=wbf[b][:, kc, :], in_=stg[b][:, kc, :], func=AF.Copy, scale=gt[:, kc:kc + 1]),
                            [t_stg[b]], [t_wbf[b]])
            P.dma(P.sp, wsc[ui, :, 0:nk * 512], wbf[b][:, 0:nk, :].rearrange("p a b -> p (a b)"), [t_wbf[b]],
                  [t_unit[ui]], dq_wbf[b])

        conv_load(0)
        conv_load(1)
        for oi in range(4):
            conv_cast_store(oi, act_only=False)
            conv_load(oi + 2)
        conv_next = [4]

        wk = AR.alloc([128, 8, 1024], BF16)
        wv = AR.alloc([128, 8, 1024], BF16)
        t_wk = T("wk")
        t_wv = T("wv")
        dq_wk = P.dmaq("wk")
        dq_wv = P.dmaq("wv")
        for i in range(2):
            P.dma(P.pool, wk[:, :, i * 512:(i + 1) * 512], wsc[UIDX["k%d" % i]].rearrange("p (a b) -> p a b", b=512),
                  [t_unit[UIDX["k%d" % i]]], [t_wk], dq_wk)
            P.dma(P.pool, wv[:, :, i * 512:(i + 1) * 512], wsc[UIDX["v%d" % i]].rearrange("p (a b) -> p a b", b=512),
                  [t_unit[UIDX["v%d" % i]]], [t_wv], dq_wv)
        xin1 = [AR.alloc([128, 4, 1024], F32) for _ in range(2)]
        t_xin1 = [T("xin1_0"), T("xin1_1")]
        dq_xin1 = [P.dmaq("xin1_0"), P.dmaq("xin1_1")]
        junk_cur = [AR.alloc([128, 1024], BF16)]
        t_junk = T("junk")
        ssq = [AR.alloc([128, 4], F32) for _ in range(2)]
        t_ssq = [T("ssq0"), T("ssq1")]
        rinv = [AR.alloc([128, 4], F32) for _ in range(2)]
        t_rinv = [T("rinv0"), T("rinv1")]
        xn = [AR.alloc([128, 1024], BF16) for _ in range(2)]
        t_xn = [T("xn0"), T("xn1")]
        xnT1 = [AR.alloc([128, 8, 512], BF16) for _ in range(2)]
        t_xnT1 = [T("xnT1_0"), T("xnT1_1")]
        sqb = [AR.alloc([128, 512], BF16) for _ in range(2)]
        t_sqb = [T("sqb0"), T("sqb1")]
        rkf = [AR.alloc([128, 512], F32) for _ in range(2)]
        t_rkf = [T("rkf0"), T("rkf1")]
        kst = [AR.alloc([128, 8, 512], BF16) for _ in range(2)]
        t_kst = [T("kst0"), T("kst1")]
        dq_kst = [P.dmaq("kst0"), P.dmaq("kst1")]
        vst = [AR.alloc([128, 2, 8, 2, 129], BF16) for _ in range(2)]
        t_vst = [T("vst0"), T("vst1")]
        dq_vst = [P.dmaq("vst0"), P.dmaq("vst1")]
        t_kt = T("kt_dram")
        t_vv = T("vv_dram")
        for b in range(2):
            P.emit(P.dve, lambda e, b=b: e.memset(vst[b].rearrange("p a b c d -> p (a b c d)"), 1.0), [], [t_vst[b]])

        def norm_tile(src, np_, t_src, ssq_ap, rinv_ap, t_s, t_r, xn_ap, t_x):
            jv = junk_cur[0][0:np_, :]
            P.emit(P.dve, lambda e: e.memset(ssq_ap, 0.0), [], [t_s])
            P.emit(P.act, lambda e: e.activation(out=jv, in_=src, func=AF.Square, accum_out=ssq_ap),
                   [t_src], [t_junk, t_s])
            P.emit(P.act, lambda e: e.activation(out=rinv_ap, in_=ssq_ap, func=AF.Ln, scale=1.0 / D,
                                                 bias=epsc[0:np_, :]), [t_s], [t_r])
            P.emit(P.act, lambda e: e.activation(out=rinv_ap, in_=rinv_ap, func=AF.Exp, scale=-0.5), [t_r], [t_r])
            P.emit(P.dve, lambda e: e.tensor_scalar(out=xn_ap, in0=src, scalar1=rinv_ap, scalar2=None, op0=ALU.mult),
                   [t_src, t_r], [t_x])

        def transpose_tile(xn_ap, np_, t_x, dst, t_dst, use_act):
            bi = nbank()
            pv = bf_view(bi)[:, 0:8 * np_].rearrange("p (a b) -> p a b", b=np_)
            for kc in range(8):
                P.emit(P.pe, lambda e, kc=kc: e.transpose(out=pv[:, kc, :], in_=xn_ap[:, kc * 128:(kc + 1) * 128],
                                                          identity=identb[0:np_, 0:np_]),
                       [t_x], [tb[bi]], ms=(kc == 7))
            if use_act:
                P.emit(P.act, lambda e: e.copy(out=dst, in_=pv), [tb[bi]], [t_dst])
            else:
                P.emit(P.dve, lambda e: e.tensor_copy(out=dst, in_=pv), [tb[bi]], [t_dst])

        def x1_load(c):
            b = c % 2
            P.dma(P.pool, xin1[b], x_all[c * 512:(c + 1) * 512, :].rearrange("(t p) d -> p t d", p=128), [],
                  [t_xin1[b]], dq_xin1[b])

        xn4 = [AR.alloc([128, 1024], BF16) for _ in range(4)]
        t_xn4 = [T("xn4_%d" % i) for i in range(4)]

        def p1_norm(c):
            b = c % 2
            for t in range(4):
                norm_tile(xin1[b][:, t, :], 128, t_xin1[b], ssq[b][:, t:t + 1], rinv[b][:, t:t + 1], t_ssq[b], t_rinv[b],
                          xn4[t], t_xn4[t])

        def p1_transpose(c):
            b = c % 2
            for t in range(4):
                transpose_tile(xn4[t], 128, t_xn4[t], xnT1[b][:, :, t * 128:(t + 1) * 128], t_xnT1[b],
                               use_act=(t % 2 == 0))

        def p1_norm_transpose(c):
            p1_norm(c)
            p1_transpose(c)

        x1_load(0)
        if nkv > 1:
            x1_load(1)
        p1_norm_transpose(0)
        for c in range(nkv):
            b = c % 2
            if conv_next[0] < len(conv_order):
                oi = conv_next[0]
                conv_cast_store(oi, act_only=True)
                if oi + 2 < len(conv_order):
                    conv_load(oi + 2)
                conv_next[0] += 1
            if c + 1 < nkv:
                p1_norm(c + 1)
            pend = None

            def k_tail(h, bi, pk, b=b, c=c):
                sb = h % 2
                b2i = nbank()
                p2 = banks[b2i][:, :]
                P.emit(P.pe, lambda e, p2=p2, sb=sb: e.matmul(out=p2, lhsT=onesb, rhs=sqb[sb], start=True, stop=True),
                       [t_sqb[sb]], [tb[b2i]])
                P.emit(P.act, lambda e, p2=p2, sb=sb: e.activation(out=rkf[sb], in_=p2, func=AF.Ln, scale=1.0 / DH,
                                                                   bias=epsc), [tb[b2i]], [t_rkf[sb]])
                P.emit(P.act, lambda e, sb=sb: e.activation(out=rkf[sb], in_=rkf[sb], func=AF.Exp, scale=-0.5),
                       [t_rkf[sb]], [t_rkf[sb]])
                P.emit(P.dve, lambda e, pk=pk, sb=sb, h=h, b=b: e.scalar_tensor_tensor(
                    out=kst[b][:, h, :], in0=pk, scalar=kg[:, 0:1], in1=rkf[sb], op0=ALU.mult, op1=ALU.mult),
                    [tb[bi], t_rkf[sb]], [t_kst[b]])
                P.emit(P.dve, lambda e, h=h, b=b, c=c: e.tensor_reduce(
                    out=kmean[:, h, 2 * c:2 * c + 2], in_=kst[b][:, h, :].rearrange("p (a l) -> p a l", l=256),
                    axis=AX.X, op=ALU.add), [t_kst[b]], [t_kmean])

            for h in range(NH):
                bi = nbank()
                pk = banks[bi][:, :]
                for kc in range(8):
                    P.emit(P.pe, lambda e, kc=kc, h=h, pk=pk, b=b: e.matmul(out=pk, lhsT=wk[:, kc, h * 128:(h + 1) * 128],
                                                                           rhs=xnT1[b][:, kc, :], start=(kc == 0),
                                                                           stop=(kc == 7)),
                           [t_wk, t_xnT1[b]], [tb[bi]], ms=(kc == 7))
                sb = h % 2
                P.emit(P.act, lambda e, pk=pk, sb=sb: e.activation(out=sqb[sb], in_=pk, func=AF.Square),
                       [tb[bi]], [t_sqb[sb]])
                held.add(bi)
                if pend is not None:
                    k_tail(*pend)
                    held.discard(pend[1])
                pend = (h, bi, pk)
            k_tail(*pend)
            held.discard(pend[1])
            P.dma(P.pool, kt_d[:, :, c * 512:(c + 1) * 512].rearrange("h p t -> p h t"), kst[b], [t_kst[b]], [t_kt],
                  dq_kst[b])
            if c + 2 < nkv:
                x1_load(c + 2)
            for blk in range(2):
                for kh in range(2):
                    for hf in range(2):
                        bi = nbank()
                        pv_ = banks[bi][:, :]
                        for kc in range(8):
                            P.emit(P.pe, lambda e, kc=kc, blk=blk, kh=kh, hf=hf, pv_=pv_, b=b: e.matmul(
                                out=pv_, lhsT=xnT1[b][:, kc, blk * 256 + kh:blk * 256 + 256:2],
                                rhs=wv[:, kc, hf * 512:(hf + 1) * 512], start=(kc == 0), stop=(kc == 7)),
                                [t_wv, t_xnT1[b]], [tb[bi]], ms=(kc == 7))
                        dst = vst[b][:, blk, hf * 4:(hf + 1) * 4, kh, 0:128]
                        src = pv_.rearrange("p (a d) -> p a d", d=128)
                        if hf == 0:
                            P.emit(P.act, lambda e, dst=dst, src=src: e.copy(out=dst, in_=src), [tb[bi]], [t_vst[b]])
                        else:
                            P.emit(P.dve, lambda e, dst=dst, src=src: e.tensor_copy(out=dst, in_=src), [tb[bi]],
                                   [t_vst[b]])
                if blk == 0 and c + 1 < nkv:
                    p1_transpose(c + 1)
            P.dma(P.pool, vv_d[:, 2 * c:2 * c + 2].rearrange("p a b c d -> p (a b c d)"),
                  vst[b].rearrange("p a b c d -> p (a b c d)"), [t_vst[b]], [t_vv], dq_vst[b])
        while conv_next[0] < len(conv_order):
            oi = conv_next[0]
            conv_cast_store(oi, act_only=False)
            if oi + 2 < len(conv_order):
                conv_load(oi + 2)
            conv_next[0] += 1
        P.emit(P.dve, lambda e: e.tensor_copy(out=kmean_b.rearrange("p a b -> p (a b)"),
                                              in_=kmean.rearrange("p a b -> p (a b)")), [t_kmean], [t_kmean])
        ph1 = t_xn4 + t_xin1 + t_ssq + t_rinv + t_xn + t_sqb + t_rkf + t_kst + t_vst + t_xnT1 + [t_junk, t_wk, t_wv, t_kt, t_vv,
                                                                                         t_kmean] + tb + t_stg + t_wbf
        P.barrier(ph1)
        AR.off = phase_mark

        NSLOT = 3
        wr = [AR.alloc([128, 8, 512], BF16) for _ in range(NSLOT)]
        t_wr = [T("wr%d" % i) for i in range(NSLOT)]
        dq_wr = [P.dmaq("wr%d" % i) for i in range(NSLOT)]
        seq_all = []
        for g in range(nch):
            seq_all += CHUNK_SEQ
        wstate = {"issued": 0, "used": 0}

        def w_issue():
            i = wstate["issued"]
            if i >= len(seq_all):
                return
            nm = seq_all[i]
            s_ = i % NSLOT
            ui = UIDX[nm]
            nk = UNITS[nm][0][2]
            P.dma(P.sp, wr[s_][:, 0:nk, :].rearrange("p a b -> p (a b)"), wsc[ui, :, 0:nk * 512], [t_unit[ui]],
                  [t_wr[s_]], dq_wr[s_])
            wstate["issued"] += 1

        def w_next(expect, ahead=NSLOT - 1):
            i = wstate["used"]
            assert seq_all[i] == expect, (seq_all[i], expect)
            while wstate["issued"] < min(i + ahead + 1, len(seq_all)):
                w_issue()
            wstate["used"] += 1
            s_ = i % NSLOT
            return wr[s_], t_wr[s_]

        xin = AR.alloc([128, 4, 1024], F32)
        t_xin = T("xin")
        dq_xin = P.dmaq("xin")
        xh = AR.alloc([32, 2, 1024], F32)
        t_xh = T("xh")
        dq_xh = P.dmaq("xh")
        a2t = AR.alloc([128, 2, 8, 64], F32)
        b2t = AR.alloc([128, 2, 64], F32)
        gbt = AR.alloc([128, 2, 64], F32)
        t_tab = T("tab")
        dq_tab = P.dmaq("tab")
        junk_cur[0] = AR.alloc([128, 1024], BF16)
        ssq2 = AR.alloc([128, 8], F32)
        rinv2 = AR.alloc([128, 8], F32)
        t_ssq2 = T("ssq2")
        t_rinv2 = T("rinv2")
        xn = [AR.alloc([128, 1024], BF16) for _ in range(2)]
        xnT_raw = AR.alloc([128, 8 * 576], BF16)
        xnT = xnT_raw.rearrange("p (a b) -> p a b", b=576)
        t_xnT = T("xnT")
        R1 = AR.alloc([128, 8, 576 + 512], F32)
        a_t = R1[:, :, 0:576]
        y_t = R1[:, :, 576:1088]
        act_t = R1.rearrange("p a b -> p (a b)").bitcast(BF16)[:, 0:FC * 512].rearrange("p (a b) -> p a b", b=512)
        t_a = T("a")
        t_y = T("y")
        t_actt = T("act")
        ybf = [AR.alloc([128, 512], BF16) for _ in range(2)]
        t_ybf = [T("ybf0"), T("ybf1")]
        ysq = [AR.alloc([128, 512], BF16) for _ in range(2)]
        t_ysq = [T("ysq0"), T("ysq1")]
        mean_t = AR.alloc([128, 512], F32)
        rstd_t = AR.alloc([128, 512], F32)
        t_mean = T("mean")
        t_rstd = T("rstd")
        xc = [AR.alloc([128, 512], F32) for _ in range(2)]
        t_xc = [T("xc0"), T("xc1")]
        sig_t = [x_[:, 0:288].rearrange("p (a b) -> p a b", a=1) for x_ in xc]
        t_sig = t_xc
        ysl = AR.alloc([128, 8, 512], BF16)
        t_ysl = T("ysl")
        mT = ysl
        t_mT = t_ysl
        t1 = AR.alloc([128, 8, 512], BF16)
        t_t1 = T("t1")
        qT = AR.alloc([128, 8, 512], BF16)
        t_qT = T("qT")
        qf = [AR.alloc([128, 512], F32) for _ in range(2)]
        t_qf = [T("qf0"), T("qf1")]
        gcs = AR.alloc([128, 8, 512], BF16)
        gas = AR.alloc([128, 8, 512], BF16)
        t_gcs = T("gcs")
        t_gas = T("gas")
        attT = AR.alloc([128, 8, 512], BF16)
        t_attT = T("attT")
        hnT = xnT_raw[:, 0:8 * 512].rearrange("p (a b) -> p a b", b=512)
        t_hnT = t_xnT
        NKV = 2
        ktp = [AR.alloc([128, 2048], BF16) for _ in range(NKV)]
        vp = [AR.alloc([128, 8, 2, 129], BF16) for _ in range(NKV)]
        t_ktp = [T("ktp%d" % i) for i in range(NKV)]
        t_vp = [T("vp%d" % i) for i in range(NKV)]
        dq_ktp = [P.dmaq("ktp%d" % i) for i in range(NKV)]
        dq_vp = [P.dmaq("vp%d" % i) for i in range(NKV)]
        NPT = 4
        PT = [AR.alloc([128, 512], BF16) for _ in range(NPT)]
        t_PT = [T("PT%d" % i) for i in range(NPT)]
        gs_t = [AR.alloc([128, 64], F32) for _ in range(4)]
        m8_t = [AR.alloc([128, 8], F32) for _ in range(4)]
        tmp_t = [AR.alloc([128, 64], F32) for _ in range(4)]
        hiv_t = [AR.alloc([128, 64], BF16) for _ in range(4)]
        t_gs = [T("gs%d" % i) for i in range(4)]
        t_m8 = [T("m8%d" % i) for i in range(4)]
        t_tmp = [T("tmp%d" % i) for i in range(4)]
        t_hiv = [T("hiv%d" % i) for i in range(4)]
        rl_t = [AR.alloc([128, 1], F32) for _ in range(2)]
        t_rl = [T("rl0"), T("rl1")]
        atok = [AR.alloc([128, 128], BF16) for _ in range(2)]
        t_atok = [T("atok0"), T("atok1")]
        sg_t = xc
        t_sg = t_xc
        tm2 = xc
        t_tm2 = t_xc
        dq_out = P.dmaq("out")
        t_outd = T("out_dram")
        kvstate = {"n": 0}
        ptstate = {"n": 0}

        def mm_group(out_ap, bank_i, pairs, extra_reads, first=True, last=True):
            n_ = len(pairs)
            for i_, (l_, r_) in enumerate(pairs):
                P.emit(P.pe, lambda e, l_=l_, r_=r_, i_=i_: e.matmul(out=out_ap, lhsT=l_, rhs=r_,
                                                                     start=(first and i_ == 0),
                                                                     stop=(last and i_ == n_ - 1)),
                       extra_reads, [tb[bank_i]], ms=(i_ == n_ - 1))

        for g in range(nch):
            P.dma(P.pool, xin, x_own[g * 512:(g + 1) * 512, :].rearrange("(t p) d -> p t d", p=128), [], [t_xin], dq_xin)
            P.dma(P.pool, xh, x_halo[g * 64:(g + 1) * 64, :].rearrange("(o p) d -> p o d", p=32), [], [t_xh], dq_xh)
            P.dma(P.pool, a2t, a2_d[2 * g:2 * g + 2].partition_broadcast(128), [], [t_tab], dq_tab)
            P.dma(P.pool, b2t, b2_d[2 * g:2 * g + 2].partition_broadcast(128), [], [t_tab], dq_tab)
            P.dma(P.pool, gbt, gate_bias[2 * g:2 * g + 2].partition_broadcast(128), [], [t_tab], dq_tab)
            for o in range(2):
                xb = o % 2
                norm_tile(xh[0:32, o, :], 32, t_xh, ssq2[0:32, 4 + o:5 + o], rinv2[0:32, 4 + o:5 + o], t_ssq2, t_rinv2,
                          xn[xb][0:32, :], t_xn[xb])
                transpose_tile(xn[xb][0:32, :], 32, t_xn[xb], xnT[:, :, o * 288:o * 288 + 32], t_xnT, use_act=(o == 0))
            for t in range(4):
                xb = t % 2
                c0 = (t // 2) * 288 + 32 + (t % 2) * 128
                norm_tile(xin[:, t, :], 128, t_xin, ssq2[:, t:t + 1], rinv2[:, t:t + 1], t_ssq2, t_rinv2, xn[xb], t_xn[xb])
                transpose_tile(xn[xb], 128, t_xn[xb], xnT[:, :, c0:c0 + 128], t_xnT, use_act=(t % 2 == 0))
            for u in range(4):
                w_, tw = w_next("cv%d" % u)
                for ci in range(2):
                    cc = 2 * u + ci
                    bu = nbank()
                    bg = nbank()
                    bu2 = nbank()
                    bg2 = nbank()
                    for ob, (bcu, bcg) in enumerate(((bu, bg), (bu2, bg2))):
                        mm_group(banks[bcu][:, 0:288],
                                 bcu, [(w_[:, kc, ci * 128:(ci + 1) * 128], xnT[:, kc, ob * 288:(ob + 1) * 288])
                                       for kc in range(8)], [tw, t_xnT])
                        mm_group(banks[bcg][:, 0:288],
                                 bcg, [(w_[:, kc, 256 + ci * 128:256 + (ci + 1) * 128], xnT[:, kc, ob * 288:(ob + 1) * 288])
                                       for kc in range(8)], [tw, t_xnT])
                        sb = ob
                        P.emit(P.act, lambda e, bcg=bcg, sb=sb, ob=ob: e.activation(out=sig_t[sb][:, 0, :],
                                                                                   in_=banks[bcg][:, 0:288],
                                                                                   func=AF.Sigmoid),
                               [tb[bcg]], [t_sig[sb]])
                        P.emit(P.dve, lambda e, bcu=bcu, sb=sb, ob=ob, cc=cc: e.tensor_tensor(
                            out=a_t[:, cc, ob * 288:(ob + 1) * 288], in0=banks[bcu][:, 0:288], in1=sig_t[sb][:, 0, :],
                            op=ALU.mult), [tb[bcu], t_sig[sb]], [t_a])
            def q_main(h, w_, tw, hi_):
                bi = nbank()
                pq_ = banks[bi][:, :]
                for ob in range(2):
                    mm_group(pq_[:, ob * 256:(ob + 1) * 256], bi,
                             [(w_[:, kc, hi_ * 128:(hi_ + 1) * 128], xnT[:, kc, ob * 288 + 32:ob * 288 + 288])
                              for kc in range(8)], [tw, t_xnT])
                sb = h % 2
                P.emit(P.act, lambda e, pq_=pq_, sb=sb: e.activation(out=ysq[sb], in_=pq_, func=AF.Square),
                       [tb[bi]], [t_ysq[sb]])
                held.add(bi)
                return (h, bi, pq_)

            def q_stage_a(stt_):
                h, bi, pq_ = stt_
                sb = h % 2
                b2i = nbank()
                p2 = banks[b2i][:, :]
                mm_group(p2, b2i, [(onesb, ysq[sb])], [t_ysq[sb]])
                P.emit(P.act, lambda e, p2=p2, sb=sb: e.activation(out=xc[sb], in_=p2, func=AF.Ln, scale=1.0 / DH,
                                                                   bias=epsc), [tb[b2i]], [t_xc[sb]])
                P.emit(P.act, lambda e, sb=sb: e.activation(out=xc[sb], in_=xc[sb], func=AF.Exp, scale=-0.5),
                       [t_xc[sb]], [t_xc[sb]])
                P.emit(P.dve, lambda e, pq_=pq_, sb=sb: e.scalar_tensor_tensor(
                    out=qf[sb], in0=pq_, scalar=qg[:, 0:1], in1=xc[sb], op0=ALU.mult, op1=ALU.mult),
                    [tb[bi], t_xc[sb]], [t_qf[sb]])
                P.emit(P.act, lambda e, sb=sb, h=h: e.copy(out=qT[:, h, :], in_=qf[sb]), [t_qf[sb]], [t_qT])
                held.discard(bi)
                return h

            def q_stage_b(h):
                sb = h % 2
                for t in range(4):
                    ob = t // 2
                    bgi = nbank()
                    pg = banks[bgi][:, 0:64]
                    mm_group(pg, bgi, [(qT[:, h, t * 128:(t + 1) * 128], kmean_b[:, h, :])], [t_qT, t_kmean])
                    P.emit(P.dve, lambda e, pg=pg, t=t, ob=ob: e.tensor_tensor(out=gs_t[t], in0=pg,
                                                                              in1=gbt[:, ob, :], op=ALU.add),
                           [tb[bgi], t_tab], [t_gs[t]])
                    P.emit(P.dve, lambda e, t=t: e.max(out=m8_t[t], in_=gs_t[t]), [t_gs[t]], [t_m8[t]])
                    P.emit(P.dve, lambda e, t=t, ob=ob, h=h: e.scalar_tensor_tensor(
                        out=tmp_t[t], in0=gs_t[t], scalar=m8_t[t][:, 2:3], in1=a2t[:, ob, h, :], op0=ALU.is_ge,
                        op1=ALU.mult), [t_gs[t], t_m8[t], t_tab], [t_tmp[t]])
                    P.emit(P.dve, lambda e, t=t, ob=ob: e.tensor_tensor(out=hiv_t[t], in0=tmp_t[t],
                                                                       in1=b2t[:, ob, :], op=ALU.add),
                           [t_tmp[t], t_tab], [t_hiv[t]])
                return h

            def q_stage_c(h):
                for t in range(4):
                    bti = nbank()
                    ptv = bf_view(bti)[0:64, 0:128]
                    P.emit(P.pe, lambda e, ptv=ptv, t=t: e.transpose(out=ptv, in_=hiv_t[t], identity=identb),
                           [t_hiv[t]], [tb[bti]])
                    P.emit(P.act, lambda e, ptv=ptv, h=h, t=t: e.copy(out=VT[0:64, h, t * 128:(t + 1) * 128], in_=ptv),
                           [tb[bti]], [t_vt64])

            def gate_mm(w_, tw, ci, cc, dst, tdst):
                bi = nbank()
                for ob in range(2):
                    mm_group(banks[bi][:, ob * 256:(ob + 1) * 256], bi,
                             [(w_[:, kc, ci * 128:(ci + 1) * 128], xnT[:, kc, ob * 288 + 32:ob * 288 + 288])
                              for kc in range(8)], [tw, t_xnT])
                P.emit(P.act, lambda e, bi=bi, dst=dst, cc=cc: e.activation(out=dst[:, cc, :], in_=banks[bi][:, :],
                                                                           func=AF.Sigmoid), [tb[bi]], [tdst])

            pa = pb = pc = None
            for u in range(2):
                wq, twq = w_next("q%d" % u, 2)
                wg, twg = w_next("gc%d" % u, 1)
                for hi_ in range(4):
                    h = 4 * u + hi_
                    cur = q_main(h, wq, twq, hi_)
                    na = q_stage_a(pa) if pa is not None else None
                    gate_mm(wg, twg, hi_, h, gcs, t_gcs)
                    if pc is not None:
                        q_stage_c(pc)
                    nb_ = q_stage_b(pb) if pb is not None else None
                    pa, pb, pc = cur, na, nb_
            for u in range(2):
                wg, twg = w_next("ga%d" % u, 2)
                for ci in range(4):
                    gate_mm(wg, twg, ci, 4 * u + ci, gas, t_gas)
                    na = q_stage_a(pa) if pa is not None else None
                    if pc is not None:
                        q_stage_c(pc)
                    nb_ = q_stage_b(pb) if pb is not None else None
                    pa, pb, pc = None, na, nb_
            assert pa is None and pb is None and pc is None
            def conv_cc(cc):
                yv = y_t[:, cc, :].rearrange("p (o l) -> p o l", l=256)

                def av(j, cc=cc):
                    return a_t[:, cc, :].rearrange("p (o l) -> p o l", l=288)[:, :, 2 + j:2 + j + 256]
                P.emit(P.dve, lambda e, yv=yv, av=av, cc=cc: e.tensor_scalar(out=yv, in0=av(0), scalar1=cw[:, cc, 0:1],
                                                                            scalar2=cb[:, cc:cc + 1], op0=ALU.mult,
                                                                            op1=ALU.add), [t_a], [t_y])
                for j in range(1, 31):
                    P.emit(P.dve, lambda e, yv=yv, av=av, cc=cc, j=j: e.scalar_tensor_tensor(
                        out=yv, in0=av(j), scalar=cw[:, cc, j:j + 1], in1=yv, op0=ALU.mult, op1=ALU.add), [t_a, t_y], [t_y])

            def normalize_head(h):
                for t in range(4):
                    ob_i = 4 + t
                    o_ap = banks[ob_i][:, 0:129]
                    mb = t % 2
                    P.emit(P.dve, lambda e, o_ap=o_ap, mb=mb: e.reciprocal(out=rl_t[mb], in_=o_ap[:, 128:129]),
                           [tb[ob_i]], [t_rl[mb]])
                    P.emit(P.dve, lambda e, o_ap=o_ap, mb=mb: e.tensor_scalar(out=atok[mb], in0=o_ap[:, 0:128],
                                                                              scalar1=rl_t[mb][:, 0:1], scalar2=None,
                                                                              op0=ALU.mult),
                           [tb[ob_i], t_rl[mb]], [t_atok[mb]])
                    bti = (srot[0] - 3) % 4 if t % 2 == 0 else srot[0] % 4
                    ptv = bf_view(bti)[:, 0:128]
                    P.emit(P.pe, lambda e, ptv=ptv, mb=mb: e.transpose(out=ptv, in_=atok[mb], identity=identb),
                           [t_atok[mb]], [tb[bti]])
                    P.emit(P.act, lambda e, ptv=ptv, h=h, t=t: e.copy(out=attT[:, h, t * 128:(t + 1) * 128], in_=ptv),
                           [tb[bti]], [t_attT])

            nblk = 8 * g + 8
            npp = nblk // 8
            units = [(h, n, kh) for h in range(NH) for n in range(nblk) for kh in range(2)]
            piece_list = [(h, n0) for h in range(NH) for n0 in range(0, nblk, 8)]
            piece_slot = {}
            piece_issued = [0]

            def ensure_piece(k):
                while piece_issued[0] <= k and piece_issued[0] < len(piece_list):
                    h_, n0_ = piece_list[piece_issued[0]]
                    s_ = kvstate["n"] % NKV
                    kvstate["n"] += 1
                    P.dma(P.pool, ktp[s_], kt_d[h_, :, n0_ * 256:(n0_ + 8) * 256], [t_kt], [t_ktp[s_]], dq_ktp[s_])
                    P.dma(P.pool, vp[s_], vv_d[:, n0_:n0_ + 8, h_, :, :], [t_vv], [t_vp[s_]], dq_vp[s_])
                    piece_slot[piece_issued[0]] = s_
                    piece_issued[0] += 1

            LA = 2
            srot = [0]
            ensure_piece(1)
            st = {}
            for idx in range(len(units) + LA):
                if idx < len(units):
                    h, n, kh = units[idx]
                    s_ = piece_slot[h * npp + n // 8]
                    nl = n % 8
                    bi = srot[0]
                    srot[0] = (srot[0] + 1) % 4
                    S_ = banks[bi][:, :]
                    cand = n >= 8 * g
                    P.emit(P.pe, lambda e, S_=S_, s_=s_, nl=nl, kh=kh, h=h: e.matmul(
                        out=S_, lhsT=ktp[s_][:, nl * 256 + kh:nl * 256 + 256:2], rhs=qT[:, h, :], start=True,
                        stop=False), [t_ktp[s_], t_qT], [tb[bi]], ms=False)
                    P.emit(P.pe, lambda e, S_=S_, n=n, h=h, cand=cand: e.matmul(
                        out=S_, lhsT=esel_lhsT(n), rhs=VT[0:65, h, :], start=False, stop=(not cand)),
                        [t_vt64], [tb[bi]], ms=(not cand))
                    if cand:
                        ob = (n - 8 * g) // 4
                        cnd = (n - 8 * g) % 4
                        P.emit(P.pe, lambda e, S_=S_, ob=ob, cnd=cnd, kh=kh: e.matmul(
                            out=S_[:, ob * 256:(ob + 1) * 256], lhsT=identb, rhs=cms[:, cnd, kh, :], start=False,
                            stop=True), [], [tb[bi]], ms=True)
                    st[idx] = (bi, s_)
                j = idx - LA
                if j >= 0:
                    h, n, kh = units[j]
                    bi, s_ = st.pop(j)
                    S_ = banks[bi][:, :]
                    nl = n % 8
                    if n % 8 == 0 and kh == 0:
                        ensure_piece(h * npp + n // 8 + 1)
                    first = (n == 0 and kh == 0)
                    lastblk = (n == nblk - 1 and kh == 1)
                    r_ = ptstate["n"] % NPT
                    ptstate["n"] += 1
                    P.emit(P.act, lambda e, S_=S_, r_=r_, h=h, kh=kh: e.activation(
                        out=PT[r_], in_=S_, func=AF.Exp, bias=kb[:, 2 * h + kh:2 * h + kh + 1]),
                        [tb[bi]], [t_PT[r_]])
                    for t in range(4):
                        ob_i = 4 + t
                        o_ap = banks[ob_i][:, 0:129]
                        P.emit(P.pe, lambda e, o_ap=o_ap, r_=r_, t=t, s_=s_, nl=nl, kh=kh, first=first,
                               lastblk=lastblk: e.matmul(out=o_ap, lhsT=PT[r_][:, t * 128:(t + 1) * 128],
                                                         rhs=vp[s_][:, nl, kh, :], start=first, stop=lastblk),
                               [t_PT[r_], t_vp[s_]], [tb[ob_i]], ms=(t == 3))
                    if lastblk:
                        normalize_head(h)
                        conv_cc(h)
            for u in range(2):
                w_, tw = w_next("ao%d" % u)
                for ci in range(4):
                    cc = 4 * u + ci
                    bi = nbank()
                    mm_group(banks[bi][:, :], bi, [(w_[:, kc, ci * 128:(ci + 1) * 128], attT[:, kc, :]) for kc in range(8)],
                             [tw, t_attT])
                    P.emit(P.dve, lambda e, bi=bi, cc=cc: e.tensor_tensor(out=t1[:, cc, :], in0=banks[bi][:, :],
                                                                          in1=gas[:, cc, :], op=ALU.mult),
                           [tb[bi], t_gas], [t_t1])
            bs1 = nbank()
            bs2 = nbank()
            for cc in range(8):
                sb = cc % 2
                P.emit(P.act, lambda e, sb=sb, cc=cc: e.copy(out=ybf[sb], in_=y_t[:, cc, :]), [t_y], [t_ybf[sb]])
                P.emit(P.act, lambda e, sb=sb, cc=cc: e.activation(out=ysq[sb], in_=y_t[:, cc, :], func=AF.Square),
                       [t_y], [t_ysq[sb]])
                P.emit(P.pe, lambda e, sb=sb, cc=cc, bs1=bs1: e.matmul(out=banks[bs1][:, :], lhsT=onesb, rhs=ybf[sb],
                                                              start=(cc == 0), stop=(cc == 7)),
                       [t_ybf[sb]], [tb[bs1]], ms=True)
                P.emit(P.pe, lambda e, sb=sb, cc=cc, bs2=bs2: e.matmul(out=banks[bs2][:, :], lhsT=onesb, rhs=ysq[sb],
                                                              start=(cc == 0), stop=(cc == 7)),
                       [t_ysq[sb]], [tb[bs2]], ms=True)
            P.emit(P.dve, lambda e, bs1=bs1: e.tensor_scalar(out=mean_t, in0=banks[bs1][:, :], scalar1=1.0 / D, scalar2=None,
                                                    op0=ALU.mult), [tb[bs1]], [t_mean])
            P.emit(P.dve, lambda e: e.tensor_tensor(out=rstd_t, in0=mean_t, in1=mean_t, op=ALU.mult), [t_mean], [t_rstd])
            P.emit(P.dve, lambda e, bs2=bs2: e.scalar_tensor_tensor(out=rstd_t, in0=banks[bs2][:, :], scalar=1.0 / D, in1=rstd_t,
                                                           op0=ALU.mult, op1=ALU.subtract), [tb[bs2], t_rstd], [t_rstd])
            P.emit(P.act, lambda e: e.activation(out=rstd_t, in_=rstd_t, func=AF.Ln, bias=epsc), [t_rstd], [t_rstd])
            P.emit(P.act, lambda e: e.activation(out=rstd_t, in_=rstd_t, func=AF.Exp, scale=-0.5), [t_rstd], [t_rstd])
            for cc in range(8):
                sb = cc % 2
                P.emit(P.dve, lambda e, sb=sb, cc=cc: e.tensor_tensor(out=xc[sb], in0=y_t[:, cc, :], in1=mean_t,
                                                                      op=ALU.subtract), [t_y, t_mean], [t_xc[sb]])
                P.emit(P.dve, lambda e, sb=sb: e.tensor_tensor(out=xc[sb], in0=xc[sb], in1=rstd_t, op=ALU.mult),
                       [t_xc[sb], t_rstd], [t_xc[sb]])
                P.emit(P.act, lambda e, sb=sb, cc=cc: e.activation(out=ysl[:, cc, :], in_=xc[sb], func=AF.Silu,
                                                                   scale=lg[:, cc:cc + 1], bias=lb[:, cc:cc + 1]),
                       [t_xc[sb]], [t_ysl])
            for u in range(2):
                w_, tw = w_next("co%d" % u)
                for ci in range(4):
                    cc = 4 * u + ci
                    bi = nbank()
                    sb = cc % 2
                    mm_group(banks[bi][:, :], bi, [(w_[:, kc, ci * 128:(ci + 1) * 128], ysl[:, kc, :]) for kc in range(8)],
                             [tw, t_ysl])
                    P.emit(P.dve, lambda e, bi=bi, cc=cc, sb=sb: e.tensor_tensor(out=tm2[sb], in0=banks[bi][:, :],
                                                                                 in1=gcs[:, cc, :], op=ALU.mult),
                           [tb[bi], t_gcs], [t_tm2[sb]])
                    P.emit(P.dve, lambda e, cc=cc, sb=sb: e.tensor_tensor(out=t1[:, cc, :], in0=tm2[sb], in1=t1[:, cc, :],
                                                                          op=ALU.add), [t_tm2[sb], t_t1], [t_t1])
            for hf in range(2):
                w_, tw = w_next("wo%d" % hf)
                for t in range(4):
                    bi = nbank()
                    mm_group(banks[bi][:, :], bi, [(t1[:, kc, t * 128:(t + 1) * 128], w_[:, kc, :]) for kc in range(8)],
                             [tw, t_t1])
                    P.emit(P.dve, lambda e, bi=bi, t=t, hf=hf: e.tensor_tensor(
                        out=xin[:, t, hf * 512:(hf + 1) * 512], in0=banks[bi][:, :], in1=xin[:, t, hf * 512:(hf + 1) * 512],
                        op=ALU.add), [tb[bi], t_xin], [t_xin])
            for t in range(4):
                xb = t % 2
                norm_tile(xin[:, t, :], 128, t_xin, ssq2[:, t:t + 1], rinv2[:, t:t + 1], t_ssq2, t_rinv2, xn[xb], t_xn[xb])
                transpose_tile(xn[xb], 128, t_xn[xb], hnT[:, :, t * 128:(t + 1) * 128], t_hnT, use_act=(t % 2 == 0))
            for u in range(11):
                w_, tw = w_next("gu%d" % u)
                for ci in range(2):
                    fc = 2 * u + ci
                    sb = fc % 2
                    bgt = nbank()
                    mm_group(banks[bgt][:, :], bgt, [(w_[:, kc, ci * 128:(ci + 1) * 128], hnT[:, kc, :]) for kc in range(8)],
                             [tw, t_hnT])
                    but = nbank()
                    mm_group(banks[but][:, :], but,
                             [(w_[:, kc, 256 + ci * 128:256 + (ci + 1) * 128], hnT[:, kc, :]) for kc in range(8)],
                             [tw, t_hnT])
                    P.emit(P.act, lambda e, bgt=bgt, sb=sb: e.activation(out=sg_t[sb], in_=banks[bgt][:, :], func=AF.Silu),
                           [tb[bgt]], [t_sg[sb]])
                    P.emit(P.dve, lambda e, but=but, sb=sb, fc=fc: e.tensor_tensor(out=act_t[:, fc, :], in0=banks[but][:, :],
                                                                                   in1=sg_t[sb], op=ALU.mult),
                           [tb[but], t_sg[sb]], [t_actt, t_a, t_y])
            for hf in range(2):
                for gi, (f0, nf) in enumerate(((0, 8), (8, 8), (16, 6))):
                    w_, tw = w_next("dn%d_%d" % (hf, gi))
                    for t in range(4):
                        bi = 4 + t
                        for fl in range(nf):
                            fc = f0 + fl
                            P.emit(P.pe, lambda e, bi=bi, t=t, fc=fc, fl=fl, w_=w_: e.matmul(
                                out=banks[bi][:, :], lhsT=act_t[:, fc, t * 128:(t + 1) * 128], rhs=w_[:, fl, :],
                                start=(fc == 0), stop=(fc == FC - 1)), [tw, t_actt], [tb[bi]], ms=(fl == nf - 1))
                for t in range(4):
                    bi = 4 + t
                    P.emit(P.dve, lambda e, bi=bi, t=t, hf=hf: e.tensor_tensor(
                        out=xin[:, t, hf * 512:(hf + 1) * 512], in0=banks[bi][:, :], in1=xin[:, t, hf * 512:(hf + 1) * 512],
                        op=ALU.add), [tb[bi], t_xin], [t_xin])
            P.dma(P.pool, out_d[g * 512:(g + 1) * 512, :].rearrange("(t p) d -> p t d", p=128), xin, [t_xin], [t_outd],
                  dq_out)
            t_a.r += t_actt.r
            t_y.r += t_actt.r
            if t_actt.w is not None:
                t_a.r.append(t_actt.w)
                t_y.r.append(t_actt.w)
        P.wait_all(P.pool, [t_outd])
        P.wait_all(P.sp, [t_outd])
        P.run()
    return nc


def host_tables(j):
    slopes = 2.0 ** (-8.0 * np.arange(1, NH + 1) / NH)
    gate_bias = np.zeros((16, 64), np.float32)
    a2 = np.zeros((16, 8, 64), np.float32)
    b2 = np.full((16, 64), NEG, np.float32)
    for i in range(16):
        cur = 4 * i + j
        gate_bias[i, cur:] = -1e30
        b2[i, cur] = 0.0
        for n in range(cur):
            a2[i, :, n] = -NEG - slopes * 256.0 * (cur - n)
    cmsel = np.zeros((128, 4, 2, 256), np.float32)
    p = np.arange(128)[:, None]
    rq = np.arange(256)[None, :]
    for kh in range(2):
        rk = 2 * p + kh
        cmsel[:, j, kh, :] = np.where(rq >= rk, 0.0, NEG)
    lo = np.zeros((1, 8, 512), np.float32)
    for h in range(8):
        lo[0, h, :] = -slopes[h] * (np.arange(512) % 256)
    kbias = np.zeros((128, 16), np.float32)
    for h in range(8):
        for kh in range(2):
            kbias[:, 2 * h + kh] = slopes[h] * (2 * np.arange(128) + kh)
    return {"gate_bias": gate_bias, "a2": a2, "b2": b2, "cmsel": cmsel.reshape(128, -1),
            "lo": lo.reshape(1, -1), "kbias": kbias, "ident": np.eye(128, dtype=np.float32)}


def make_in_maps(inputs):
    x = np.asarray(inputs["x"], np.float32)
    shared = {
        "w_in": np.ascontiguousarray(inputs["w_in"][0]), "w_conv_out": np.ascontiguousarray(inputs["w_conv_out"][0]),
        "w_attn_out": np.ascontiguousarray(inputs["w_attn_out"][0]), "w_out": np.ascontiguousarray(inputs["w_out"][0]),
        "w_ffn_gate": np.ascontiguousarray(inputs["w_ffn_gate"][0]), "w_ffn_up": np.ascontiguousarray(inputs["w_ffn_up"][0]),
        "w_ffn_down": np.ascontiguousarray(inputs["w_ffn_down"][0]),
        "norm1_g": np.asarray(inputs["norm1_g"], np.float32).reshape(1, D),
        "norm2_g": np.asarray(inputs["norm2_g"], np.float32).reshape(1, D),
        "dw_w": np.ascontiguousarray(inputs["dw_w"][0]), "dw_b": np.asarray(inputs["dw_b"], np.float32).reshape(1, D),
        "conv_ln_g": np.asarray(inputs["conv_ln_g"], np.float32).reshape(1, D),
        "conv_ln_b": np.asarray(inputs["conv_ln_b"], np.float32).reshape(1, D),
        "q_norm_g": np.asarray(inputs["q_norm_g"], np.float32).reshape(1, DH),
        "k_norm_g": np.asarray(inputs["k_norm_g"], np.float32).reshape(1, DH),
    }
    shared = {k: np.asarray(v, np.float32) for k, v in shared.items()}
    maps = []
    for c in range(8):
        b, j = c // 4, c % 4
        xb = x[b]
        xblk = xb.reshape(NBK, L, D)
        own = [4 * i + j for i in range(16)]
        x_own = np.ascontiguousarray(xblk[own].reshape(4096, D))
        halo = np.zeros((16, 32, D), np.float32)
        for i, n in enumerate(own):
            if n > 0:
                halo[i] = xb[n * L - 32:n * L]
        m = dict(shared)
        m.update(host_tables(j))
        m["x_all"] = np.ascontiguousarray(xb)
        m["x_own"] = x_own
        m["x_halo"] = halo.reshape(512, D)
        maps.append(m)
    return maps


_NC_CACHE = {}


def kernel(**inputs):
    if "nc" not in _NC_CACHE:
        _NC_CACHE["nc"] = build_nc()
    nc = _NC_CACHE["nc"]
    maps = make_in_maps(inputs)
    res = run_bass_kernel_spmd(nc, maps, core_ids=list(range(8)))
    out = np.zeros((2, S, D), np.float32)
    ov = out.reshape(2, NBK, L, D)
    for c in range(8):
        b, j = c // 4, c % 4
        o = np.asarray(res.results[c]["out_own"], np.float32).reshape(16, L, D)
        for i in range(16):
            ov[b, 4 * i + j] = o[i]
    return out
```

```python
import numpy as np
from contextlib import ExitStack
import concourse.bass as bass
import concourse.mybir as mybir
from concourse.bass_utils import run_bass_kernel_spmd

F32 = mybir.dt.float32
BF16 = mybir.dt.bfloat16
AF = mybir.ActivationFunctionType
ALU = mybir.AluOpType
AX = mybir.AxisListType

D = 1024
KC = 8
NH = 8
DH = 128
FF = 2816
FC = 22
S = 16384
L = 256
NBK = 64
EPS = 1e-6
NEG = -30000.0


class T:
    __slots__ = ("name", "w", "r")

    def __init__(self, name):
        self.name = name
        self.w = None
        self.r = []


class Q:
    def __init__(self, name, sem, scale=1):
        self.name = name
        self.sem = sem
        self.scale = scale
        self.count = 0
        self.ops = []
        self.seen = {}


class Prog:
    def __init__(self, nc, es):
        self.nc = nc
        self.es = es
        self.pe = Q("pe", self.newsem("s_pe"))
        self.act = Q("act", self.newsem("s_act"))
        self.dve = Q("dve", self.newsem("s_dve"))
        self.pool = Q("pool", self.newsem("s_pool"))
        self.sp = Q("sp", self.newsem("s_sp"))
        self.engines = [self.pe, self.act, self.dve, self.pool, self.sp]

    def newsem(self, name):
        return self.es.enter_context(self.nc.semaphore(name))

    def dmaq(self, name):
        return Q(name, self.newsem("d_" + name), 16)

    def _wait(self, q, tok):
        sq, cnt = tok
        if q.seen.get(sq, 0) >= cnt:
            return
        q.seen[sq] = cnt
        q.ops.append(lambda e, s=sq.sem, v=cnt * sq.scale: e.wait_ge(s, v))

    def emit(self, q, fn, reads=(), writes=(), ms=True, sig=None):
        sig = sig or q
        for t in reads:
            if t.w is not None:
                self._wait(q, t.w)
        for t in writes:
            if t.w is not None and t.w[0] is not q:
                self._wait(q, t.w)
            for tok in t.r:
                if tok[0] is not q:
                    self._wait(q, tok)
        if ms:
            sig.count += 1
            tok = (sig, sig.count)
            q.ops.append(lambda e, s=sig.sem, v=sig.scale: fn(e).then_inc(s, v))
        else:
            tok = (sig, sig.count + 1)
            q.ops.append(lambda e: fn(e))
        for t in writes:
            t.w = tok
            t.r = []
        for t in reads:
            t.r.append(tok)
            if len(t.r) > 16:
                best = {}
                for sq, c in t.r:
                    if sq not in best or best[sq] < c:
                        best[sq] = c
                t.r = list(best.items())
        return tok

    def dma(self, q, out, in_, reads, writes, sig, **kw):
        return self.emit(q, lambda e: e.dma_start(out=out, in_=in_, **kw), reads, writes, sig=sig)

    def wait_all(self, q, ts):
        for t in ts:
            if t.w is not None:
                self._wait(q, t.w)
            for tok in t.r:
                self._wait(q, tok)

    def barrier(self, ts):
        for q in self.engines:
            self.wait_all(q, ts)

    def run(self):
        nc = self.nc
        with nc.Block() as block:
            @block.tensor
            def _(e):
                for op in self.pe.ops:
                    op(e)

            @block.scalar
            def _(e):
                for op in self.act.ops:
                    op(e)

            @block.vector
            def _(e):
                for op in self.dve.ops:
                    op(e)

            @block.gpsimd
            def _(e):
                for op in self.pool.ops:
                    op(e)

            @block.sync
            def _(e):
                for op in self.sp.ops:
                    op(e)


class Arena:
    def __init__(self, ap, nwords):
        self.ap = ap
        self.n = nwords
        self.off = 0

    def alloc(self, shape, dtype):
        shape = list(shape)
        np_ = shape[0]
        free = 1
        for s_ in shape[1:]:
            free *= s_
        esz = 4 if dtype == F32 else 2
        words = (free * esz + 3) // 4
        words = (words + 7) // 8 * 8
        assert self.off + words <= self.n, ("arena overflow", self.off, words, self.n)
        v = self.ap[0:np_, self.off:self.off + words]
        self.off += words
        if dtype != F32:
            v = v.bitcast(dtype)
        v = v[:, 0:free]
        if len(shape) == 2:
            return v
        names = " ".join("d%d" % i for i in range(len(shape) - 1))
        kw = {"d%d" % i: shape[i + 1] for i in range(len(shape) - 1)}
        return v.rearrange("p (%s) -> p %s" % (names, names), **kw)


def unit_table():
    u = {}
    for i in range(4):
        u["cv%d" % i] = [("w_in", 0, 8, (2 * i) * 128, 128, 0, "g1"), ("w_in", 0, 8, (2 * i + 1) * 128, 128, 128, "g1"),
                         ("w_in", 0, 8, 1024 + (2 * i) * 128, 128, 256, "g1"),
                         ("w_in", 0, 8, 1024 + (2 * i + 1) * 128, 128, 384, "g1")]
    for nm, c0 in (("q", 2048), ("k", 3072), ("v", 4096), ("gc", 5120), ("ga", 6144)):
        for i in range(2):
            u["%s%d" % (nm, i)] = [("w_in", 0, 8, c0 + i * 512, 512, 0, "g1")]
    for nm, src in (("co", "w_conv_out"), ("ao", "w_attn_out"), ("wo", "w_out")):
        for i in range(2):
            u["%s%d" % (nm, i)] = [(src, 0, 8, i * 512, 512, 0, None)]
    for i in range(11):
        u["gu%d" % i] = [("w_ffn_gate", 0, 8, (2 * i) * 128, 128, 0, "g2"),
                         ("w_ffn_gate", 0, 8, (2 * i + 1) * 128, 128, 128, "g2"),
                         ("w_ffn_up", 0, 8, (2 * i) * 128, 128, 256, "g2"),
                         ("w_ffn_up", 0, 8, (2 * i + 1) * 128, 128, 384, "g2")]
    for hf in range(2):
        for gi, (f0, nf) in enumerate(((0, 8), (8, 8), (16, 6))):
            u["dn%d_%d" % (hf, gi)] = [("w_ffn_down", f0 * 128, nf, hf * 512, 512, 0, None)]
    return u


UNITS = unit_table()
UNAMES = list(UNITS.keys())
UIDX = {n: i for i, n in enumerate(UNAMES)}
NU = len(UNAMES)
CHUNK_SEQ = (["cv%d" % i for i in range(4)] + ["q0", "gc0", "q1", "gc1", "ga0", "ga1", "ao0", "ao1", "co0", "co1",
                                               "wo0", "wo1"] + ["gu%d" % i for i in range(11)] +
             ["dn0_0", "dn0_1", "dn0_2", "dn1_0", "dn1_1", "dn1_2"])


def build_nc(nch=8, nkv=32):
    nc = bass.Bass("TRN2", target_bir_lowering=False)

    def din(name, shape):
        return nc.dram_tensor(name, list(shape), F32, kind="ExternalInput").ap()

    x_all = din("x_all", [S, D])
    x_own = din("x_own", [4096, D])
    x_halo = din("x_halo", [512, D])
    gate_bias = din("gate_bias", [16, 64])
    a2_d = din("a2", [16, 8, 64])
    b2_d = din("b2", [16, 64])
    cms_d = din("cmsel", [128, 4 * 2 * 256])
    lo_d = din("lo", [1, 8 * 512])
    kb_d = din("kbias", [128, 16])
    id_d = din("ident", [128, 128])
    wsrc = {
        "w_in": din("w_in", [D, 7168]), "w_conv_out": din("w_conv_out", [D, D]), "w_attn_out": din("w_attn_out", [D, D]),
        "w_out": din("w_out", [D, D]), "w_ffn_gate": din("w_ffn_gate", [D, FF]), "w_ffn_up": din("w_ffn_up", [D, FF]),
        "w_ffn_down": din("w_ffn_down", [FF, D]),
    }
    norm1_g = din("norm1_g", [1, D])
    norm2_g = din("norm2_g", [1, D])
    dw_w = din("dw_w", [31, D])
    dw_b = din("dw_b", [1, D])
    ln_g = din("conv_ln_g", [1, D])
    ln_b = din("conv_ln_b", [1, D])
    qg_d = din("q_norm_g", [1, DH])
    kg_d = din("k_norm_g", [1, DH])
    out_d = nc.dram_tensor("out_own", [4096, D], F32, kind="ExternalOutput").ap()
    wsc = nc.dram_tensor("wsc", [NU, 128, 4096], BF16).ap()
    kt_d = nc.dram_tensor("kt_s", [NH, 128, S], BF16).ap()
    vv_d = nc.dram_tensor("vv_s", [128, NBK, NH, 2, 129], BF16).ap()

    with ExitStack() as es:
        P = Prog(nc, es)
        NW = 53200
        arena_t = es.enter_context(nc.sbuf_tensor("arena", [128, NW], F32))
        AR = Arena(arena_t[:, :], NW)
        banks = [es.enter_context(nc.psum_tensor("pb%d" % i, [128, 512], F32)) for i in range(8)]
        tb = [T("pb%d" % i) for i in range(8)]
        rot = [0]

        held = set()

        def nbank():
            while rot[0] in held:
                rot[0] = (rot[0] + 1) % 8
            i = rot[0]
            rot[0] = (rot[0] + 1) % 8
            return i

        def bf_view(i):
            return banks[i][:, :].bitcast(BF16)

        identf = AR.alloc([128, 128], F32)
        identb = AR.alloc([128, 128], BF16)
        onesb = AR.alloc([128, 128], BF16)
        esel = AR.alloc([65, 64], BF16)
        cms = AR.alloc([128, 4, 2, 256], BF16)
        kb = AR.alloc([128, 16], F32)
        kmean = AR.alloc([128, 8, 64], F32)
        kmean_b = AR.alloc([128, 8, 64], BF16)
        VT = AR.alloc([65, 8, 512], BF16)
        g1 = AR.alloc([128, 8], F32)
        g2 = AR.alloc([128, 8], F32)
        cw = AR.alloc([128, 8, 31], F32)
        cb = AR.alloc([128, 8], F32)
        lg = AR.alloc([128, 8], F32)
        lb = AR.alloc([128, 8], F32)
        qg = AR.alloc([128, 1], F32)
        kg = AR.alloc([128, 1], F32)
        epsc = AR.alloc([128, 1], F32)
        t_const = T("const")
        t_kmean = T("kmean")
        t_vt64 = T("vt64")
        dq_c = P.dmaq("const")
        const_mark = AR.off

        cms_f = AR.alloc([128, 2048], F32)
        lo_f = AR.alloc([65, 4096], F32)
        t_cst = T("cst")
        pq = P.pool
        P.dma(pq, identf, id_d, [], [t_cst], dq_c)
        P.dma(pq, cms_f, cms_d, [], [t_cst], dq_c)
        P.dma(pq, lo_f[64:65, :], lo_d, [], [t_cst], dq_c)
        P.dma(pq, kb, kb_d, [], [t_cst], dq_c)
        P.dma(pq, g1, norm1_g[0].rearrange("(kc p) -> p kc", p=128), [], [t_cst], dq_c, allow_slow_non_contiguous=True)
        P.dma(pq, g2, norm2_g[0].rearrange("(kc p) -> p kc", p=128), [], [t_cst], dq_c, allow_slow_non_contiguous=True)
        P.dma(pq, cb, dw_b[0].rearrange("(kc p) -> p kc", p=128), [], [t_cst], dq_c, allow_slow_non_contiguous=True)
        P.dma(pq, lg, ln_g[0].rearrange("(kc p) -> p kc", p=128), [], [t_cst], dq_c, allow_slow_non_contiguous=True)
        P.dma(pq, lb, ln_b[0].rearrange("(kc p) -> p kc", p=128), [], [t_cst], dq_c, allow_slow_non_contiguous=True)
        for kc_ in range(8):
            P.dma(pq, cw[:, kc_, :], dw_w[:, kc_ * 128:(kc_ + 1) * 128].rearrange("j p -> p j"), [], [t_cst], dq_c,
                  allow_slow_non_contiguous=True)
        P.dma(pq, qg, qg_d.rearrange("o p -> p o"), [], [t_cst], dq_c, allow_slow_non_contiguous=True)
        P.dma(pq, kg, kg_d.rearrange("o p -> p o"), [], [t_cst], dq_c, allow_slow_non_contiguous=True)
        dv = P.dve
        P.emit(dv, lambda e: e.tensor_copy(out=identb, in_=identf), [t_cst], [t_const])
        P.emit(dv, lambda e: e.memset(onesb, 1.0), [], [t_const])
        P.emit(dv, lambda e: e.memset(kmean.rearrange("p a b -> p (a b)"), 0.0), [], [t_const])
        P.emit(dv, lambda e: e.memset(epsc, EPS), [], [t_const])
        P.emit(dv, lambda e: e.tensor_copy(out=esel[0:64, :], in_=identf[0:64, 0:64]), [t_cst], [t_const])
        P.emit(dv, lambda e: e.memset(esel[64:65, :], 1.0), [], [t_const])
        P.emit(dv, lambda e: e.tensor_copy(out=cms.rearrange("p a b c -> p (a b c)"), in_=cms_f), [t_cst], [t_const])
        P.emit(dv, lambda e: e.tensor_copy(out=VT[64:65, :, :].rearrange("p a b -> p (a b)"), in_=lo_f[64:65, :]),
               [t_cst], [t_const])
        P.emit(dv, lambda e: e.tensor_scalar(out=qg, in0=qg, scalar1=float(DH) ** -0.5, scalar2=None, op0=ALU.mult),
               [t_cst], [t_const])
        P.barrier([t_cst, t_const])
        AR.off = const_mark
        phase_mark = AR.off

        def esel_lhsT(n):
            a = esel[0:65, n:n + 1]
            return bass.AP(a.tensor, a.offset, [[a.ap[0][0], 65], [0, 128]])

        stg = [AR.alloc([128, 8, 512], F32) for _ in range(2)]
        wbf = [AR.alloc([128, 8, 512], BF16) for _ in range(2)]
        t_stg = [T("stg0"), T("stg1")]
        t_wbf = [T("wbf0"), T("wbf1")]
        dq_stg = [P.dmaq("stg0"), P.dmaq("stg1")]
        dq_wbf = [P.dmaq("wbf0"), P.dmaq("wbf1")]
        t_unit = [T("unit%d" % i) for i in range(NU)]
        gains = {"g1": g1, "g2": g2}
        kv_units = ["k0", "k1", "v0", "v1"]
        conv_order = kv_units + [n_ for n_ in UNAMES if n_ not in kv_units]

        def conv_load(oi):
            nm = conv_order[oi]
            b = oi % 2
            for (src, row0, nk_, c0, ncol, dst, _g) in UNITS[nm]:
                sap = wsrc[src][row0:row0 + nk_ * 128, c0:c0 + ncol].rearrange("(kc p) n -> p kc n", p=128)
                P.dma(P.sp, stg[b][:, 0:nk_, dst:dst + ncol], sap, [], [t_stg[b]], dq_stg[b])

        def conv_cast_store(oi, act_only):
            nm = conv_order[oi]
            ui = UIDX[nm]
            b = oi % 2
            segs = UNITS[nm]
            nk = segs[0][2]
            gname = segs[0][6]
            for kc in range(nk):
                use_dve = (kc % 2 == 0) and not act_only
                if gname is None:
                    if use_dve:
                        P.emit(P.dve, lambda e, b=b, kc=kc: e.tensor_copy(out=wbf[b][:, kc, :], in_=stg[b][:, kc, :]),
                               [t_stg[b]], [t_wbf[b]])
                    else:
                        P.emit(P.act, lambda e, b=b, kc=kc: e.copy(out=wbf[b][:, kc, :], in_=stg[b][:, kc, :]),
                               [t_stg[b]], [t_wbf[b]])
                else:
                    gt = gains[gname]
                    if use_dve:
                        P.emit(P.dve, lambda e, b=b, kc=kc, gt=gt: e.tensor_scalar(
                            out=wbf[b][:, kc, :], in0=stg[b][:, kc, :], scalar1=gt[:, kc:kc + 1], scalar2=None,
                            op0=ALU.mult), [t_stg[b]], [t_wbf[b]])
                    else:
                        P.emit(P.act, lambda e, b=b, kc=kc, gt=gt: e.activation(
                            out=wbf[b][:, kc, :], in_=stg[b][:, kc, :], func=AF.Copy, scale=gt[:, kc:kc + 1]),
                            [t_stg[b]], [t_wbf[b]])
            P.dma(P.sp, wsc[ui, :, 0:nk * 512], wbf[b][:, 0:nk, :].rearrange("p a b -> p (a b)"), [t_wbf[b]],
                  [t_unit[ui]], dq_wbf[b])

        conv_load(0)
        conv_load(1)
        for oi in range(4):
            conv_cast_store(oi, act_only=False)
            conv_load(oi + 2)
        conv_next = [4]

        wk = AR.alloc([128, 8, 1024], BF16)
        wv = AR.alloc([128, 8, 1024], BF16)
        t_wk = T("wk")
        t_wv = T("wv")
        dq_wk = P.dmaq("wk")
        dq_wv = P.dmaq("wv")
        for i in range(2):
            P.dma(P.pool, wk[:, :, i * 512:(i + 1) * 512], wsc[UIDX["k%d" % i]].rearrange("p (a b) -> p a b", b=512),
                  [t_unit[UIDX["k%d" % i]]], [t_wk], dq_wk)
            P.dma(P.pool, wv[:, :, i * 512:(i + 1) * 512], wsc[UIDX["v%d" % i]].rearrange("p (a b) -> p a b", b=512),
                  [t_unit[UIDX["v%d" % i]]], [t_wv], dq_wv)
        xin1 = [AR.alloc([128, 4, 1024], F32) for _ in range(2)]
        t_xin1 = [T("xin1_0"), T("xin1_1")]
        dq_xin1 = [P.dmaq("xin1_0"), P.dmaq("xin1_1")]
        junk_cur = [AR.alloc([128, 1024], BF16)]
        t_junk = T("junk")
        ssq = [AR.alloc([128, 4], F32) for _ in range(2)]
        t_ssq = [T("ssq0"), T("ssq1")]
        rinv = [AR.alloc([128, 4], F32) for _ in range(2)]
        t_rinv = [T("rinv0"), T("rinv1")]
        xn = [AR.alloc([128, 1024], BF16) for _ in range(2)]
        t_xn = [T("xn0"), T("xn1")]
        xnT1 = [AR.alloc([128, 8, 512], BF16) for _ in range(2)]
        t_xnT1 = [T("xnT1_0"), T("xnT1_1")]
        sqb = [AR.alloc([128, 512], BF16) for _ in range(2)]
        t_sqb = [T("sqb0"), T("sqb1")]
        rkf = [AR.alloc([128, 512], F32) for _ in range(2)]
        t_rkf = [T("rkf0"), T("rkf1")]
        kst = [AR.alloc([128, 8, 512], BF16) for _ in range(2)]
        t_kst = [T("kst0"), T("kst1")]
        dq_kst = [P.dmaq("kst0"), P.dmaq("kst1")]
        vst = [AR.alloc([128, 2, 8, 2, 129], BF16) for _ in range(2)]
        t_vst = [T("vst0"), T("vst1")]
        dq_vst = [P.dmaq("vst0"), P.dmaq("vst1")]
        t_kt = T("kt_dram")
        t_vv = T("vv_dram")
        for b in range(2):
            P.emit(P.dve, lambda e, b=b: e.memset(vst[b].rearrange("p a b c d -> p (a b c d)"), 1.0), [], [t_vst[b]])

        def norm_tile(src, np_, t_src, ssq_ap, rinv_ap, t_s, t_r, xn_ap, t_x):
            jv = junk_cur[0][0:np_, :]
            P.emit(P.dve, lambda e: e.memset(ssq_ap, 0.0), [], [t_s])
            P.emit(P.act, lambda e: e.activation(out=jv, in_=src, func=AF.Square, accum_out=ssq_ap),
                   [t_src], [t_junk, t_s])
            P.emit(P.act, lambda e: e.activation(out=rinv_ap, in_=ssq_ap, func=AF.Ln, scale=1.0 / D,
                                                 bias=epsc[0:np_, :]), [t_s], [t_r])
            P.emit(P.act, lambda e: e.activation(out=rinv_ap, in_=rinv_ap, func=AF.Exp, scale=-0.5), [t_r], [t_r])
            P.emit(P.dve, lambda e: e.tensor_scalar(out=xn_ap, in0=src, scalar1=rinv_ap, scalar2=None, op0=ALU.mult),
                   [t_src, t_r], [t_x])

        def transpose_tile(xn_ap, np_, t_x, dst, t_dst, use_act):
            bi = nbank()
            pv = bf_view(bi)[:, 0:8 * np_].rearrange("p (a b) -> p a b", b=np_)
            for kc in range(8):
                P.emit(P.pe, lambda e, kc=kc: e.transpose(out=pv[:, kc, :], in_=xn_ap[:, kc * 128:(kc + 1) * 128],
                                                          identity=identb[0:np_, 0:np_]),
                       [t_x], [tb[bi]], ms=(kc == 7))
            if use_act:
                P.emit(P.act, lambda e: e.copy(out=dst, in_=pv), [tb[bi]], [t_dst])
            else:
                P.emit(P.dve, lambda e: e.tensor_copy(out=dst, in_=pv), [tb[bi]], [t_dst])

        def x1_load(c):
            b = c % 2
            P.dma(P.pool, xin1[b], x_all[c * 512:(c + 1) * 512, :].rearrange("(t p) d -> p t d", p=128), [],
                  [t_xin1[b]], dq_xin1[b])

        xn4 = [AR.alloc([128, 1024], BF16) for _ in range(4)]
        t_xn4 = [T("xn4_%d" % i) for i in range(4)]

        def p1_norm(c):
            b = c % 2
            for t in range(4):
                norm_tile(xin1[b][:, t, :], 128, t_xin1[b], ssq[b][:, t:t + 1], rinv[b][:, t:t + 1], t_ssq[b], t_rinv[b],
                          xn4[t], t_xn4[t])

        def p1_transpose(c):
            b = c % 2
            for t in range(4):
                transpose_tile(xn4[t], 128, t_xn4[t], xnT1[b][:, :, t * 128:(t + 1) * 128], t_xnT1[b],
                               use_act=(t % 2 == 0))

        def p1_norm_transpose(c):
            p1_norm(c)
            p1_transpose(c)

        x1_load(0)
        if nkv > 1:
            x1_load(1)
        p1_norm_transpose(0)
        for c in range(nkv):
            b = c % 2
            if conv_next[0] < len(conv_order):
                oi = conv_next[0]
                conv_cast_store(oi, act_only=True)
                if oi + 2 < len(conv_order):
                    conv_load(oi + 2)
                conv_next[0] += 1
            if c + 1 < nkv:
                p1_norm(c + 1)
            pend = None

            def k_tail(h, bi, pk, b=b, c=c):
                sb = h % 2
                b2i = nbank()
                p2 = banks[b2i][:, :]
                P.emit(P.pe, lambda e, p2=p2, sb=sb: e.matmul(out=p2, lhsT=onesb, rhs=sqb[sb], start=True, stop=True),
                       [t_sqb[sb]], [tb[b2i]])
                P.emit(P.act, lambda e, p2=p2, sb=sb: e.activation(out=rkf[sb], in_=p2, func=AF.Ln, scale=1.0 / DH,
                                                                   bias=epsc), [tb[b2i]], [t_rkf[sb]])
                P.emit(P.act, lambda e, sb=sb: e.activation(out=rkf[sb], in_=rkf[sb], func=AF.Exp, scale=-0.5),
                       [t_rkf[sb]], [t_rkf[sb]])
                P.emit(P.dve, lambda e, pk=pk, sb=sb, h=h, b=b: e.scalar_tensor_tensor(
                    out=kst[b][:, h, :], in0=pk, scalar=kg[:, 0:1], in1=rkf[sb], op0=ALU.mult, op1=ALU.mult),
                    [tb[bi], t_rkf[sb]], [t_kst[b]])
                P.emit(P.dve, lambda e, h=h, b=b, c=c: e.tensor_reduce(
                    out=kmean[:, h, 2 * c:2 * c + 2], in_=kst[b][:, h, :].rearrange("p (a l) -> p a l", l=256),
                    axis=AX.X, op=ALU.add), [t_kst[b]], [t_kmean])

            for h in range(NH):
                bi = nbank()
                pk = banks[bi][:, :]
                for kc in range(8):
                    P.emit(P.pe, lambda e, kc=kc, h=h, pk=pk, b=b: e.matmul(out=pk, lhsT=wk[:, kc, h * 128:(h + 1) * 128],
                                                                           rhs=xnT1[b][:, kc, :], start=(kc == 0),
                                                                           stop=(kc == 7)),
                           [t_wk, t_xnT1[b]], [tb[bi]], ms=(kc == 7))
                sb = h % 2
                P.emit(P.act, lambda e, pk=pk, sb=sb: e.activation(out=sqb[sb], in_=pk, func=AF.Square),
                       [tb[bi]], [t_sqb[sb]])
                held.add(bi)
                if pend is not None:
                    k_tail(*pend)
                    held.discard(pend[1])
                pend = (h, bi, pk)
            k_tail(*pend)
            held.discard(pend[1])
            P.dma(P.pool, kt_d[:, :, c * 512:(c + 1) * 512].rearrange("h p t -> p h t"), kst[b], [t_kst[b]], [t_kt],
                  dq_kst[b])
            if c + 2 < nkv:
                x1_load(c + 2)
            for blk in range(2):
                for kh in range(2):
                    for hf in range(2):
                        bi = nbank()
                        pv_ = banks[bi][:, :]
                        for kc in range(8):
                            P.emit(P.pe, lambda e, kc=kc, blk=blk, kh=kh, hf=hf, pv_=pv_, b=b: e.matmul(
                                out=pv_, lhsT=xnT1[b][:, kc, blk * 256 + kh:blk * 256 + 256:2],
                                rhs=wv[:, kc, hf * 512:(hf + 1) * 512], start=(kc == 0), stop=(kc == 7)),
                                [t_wv, t_xnT1[b]], [tb[bi]], ms=(kc == 7))
                        dst = vst[b][:, blk, hf * 4:(hf + 1) * 4, kh, 0:128]
                        src = pv_.rearrange("p (a d) -> p a d", d=128)
                        if hf == 0:
                            P.emit(P.act, lambda e, dst=dst, src=src: e.copy(out=dst, in_=src), [tb[bi]], [t_vst[b]])
                        else:
                            P.emit(P.dve, lambda e, dst=dst, src=src: e.tensor_copy(out=dst, in_=src), [tb[bi]],
                                   [t_vst[b]])
                if blk == 0 and c + 1 < nkv:
                    p1_transpose(c + 1)
            P.dma(P.pool, vv_d[:, 2 * c:2 * c + 2].rearrange("p a b c d -> p (a b c d)"),
                  vst[b].rearrange("p a b c d -> p (a b c d)"), [t_vst[b]], [t_vv], dq_vst[b])
        while conv_next[0] < len(conv_order):
            oi = conv_next[0]
            conv_cast_store(oi, act_only=False)
            if oi + 2 < len(conv_order):
                conv_load(oi + 2)
            conv_next[0] += 1
        P.emit(P.dve, lambda e: e.tensor_copy(out=kmean_b.rearrange("p a b -> p (a b)"),
                                              in_=kmean.rearrange("p a b -> p (a b)")), [t_kmean], [t_kmean])
        ph1 = t_xn4 + t_xin1 + t_ssq + t_rinv + t_xn + t_sqb + t_rkf + t_kst + t_vst + t_xnT1 + [t_junk, t_wk, t_wv, t_kt, t_vv,
                                                                                         t_kmean] + tb + t_stg + t_wbf
        P.barrier(ph1)
        AR.off = phase_mark

        NSLOT = 3
        wr = [AR.alloc([128, 8, 512], BF16) for _ in range(NSLOT)]
        t_wr = [T("wr%d" % i) for i in range(NSLOT)]
        dq_wr = [P.dmaq("wr%d" % i) for i in range(NSLOT)]
        seq_all = []
        for g in range(nch):
            seq_all += CHUNK_SEQ
        wstate = {"issued": 0, "used": 0}

        def w_issue():
            i = wstate["issued"]
            if i >= len(seq_all):
                return
            nm = seq_all[i]
            s_ = i % NSLOT
            ui = UIDX[nm]
            nk = UNITS[nm][0][2]
            P.dma(P.sp, wr[s_][:, 0:nk, :].rearrange("p a b -> p (a b)"), wsc[ui, :, 0:nk * 512], [t_unit[ui]],
                  [t_wr[s_]], dq_wr[s_])
            wstate["issued"] += 1

        def w_next(expect, ahead=NSLOT - 1):
            i = wstate["used"]
            assert seq_all[i] == expect, (seq_all[i], expect)
            while wstate["issued"] < min(i + ahead + 1, len(seq_all)):
                w_issue()
            wstate["used"] += 1
            s_ = i % NSLOT
            return wr[s_], t_wr[s_]

        xin = AR.alloc([128, 4, 1024], F32)
        t_xin = T("xin")
        dq_xin = P.dmaq("xin")
        xh = AR.alloc([32, 2, 1024], F32)
        t_xh = T("xh")
        dq_xh = P.dmaq("xh")
        a2t = AR.alloc([128, 2, 8, 64], F32)
        b2t = AR.alloc([128, 2, 64], F32)
        gbt = AR.alloc([128, 2, 64], F32)
        t_tab = T("tab")
        dq_tab = P.dmaq("tab")
        junk_cur[0] = AR.alloc([128, 1024], BF16)
        ssq2 = AR.alloc([128, 8], F32)
        rinv2 = AR.alloc([128, 8], F32)
        t_ssq2 = [T("ssq2_%d" % i) for i in range(6)]
        t_rinv2 = [T("rinv2_%d" % i) for i in range(6)]
        xn = [AR.alloc([128, 1024], BF16) for _ in range(2)]
        xnT_raw = AR.alloc([128, 8 * 576], BF16)
        xnT = xnT_raw.rearrange("p (a b) -> p a b", b=576)
        t_xnT = T("xnT")
        R1 = AR.alloc([128, 8, 576 + 512], F32)
        a_t = R1[:, :, 0:576]
        y_t = R1[:, :, 576:1088]
        act_t = R1.rearrange("p a b -> p (a b)").bitcast(BF16)[:, 0:FC * 512].rearrange("p (a b) -> p a b", b=512)
        t_a = T("a")
        t_y = T("y")
        t_actt = T("act")
        ybf = [AR.alloc([128, 512], BF16) for _ in range(2)]
        t_ybf = [T("ybf0"), T("ybf1")]
        ysq = [AR.alloc([128, 512], BF16) for _ in range(2)]
        t_ysq = [T("ysq0"), T("ysq1")]
        mean_t = AR.alloc([128, 512], F32)
        rstd_t = AR.alloc([128, 512], F32)
        t_mean = T("mean")
        t_rstd = T("rstd")
        xc = [AR.alloc([128, 512], F32) for _ in range(2)]
        t_xc = [T("xc0"), T("xc1")]
        sig_t = [x_[:, 0:288].rearrange("p (a b) -> p a b", a=1) for x_ in xc]
        t_sig = t_xc
        ysl = AR.alloc([128, 8, 512], BF16)
        t_ysl = T("ysl")
        mT = ysl
        t_mT = t_ysl
        t1 = AR.alloc([128, 8, 512], BF16)
        t_t1 = T("t1")
        qT = AR.alloc([128, 8, 512], BF16)
        t_qT = T("qT")
        qf = [AR.alloc([128, 512], F32) for _ in range(2)]
        t_qf = [T("qf0"), T("qf1")]
        off_g = AR.off
        gcs = AR.alloc([128, 8, 512], BF16)
        gas = AR.alloc([128, 8, 512], BF16)
        assert AR.off - off_g == 4096
        outbuf = AR.ap[:, off_g:off_g + 4096].rearrange("p (t d) -> p t d", d=1024)
        t_gcs = T("gcs")
        t_gas = T("gas")
        attT = AR.alloc([128, 8, 512], BF16)
        t_attT = T("attT")
        hnT = xnT_raw[:, 0:8 * 512].rearrange("p (a b) -> p a b", b=512)
        t_hnT = t_xnT
        NKV = 2
        ktp = [AR.alloc([128, 2048], BF16) for _ in range(NKV)]
        vp = [AR.alloc([128, 8, 2, 129], BF16) for _ in range(NKV)]
        t_ktp = [T("ktp%d" % i) for i in range(NKV)]
        t_vp = [T("vp%d" % i) for i in range(NKV)]
        dq_ktp = [P.dmaq("ktp%d" % i) for i in range(NKV)]
        dq_vp = [P.dmaq("vp%d" % i) for i in range(NKV)]
        NPT = 4
        PT = [AR.alloc([128, 512], BF16) for _ in range(NPT)]
        t_PT = [T("PT%d" % i) for i in range(NPT)]
        gs_t = [AR.alloc([128, 64], F32) for _ in range(4)]
        m8_t = [AR.alloc([128, 8], F32) for _ in range(4)]
        tmp_t = [AR.alloc([128, 64], F32) for _ in range(4)]
        hiv_t = [AR.alloc([128, 64], BF16) for _ in range(4)]
        t_gs = [T("gs%d" % i) for i in range(4)]
        t_m8 = [T("m8%d" % i) for i in range(4)]
        t_tmp = [T("tmp%d" % i) for i in range(4)]
        t_hiv = [T("hiv%d" % i) for i in range(4)]
        rl_t = [AR.alloc([128, 1], F32) for _ in range(2)]
        t_rl = [T("rl0"), T("rl1")]
        atok = [AR.alloc([128, 128], BF16) for _ in range(2)]
        t_atok = [T("atok0"), T("atok1")]
        sg_t = xc
        t_sg = t_xc
        tm2 = xc
        t_tm2 = t_xc
        dq_out = P.dmaq("out")
        t_outd = T("out_dram")
        kvstate = {"n": 0}
        ptstate = {"n": 0}

        def mm_group(out_ap, bank_i, pairs, extra_reads, first=True, last=True):
            n_ = len(pairs)
            for i_, (l_, r_) in enumerate(pairs):
                P.emit(P.pe, lambda e, l_=l_, r_=r_, i_=i_: e.matmul(out=out_ap, lhsT=l_, rhs=r_,
                                                                     start=(first and i_ == 0),
                                                                     stop=(last and i_ == n_ - 1)),
                       extra_reads, [tb[bank_i]], ms=(i_ == n_ - 1))

        for g in range(nch):
            P.dma(P.pool, xin, x_own[g * 512:(g + 1) * 512, :].rearrange("(t p) d -> p t d", p=128), [], [t_xin], dq_xin)
            P.dma(P.pool, xh, x_halo[g * 64:(g + 1) * 64, :].rearrange("(o p) d -> p o d", p=32), [], [t_xh], dq_xh)
            P.dma(P.pool, a2t, a2_d[2 * g:2 * g + 2].partition_broadcast(128), [], [t_tab], dq_tab)
            P.dma(P.pool, b2t, b2_d[2 * g:2 * g + 2].partition_broadcast(128), [], [t_tab], dq_tab)
            P.dma(P.pool, gbt, gate_bias[2 * g:2 * g + 2].partition_broadcast(128), [], [t_tab], dq_tab)
            nbuf = [(xn[0], t_xn[0]), (xn[1], t_xn[1]), (qf[0].bitcast(BF16), t_qf[0]), (qf[1].bitcast(BF16), t_qf[1])]
            for o in range(2):
                nb_ap, nb_t = nbuf[o]
                norm_tile(xh[0:32, o, :], 32, t_xh, ssq2[0:32, 4 + o:5 + o], rinv2[0:32, 4 + o:5 + o], t_ssq2[4 + o],
                          t_rinv2[4 + o], nb_ap[0:32, :], nb_t)
            for t in range(4):
                nb_ap, nb_t = nbuf[(t + 2) % 4]
                norm_tile(xin[:, t, :], 128, t_xin, ssq2[:, t:t + 1], rinv2[:, t:t + 1], t_ssq2[t], t_rinv2[t], nb_ap, nb_t)
                if t == 1:
                    for o in range(2):
                        nb_ap2, nb_t2 = nbuf[o]
                        transpose_tile(nb_ap2[0:32, :], 32, nb_t2, xnT[:, :, o * 288:o * 288 + 32], t_xnT,
                                       use_act=(o == 0))
            for t in range(4):
                nb_ap, nb_t = nbuf[(t + 2) % 4]
                c0 = (t // 2) * 288 + 32 + (t % 2) * 128
                transpose_tile(nb_ap, 128, nb_t, xnT[:, :, c0:c0 + 128], t_xnT, use_act=(t % 2 == 0))
            for u in range(4):
                w_, tw = w_next("cv%d" % u)
                for ci in range(2):
                    cc = 2 * u + ci
                    bu = nbank()
                    bg = nbank()
                    bu2 = nbank()
                    bg2 = nbank()
                    for ob, (bcu, bcg) in enumerate(((bu, bg), (bu2, bg2))):
                        mm_group(banks[bcu][:, 0:288],
                                 bcu, [(w_[:, kc, ci * 128:(ci + 1) * 128], xnT[:, kc, ob * 288:(ob + 1) * 288])
                                       for kc in range(8)], [tw, t_xnT])
                        mm_group(banks[bcg][:, 0:288],
                                 bcg, [(w_[:, kc, 256 + ci * 128:256 + (ci + 1) * 128], xnT[:, kc, ob * 288:(ob + 1) * 288])
                                       for kc in range(8)], [tw, t_xnT])
                        sb = ob
                        P.emit(P.act, lambda e, bcg=bcg, sb=sb, ob=ob: e.activation(out=sig_t[sb][:, 0, :],
                                                                                   in_=banks[bcg][:, 0:288],
                                                                                   func=AF.Sigmoid),
                               [tb[bcg]], [t_sig[sb]])
                        P.emit(P.dve, lambda e, bcu=bcu, sb=sb, ob=ob, cc=cc: e.tensor_tensor(
                            out=a_t[:, cc, ob * 288:(ob + 1) * 288], in0=banks[bcu][:, 0:288], in1=sig_t[sb][:, 0, :],
                            op=ALU.mult), [tb[bcu], t_sig[sb]], [t_a])
            def q_main(h, w_, tw, hi_):
                bi = nbank()
                pq_ = banks[bi][:, :]
                for ob in range(2):
                    mm_group(pq_[:, ob * 256:(ob + 1) * 256], bi,
                             [(w_[:, kc, hi_ * 128:(hi_ + 1) * 128], xnT[:, kc, ob * 288 + 32:ob * 288 + 288])
                              for kc in range(8)], [tw, t_xnT])
                sb = h % 2
                P.emit(P.act, lambda e, pq_=pq_, sb=sb: e.activation(out=ysq[sb], in_=pq_, func=AF.Square),
                       [tb[bi]], [t_ysq[sb]])
                held.add(bi)
                return (h, bi, pq_)

            def q_stage_a(stt_):
                h, bi, pq_ = stt_
                sb = h % 2
                b2i = nbank()
                p2 = banks[b2i][:, :]
                mm_group(p2, b2i, [(onesb, ysq[sb])], [t_ysq[sb]])
                P.emit(P.act, lambda e, p2=p2, sb=sb: e.activation(out=xc[sb], in_=p2, func=AF.Ln, scale=1.0 / DH,
                                                                   bias=epsc), [tb[b2i]], [t_xc[sb]])
                P.emit(P.act, lambda e, sb=sb: e.activation(out=xc[sb], in_=xc[sb], func=AF.Exp, scale=-0.5),
                       [t_xc[sb]], [t_xc[sb]])
                P.emit(P.dve, lambda e, pq_=pq_, sb=sb: e.scalar_tensor_tensor(
                    out=qf[sb], in0=pq_, scalar=qg[:, 0:1], in1=xc[sb], op0=ALU.mult, op1=ALU.mult),
                    [tb[bi], t_xc[sb]], [t_qf[sb]])
                P.emit(P.act, lambda e, sb=sb, h=h: e.copy(out=qT[:, h, :], in_=qf[sb]), [t_qf[sb]], [t_qT])
                held.discard(bi)
                return h

            def q_stage_b(h):
                sb = h % 2
                for t in range(4):
                    ob = t // 2
                    bgi = nbank()
                    pg = banks[bgi][:, 0:64]
                    mm_group(pg, bgi, [(qT[:, h, t * 128:(t + 1) * 128], kmean_b[:, h, :])], [t_qT, t_kmean])
                    P.emit(P.dve, lambda e, pg=pg, t=t, ob=ob: e.tensor_tensor(out=gs_t[t], in0=pg,
                                                                              in1=gbt[:, ob, :], op=ALU.add),
                           [tb[bgi], t_tab], [t_gs[t]])
                    P.emit(P.dve, lambda e, t=t: e.max(out=m8_t[t], in_=gs_t[t]), [t_gs[t]], [t_m8[t]])
                    P.emit(P.dve, lambda e, t=t, ob=ob, h=h: e.scalar_tensor_tensor(
                        out=tmp_t[t], in0=gs_t[t], scalar=m8_t[t][:, 2:3], in1=a2t[:, ob, h, :], op0=ALU.is_ge,
                        op1=ALU.mult), [t_gs[t], t_m8[t], t_tab], [t_tmp[t]])
                    P.emit(P.dve, lambda e, t=t, ob=ob: e.tensor_tensor(out=hiv_t[t], in0=tmp_t[t],
                                                                       in1=b2t[:, ob, :], op=ALU.add),
                           [t_tmp[t], t_tab], [t_hiv[t]])
                return h

            def q_stage_c(h):
                for t in range(4):
                    bti = nbank()
                    ptv = bf_view(bti)[0:64, 0:128]
                    P.emit(P.pe, lambda e, ptv=ptv, t=t: e.transpose(out=ptv, in_=hiv_t[t], identity=identb),
                           [t_hiv[t]], [tb[bti]])
                    P.emit(P.act, lambda e, ptv=ptv, h=h, t=t: e.copy(out=VT[0:64, h, t * 128:(t + 1) * 128], in_=ptv),
                           [tb[bti]], [t_vt64])

            def gate_mm(w_, tw, ci, cc, dst, tdst):
                bi = nbank()
                for ob in range(2):
                    mm_group(banks[bi][:, ob * 256:(ob + 1) * 256], bi,
                             [(w_[:, kc, ci * 128:(ci + 1) * 128], xnT[:, kc, ob * 288 + 32:ob * 288 + 288])
                              for kc in range(8)], [tw, t_xnT])
                P.emit(P.act, lambda e, bi=bi, dst=dst, cc=cc: e.activation(out=dst[:, cc, :], in_=banks[bi][:, :],
                                                                           func=AF.Sigmoid), [tb[bi]], [tdst])

            pa = pb = pc = None
            for u in range(2):
                wq, twq = w_next("q%d" % u, 2)
                wg, twg = w_next("gc%d" % u, 1)
                for hi_ in range(4):
                    h = 4 * u + hi_
                    cur = q_main(h, wq, twq, hi_)
                    na = q_stage_a(pa) if pa is not None else None
                    gate_mm(wg, twg, hi_, h, gcs, t_gcs)
                    if pc is not None:
                        q_stage_c(pc)
                    nb_ = q_stage_b(pb) if pb is not None else None
                    pa, pb, pc = cur, na, nb_
            for u in range(2):
                wg, twg = w_next("ga%d" % u, 2)
                for ci in range(4):
                    gate_mm(wg, twg, ci, 4 * u + ci, gas, t_gas)
                    na = q_stage_a(pa) if pa is not None else None
                    if pc is not None:
                        q_stage_c(pc)
                    nb_ = q_stage_b(pb) if pb is not None else None
                    pa, pb, pc = None, na, nb_
            assert pa is None and pb is None and pc is None
            def conv_cc(cc):
                yv = y_t[:, cc, :].rearrange("p (o l) -> p o l", l=256)

                def av(j, cc=cc):
                    return a_t[:, cc, :].rearrange("p (o l) -> p o l", l=288)[:, :, 2 + j:2 + j + 256]
                P.emit(P.dve, lambda e, yv=yv, av=av, cc=cc: e.tensor_scalar(out=yv, in0=av(0), scalar1=cw[:, cc, 0:1],
                                                                            scalar2=cb[:, cc:cc + 1], op0=ALU.mult,
                                                                            op1=ALU.add), [t_a], [t_y])
                for j in range(1, 31):
                    P.emit(P.dve, lambda e, yv=yv, av=av, cc=cc, j=j: e.scalar_tensor_tensor(
                        out=yv, in0=av(j), scalar=cw[:, cc, j:j + 1], in1=yv, op0=ALU.mult, op1=ALU.add), [t_a, t_y], [t_y])

            def normalize_head(h):
                for t in range(4):
                    ob_i = 4 + t
                    o_ap = banks[ob_i][:, 0:129]
                    mb = t % 2
                    P.emit(P.dve, lambda e, o_ap=o_ap, mb=mb: e.reciprocal(out=rl_t[mb], in_=o_ap[:, 128:129]),
                           [tb[ob_i]], [t_rl[mb]])
                    P.emit(P.dve, lambda e, o_ap=o_ap, mb=mb: e.tensor_scalar(out=atok[mb], in0=o_ap[:, 0:128],
                                                                              scalar1=rl_t[mb][:, 0:1], scalar2=None,
                                                                              op0=ALU.mult),
                           [tb[ob_i], t_rl[mb]], [t_atok[mb]])
                    bti = (srot[0] - 3) % 4 if t % 2 == 0 else srot[0] % 4
                    ptv = bf_view(bti)[:, 0:128]
                    P.emit(P.pe, lambda e, ptv=ptv, mb=mb: e.transpose(out=ptv, in_=atok[mb], identity=identb),
                           [t_atok[mb]], [tb[bti]])
                    P.emit(P.act, lambda e, ptv=ptv, h=h, t=t: e.copy(out=attT[:, h, t * 128:(t + 1) * 128], in_=ptv),
                           [tb[bti]], [t_attT])

            nblk = 8 * g + 8
            npp = nblk // 8
            units = [(h, n, kh) for h in range(NH) for n in range(nblk) for kh in range(2)]
            piece_list = [(h, n0) for h in range(NH) for n0 in range(0, nblk, 8)]
            piece_slot = {}
            piece_issued = [0]

            def ensure_piece(k):
                while piece_issued[0] <= k and piece_issued[0] < len(piece_list):
                    h_, n0_ = piece_list[piece_issued[0]]
                    s_ = kvstate["n"] % NKV
                    kvstate["n"] += 1
                    P.dma(P.pool, ktp[s_], kt_d[h_, :, n0_ * 256:(n0_ + 8) * 256], [t_kt], [t_ktp[s_]], dq_ktp[s_])
                    P.dma(P.pool, vp[s_], vv_d[:, n0_:n0_ + 8, h_, :, :], [t_vv], [t_vp[s_]], dq_vp[s_])
                    piece_slot[piece_issued[0]] = s_
                    piece_issued[0] += 1

            LA = 2
            srot = [0]
            ensure_piece(1)
            st = {}
            for idx in range(len(units) + LA):
                if idx < len(units):
                    h, n, kh = units[idx]
                    s_ = piece_slot[h * npp + n // 8]
                    nl = n % 8
                    bi = srot[0]
                    srot[0] = (srot[0] + 1) % 4
                    S_ = banks[bi][:, :]
                    cand = n >= 8 * g
                    P.emit(P.pe, lambda e, S_=S_, s_=s_, nl=nl, kh=kh, h=h: e.matmul(
                        out=S_, lhsT=ktp[s_][:, nl * 256 + kh:nl * 256 + 256:2], rhs=qT[:, h, :], start=True,
                        stop=False), [t_ktp[s_], t_qT], [tb[bi]], ms=False)
                    P.emit(P.pe, lambda e, S_=S_, n=n, h=h, cand=cand: e.matmul(
                        out=S_, lhsT=esel_lhsT(n), rhs=VT[0:65, h, :], start=False, stop=(not cand)),
                        [t_vt64], [tb[bi]], ms=(not cand))
                    if cand:
                        ob = (n - 8 * g) // 4
                        cnd = (n - 8 * g) % 4
                        P.emit(P.pe, lambda e, S_=S_, ob=ob, cnd=cnd, kh=kh: e.matmul(
                            out=S_[:, ob * 256:(ob + 1) * 256], lhsT=identb, rhs=cms[:, cnd, kh, :], start=False,
                            stop=True), [], [tb[bi]], ms=True)
                    st[idx] = (bi, s_)
                j = idx - LA
                if j >= 0:
                    h, n, kh = units[j]
                    bi, s_ = st.pop(j)
                    S_ = banks[bi][:, :]
                    nl = n % 8
                    if n % 8 == 0 and kh == 0:
                        ensure_piece(h * npp + n // 8 + 1)
                    first = (n == 0 and kh == 0)
                    lastblk = (n == nblk - 1 and kh == 1)
                    r_ = ptstate["n"] % NPT
                    ptstate["n"] += 1
                    P.emit(P.act, lambda e, S_=S_, r_=r_, h=h, kh=kh: e.activation(
                        out=PT[r_], in_=S_, func=AF.Exp, bias=kb[:, 2 * h + kh:2 * h + kh + 1]),
                        [tb[bi]], [t_PT[r_]])
                    for t in range(4):
                        ob_i = 4 + t
                        o_ap = banks[ob_i][:, 0:129]
                        P.emit(P.pe, lambda e, o_ap=o_ap, r_=r_, t=t, s_=s_, nl=nl, kh=kh, first=first,
                               lastblk=lastblk: e.matmul(out=o_ap, lhsT=PT[r_][:, t * 128:(t + 1) * 128],
                                                         rhs=vp[s_][:, nl, kh, :], start=first, stop=lastblk),
                               [t_PT[r_], t_vp[s_]], [tb[ob_i]], ms=(t == 3))
                    if lastblk:
                        normalize_head(h)
                        conv_cc(h)
            for u in range(2):
                w_, tw = w_next("ao%d" % u)
                for ci in range(4):
                    cc = 4 * u + ci
                    bi = nbank()
                    mm_group(banks[bi][:, :], bi, [(w_[:, kc, ci * 128:(ci + 1) * 128], attT[:, kc, :]) for kc in range(8)],
                             [tw, t_attT])
                    P.emit(P.dve, lambda e, bi=bi, cc=cc: e.tensor_tensor(out=t1[:, cc, :], in0=banks[bi][:, :],
                                                                          in1=gas[:, cc, :], op=ALU.mult),
                           [tb[bi], t_gas], [t_t1])
            bs1 = nbank()
            bs2 = nbank()
            for cc in range(8):
                sb = cc % 2
                P.emit(P.act, lambda e, sb=sb, cc=cc: e.copy(out=ybf[sb], in_=y_t[:, cc, :]), [t_y], [t_ybf[sb]])
                P.emit(P.act, lambda e, sb=sb, cc=cc: e.activation(out=ysq[sb], in_=y_t[:, cc, :], func=AF.Square),
                       [t_y], [t_ysq[sb]])
                P.emit(P.pe, lambda e, sb=sb, cc=cc, bs1=bs1: e.matmul(out=banks[bs1][:, :], lhsT=onesb, rhs=ybf[sb],
                                                              start=(cc == 0), stop=(cc == 7)),
                       [t_ybf[sb]], [tb[bs1]], ms=True)
                P.emit(P.pe, lambda e, sb=sb, cc=cc, bs2=bs2: e.matmul(out=banks[bs2][:, :], lhsT=onesb, rhs=ysq[sb],
                                                              start=(cc == 0), stop=(cc == 7)),
                       [t_ysq[sb]], [tb[bs2]], ms=True)
            P.emit(P.dve, lambda e, bs1=bs1: e.tensor_scalar(out=mean_t, in0=banks[bs1][:, :], scalar1=1.0 / D, scalar2=None,
                                                    op0=ALU.mult), [tb[bs1]], [t_mean])
            P.emit(P.dve, lambda e: e.tensor_tensor(out=rstd_t, in0=mean_t, in1=mean_t, op=ALU.mult), [t_mean], [t_rstd])
            P.emit(P.dve, lambda e, bs2=bs2: e.scalar_tensor_tensor(out=rstd_t, in0=banks[bs2][:, :], scalar=1.0 / D, in1=rstd_t,
                                                           op0=ALU.mult, op1=ALU.subtract), [tb[bs2], t_rstd], [t_rstd])
            P.emit(P.act, lambda e: e.activation(out=rstd_t, in_=rstd_t, func=AF.Ln, bias=epsc), [t_rstd], [t_rstd])
            P.emit(P.act, lambda e: e.activation(out=rstd_t, in_=rstd_t, func=AF.Exp, scale=-0.5), [t_rstd], [t_rstd])
            for cc in range(8):
                sb = cc % 2
                P.emit(P.dve, lambda e, sb=sb, cc=cc: e.tensor_tensor(out=xc[sb], in0=y_t[:, cc, :], in1=mean_t,
                                                                      op=ALU.subtract), [t_y, t_mean], [t_xc[sb]])
                P.emit(P.dve, lambda e, sb=sb: e.tensor_tensor(out=xc[sb], in0=xc[sb], in1=rstd_t, op=ALU.mult),
                       [t_xc[sb], t_rstd], [t_xc[sb]])
                P.emit(P.act, lambda e, sb=sb, cc=cc: e.activation(out=ysl[:, cc, :], in_=xc[sb], func=AF.Silu,
                                                                   scale=lg[:, cc:cc + 1], bias=lb[:, cc:cc + 1]),
                       [t_xc[sb]], [t_ysl])
            for u in range(2):
                w_, tw = w_next("co%d" % u)
                for ci in range(4):
                    cc = 4 * u + ci
                    bi = nbank()
                    sb = cc % 2
                    mm_group(banks[bi][:, :], bi, [(w_[:, kc, ci * 128:(ci + 1) * 128], ysl[:, kc, :]) for kc in range(8)],
                             [tw, t_ysl])
                    P.emit(P.dve, lambda e, bi=bi, cc=cc, sb=sb: e.tensor_tensor(out=tm2[sb], in0=banks[bi][:, :],
                                                                                 in1=gcs[:, cc, :], op=ALU.mult),
                           [tb[bi], t_gcs], [t_tm2[sb]])
                    P.emit(P.dve, lambda e, cc=cc, sb=sb: e.tensor_tensor(out=t1[:, cc, :], in0=tm2[sb], in1=t1[:, cc, :],
                                                                          op=ALU.add), [t_tm2[sb], t_t1], [t_t1])
            for hf in range(2):
                w_, tw = w_next("wo%d" % hf)
                for t in range(4):
                    bi = nbank()
                    mm_group(banks[bi][:, :], bi, [(t1[:, kc, t * 128:(t + 1) * 128], w_[:, kc, :]) for kc in range(8)],
                             [tw, t_t1])
                    P.emit(P.dve, lambda e, bi=bi, t=t, hf=hf: e.tensor_tensor(
                        out=xin[:, t, hf * 512:(hf + 1) * 512], in0=banks[bi][:, :], in1=xin[:, t, hf * 512:(hf + 1) * 512],
                        op=ALU.add), [tb[bi], t_xin], [t_xin])
            for t in range(4):
                nb_ap, nb_t = nbuf[t]
                norm_tile(xin[:, t, :], 128, t_xin, ssq2[:, t:t + 1], rinv2[:, t:t + 1], t_ssq2[t], t_rinv2[t], nb_ap, nb_t)
            for t in range(4):
                nb_ap, nb_t = nbuf[t]
                transpose_tile(nb_ap, 128, nb_t, hnT[:, :, t * 128:(t + 1) * 128], t_hnT, use_act=(t % 2 == 0))
            for u in range(11):
                w_, tw = w_next("gu%d" % u)
                for ci in range(2):
                    fc = 2 * u + ci
                    sb = fc % 2
                    bgt = nbank()
                    mm_group(banks[bgt][:, :], bgt, [(w_[:, kc, ci * 128:(ci + 1) * 128], hnT[:, kc, :]) for kc in range(8)],
                             [tw, t_hnT])
                    but = nbank()
                    mm_group(banks[but][:, :], but,
                             [(w_[:, kc, 256 + ci * 128:256 + (ci + 1) * 128], hnT[:, kc, :]) for kc in range(8)],
                             [tw, t_hnT])
                    P.emit(P.act, lambda e, bgt=bgt, sb=sb: e.activation(out=sg_t[sb], in_=banks[bgt][:, :], func=AF.Silu),
                           [tb[bgt]], [t_sg[sb]])
                    P.emit(P.dve, lambda e, but=but, sb=sb, fc=fc: e.tensor_tensor(out=act_t[:, fc, :], in0=banks[but][:, :],
                                                                                   in1=sg_t[sb], op=ALU.mult),
                           [tb[but], t_sg[sb]], [t_actt, t_a, t_y])
            for hf in range(2):
                for gi, (f0, nf) in enumerate(((0, 8), (8, 8), (16, 6))):
                    w_, tw = w_next("dn%d_%d" % (hf, gi))
                    for t in range(4):
                        bi = 4 + t
                        for fl in range(nf):
                            fc = f0 + fl
                            P.emit(P.pe, lambda e, bi=bi, t=t, fc=fc, fl=fl, w_=w_: e.matmul(
                                out=banks[bi][:, :], lhsT=act_t[:, fc, t * 128:(t + 1) * 128], rhs=w_[:, fl, :],
                                start=(fc == 0), stop=(fc == FC - 1)), [tw, t_actt], [tb[bi]], ms=(fl == nf - 1))
                for t in range(4):
                    bi = 4 + t
                    P.emit(P.dve, lambda e, bi=bi, t=t, hf=hf: e.tensor_tensor(
                        out=outbuf[:, t, hf * 512:(hf + 1) * 512], in0=banks[bi][:, :],
                        in1=xin[:, t, hf * 512:(hf + 1) * 512], op=ALU.add), [tb[bi], t_xin], [t_gcs, t_gas])
            P.dma(P.pool, out_d[g * 512:(g + 1) * 512, :].rearrange("(t p) d -> p t d", p=128), outbuf, [t_gcs, t_gas],
                  [t_outd], dq_out)
            t_a.r += t_actt.r
            t_y.r += t_actt.r
            if t_actt.w is not None:
                t_a.r.append(t_actt.w)
                t_y.r.append(t_actt.w)
        P.wait_all(P.pool, [t_outd])
        P.wait_all(P.sp, [t_outd])
        P.run()
    return nc


def host_tables(j):
    slopes = 2.0 ** (-8.0 * np.arange(1, NH + 1) / NH)
    gate_bias = np.zeros((16, 64), np.float32)
    a2 = np.zeros((16, 8, 64), np.float32)
    b2 = np.full((16, 64), NEG, np.float32)
    for i in range(16):
        cur = 4 * i + j
        gate_bias[i, cur:] = -1e30
        b2[i, cur] = 0.0
        for n in range(cur):
            a2[i, :, n] = -NEG - slopes * 256.0 * (cur - n)
    cmsel = np.zeros((128, 4, 2, 256), np.float32)
    p = np.arange(128)[:, None]
    rq = np.arange(256)[None, :]
    for kh in range(2):
        rk = 2 * p + kh
        cmsel[:, j, kh, :] = np.where(rq >= rk, 0.0, NEG)
    lo = np.zeros((1, 8, 512), np.float32)
    for h in range(8):
        lo[0, h, :] = -slopes[h] * (np.arange(512) % 256)
    kbias = np.zeros((128, 16), np.float32)
    for h in range(8):
        for kh in range(2):
            kbias[:, 2 * h + kh] = slopes[h] * (2 * np.arange(128) + kh)
    return {"gate_bias": gate_bias, "a2": a2, "b2": b2, "cmsel": cmsel.reshape(128, -1),
            "lo": lo.reshape(1, -1), "kbias": kbias, "ident": np.eye(128, dtype=np.float32)}


def make_in_maps(inputs):
    x = np.asarray(inputs["x"], np.float32)
    shared = {
        "w_in": np.ascontiguousarray(inputs["w_in"][0]), "w_conv_out": np.ascontiguousarray(inputs["w_conv_out"][0]),
        "w_attn_out": np.ascontiguousarray(inputs["w_attn_out"][0]), "w_out": np.ascontiguousarray(inputs["w_out"][0]),
        "w_ffn_gate": np.ascontiguousarray(inputs["w_ffn_gate"][0]), "w_ffn_up": np.ascontiguousarray(inputs["w_ffn_up"][0]),
        "w_ffn_down": np.ascontiguousarray(inputs["w_ffn_down"][0]),
        "norm1_g": np.asarray(inputs["norm1_g"], np.float32).reshape(1, D),
        "norm2_g": np.asarray(inputs["norm2_g"], np.float32).reshape(1, D),
        "dw_w": np.ascontiguousarray(inputs["dw_w"][0]), "dw_b": np.asarray(inputs["dw_b"], np.float32).reshape(1, D),
        "conv_ln_g": np.asarray(inputs["conv_ln_g"], np.float32).reshape(1, D),
        "conv_ln_b": np.asarray(inputs["conv_ln_b"], np.float32).reshape(1, D),
        "q_norm_g": np.asarray(inputs["q_norm_g"], np.float32).reshape(1, DH),
        "k_norm_g": np.asarray(inputs["k_norm_g"], np.float32).reshape(1, DH),
    }
    shared = {k: np.asarray(v, np.float32) for k, v in shared.items()}
    maps = []
    for c in range(8):
        b, j = c // 4, c % 4
        xb = x[b]
        xblk = xb.reshape(NBK, L, D)
        own = [4 * i + j for i in range(16)]
        x_own = np.ascontiguousarray(xblk[own].reshape(4096, D))
        halo = np.zeros((16, 32, D), np.float32)
        for i, n in enumerate(own):
            if n > 0:
                halo[i] = xb[n * L - 32:n * L]
        m = dict(shared)
        m.update(host_tables(j))
        m["x_all"] = np.ascontiguousarray(xb)
        m["x_own"] = x_own
        m["x_halo"] = halo.reshape(512, D)
        maps.append(m)
    return maps


_NC_CACHE = {}


def kernel(**inputs):
    if "nc" not in _NC_CACHE:
        _NC_CACHE["nc"] = build_nc()
    nc = _NC_CACHE["nc"]
    maps = make_in_maps(inputs)
    res = run_bass_kernel_spmd(nc, maps, core_ids=list(range(8)))
    out = np.zeros((2, S, D), np.float32)
    ov = out.reshape(2, NBK, L, D)
    for c in range(8):
        b, j = c // 4, c % 4
        o = np.asarray(res.results[c]["out_own"], np.float32).reshape(16, L, D)
        for i in range(16):
            ov[b, 4 * i + j] = o[i]
    return out
```

```python
import numpy as np
from contextlib import ExitStack
import concourse.bass as bass
import concourse.mybir as mybir
from concourse.bass_utils import run_bass_kernel_spmd

F32 = mybir.dt.float32
BF16 = mybir.dt.bfloat16
AF = mybir.ActivationFunctionType
ALU = mybir.AluOpType
AX = mybir.AxisListType

D = 1024
KC = 8
NH = 8
DH = 128
FF = 2816
FC = 22
S = 16384
L = 256
NBK = 64
EPS = 1e-6
NEG = -30000.0


class T:
    __slots__ = ("name", "w", "r")

    def __init__(self, name):
        self.name = name
        self.w = None
        self.r = []


class Q:
    def __init__(self, name, sem, scale=1):
        self.name = name
        self.sem = sem
        self.scale = scale
        self.count = 0
        self.ops = []
        self.seen = {}


class Prog:
    def __init__(self, nc, es):
        self.nc = nc
        self.es = es
        self.pe = Q("pe", self.newsem("s_pe"))
        self.act = Q("act", self.newsem("s_act"))
        self.dve = Q("dve", self.newsem("s_dve"))
        self.pool = Q("pool", self.newsem("s_pool"))
        self.sp = Q("sp", self.newsem("s_sp"))
        self.engines = [self.pe, self.act, self.dve, self.pool, self.sp]

    def newsem(self, name):
        return self.es.enter_context(self.nc.semaphore(name))

    def dmaq(self, name):
        return Q(name, self.newsem("d_" + name), 16)

    def _wait(self, q, tok):
        sq, cnt = tok
        if q.seen.get(sq, 0) >= cnt:
            return
        q.seen[sq] = cnt
        q.ops.append(lambda e, s=sq.sem, v=cnt * sq.scale: e.wait_ge(s, v))

    def emit(self, q, fn, reads=(), writes=(), ms=True, sig=None):
        sig = sig or q
        for t in reads:
            if t.w is not None:
                self._wait(q, t.w)
        for t in writes:
            if t.w is not None and t.w[0] is not q:
                self._wait(q, t.w)
            for tok in t.r:
                if tok[0] is not q:
                    self._wait(q, tok)
        if ms:
            sig.count += 1
            tok = (sig, sig.count)
            q.ops.append(lambda e, s=sig.sem, v=sig.scale: fn(e).then_inc(s, v))
        else:
            tok = (sig, sig.count + 1)
            q.ops.append(lambda e: fn(e))
        for t in writes:
            t.w = tok
            t.r = []
        for t in reads:
            t.r.append(tok)
            if len(t.r) > 16:
                best = {}
                for sq, c in t.r:
                    if sq not in best or best[sq] < c:
                        best[sq] = c
                t.r = list(best.items())
        return tok

    def dma(self, q, out, in_, reads, writes, sig, **kw):
        return self.emit(q, lambda e: e.dma_start(out=out, in_=in_, **kw), reads, writes, sig=sig)

    def wait_all(self, q, ts):
        for t in ts:
            if t.w is not None:
                self._wait(q, t.w)
            for tok in t.r:
                self._wait(q, tok)

    def barrier(self, ts):
        for q in self.engines:
            self.wait_all(q, ts)

    def run(self):
        nc = self.nc
        with nc.Block() as block:
            @block.tensor
            def _(e):
                for op in self.pe.ops:
                    op(e)

            @block.scalar
            def _(e):
                for op in self.act.ops:
                    op(e)

            @block.vector
            def _(e):
                for op in self.dve.ops:
                    op(e)

            @block.gpsimd
            def _(e):
                for op in self.pool.ops:
                    op(e)

            @block.sync
            def _(e):
                for op in self.sp.ops:
                    op(e)


class Arena:
    def __init__(self, ap, nwords):
        self.ap = ap
        self.n = nwords
        self.off = 0

    def alloc(self, shape, dtype):
        shape = list(shape)
        np_ = shape[0]
        free = 1
        for s_ in shape[1:]:
            free *= s_
        esz = 4 if dtype == F32 else 2
        words = (free * esz + 3) // 4
        words = (words + 7) // 8 * 8
        assert self.off + words <= self.n, ("arena overflow", self.off, words, self.n)
        v = self.ap[0:np_, self.off:self.off + words]
        self.off += words
        if dtype != F32:
            v = v.bitcast(dtype)
        v = v[:, 0:free]
        if len(shape) == 2:
            return v
        names = " ".join("d%d" % i for i in range(len(shape) - 1))
        kw = {"d%d" % i: shape[i + 1] for i in range(len(shape) - 1)}
        return v.rearrange("p (%s) -> p %s" % (names, names), **kw)


def unit_table():
    u = {}
    for i in range(4):
        u["cv%d" % i] = [("w_in", 0, 8, (2 * i) * 128, 128, 0, "g1"), ("w_in", 0, 8, (2 * i + 1) * 128, 128, 128, "g1"),
                         ("w_in", 0, 8, 1024 + (2 * i) * 128, 128, 256, "g1"),
                         ("w_in", 0, 8, 1024 + (2 * i + 1) * 128, 128, 384, "g1")]
    for nm, c0 in (("q", 2048), ("k", 3072), ("v", 4096), ("gc", 5120), ("ga", 6144)):
        for i in range(2):
            u["%s%d" % (nm, i)] = [("w_in", 0, 8, c0 + i * 512, 512, 0, "g1")]
    for nm, src in (("co", "w_conv_out"), ("ao", "w_attn_out"), ("wo", "w_out")):
        for i in range(2):
            u["%s%d" % (nm, i)] = [(src, 0, 8, i * 512, 512, 0, None)]
    for i in range(11):
        u["gu%d" % i] = [("w_ffn_gate", 0, 8, (2 * i) * 128, 128, 0, "g2"),
                         ("w_ffn_gate", 0, 8, (2 * i + 1) * 128, 128, 128, "g2"),
                         ("w_ffn_up", 0, 8, (2 * i) * 128, 128, 256, "g2"),
                         ("w_ffn_up", 0, 8, (2 * i + 1) * 128, 128, 384, "g2")]
    for hf in range(2):
        for gi, (f0, nf) in enumerate(((0, 8), (8, 8), (16, 6))):
            u["dn%d_%d" % (hf, gi)] = [("w_ffn_down", f0 * 128, nf, hf * 512, 512, 0, None)]
    return u


UNITS = unit_table()
UNAMES = list(UNITS.keys())
UIDX = {n: i for i, n in enumerate(UNAMES)}
NU = len(UNAMES)
CHUNK_SEQ = (["cv%d" % i for i in range(4)] + ["q0", "gc0", "q1", "gc1", "ga0", "ga1", "ao0", "ao1", "co0", "co1",
                                               "wo0", "wo1"] + ["gu%d" % i for i in range(11)] +
             ["dn0_0", "dn0_1", "dn0_2", "dn1_0", "dn1_1", "dn1_2"])


def build_nc(nch=8, nkv=32):
    nc = bass.Bass("TRN2", target_bir_lowering=False)

    def din(name, shape):
        return nc.dram_tensor(name, list(shape), F32, kind="ExternalInput").ap()

    x_all = din("x_all", [S, D])
    x_own = din("x_own", [4096, D])
    x_halo = din("x_halo", [512, D])
    gate_bias = din("gate_bias", [16, 64])
    a2_d = din("a2", [16, 8, 64])
    b2_d = din("b2", [16, 64])
    cms_d = din("cmsel", [128, 4 * 2 * 256])
    lo_d = din("lo", [1, 8 * 512])
    kb_d = din("kbias", [128, 16])
    id_d = din("ident", [128, 128])
    wsrc = {
        "w_in": din("w_in", [D, 7168]), "w_conv_out": din("w_conv_out", [D, D]), "w_attn_out": din("w_attn_out", [D, D]),
        "w_out": din("w_out", [D, D]), "w_ffn_gate": din("w_ffn_gate", [D, FF]), "w_ffn_up": din("w_ffn_up", [D, FF]),
        "w_ffn_down": din("w_ffn_down", [FF, D]),
    }
    norm1_g = din("norm1_g", [1, D])
    norm2_g = din("norm2_g", [1, D])
    dw_w = din("dw_w", [31, D])
    dw_b = din("dw_b", [1, D])
    ln_g = din("conv_ln_g", [1, D])
    ln_b = din("conv_ln_b", [1, D])
    qg_d = din("q_norm_g", [1, DH])
    kg_d = din("k_norm_g", [1, DH])
    out_d = nc.dram_tensor("out_own", [4096, D], F32, kind="ExternalOutput").ap()
    wsc = nc.dram_tensor("wsc", [NU, 128, 4096], BF16).ap()
    kt_d = nc.dram_tensor("kt_s", [NH, 128, S], BF16).ap()
    vv_d = nc.dram_tensor("vv_s", [128, NBK, NH, 2, 129], BF16).ap()

    with ExitStack() as es:
        P = Prog(nc, es)
        NW = 53200
        arena_t = es.enter_context(nc.sbuf_tensor("arena", [128, NW], F32))
        AR = Arena(arena_t[:, :], NW)
        banks = [es.enter_context(nc.psum_tensor("pb%d" % i, [128, 512], F32)) for i in range(8)]
        tb = [T("pb%d" % i) for i in range(8)]
        rot = [0]

        held = set()

        def nbank():
            while rot[0] in held:
                rot[0] = (rot[0] + 1) % 8
            i = rot[0]
            rot[0] = (rot[0] + 1) % 8
            return i

        def bf_view(i):
            return banks[i][:, :].bitcast(BF16)

        identf = AR.alloc([128, 128], F32)
        identb = AR.alloc([128, 128], BF16)
        onesb = AR.alloc([128, 128], BF16)
        esel = AR.alloc([65, 64], BF16)
        cms = AR.alloc([128, 4, 2, 256], BF16)
        kb = AR.alloc([128, 16], F32)
        kmean = AR.alloc([128, 8, 64], F32)
        kmean_b = AR.alloc([128, 8, 64], BF16)
        VT = AR.alloc([65, 8, 512], BF16)
        g1 = AR.alloc([128, 8], F32)
        g2 = AR.alloc([128, 8], F32)
        cw = AR.alloc([128, 8, 31], F32)
        cb = AR.alloc([128, 8], F32)
        lg = AR.alloc([128, 8], F32)
        lb = AR.alloc([128, 8], F32)
        qg = AR.alloc([128, 1], F32)
        kg = AR.alloc([128, 1], F32)
        epsc = AR.alloc([128, 1], F32)
        t_const = T("const")
        t_kmean = T("kmean")
        t_vt64 = T("vt64")
        dq_c = P.dmaq("const")
        const_mark = AR.off

        cms_f = AR.alloc([128, 2048], F32)
        lo_f = AR.alloc([65, 4096], F32)
        t_cst = T("cst")
        pq = P.pool
        P.dma(pq, identf, id_d, [], [t_cst], dq_c)
        P.dma(pq, cms_f, cms_d, [], [t_cst], dq_c)
        P.dma(pq, lo_f[64:65, :], lo_d, [], [t_cst], dq_c)
        P.dma(pq, kb, kb_d, [], [t_cst], dq_c)
        P.dma(pq, g1, norm1_g[0].rearrange("(kc p) -> p kc", p=128), [], [t_cst], dq_c, allow_slow_non_contiguous=True)
        P.dma(pq, g2, norm2_g[0].rearrange("(kc p) -> p kc", p=128), [], [t_cst], dq_c, allow_slow_non_contiguous=True)
        P.dma(pq, cb, dw_b[0].rearrange("(kc p) -> p kc", p=128), [], [t_cst], dq_c, allow_slow_non_contiguous=True)
        P.dma(pq, lg, ln_g[0].rearrange("(kc p) -> p kc", p=128), [], [t_cst], dq_c, allow_slow_non_contiguous=True)
        P.dma(pq, lb, ln_b[0].rearrange("(kc p) -> p kc", p=128), [], [t_cst], dq_c, allow_slow_non_contiguous=True)
        for kc_ in range(8):
            P.dma(pq, cw[:, kc_, :], dw_w[:, kc_ * 128:(kc_ + 1) * 128].rearrange("j p -> p j"), [], [t_cst], dq_c,
                  allow_slow_non_contiguous=True)
        P.dma(pq, qg, qg_d.rearrange("o p -> p o"), [], [t_cst], dq_c, allow_slow_non_contiguous=True)
        P.dma(pq, kg, kg_d.rearrange("o p -> p o"), [], [t_cst], dq_c, allow_slow_non_contiguous=True)
        dv = P.dve
        P.emit(dv, lambda e: e.tensor_copy(out=identb, in_=identf), [t_cst], [t_const])
        P.emit(dv, lambda e: e.memset(onesb, 1.0), [], [t_const])
        P.emit(dv, lambda e: e.memset(kmean.rearrange("p a b -> p (a b)"), 0.0), [], [t_const])
        P.emit(dv, lambda e: e.memset(epsc, EPS), [], [t_const])
        P.emit(dv, lambda e: e.tensor_copy(out=esel[0:64, :], in_=identf[0:64, 0:64]), [t_cst], [t_const])
        P.emit(dv, lambda e: e.memset(esel[64:65, :], 1.0), [], [t_const])
        P.emit(dv, lambda e: e.tensor_copy(out=cms.rearrange("p a b c -> p (a b c)"), in_=cms_f), [t_cst], [t_const])
        P.emit(dv, lambda e: e.tensor_copy(out=VT[64:65, :, :].rearrange("p a b -> p (a b)"), in_=lo_f[64:65, :]),
               [t_cst], [t_const])
        P.emit(dv, lambda e: e.tensor_scalar(out=qg, in0=qg, scalar1=float(DH) ** -0.5, scalar2=None, op0=ALU.mult),
               [t_cst], [t_const])
        P.barrier([t_cst, t_const])
        AR.off = const_mark
        phase_mark = AR.off

        def esel_lhsT(n):
            a = esel[0:65, n:n + 1]
            return bass.AP(a.tensor, a.offset, [[a.ap[0][0], 65], [0, 128]])

        stg = [AR.alloc([128, 8, 512], F32) for _ in range(2)]
        wbf = [AR.alloc([128, 8, 512], BF16) for _ in range(2)]
        t_stg = [T("stg0"), T("stg1")]
        t_wbf = [T("wbf0"), T("wbf1")]
        dq_stg = [P.dmaq("stg0"), P.dmaq("stg1")]
        dq_wbf = [P.dmaq("wbf0"), P.dmaq("wbf1")]
        t_unit = [T("unit%d" % i) for i in range(NU)]
        gains = {"g1": g1, "g2": g2}
        kv_units = ["k0", "k1", "v0", "v1"]
        conv_order = kv_units + [n_ for n_ in UNAMES if n_ not in kv_units]

        def conv_load(oi):
            nm = conv_order[oi]
            b = oi % 2
            for (src, row0, nk_, c0, ncol, dst, _g) in UNITS[nm]:
                sap = wsrc[src][row0:row0 + nk_ * 128, c0:c0 + ncol].rearrange("(kc p) n -> p kc n", p=128)
                P.dma(P.sp, stg[b][:, 0:nk_, dst:dst + ncol], sap, [], [t_stg[b]], dq_stg[b])

        def conv_cast_store(oi, act_only):
            nm = conv_order[oi]
            ui = UIDX[nm]
            b = oi % 2
            segs = UNITS[nm]
            nk = segs[0][2]
            gname = segs[0][6]
            for kc in range(nk):
                use_dve = (kc % 2 == 0) and not act_only
                if gname is None:
                    if use_dve:
                        P.emit(P.dve, lambda e, b=b, kc=kc: e.tensor_copy(out=wbf[b][:, kc, :], in_=stg[b][:, kc, :]),
                               [t_stg[b]], [t_wbf[b]])
                    else:
                        P.emit(P.act, lambda e, b=b, kc=kc: e.copy(out=wbf[b][:, kc, :], in_=stg[b][:, kc, :]),
                               [t_stg[b]], [t_wbf[b]])
                else:
                    gt = gains[gname]
                    if use_dve:
                        P.emit(P.dve, lambda e, b=b, kc=kc, gt=gt: e.tensor_scalar(
                            out=wbf[b][:, kc, :], in0=stg[b][:, kc, :], scalar1=gt[:, kc:kc + 1], scalar2=None,
                            op0=ALU.mult), [t_stg[b]], [t_wbf[b]])
                    else:
                        P.emit(P.act, lambda e, b=b, kc=kc, gt=gt: e.activation(
                            out=wbf[b][:, kc, :], in_=stg[b][:, kc, :], func=AF.Copy, scale=gt[:, kc:kc + 1]),
                            [t_stg[b]], [t_wbf[b]])
            P.dma(P.sp, wsc[ui, :, 0:nk * 512], wbf[b][:, 0:nk, :].rearrange("p a b -> p (a b)"), [t_wbf[b]],
                  [t_unit[ui]], dq_wbf[b])

        conv_load(0)
        conv_load(1)
        for oi in range(4):
            conv_cast_store(oi, act_only=False)
            conv_load(oi + 2)
        conv_next = [4]

        wk = AR.alloc([128, 8, 1024], BF16)
        wv = AR.alloc([128, 8, 1024], BF16)
        t_wk = T("wk")
        t_wv = T("wv")
        dq_wk = P.dmaq("wk")
        dq_wv = P.dmaq("wv")
        for i in range(2):
            P.dma(P.pool, wk[:, :, i * 512:(i + 1) * 512], wsc[UIDX["k%d" % i]].rearrange("p (a b) -> p a b", b=512),
                  [t_unit[UIDX["k%d" % i]]], [t_wk], dq_wk)
            P.dma(P.pool, wv[:, :, i * 512:(i + 1) * 512], wsc[UIDX["v%d" % i]].rearrange("p (a b) -> p a b", b=512),
                  [t_unit[UIDX["v%d" % i]]], [t_wv], dq_wv)
        xin1 = [AR.alloc([128, 4, 1024], F32) for _ in range(2)]
        t_xin1 = [T("xin1_0"), T("xin1_1")]
        dq_xin1 = [P.dmaq("xin1_0"), P.dmaq("xin1_1")]
        junk_cur = [AR.alloc([128, 1024], BF16)]
        t_junk = T("junk")
        ssq = [AR.alloc([128, 4], F32) for _ in range(2)]
        t_ssq = [T("ssq0"), T("ssq1")]
        rinv = [AR.alloc([128, 4], F32) for _ in range(2)]
        t_rinv = [T("rinv0"), T("rinv1")]
        xn = [AR.alloc([128, 1024], BF16) for _ in range(2)]
        t_xn = [T("xn0"), T("xn1")]
        xnT1 = [AR.alloc([128, 8, 512], BF16) for _ in range(2)]
        t_xnT1 = [T("xnT1_0"), T("xnT1_1")]
        sqb = [AR.alloc([128, 512], BF16) for _ in range(2)]
        t_sqb = [T("sqb0"), T("sqb1")]
        rkf = [AR.alloc([128, 512], F32) for _ in range(2)]
        t_rkf = [T("rkf0"), T("rkf1")]
        kst = [AR.alloc([128, 8, 512], BF16) for _ in range(2)]
        t_kst = [T("kst0"), T("kst1")]
        dq_kst = [P.dmaq("kst0"), P.dmaq("kst1")]
        vst = [AR.alloc([128, 2, 8, 2, 129], BF16) for _ in range(2)]
        t_vst = [T("vst0"), T("vst1")]
        dq_vst = [P.dmaq("vst0"), P.dmaq("vst1")]
        t_kt = T("kt_dram")
        t_vv = T("vv_dram")
        for b in range(2):
            P.emit(P.dve, lambda e, b=b: e.memset(vst[b].rearrange("p a b c d -> p (a b c d)"), 1.0), [], [t_vst[b]])

        def norm_tile(src, np_, t_src, ssq_ap, rinv_ap, t_s, t_r, xn_ap, t_x):
            jv = junk_cur[0][0:np_, :]
            P.emit(P.dve, lambda e: e.memset(ssq_ap, 0.0), [], [t_s])
            P.emit(P.act, lambda e: e.activation(out=jv, in_=src, func=AF.Square, accum_out=ssq_ap),
                   [t_src], [t_junk, t_s])
            P.emit(P.act, lambda e: e.activation(out=rinv_ap, in_=ssq_ap, func=AF.Ln, scale=1.0 / D,
                                                 bias=epsc[0:np_, :]), [t_s], [t_r])
            P.emit(P.act, lambda e: e.activation(out=rinv_ap, in_=rinv_ap, func=AF.Exp, scale=-0.5), [t_r], [t_r])
            P.emit(P.dve, lambda e: e.tensor_scalar(out=xn_ap, in0=src, scalar1=rinv_ap, scalar2=None, op0=ALU.mult),
                   [t_src, t_r], [t_x])

        def transpose_tile(xn_ap, np_, t_x, dst, t_dst, use_act):
            bi = nbank()
            pv = bf_view(bi)[:, 0:8 * np_].rearrange("p (a b) -> p a b", b=np_)
            for kc in range(8):
                P.emit(P.pe, lambda e, kc=kc: e.transpose(out=pv[:, kc, :], in_=xn_ap[:, kc * 128:(kc + 1) * 128],
                                                          identity=identb[0:np_, 0:np_]),
                       [t_x], [tb[bi]], ms=(kc == 7))
            if use_act:
                P.emit(P.act, lambda e: e.copy(out=dst, in_=pv), [tb[bi]], [t_dst])
            else:
                P.emit(P.dve, lambda e: e.tensor_copy(out=dst, in_=pv), [tb[bi]], [t_dst])

        def x1_load(c):
            b = c % 2
            P.dma(P.pool, xin1[b], x_all[c * 512:(c + 1) * 512, :].rearrange("(t p) d -> p t d", p=128), [],
                  [t_xin1[b]], dq_xin1[b])

        xn4 = [AR.alloc([128, 1024], BF16) for _ in range(4)]
        t_xn4 = [T("xn4_%d" % i) for i in range(4)]

        def p1_norm(c):
            b = c % 2
            for t in range(4):
                norm_tile(xin1[b][:, t, :], 128, t_xin1[b], ssq[b][:, t:t + 1], rinv[b][:, t:t + 1], t_ssq[b], t_rinv[b],
                          xn4[t], t_xn4[t])

        def p1_transpose(c):
            b = c % 2
            for t in range(4):
                transpose_tile(xn4[t], 128, t_xn4[t], xnT1[b][:, :, t * 128:(t + 1) * 128], t_xnT1[b],
                               use_act=(t % 2 == 0))

        def p1_norm_transpose(c):
            p1_norm(c)
            p1_transpose(c)

        x1_load(0)
        if nkv > 1:
            x1_load(1)
        p1_norm_transpose(0)
        for c in range(nkv):
            b = c % 2
            if conv_next[0] < len(conv_order):
                oi = conv_next[0]
                conv_cast_store(oi, act_only=True)
                if oi + 2 < len(conv_order):
                    conv_load(oi + 2)
                conv_next[0] += 1
            if c + 1 < nkv:
                p1_norm(c + 1)
            pend = None

            def k_tail(h, bi, pk, b=b, c=c):
                sb = h % 2
                b2i = nbank()
                p2 = banks[b2i][:, :]
                P.emit(P.pe, lambda e, p2=p2, sb=sb: e.matmul(out=p2, lhsT=onesb, rhs=sqb[sb], start=True, stop=True),
                       [t_sqb[sb]], [tb[b2i]])
                P.emit(P.act, lambda e, p2=p2, sb=sb: e.activation(out=rkf[sb], in_=p2, func=AF.Ln, scale=1.0 / DH,
                                                                   bias=epsc), [tb[b2i]], [t_rkf[sb]])
                P.emit(P.act, lambda e, sb=sb: e.activation(out=rkf[sb], in_=rkf[sb], func=AF.Exp, scale=-0.5),
                       [t_rkf[sb]], [t_rkf[sb]])
                P.emit(P.dve, lambda e, pk=pk, sb=sb, h=h, b=b: e.scalar_tensor_tensor(
                    out=kst[b][:, h, :], in0=pk, scalar=kg[:, 0:1], in1=rkf[sb], op0=ALU.mult, op1=ALU.mult),
                    [tb[bi], t_rkf[sb]], [t_kst[b]])
                P.emit(P.dve, lambda e, h=h, b=b, c=c: e.tensor_reduce(
                    out=kmean[:, h, 2 * c:2 * c + 2], in_=kst[b][:, h, :].rearrange("p (a l) -> p a l", l=256),
                    axis=AX.X, op=ALU.add), [t_kst[b]], [t_kmean])

            for h in range(NH):
                bi = nbank()
                pk = banks[bi][:, :]
                for kc in range(8):
                    P.emit(P.pe, lambda e, kc=kc, h=h, pk=pk, b=b: e.matmul(out=pk, lhsT=wk[:, kc, h * 128:(h + 1) * 128],
                                                                           rhs=xnT1[b][:, kc, :], start=(kc == 0),
                                                                           stop=(kc == 7)),
                           [t_wk, t_xnT1[b]], [tb[bi]], ms=(kc == 7))
                sb = h % 2
                P.emit(P.act, lambda e, pk=pk, sb=sb: e.activation(out=sqb[sb], in_=pk, func=AF.Square),
                       [tb[bi]], [t_sqb[sb]])
                held.add(bi)
                if pend is not None:
                    k_tail(*pend)
                    held.discard(pend[1])
                pend = (h, bi, pk)
            k_tail(*pend)
            held.discard(pend[1])
            P.dma(P.pool, kt_d[:, :, c * 512:(c + 1) * 512].rearrange("h p t -> p h t"), kst[b], [t_kst[b]], [t_kt],
                  dq_kst[b])
            if c + 2 < nkv:
                x1_load(c + 2)
            for blk in range(2):
                for kh in range(2):
                    for hf in range(2):
                        bi = nbank()
                        pv_ = banks[bi][:, :]
                        for kc in range(8):
                            P.emit(P.pe, lambda e, kc=kc, blk=blk, kh=kh, hf=hf, pv_=pv_, b=b: e.matmul(
                                out=pv_, lhsT=xnT1[b][:, kc, blk * 256 + kh:blk * 256 + 256:2],
                                rhs=wv[:, kc, hf * 512:(hf + 1) * 512], start=(kc == 0), stop=(kc == 7)),
                                [t_wv, t_xnT1[b]], [tb[bi]], ms=(kc == 7))
                        dst = vst[b][:, blk, hf * 4:(hf + 1) * 4, kh, 0:128]
                        src = pv_.rearrange("p (a d) -> p a d", d=128)
                        if hf == 0:
                            P.emit(P.act, lambda e, dst=dst, src=src: e.copy(out=dst, in_=src), [tb[bi]], [t_vst[b]])
                        else:
                            P.emit(P.dve, lambda e, dst=dst, src=src: e.tensor_copy(out=dst, in_=src), [tb[bi]],
                                   [t_vst[b]])
                if blk == 0 and c + 1 < nkv:
                    p1_transpose(c + 1)
            P.dma(P.pool, vv_d[:, 2 * c:2 * c + 2].rearrange("p a b c d -> p (a b c d)"),
                  vst[b].rearrange("p a b c d -> p (a b c d)"), [t_vst[b]], [t_vv], dq_vst[b])
        while conv_next[0] < len(conv_order):
            oi = conv_next[0]
            conv_cast_store(oi, act_only=False)
            if oi + 2 < len(conv_order):
                conv_load(oi + 2)
            conv_next[0] += 1
        P.emit(P.dve, lambda e: e.tensor_copy(out=kmean_b.rearrange("p a b -> p (a b)"),
                                              in_=kmean.rearrange("p a b -> p (a b)")), [t_kmean], [t_kmean])
        ph1 = t_xn4 + t_xin1 + t_ssq + t_rinv + t_xn + t_sqb + t_rkf + t_kst + t_vst + t_xnT1 + [t_junk, t_wk, t_wv, t_kt, t_vv,
                                                                                         t_kmean] + tb + t_stg + t_wbf
        P.barrier(ph1)
        AR.off = phase_mark

        NSLOT = 3
        wr = [AR.alloc([128, 8, 512], BF16) for _ in range(NSLOT)]
        t_wr = [T("wr%d" % i) for i in range(NSLOT)]
        dq_wr = [P.dmaq("wr%d" % i) for i in range(NSLOT)]
        seq_all = []
        for g in range(nch):
            seq_all += CHUNK_SEQ
        wstate = {"issued": 0, "used": 0}

        def w_issue():
            i = wstate["issued"]
            if i >= len(seq_all):
                return
            nm = seq_all[i]
            s_ = i % NSLOT
            ui = UIDX[nm]
            nk = UNITS[nm][0][2]
            P.dma(P.sp, wr[s_][:, 0:nk, :].rearrange("p a b -> p (a b)"), wsc[ui, :, 0:nk * 512], [t_unit[ui]],
                  [t_wr[s_]], dq_wr[s_])
            wstate["issued"] += 1

        def w_next(expect, ahead=NSLOT - 1):
            i = wstate["used"]
            assert seq_all[i] == expect, (seq_all[i], expect)
            while wstate["issued"] < min(i + ahead + 1, len(seq_all)):
                w_issue()
            wstate["used"] += 1
            s_ = i % NSLOT
            return wr[s_], t_wr[s_]

        xin = AR.alloc([128, 4, 1024], F32)
        t_xin = T("xin")
        dq_xin = P.dmaq("xin")
        xh = AR.alloc([32, 2, 1024], F32)
        t_xh = T("xh")
        dq_xh = P.dmaq("xh")
        a2t = AR.alloc([128, 2, 8, 64], F32)
        b2t = AR.alloc([128, 2, 64], F32)
        gbt = AR.alloc([128, 2, 64], F32)
        t_tab = T("tab")
        dq_tab = P.dmaq("tab")
        junk_cur[0] = AR.alloc([128, 1024], BF16)
        ssq2 = AR.alloc([128, 8], F32)
        rinv2 = AR.alloc([128, 8], F32)
        t_ssq2 = [T("ssq2_%d" % i) for i in range(6)]
        t_rinv2 = [T("rinv2_%d" % i) for i in range(6)]
        xn = [AR.alloc([128, 1024], BF16) for _ in range(2)]
        xnT_raw = AR.alloc([128, 8 * 576], BF16)
        xnT = xnT_raw.rearrange("p (a b) -> p a b", b=576)
        t_xnT = T("xnT")
        R1 = AR.alloc([128, 8, 576 + 512], F32)
        a_t = R1[:, :, 0:576]
        y_t = R1[:, :, 576:1088]
        act_t = R1.rearrange("p a b -> p (a b)").bitcast(BF16)[:, 0:FC * 512].rearrange("p (a b) -> p a b", b=512)
        t_a = T("a")
        t_y = T("y")
        t_actt = T("act")
        ybf = [AR.alloc([128, 512], BF16) for _ in range(2)]
        t_ybf = [T("ybf0"), T("ybf1")]
        ysq = [AR.alloc([128, 512], BF16) for _ in range(2)]
        t_ysq = [T("ysq0"), T("ysq1")]
        mean_t = AR.alloc([128, 512], F32)
        rstd_t = AR.alloc([128, 512], F32)
        t_mean = T("mean")
        t_rstd = T("rstd")
        xc = [AR.alloc([128, 512], F32) for _ in range(2)]
        t_xc = [T("xc0"), T("xc1")]
        sig_t = [x_[:, 0:288].rearrange("p (a b) -> p a b", a=1) for x_ in xc]
        t_sig = t_xc
        ysl = AR.alloc([128, 8, 512], BF16)
        t_ysl = T("ysl")
        mT = ysl
        t_mT = t_ysl
        t1 = AR.alloc([128, 8, 512], BF16)
        t_t1 = T("t1")
        qT = AR.alloc([128, 8, 512], BF16)
        t_qT = T("qT")
        qf = [AR.alloc([128, 512], F32) for _ in range(2)]
        t_qf = [T("qf0"), T("qf1")]
        off_g = AR.off
        gcs = AR.alloc([128, 8, 512], BF16)
        gas = AR.alloc([128, 8, 512], BF16)
        assert AR.off - off_g == 4096
        outbuf = AR.ap[:, off_g:off_g + 4096].rearrange("p (t d) -> p t d", d=1024)
        t_gcs = T("gcs")
        t_gas = T("gas")
        attT = AR.alloc([128, 8, 512], BF16)
        t_attT = T("attT")
        hnT = xnT_raw[:, 0:8 * 512].rearrange("p (a b) -> p a b", b=512)
        t_hnT = t_xnT
        NKV = 2
        ktp = [AR.alloc([128, 2048], BF16) for _ in range(NKV)]
        vp = [AR.alloc([128, 8, 2, 129], BF16) for _ in range(NKV)]
        t_ktp = [T("ktp%d" % i) for i in range(NKV)]
        t_vp = [T("vp%d" % i) for i in range(NKV)]
        dq_ktp = [P.dmaq("ktp%d" % i) for i in range(NKV)]
        dq_vp = [P.dmaq("vp%d" % i) for i in range(NKV)]
        NPT = 4
        PT = [AR.alloc([128, 512], BF16) for _ in range(NPT)]
        t_PT = [T("PT%d" % i) for i in range(NPT)]
        gs_t = [AR.alloc([128, 64], F32) for _ in range(4)]
        m8_t = [AR.alloc([128, 8], F32) for _ in range(4)]
        tmp_t = [AR.alloc([128, 64], F32) for _ in range(4)]
        hiv_t = [AR.alloc([128, 64], BF16) for _ in range(4)]
        t_gs = [T("gs%d" % i) for i in range(4)]
        t_m8 = [T("m8%d" % i) for i in range(4)]
        t_tmp = [T("tmp%d" % i) for i in range(4)]
        t_hiv = [T("hiv%d" % i) for i in range(4)]
        rl_t = [AR.alloc([128, 1], F32) for _ in range(2)]
        t_rl = [T("rl0"), T("rl1")]
        atok = [AR.alloc([128, 128], BF16) for _ in range(2)]
        t_atok = [T("atok0"), T("atok1")]
        sg_t = xc
        t_sg = t_xc
        tm2 = xc
        t_tm2 = t_xc
        dq_out = P.dmaq("out")
        t_outd = T("out_dram")
        kvstate = {"n": 0}
        ptstate = {"n": 0}

        def mm_group(out_ap, bank_i, pairs, extra_reads, first=True, last=True):
            n_ = len(pairs)
            for i_, (l_, r_) in enumerate(pairs):
                P.emit(P.pe, lambda e, l_=l_, r_=r_, i_=i_: e.matmul(out=out_ap, lhsT=l_, rhs=r_,
                                                                     start=(first and i_ == 0),
                                                                     stop=(last and i_ == n_ - 1)),
                       extra_reads, [tb[bank_i]], ms=(i_ == n_ - 1))

        for g in range(nch):
            P.dma(P.pool, xin, x_own[g * 512:(g + 1) * 512, :].rearrange("(t p) d -> p t d", p=128), [], [t_xin], dq_xin)
            P.dma(P.pool, xh, x_halo[g * 64:(g + 1) * 64, :].rearrange("(o p) d -> p o d", p=32), [], [t_xh], dq_xh)
            P.dma(P.pool, a2t, a2_d[2 * g:2 * g + 2].partition_broadcast(128), [], [t_tab], dq_tab)
            P.dma(P.pool, b2t, b2_d[2 * g:2 * g + 2].partition_broadcast(128), [], [t_tab], dq_tab)
            P.dma(P.pool, gbt, gate_bias[2 * g:2 * g + 2].partition_broadcast(128), [], [t_tab], dq_tab)
            nbuf = [(xn[0], t_xn[0]), (xn[1], t_xn[1]), (qf[0].bitcast(BF16), t_qf[0]), (qf[1].bitcast(BF16), t_qf[1])]
            for o in range(2):
                nb_ap, nb_t = nbuf[o]
                norm_tile(xh[0:32, o, :], 32, t_xh, ssq2[0:32, 4 + o:5 + o], rinv2[0:32, 4 + o:5 + o], t_ssq2[4 + o],
                          t_rinv2[4 + o], nb_ap[0:32, :], nb_t)
            for t in range(4):
                nb_ap, nb_t = nbuf[(t + 2) % 4]
                norm_tile(xin[:, t, :], 128, t_xin, ssq2[:, t:t + 1], rinv2[:, t:t + 1], t_ssq2[t], t_rinv2[t], nb_ap, nb_t)
                if t == 1:
                    for o in range(2):
                        nb_ap2, nb_t2 = nbuf[o]
                        transpose_tile(nb_ap2[0:32, :], 32, nb_t2, xnT[:, :, o * 288:o * 288 + 32], t_xnT,
                                       use_act=(o == 0))
            for t in range(4):
                nb_ap, nb_t = nbuf[(t + 2) % 4]
                c0 = (t // 2) * 288 + 32 + (t % 2) * 128
                transpose_tile(nb_ap, 128, nb_t, xnT[:, :, c0:c0 + 128], t_xnT, use_act=(t % 2 == 0))
            for u in range(4):
                w_, tw = w_next("cv%d" % u)
                for ci in range(2):
                    cc = 2 * u + ci
                    bu = nbank()
                    bg = nbank()
                    bu2 = nbank()
                    bg2 = nbank()
                    for ob, (bcu, bcg) in enumerate(((bu, bg), (bu2, bg2))):
                        mm_group(banks[bcu][:, 0:288],
                                 bcu, [(w_[:, kc, ci * 128:(ci + 1) * 128], xnT[:, kc, ob * 288:(ob + 1) * 288])
                                       for kc in range(8)], [tw, t_xnT])
                        mm_group(banks[bcg][:, 0:288],
                                 bcg, [(w_[:, kc, 256 + ci * 128:256 + (ci + 1) * 128], xnT[:, kc, ob * 288:(ob + 1) * 288])
                                       for kc in range(8)], [tw, t_xnT])
                        sb = ob
                        P.emit(P.act, lambda e, bcg=bcg, sb=sb, ob=ob: e.activation(out=sig_t[sb][:, 0, :],
                                                                                   in_=banks[bcg][:, 0:288],
                                                                                   func=AF.Sigmoid),
                               [tb[bcg]], [t_sig[sb]])
                        P.emit(P.dve, lambda e, bcu=bcu, sb=sb, ob=ob, cc=cc: e.tensor_tensor(
                            out=a_t[:, cc, ob * 288:(ob + 1) * 288], in0=banks[bcu][:, 0:288], in1=sig_t[sb][:, 0, :],
                            op=ALU.mult), [tb[bcu], t_sig[sb]], [t_a])
            def q_main(h, w_, tw, hi_):
                bi = nbank()
                pq_ = banks[bi][:, :]
                for ob in range(2):
                    mm_group(pq_[:, ob * 256:(ob + 1) * 256], bi,
                             [(w_[:, kc, hi_ * 128:(hi_ + 1) * 128], xnT[:, kc, ob * 288 + 32:ob * 288 + 288])
                              for kc in range(8)], [tw, t_xnT])
                sb = h % 2
                P.emit(P.act, lambda e, pq_=pq_, sb=sb: e.activation(out=ysq[sb], in_=pq_, func=AF.Square),
                       [tb[bi]], [t_ysq[sb]])
                held.add(bi)
                return (h, bi, pq_)

            def q_stage_a(stt_):
                h, bi, pq_ = stt_
                sb = h % 2
                b2i = nbank()
                p2 = banks[b2i][:, :]
                mm_group(p2, b2i, [(onesb, ysq[sb])], [t_ysq[sb]])
                P.emit(P.act, lambda e, p2=p2, sb=sb: e.activation(out=xc[sb], in_=p2, func=AF.Ln, scale=1.0 / DH,
                                                                   bias=epsc), [tb[b2i]], [t_xc[sb]])
                P.emit(P.act, lambda e, sb=sb: e.activation(out=xc[sb], in_=xc[sb], func=AF.Exp, scale=-0.5),
                       [t_xc[sb]], [t_xc[sb]])
                P.emit(P.dve, lambda e, pq_=pq_, sb=sb: e.scalar_tensor_tensor(
                    out=qf[sb], in0=pq_, scalar=qg[:, 0:1], in1=xc[sb], op0=ALU.mult, op1=ALU.mult),
                    [tb[bi], t_xc[sb]], [t_qf[sb]])
                P.emit(P.act, lambda e, sb=sb, h=h: e.copy(out=qT[:, h, :], in_=qf[sb]), [t_qf[sb]], [t_qT])
                held.discard(bi)
                return h

            def q_stage_b(h):
                sb = h % 2
                for t in range(4):
                    ob = t // 2
                    bgi = nbank()
                    pg = banks[bgi][:, 0:64]
                    mm_group(pg, bgi, [(qT[:, h, t * 128:(t + 1) * 128], kmean_b[:, h, :])], [t_qT, t_kmean])
                    P.emit(P.dve, lambda e, pg=pg, t=t, ob=ob: e.tensor_tensor(out=gs_t[t], in0=pg,
                                                                              in1=gbt[:, ob, :], op=ALU.add),
                           [tb[bgi], t_tab], [t_gs[t]])
                    P.emit(P.dve, lambda e, t=t: e.max(out=m8_t[t], in_=gs_t[t]), [t_gs[t]], [t_m8[t]])
                    P.emit(P.dve, lambda e, t=t, ob=ob, h=h: e.scalar_tensor_tensor(
                        out=tmp_t[t], in0=gs_t[t], scalar=m8_t[t][:, 2:3], in1=a2t[:, ob, h, :], op0=ALU.is_ge,
                        op1=ALU.mult), [t_gs[t], t_m8[t], t_tab], [t_tmp[t]])
                    P.emit(P.dve, lambda e, t=t, ob=ob: e.tensor_tensor(out=hiv_t[t], in0=tmp_t[t],
                                                                       in1=b2t[:, ob, :], op=ALU.add),
                           [t_tmp[t], t_tab], [t_hiv[t]])
                return h

            def q_stage_c(h):
                for t in range(4):
                    bti = nbank()
                    ptv = bf_view(bti)[0:64, 0:128]
                    P.emit(P.pe, lambda e, ptv=ptv, t=t: e.transpose(out=ptv, in_=hiv_t[t], identity=identb),
                           [t_hiv[t]], [tb[bti]])
                    P.emit(P.act, lambda e, ptv=ptv, h=h, t=t: e.copy(out=VT[0:64, h, t * 128:(t + 1) * 128], in_=ptv),
                           [tb[bti]], [t_vt64])

            def gate_mm(w_, tw, ci, cc, dst, tdst):
                bi = nbank()
                for ob in range(2):
                    mm_group(banks[bi][:, ob * 256:(ob + 1) * 256], bi,
                             [(w_[:, kc, ci * 128:(ci + 1) * 128], xnT[:, kc, ob * 288 + 32:ob * 288 + 288])
                              for kc in range(8)], [tw, t_xnT])
                P.emit(P.act, lambda e, bi=bi, dst=dst, cc=cc: e.activation(out=dst[:, cc, :], in_=banks[bi][:, :],
                                                                           func=AF.Sigmoid), [tb[bi]], [tdst])

            pa = pb = pc = None
            for u in range(2):
                wq, twq = w_next("q%d" % u, 2)
                wg, twg = w_next("gc%d" % u, 1)
                for hi_ in range(4):
                    h = 4 * u + hi_
                    cur = q_main(h, wq, twq, hi_)
                    na = q_stage_a(pa) if pa is not None else None
                    gate_mm(wg, twg, hi_, h, gcs, t_gcs)
                    if pc is not None:
                        q_stage_c(pc)
                    nb_ = q_stage_b(pb) if pb is not None else None
                    pa, pb, pc = cur, na, nb_
            for u in range(2):
                wg, twg = w_next("ga%d" % u, 2)
                for ci in range(4):
                    gate_mm(wg, twg, ci, 4 * u + ci, gas, t_gas)
                    na = q_stage_a(pa) if pa is not None else None
                    if pc is not None:
                        q_stage_c(pc)
                    nb_ = q_stage_b(pb) if pb is not None else None
                    pa, pb, pc = None, na, nb_
            assert pa is None and pb is None and pc is None
            def conv_cc(cc):
                yv = y_t[:, cc, :].rearrange("p (o l) -> p o l", l=256)

                def av(j, cc=cc):
                    return a_t[:, cc, :].rearrange("p (o l) -> p o l", l=288)[:, :, 2 + j:2 + j + 256]
                P.emit(P.dve, lambda e, yv=yv, av=av, cc=cc: e.tensor_scalar(out=yv, in0=av(0), scalar1=cw[:, cc, 0:1],
                                                                            scalar2=cb[:, cc:cc + 1], op0=ALU.mult,
                                                                            op1=ALU.add), [t_a], [t_y])
                for j in range(1, 31):
                    P.emit(P.dve, lambda e, yv=yv, av=av, cc=cc, j=j: e.scalar_tensor_tensor(
                        out=yv, in0=av(j), scalar=cw[:, cc, j:j + 1], in1=yv, op0=ALU.mult, op1=ALU.add), [t_a, t_y], [t_y])

            def normalize_head(h):
                for t in range(4):
                    ob_i = 4 + t
                    o_ap = banks[ob_i][:, 0:129]
                    mb = t % 2
                    P.emit(P.dve, lambda e, o_ap=o_ap, mb=mb: e.reciprocal(out=rl_t[mb], in_=o_ap[:, 128:129]),
                           [tb[ob_i]], [t_rl[mb]])
                    P.emit(P.dve, lambda e, o_ap=o_ap, mb=mb: e.tensor_scalar(out=atok[mb], in0=o_ap[:, 0:128],
                                                                              scalar1=rl_t[mb][:, 0:1], scalar2=None,
                                                                              op0=ALU.mult),
                           [tb[ob_i], t_rl[mb]], [t_atok[mb]])
                    bti = (srot[0] - 3) % 4 if t % 2 == 0 else srot[0] % 4
                    ptv = bf_view(bti)[:, 0:128]
                    P.emit(P.pe, lambda e, ptv=ptv, mb=mb: e.transpose(out=ptv, in_=atok[mb], identity=identb),
                           [t_atok[mb]], [tb[bti]])
                    P.emit(P.act, lambda e, ptv=ptv, h=h, t=t: e.copy(out=attT[:, h, t * 128:(t + 1) * 128], in_=ptv),
                           [tb[bti]], [t_attT])

            nblk = 8 * g + 8
            npp = nblk // 8
            units = [(h, n, kh) for h in range(NH) for n in range(nblk) for kh in range(2)]
            piece_list = [(h, n0) for h in range(NH) for n0 in range(0, nblk, 8)]
            piece_slot = {}
            piece_issued = [0]

            def ensure_piece(k):
                while piece_issued[0] <= k and piece_issued[0] < len(piece_list):
                    h_, n0_ = piece_list[piece_issued[0]]
                    s_ = kvstate["n"] % NKV
                    kvstate["n"] += 1
                    P.dma(P.pool, ktp[s_], kt_d[h_, :, n0_ * 256:(n0_ + 8) * 256], [t_kt], [t_ktp[s_]], dq_ktp[s_])
                    P.dma(P.pool, vp[s_], vv_d[:, n0_:n0_ + 8, h_, :, :], [t_vv], [t_vp[s_]], dq_vp[s_])
                    piece_slot[piece_issued[0]] = s_
                    piece_issued[0] += 1

            LA = 2
            srot = [0]
            ensure_piece(1)
            conv_cc(0)
            st = {}
            for idx in range(len(units) + LA):
                if idx < len(units):
                    h, n, kh = units[idx]
                    s_ = piece_slot[h * npp + n // 8]
                    nl = n % 8
                    bi = srot[0]
                    srot[0] = (srot[0] + 1) % 4
                    S_ = banks[bi][:, :]
                    cand = n >= 8 * g
                    P.emit(P.pe, lambda e, S_=S_, s_=s_, nl=nl, kh=kh, h=h: e.matmul(
                        out=S_, lhsT=ktp[s_][:, nl * 256 + kh:nl * 256 + 256:2], rhs=qT[:, h, :], start=True,
                        stop=False), [t_ktp[s_], t_qT], [tb[bi]], ms=False)
                    P.emit(P.pe, lambda e, S_=S_, n=n, h=h, cand=cand: e.matmul(
                        out=S_, lhsT=esel_lhsT(n), rhs=VT[0:65, h, :], start=False, stop=(not cand)),
                        [t_vt64], [tb[bi]], ms=(not cand))
                    if cand:
                        ob = (n - 8 * g) // 4
                        cnd = (n - 8 * g) % 4
                        P.emit(P.pe, lambda e, S_=S_, ob=ob, cnd=cnd, kh=kh: e.matmul(
                            out=S_[:, ob * 256:(ob + 1) * 256], lhsT=identb, rhs=cms[:, cnd, kh, :], start=False,
                            stop=True), [], [tb[bi]], ms=True)
                    st[idx] = (bi, s_)
                j = idx - LA
                if j >= 0:
                    h, n, kh = units[j]
                    bi, s_ = st.pop(j)
                    S_ = banks[bi][:, :]
                    nl = n % 8
                    if n % 8 == 0 and kh == 0:
                        ensure_piece(h * npp + n // 8 + 1)
                    first = (n == 0 and kh == 0)
                    lastblk = (n == nblk - 1 and kh == 1)
                    r_ = ptstate["n"] % NPT
                    ptstate["n"] += 1
                    P.emit(P.act, lambda e, S_=S_, r_=r_, h=h, kh=kh: e.activation(
                        out=PT[r_], in_=S_, func=AF.Exp, bias=kb[:, 2 * h + kh:2 * h + kh + 1]),
                        [tb[bi]], [t_PT[r_]])
                    for t in range(4):
                        ob_i = 4 + t
                        o_ap = banks[ob_i][:, 0:129]
                        P.emit(P.pe, lambda e, o_ap=o_ap, r_=r_, t=t, s_=s_, nl=nl, kh=kh, first=first,
                               lastblk=lastblk: e.matmul(out=o_ap, lhsT=PT[r_][:, t * 128:(t + 1) * 128],
                                                         rhs=vp[s_][:, nl, kh, :], start=first, stop=lastblk),
                               [t_PT[r_], t_vp[s_]], [tb[ob_i]], ms=(t == 3))
                    if lastblk:
                        normalize_head(h)
                        if h + 1 < NH:
                            conv_cc(h + 1)
            for u in range(2):
                w_, tw = w_next("ao%d" % u)
                for ci in range(4):
                    cc = 4 * u + ci
                    bi = nbank()
                    mm_group(banks[bi][:, :], bi, [(w_[:, kc, ci * 128:(ci + 1) * 128], attT[:, kc, :]) for kc in range(8)],
                             [tw, t_attT])
                    P.emit(P.dve, lambda e, bi=bi, cc=cc: e.tensor_tensor(out=t1[:, cc, :], in0=banks[bi][:, :],
                                                                          in1=gas[:, cc, :], op=ALU.mult),
                           [tb[bi], t_gas], [t_t1])
            bs1 = nbank()
            bs2 = nbank()
            for cc in range(8):
                sb = cc % 2
                P.emit(P.act, lambda e, sb=sb, cc=cc: e.copy(out=ybf[sb], in_=y_t[:, cc, :]), [t_y], [t_ybf[sb]])
                P.emit(P.act, lambda e, sb=sb, cc=cc: e.activation(out=ysq[sb], in_=y_t[:, cc, :], func=AF.Square),
                       [t_y], [t_ysq[sb]])
                P.emit(P.pe, lambda e, sb=sb, cc=cc, bs1=bs1: e.matmul(out=banks[bs1][:, :], lhsT=onesb, rhs=ybf[sb],
                                                              start=(cc == 0), stop=(cc == 7)),
                       [t_ybf[sb]], [tb[bs1]], ms=True)
                P.emit(P.pe, lambda e, sb=sb, cc=cc, bs2=bs2: e.matmul(out=banks[bs2][:, :], lhsT=onesb, rhs=ysq[sb],
                                                              start=(cc == 0), stop=(cc == 7)),
                       [t_ysq[sb]], [tb[bs2]], ms=True)
            P.emit(P.dve, lambda e, bs1=bs1: e.tensor_scalar(out=mean_t, in0=banks[bs1][:, :], scalar1=1.0 / D, scalar2=None,
                                                    op0=ALU.mult), [tb[bs1]], [t_mean])
            P.emit(P.dve, lambda e: e.tensor_tensor(out=rstd_t, in0=mean_t, in1=mean_t, op=ALU.mult), [t_mean], [t_rstd])
            P.emit(P.dve, lambda e, bs2=bs2: e.scalar_tensor_tensor(out=rstd_t, in0=banks[bs2][:, :], scalar=1.0 / D, in1=rstd_t,
                                                           op0=ALU.mult, op1=ALU.subtract), [tb[bs2], t_rstd], [t_rstd])
            P.emit(P.act, lambda e: e.activation(out=rstd_t, in_=rstd_t, func=AF.Ln, bias=epsc), [t_rstd], [t_rstd])
            P.emit(P.act, lambda e: e.activation(out=rstd_t, in_=rstd_t, func=AF.Exp, scale=-0.5), [t_rstd], [t_rstd])
            for cc in range(8):
                sb = cc % 2
                P.emit(P.dve, lambda e, sb=sb, cc=cc: e.tensor_tensor(out=xc[sb], in0=y_t[:, cc, :], in1=mean_t,
                                                                      op=ALU.subtract), [t_y, t_mean], [t_xc[sb]])
                P.emit(P.dve, lambda e, sb=sb: e.tensor_tensor(out=xc[sb], in0=xc[sb], in1=rstd_t, op=ALU.mult),
                       [t_xc[sb], t_rstd], [t_xc[sb]])
                P.emit(P.act, lambda e, sb=sb, cc=cc: e.activation(out=ysl[:, cc, :], in_=xc[sb], func=AF.Silu,
                                                                   scale=lg[:, cc:cc + 1], bias=lb[:, cc:cc + 1]),
                       [t_xc[sb]], [t_ysl])
            for u in range(2):
                w_, tw = w_next("co%d" % u)
                for ci in range(4):
                    cc = 4 * u + ci
                    bi = nbank()
                    sb = cc % 2
                    mm_group(banks[bi][:, :], bi, [(w_[:, kc, ci * 128:(ci + 1) * 128], ysl[:, kc, :]) for kc in range(8)],
                             [tw, t_ysl])
                    P.emit(P.dve, lambda e, bi=bi, cc=cc, sb=sb: e.tensor_tensor(out=tm2[sb], in0=banks[bi][:, :],
                                                                                 in1=gcs[:, cc, :], op=ALU.mult),
                           [tb[bi], t_gcs], [t_tm2[sb]])
                    P.emit(P.dve, lambda e, cc=cc, sb=sb: e.tensor_tensor(out=t1[:, cc, :], in0=tm2[sb], in1=t1[:, cc, :],
                                                                          op=ALU.add), [t_tm2[sb], t_t1], [t_t1])
            for hf in range(2):
                w_, tw = w_next("wo%d" % hf)
                for t in range(4):
                    bi = nbank()
                    mm_group(banks[bi][:, :], bi, [(t1[:, kc, t * 128:(t + 1) * 128], w_[:, kc, :]) for kc in range(8)],
                             [tw, t_t1])
                    P.emit(P.dve, lambda e, bi=bi, t=t, hf=hf: e.tensor_tensor(
                        out=xin[:, t, hf * 512:(hf + 1) * 512], in0=banks[bi][:, :], in1=xin[:, t, hf * 512:(hf + 1) * 512],
                        op=ALU.add), [tb[bi], t_xin], [t_xin])
            for t in range(4):
                nb_ap, nb_t = nbuf[t]
                norm_tile(xin[:, t, :], 128, t_xin, ssq2[:, t:t + 1], rinv2[:, t:t + 1], t_ssq2[t], t_rinv2[t], nb_ap, nb_t)
            for t in range(4):
                nb_ap, nb_t = nbuf[t]
                transpose_tile(nb_ap, 128, nb_t, hnT[:, :, t * 128:(t + 1) * 128], t_hnT, use_act=(t % 2 == 0))
            for u in range(11):
                w_, tw = w_next("gu%d" % u)
                for ci in range(2):
                    fc = 2 * u + ci
                    sb = fc % 2
                    bgt = nbank()
                    mm_group(banks[bgt][:, :], bgt, [(w_[:, kc, ci * 128:(ci + 1) * 128], hnT[:, kc, :]) for kc in range(8)],
                             [tw, t_hnT])
                    but = nbank()
                    mm_group(banks[but][:, :], but,
                             [(w_[:, kc, 256 + ci * 128:256 + (ci + 1) * 128], hnT[:, kc, :]) for kc in range(8)],
                             [tw, t_hnT])
                    P.emit(P.act, lambda e, bgt=bgt, sb=sb: e.activation(out=sg_t[sb], in_=banks[bgt][:, :], func=AF.Silu),
                           [tb[bgt]], [t_sg[sb]])
                    P.emit(P.dve, lambda e, but=but, sb=sb, fc=fc: e.tensor_tensor(out=act_t[:, fc, :], in0=banks[but][:, :],
                                                                                   in1=sg_t[sb], op=ALU.mult),
                           [tb[but], t_sg[sb]], [t_actt, t_a, t_y])
            for hf in range(2):
                for gi, (f0, nf) in enumerate(((0, 8), (8, 8), (16, 6))):
                    w_, tw = w_next("dn%d_%d" % (hf, gi))
                    for t in range(4):
                        bi = 4 + t
                        for fl in range(nf):
                            fc = f0 + fl
                            P.emit(P.pe, lambda e, bi=bi, t=t, fc=fc, fl=fl, w_=w_: e.matmul(
                                out=banks[bi][:, :], lhsT=act_t[:, fc, t * 128:(t + 1) * 128], rhs=w_[:, fl, :],
                                start=(fc == 0), stop=(fc == FC - 1)), [tw, t_actt], [tb[bi]], ms=(fl == nf - 1))
                for t in range(4):
                    bi = 4 + t
                    P.emit(P.dve, lambda e, bi=bi, t=t, hf=hf: e.tensor_tensor(
                        out=outbuf[:, t, hf * 512:(hf + 1) * 512], in0=banks[bi][:, :],
                        in1=xin[:, t, hf * 512:(hf + 1) * 512], op=ALU.add), [tb[bi], t_xin], [t_gcs, t_gas])
            P.dma(P.pool, out_d[g * 512:(g + 1) * 512, :].rearrange("(t p) d -> p t d", p=128), outbuf, [t_gcs, t_gas],
                  [t_outd], dq_out)
            t_a.r += t_actt.r
            t_y.r += t_actt.r
            if t_actt.w is not None:
                t_a.r.append(t_actt.w)
                t_y.r.append(t_actt.w)
        P.wait_all(P.pool, [t_outd])
        P.wait_all(P.sp, [t_outd])
        P.run()
    return nc


def host_tables(j):
    slopes = 2.0 ** (-8.0 * np.arange(1, NH + 1) / NH)
    gate_bias = np.zeros((16, 64), np.float32)
    a2 = np.zeros((16, 8, 64), np.float32)
    b2 = np.full((16, 64), NEG, np.float32)
    for i in range(16):
        cur = 4 * i + j
        gate_bias[i, cur:] = -1e30
        b2[i, cur] = 0.0
        for n in range(cur):
            a2[i, :, n] = -NEG - slopes * 256.0 * (cur - n)
    cmsel = np.zeros((128, 4, 2, 256), np.float32)
    p = np.arange(128)[:, None]
    rq = np.arange(256)[None, :]
    for kh in range(2):
        rk = 2 * p + kh
        cmsel[:, j, kh, :] = np.where(rq >= rk, 0.0, NEG)
    lo = np.zeros((1, 8, 512), np.float32)
    for h in range(8):
        lo[0, h, :] = -slopes[h] * (np.arange(512) % 256)
    kbias = np.zeros((128, 16), np.float32)
    for h in range(8):
        for kh in range(2):
            kbias[:, 2 * h + kh] = slopes[h] * (2 * np.arange(128) + kh)
    return {"gate_bias": gate_bias, "a2": a2, "b2": b2, "cmsel": cmsel.reshape(128, -1),
            "lo": lo.reshape(1, -1), "kbias": kbias, "ident": np.eye(128, dtype=np.float32)}


def make_in_maps(inputs):
    x = np.asarray(inputs["x"], np.float32)
    shared = {
        "w_in": np.ascontiguousarray(inputs["w_in"][0]), "w_conv_out": np.ascontiguousarray(inputs["w_conv_out"][0]),
        "w_attn_out": np.ascontiguousarray(inputs["w_attn_out"][0]), "w_out": np.ascontiguousarray(inputs["w_out"][0]),
        "w_ffn_gate": np.ascontiguousarray(inputs["w_ffn_gate"][0]), "w_ffn_up": np.ascontiguousarray(inputs["w_ffn_up"][0]),
        "w_ffn_down": np.ascontiguousarray(inputs["w_ffn_down"][0]),
        "norm1_g": np.asarray(inputs["norm1_g"], np.float32).reshape(1, D),
        "norm2_g": np.asarray(inputs["norm2_g"], np.float32).reshape(1, D),
        "dw_w": np.ascontiguousarray(inputs["dw_w"][0]), "dw_b": np.asarray(inputs["dw_b"], np.float32).reshape(1, D),
        "conv_ln_g": np.asarray(inputs["conv_ln_g"], np.float32).reshape(1, D),
        "conv_ln_b": np.asarray(inputs["conv_ln_b"], np.float32).reshape(1, D),
        "q_norm_g": np.asarray(inputs["q_norm_g"], np.float32).reshape(1, DH),
        "k_norm_g": np.asarray(inputs["k_norm_g"], np.float32).reshape(1, DH),
    }
    shared = {k: np.asarray(v, np.float32) for k, v in shared.items()}
    maps = []
    for c in range(8):
        b, j = c // 4, c % 4
        xb = x[b]
        xblk = xb.reshape(NBK, L, D)
        own = [4 * i + j for i in range(16)]
        x_own = np.ascontiguousarray(xblk[own].reshape(4096, D))
        halo = np.zeros((16, 32, D), np.float32)
        for i, n in enumerate(own):
            if n > 0:
                halo[i] = xb[n * L - 32:n * L]
        m = dict(shared)
        m.update(host_tables(j))
        m["x_all"] = np.ascontiguousarray(xb)
        m["x_own"] = x_own
        m["x_halo"] = halo.reshape(512, D)
        maps.append(m)
    return maps


_NC_CACHE = {}


def kernel(**inputs):
    if "nc" not in _NC_CACHE:
        _NC_CACHE["nc"] = build_nc()
    nc = _NC_CACHE["nc"]
    maps = make_in_maps(inputs)
    res = run_bass_kernel_spmd(nc, maps, core_ids=list(range(8)))
    out = np.zeros((2, S, D), np.float32)
    ov = out.reshape(2, NBK, L, D)
    for c in range(8):
        b, j = c // 4, c % 4
        o = np.asarray(res.results[c]["out_own"], np.float32).reshape(16, L, D)
        for i in range(16):
            ov[b, 4 * i + j] = o[i]
    return out
```

```python
import numpy as np
from contextlib import ExitStack
import concourse.bass as bass
import concourse.mybir as mybir
from concourse.bass_utils import run_bass_kernel_spmd

F32 = mybir.dt.float32
BF16 = mybir.dt.bfloat16
AF = mybir.ActivationFunctionType
ALU = mybir.AluOpType
AX = mybir.AxisListType

D = 1024
KC = 8
NH = 8
DH = 128
FF = 2816
FC = 22
S = 16384
L = 256
NBK = 64
EPS = 1e-6
NEG = -30000.0


class T:
    __slots__ = ("name", "w", "r")

    def __init__(self, name):
        self.name = name
        self.w = None
        self.r = []


class Q:
    def __init__(self, name, sem, scale=1):
        self.name = name
        self.sem = sem
        self.scale = scale
        self.count = 0
        self.ops = []
        self.seen = {}


class Prog:
    def __init__(self, nc, es):
        self.nc = nc
        self.es = es
        self.pe = Q("pe", self.newsem("s_pe"))
        self.act = Q("act", self.newsem("s_act"))
        self.dve = Q("dve", self.newsem("s_dve"))
        self.pool = Q("pool", self.newsem("s_pool"))
        self.sp = Q("sp", self.newsem("s_sp"))
        self.engines = [self.pe, self.act, self.dve, self.pool, self.sp]

    def newsem(self, name):
        return self.es.enter_context(self.nc.semaphore(name))

    def dmaq(self, name):
        return Q(name, self.newsem("d_" + name), 16)

    def _wait(self, q, tok):
        sq, cnt = tok
        if q.seen.get(sq, 0) >= cnt:
            return
        q.seen[sq] = cnt
        q.ops.append(lambda e, s=sq.sem, v=cnt * sq.scale: e.wait_ge(s, v))

    def emit(self, q, fn, reads=(), writes=(), ms=True, sig=None):
        sig = sig or q
        for t in reads:
            if t.w is not None:
                self._wait(q, t.w)
        for t in writes:
            if t.w is not None and t.w[0] is not q:
                self._wait(q, t.w)
            for tok in t.r:
                if tok[0] is not q:
                    self._wait(q, tok)
        if ms:
            sig.count += 1
            tok = (sig, sig.count)
            q.ops.append(lambda e, s=sig.sem, v=sig.scale: fn(e).then_inc(s, v))
        else:
            tok = (sig, sig.count + 1)
            q.ops.append(lambda e: fn(e))
        for t in writes:
            t.w = tok
            t.r = []
        for t in reads:
            t.r.append(tok)
            if len(t.r) > 16:
                best = {}
                for sq, c in t.r:
                    if sq not in best or best[sq] < c:
                        best[sq] = c
                t.r = list(best.items())
        return tok

    def dma(self, q, out, in_, reads, writes, sig, **kw):
        return self.emit(q, lambda e: e.dma_start(out=out, in_=in_, **kw), reads, writes, sig=sig)

    def wait_all(self, q, ts):
        for t in ts:
            if t.w is not None:
                self._wait(q, t.w)
            for tok in t.r:
                self._wait(q, tok)

    def barrier(self, ts):
        for q in self.engines:
            self.wait_all(q, ts)

    def run(self):
        nc = self.nc
        with nc.Block() as block:
            @block.tensor
            def _(e):
                for op in self.pe.ops:
                    op(e)

            @block.scalar
            def _(e):
                for op in self.act.ops:
                    op(e)

            @block.vector
            def _(e):
                for op in self.dve.ops:
                    op(e)

            @block.gpsimd
            def _(e):
                for op in self.pool.ops:
                    op(e)

            @block.sync
            def _(e):
                for op in self.sp.ops:
                    op(e)


class Arena:
    def __init__(self, ap, nwords):
        self.ap = ap
        self.n = nwords
        self.off = 0

    def alloc(self, shape, dtype):
        shape = list(shape)
        np_ = shape[0]
        free = 1
        for s_ in shape[1:]:
            free *= s_
        esz = 4 if dtype == F32 else 2
        words = (free * esz + 3) // 4
        words = (words + 7) // 8 * 8
        assert self.off + words <= self.n, ("arena overflow", self.off, words, self.n)
        v = self.ap[0:np_, self.off:self.off + words]
        self.off += words
        if dtype != F32:
            v = v.bitcast(dtype)
        v = v[:, 0:free]
        if len(shape) == 2:
            return v
        names = " ".join("d%d" % i for i in range(len(shape) - 1))
        kw = {"d%d" % i: shape[i + 1] for i in range(len(shape) - 1)}
        return v.rearrange("p (%s) -> p %s" % (names, names), **kw)


def unit_table():
    u = {}
    for i in range(4):
        u["cv%d" % i] = [("w_in", 0, 8, (2 * i) * 128, 128, 0, "g1"), ("w_in", 0, 8, (2 * i + 1) * 128, 128, 128, "g1"),
                         ("w_in", 0, 8, 1024 + (2 * i) * 128, 128, 256, "g1"),
                         ("w_in", 0, 8, 1024 + (2 * i + 1) * 128, 128, 384, "g1")]
    for nm, c0 in (("q", 2048), ("k", 3072), ("v", 4096), ("gc", 5120), ("ga", 6144)):
        for i in range(2):
            u["%s%d" % (nm, i)] = [("w_in", 0, 8, c0 + i * 512, 512, 0, "g1")]
    for nm, src in (("co", "w_conv_out"), ("ao", "w_attn_out"), ("wo", "w_out")):
        for i in range(2):
            u["%s%d" % (nm, i)] = [(src, 0, 8, i * 512, 512, 0, None)]
    for i in range(11):
        u["gu%d" % i] = [("w_ffn_gate", 0, 8, (2 * i) * 128, 128, 0, "g2"),
                         ("w_ffn_gate", 0, 8, (2 * i + 1) * 128, 128, 128, "g2"),
                         ("w_ffn_up", 0, 8, (2 * i) * 128, 128, 256, "g2"),
                         ("w_ffn_up", 0, 8, (2 * i + 1) * 128, 128, 384, "g2")]
    for hf in range(2):
        for gi, (f0, nf) in enumerate(((0, 8), (8, 8), (16, 6))):
            u["dn%d_%d" % (hf, gi)] = [("w_ffn_down", f0 * 128, nf, hf * 512, 512, 0, None)]
    return u


UNITS = unit_table()
UNAMES = list(UNITS.keys())
UIDX = {n: i for i, n in enumerate(UNAMES)}
NU = len(UNAMES)
CHUNK_SEQ = (["cv%d" % i for i in range(4)] + ["q0", "gc0", "q1", "gc1", "ga0", "ga1", "ao0", "ao1", "co0", "co1",
                                               "wo0", "wo1"] + ["gu%d" % i for i in range(11)] +
             ["dn0_0", "dn0_1", "dn0_2", "dn1_0", "dn1_1", "dn1_2"])


def build_nc(nch=8, nkv=32):
    nc = bass.Bass("TRN2", target_bir_lowering=False)

    def din(name, shape):
        return nc.dram_tensor(name, list(shape), F32, kind="ExternalInput").ap()

    x_all = din("x_all", [S, D])
    x_own = din("x_own", [4096, D])
    x_halo = din("x_halo", [512, D])
    gate_bias = din("gate_bias", [16, 64])
    a2_d = din("a2", [16, 8, 64])
    b2_d = din("b2", [16, 64])
    cms_d = din("cmsel", [128, 4 * 2 * 256])
    lo_d = din("lo", [1, 8 * 512])
    kb_d = din("kbias", [128, 16])
    id_d = din("ident", [128, 128])
    wsrc = {
        "w_in": din("w_in", [D, 7168]), "w_conv_out": din("w_conv_out", [D, D]), "w_attn_out": din("w_attn_out", [D, D]),
        "w_out": din("w_out", [D, D]), "w_ffn_gate": din("w_ffn_gate", [D, FF]), "w_ffn_up": din("w_ffn_up", [D, FF]),
        "w_ffn_down": din("w_ffn_down", [FF, D]),
    }
    norm1_g = din("norm1_g", [1, D])
    norm2_g = din("norm2_g", [1, D])
    dw_w = din("dw_w", [31, D])
    dw_b = din("dw_b", [1, D])
    ln_g = din("conv_ln_g", [1, D])
    ln_b = din("conv_ln_b", [1, D])
    qg_d = din("q_norm_g", [1, DH])
    kg_d = din("k_norm_g", [1, DH])
    out_d = nc.dram_tensor("out_own", [4096, D], F32, kind="ExternalOutput").ap()
    wsc = nc.dram_tensor("wsc", [NU, 128, 4096], BF16).ap()
    kt_d = nc.dram_tensor("kt_s", [NH, 128, S], BF16).ap()
    vv_d = nc.dram_tensor("vv_s", [128, NBK, NH, 2, 129], BF16).ap()

    with ExitStack() as es:
        P = Prog(nc, es)
        NW = 53200
        arena_t = es.enter_context(nc.sbuf_tensor("arena", [128, NW], F32))
        AR = Arena(arena_t[:, :], NW)
        banks = [es.enter_context(nc.psum_tensor("pb%d" % i, [128, 512], F32)) for i in range(8)]
        tb = [T("pb%d" % i) for i in range(8)]
        rot = [0]

        held = set()

        def nbank():
            while rot[0] in held:
                rot[0] = (rot[0] + 1) % 8
            i = rot[0]
            rot[0] = (rot[0] + 1) % 8
            return i

        def bf_view(i):
            return banks[i][:, :].bitcast(BF16)

        identf = AR.alloc([128, 128], F32)
        identb = AR.alloc([128, 128], BF16)
        onesb = AR.alloc([128, 128], BF16)
        esel = AR.alloc([65, 64], BF16)
        cms = AR.alloc([128, 4, 2, 256], BF16)
        kb = AR.alloc([128, 16], F32)
        kmean = AR.alloc([128, 8, 64], F32)
        kmean_b = AR.alloc([128, 8, 64], BF16)
        VT = AR.alloc([65, 8, 512], BF16)
        g1 = AR.alloc([128, 8], F32)
        g2 = AR.alloc([128, 8], F32)
        cw = AR.alloc([128, 8, 31], F32)
        cb = AR.alloc([128, 8], F32)
        lg = AR.alloc([128, 8], F32)
        lb = AR.alloc([128, 8], F32)
        qg = AR.alloc([128, 1], F32)
        kg = AR.alloc([128, 1], F32)
        epsc = AR.alloc([128, 1], F32)
        t_const = T("const")
        t_kmean = T("kmean")
        t_vt64 = T("vt64")
        dq_c = P.dmaq("const")
        const_mark = AR.off

        cms_f = AR.alloc([128, 2048], F32)
        lo_f = AR.alloc([65, 4096], F32)
        t_cst = T("cst")
        pq = P.pool
        P.dma(pq, identf, id_d, [], [t_cst], dq_c)
        P.dma(pq, cms_f, cms_d, [], [t_cst], dq_c)
        P.dma(pq, lo_f[64:65, :], lo_d, [], [t_cst], dq_c)
        P.dma(pq, kb, kb_d, [], [t_cst], dq_c)
        P.dma(pq, g1, norm1_g[0].rearrange("(kc p) -> p kc", p=128), [], [t_cst], dq_c, allow_slow_non_contiguous=True)
        P.dma(pq, g2, norm2_g[0].rearrange("(kc p) -> p kc", p=128), [], [t_cst], dq_c, allow_slow_non_contiguous=True)
        P.dma(pq, cb, dw_b[0].rearrange("(kc p) -> p kc", p=128), [], [t_cst], dq_c, allow_slow_non_contiguous=True)
        P.dma(pq, lg, ln_g[0].rearrange("(kc p) -> p kc", p=128), [], [t_cst], dq_c, allow_slow_non_contiguous=True)
        P.dma(pq, lb, ln_b[0].rearrange("(kc p) -> p kc", p=128), [], [t_cst], dq_c, allow_slow_non_contiguous=True)
        for kc_ in range(8):
            P.dma(pq, cw[:, kc_, :], dw_w[:, kc_ * 128:(kc_ + 1) * 128].rearrange("j p -> p j"), [], [t_cst], dq_c,
                  allow_slow_non_contiguous=True)
        P.dma(pq, qg, qg_d.rearrange("o p -> p o"), [], [t_cst], dq_c, allow_slow_non_contiguous=True)
        P.dma(pq, kg, kg_d.rearrange("o p -> p o"), [], [t_cst], dq_c, allow_slow_non_contiguous=True)
        dv = P.dve
        P.emit(dv, lambda e: e.tensor_copy(out=identb, in_=identf), [t_cst], [t_const])
        P.emit(dv, lambda e: e.memset(onesb, 1.0), [], [t_const])
        P.emit(dv, lambda e: e.memset(kmean.rearrange("p a b -> p (a b)"), 0.0), [], [t_const])
        P.emit(dv, lambda e: e.memset(epsc, EPS), [], [t_const])
        P.emit(dv, lambda e: e.tensor_copy(out=esel[0:64, :], in_=identf[0:64, 0:64]), [t_cst], [t_const])
        P.emit(dv, lambda e: e.memset(esel[64:65, :], 1.0), [], [t_const])
        P.emit(dv, lambda e: e.tensor_copy(out=cms.rearrange("p a b c -> p (a b c)"), in_=cms_f), [t_cst], [t_const])
        P.emit(dv, lambda e: e.tensor_copy(out=VT[64:65, :, :].rearrange("p a b -> p (a b)"), in_=lo_f[64:65, :]),
               [t_cst], [t_const])
        P.emit(dv, lambda e: e.tensor_scalar(out=qg, in0=qg, scalar1=float(DH) ** -0.5, scalar2=None, op0=ALU.mult),
               [t_cst], [t_const])
        P.barrier([t_cst, t_const])
        AR.off = const_mark
        phase_mark = AR.off

        def esel_lhsT(n):
            a = esel[0:65, n:n + 1]
            return bass.AP(a.tensor, a.offset, [[a.ap[0][0], 65], [0, 128]])

        stg = [AR.alloc([128, 8, 512], F32) for _ in range(2)]
        wbf = [AR.alloc([128, 8, 512], BF16) for _ in range(2)]
        t_stg = [T("stg0"), T("stg1")]
        t_wbf = [T("wbf0"), T("wbf1")]
        dq_stg = [P.dmaq("stg0"), P.dmaq("stg1")]
        dq_wbf = [P.dmaq("wbf0"), P.dmaq("wbf1")]
        t_unit = [T("unit%d" % i) for i in range(NU)]
        gains = {"g1": g1, "g2": g2}
        kv_units = ["k0", "k1", "v0", "v1"]
        conv_order = kv_units + [n_ for n_ in UNAMES if n_ not in kv_units]

        def conv_load(oi):
            nm = conv_order[oi]
            b = oi % 2
            for (src, row0, nk_, c0, ncol, dst, _g) in UNITS[nm]:
                sap = wsrc[src][row0:row0 + nk_ * 128, c0:c0 + ncol].rearrange("(kc p) n -> p kc n", p=128)
                P.dma(P.sp, stg[b][:, 0:nk_, dst:dst + ncol], sap, [], [t_stg[b]], dq_stg[b])

        def conv_cast_store(oi, act_only):
            nm = conv_order[oi]
            ui = UIDX[nm]
            b = oi % 2
            segs = UNITS[nm]
            nk = segs[0][2]
            gname = segs[0][6]
            for kc in range(nk):
                use_dve = (kc % 2 == 0) and not act_only
                if gname is None:
                    if use_dve:
                        P.emit(P.dve, lambda e, b=b, kc=kc: e.tensor_copy(out=wbf[b][:, kc, :], in_=stg[b][:, kc, :]),
                               [t_stg[b]], [t_wbf[b]])
                    else:
                        P.emit(P.act, lambda e, b=b, kc=kc: e.copy(out=wbf[b][:, kc, :], in_=stg[b][:, kc, :]),
                               [t_stg[b]], [t_wbf[b]])
                else:
                    gt = gains[gname]
                    if use_dve:
                        P.emit(P.dve, lambda e, b=b, kc=kc, gt=gt: e.tensor_scalar(
                            out=wbf[b][:, kc, :], in0=stg[b][:, kc, :], scalar1=gt[:, kc:kc + 1], scalar2=None,
                            op0=ALU.mult), [t_stg[b]], [t_wbf[b]])
                    else:
                        P.emit(P.act, lambda e, b=b, kc=kc, gt=gt: e.activation(
                            out=wbf[b][:, kc, :], in_=stg[b][:, kc, :], func=AF.Copy, scale=gt[:, kc:kc + 1]),
                            [t_stg[b]], [t_wbf[b]])
            P.dma(P.sp, wsc[ui, :, 0:nk * 512], wbf[b][:, 0:nk, :].rearrange("p a b -> p (a b)"), [t_wbf[b]],
                  [t_unit[ui]], dq_wbf[b])

        conv_load(0)
        conv_load(1)
        for oi in range(4):
            conv_cast_store(oi, act_only=False)
            conv_load(oi + 2)
        conv_next = [4]

        wk = AR.alloc([128, 8, 1024], BF16)
        wv = AR.alloc([128, 8, 1024], BF16)
        t_wk = T("wk")
        t_wv = T("wv")
        dq_wk = P.dmaq("wk")
        dq_wv = P.dmaq("wv")
        for i in range(2):
            P.dma(P.pool, wk[:, :, i * 512:(i + 1) * 512], wsc[UIDX["k%d" % i]].rearrange("p (a b) -> p a b", b=512),
                  [t_unit[UIDX["k%d" % i]]], [t_wk], dq_wk)
            P.dma(P.pool, wv[:, :, i * 512:(i + 1) * 512], wsc[UIDX["v%d" % i]].rearrange("p (a b) -> p a b", b=512),
                  [t_unit[UIDX["v%d" % i]]], [t_wv], dq_wv)
        xin1 = [AR.alloc([128, 4, 1024], F32) for _ in range(2)]
        t_xin1 = [T("xin1_0"), T("xin1_1")]
        dq_xin1 = [P.dmaq("xin1_0"), P.dmaq("xin1_1")]
        junk_cur = [AR.alloc([128, 1024], BF16)]
        t_junk = T("junk")
        ssq = [AR.alloc([128, 4], F32) for _ in range(2)]
        t_ssq = [T("ssq0"), T("ssq1")]
        rinv = [AR.alloc([128, 4], F32) for _ in range(2)]
        t_rinv = [T("rinv0"), T("rinv1")]
        xn = [AR.alloc([128, 1024], BF16) for _ in range(2)]
        t_xn = [T("xn0"), T("xn1")]
        xnT1 = [AR.alloc([128, 8, 512], BF16) for _ in range(2)]
        t_xnT1 = [T("xnT1_0"), T("xnT1_1")]
        sqb = [AR.alloc([128, 512], BF16) for _ in range(2)]
        t_sqb = [T("sqb0"), T("sqb1")]
        rkf = [AR.alloc([128, 512], F32) for _ in range(2)]
        t_rkf = [T("rkf0"), T("rkf1")]
        kst = [AR.alloc([128, 8, 512], BF16) for _ in range(2)]
        t_kst = [T("kst0"), T("kst1")]
        dq_kst = [P.dmaq("kst0"), P.dmaq("kst1")]
        vst = [AR.alloc([128, 2, 8, 2, 129], BF16) for _ in range(2)]
        t_vst = [T("vst0"), T("vst1")]
        dq_vst = [P.dmaq("vst0"), P.dmaq("vst1")]
        t_kt = T("kt_dram")
        t_vv = T("vv_dram")
        for b in range(2):
            P.emit(P.dve, lambda e, b=b: e.memset(vst[b].rearrange("p a b c d -> p (a b c d)"), 1.0), [], [t_vst[b]])

        def norm_tile(src, np_, t_src, ssq_ap, rinv_ap, t_s, t_r, xn_ap, t_x):
            jv = junk_cur[0][0:np_, :]
            P.emit(P.dve, lambda e: e.memset(ssq_ap, 0.0), [], [t_s])
            P.emit(P.act, lambda e: e.activation(out=jv, in_=src, func=AF.Square, accum_out=ssq_ap),
                   [t_src], [t_junk, t_s])
            P.emit(P.act, lambda e: e.activation(out=rinv_ap, in_=ssq_ap, func=AF.Ln, scale=1.0 / D,
                                                 bias=epsc[0:np_, :]), [t_s], [t_r])
            P.emit(P.act, lambda e: e.activation(out=rinv_ap, in_=rinv_ap, func=AF.Exp, scale=-0.5), [t_r], [t_r])
            P.emit(P.dve, lambda e: e.tensor_scalar(out=xn_ap, in0=src, scalar1=rinv_ap, scalar2=None, op0=ALU.mult),
                   [t_src, t_r], [t_x])

        def transpose_tile(xn_ap, np_, t_x, dst, t_dst, use_act):
            bi = nbank()
            pv = bf_view(bi)[:, 0:8 * np_].rearrange("p (a b) -> p a b", b=np_)
            for kc in range(8):
                P.emit(P.pe, lambda e, kc=kc: e.transpose(out=pv[:, kc, :], in_=xn_ap[:, kc * 128:(kc + 1) * 128],
                                                          identity=identb[0:np_, 0:np_]),
                       [t_x], [tb[bi]], ms=(kc == 7))
            if use_act:
                P.emit(P.act, lambda e: e.copy(out=dst, in_=pv), [tb[bi]], [t_dst])
            else:
                P.emit(P.dve, lambda e: e.tensor_copy(out=dst, in_=pv), [tb[bi]], [t_dst])

        def x1_load(c):
            b = c % 2
            P.dma(P.pool, xin1[b], x_all[c * 512:(c + 1) * 512, :].rearrange("(t p) d -> p t d", p=128), [],
                  [t_xin1[b]], dq_xin1[b])

        xn4 = [AR.alloc([128, 1024], BF16) for _ in range(4)]
        t_xn4 = [T("xn4_%d" % i) for i in range(4)]

        def p1_norm(c):
            b = c % 2
            for t in range(4):
                norm_tile(xin1[b][:, t, :], 128, t_xin1[b], ssq[b][:, t:t + 1], rinv[b][:, t:t + 1], t_ssq[b], t_rinv[b],
                          xn4[t], t_xn4[t])

        def p1_transpose(c):
            b = c % 2
            for t in range(4):
                transpose_tile(xn4[t], 128, t_xn4[t], xnT1[b][:, :, t * 128:(t + 1) * 128], t_xnT1[b],
                               use_act=(t % 2 == 0))

        def p1_norm_transpose(c):
            p1_norm(c)
            p1_transpose(c)

        x1_load(0)
        if nkv > 1:
            x1_load(1)
        p1_norm_transpose(0)
        for c in range(nkv):
            b = c % 2
            if conv_next[0] < len(conv_order):
                oi = conv_next[0]
                conv_cast_store(oi, act_only=True)
                if oi + 2 < len(conv_order):
                    conv_load(oi + 2)
                conv_next[0] += 1
            if c + 1 < nkv:
                p1_norm(c + 1)
            pend = None

            def k_tail(h, bi, pk, b=b, c=c):
                sb = h % 2
                b2i = nbank()
                p2 = banks[b2i][:, :]
                P.emit(P.pe, lambda e, p2=p2, sb=sb: e.matmul(out=p2, lhsT=onesb, rhs=sqb[sb], start=True, stop=True),
                       [t_sqb[sb]], [tb[b2i]])
                P.emit(P.act, lambda e, p2=p2, sb=sb: e.activation(out=rkf[sb], in_=p2, func=AF.Ln, scale=1.0 / DH,
                                                                   bias=epsc), [tb[b2i]], [t_rkf[sb]])
                P.emit(P.act, lambda e, sb=sb: e.activation(out=rkf[sb], in_=rkf[sb], func=AF.Exp, scale=-0.5),
                       [t_rkf[sb]], [t_rkf[sb]])
                P.emit(P.dve, lambda e, pk=pk, sb=sb, h=h, b=b: e.scalar_tensor_tensor(
                    out=kst[b][:, h, :], in0=pk, scalar=kg[:, 0:1], in1=rkf[sb], op0=ALU.mult, op1=ALU.mult),
                    [tb[bi], t_rkf[sb]], [t_kst[b]])
                P.emit(P.dve, lambda e, h=h, b=b, c=c: e.tensor_reduce(
                    out=kmean[:, h, 2 * c:2 * c + 2], in_=kst[b][:, h, :].rearrange("p (a l) -> p a l", l=256),
                    axis=AX.X, op=ALU.add), [t_kst[b]], [t_kmean])

            for h in range(NH):
                bi = nbank()
                pk = banks[bi][:, :]
                for kc in range(8):
                    P.emit(P.pe, lambda e, kc=kc, h=h, pk=pk, b=b: e.matmul(out=pk, lhsT=wk[:, kc, h * 128:(h + 1) * 128],
                                                                           rhs=xnT1[b][:, kc, :], start=(kc == 0),
                                                                           stop=(kc == 7)),
                           [t_wk, t_xnT1[b]], [tb[bi]], ms=(kc == 7))
                sb = h % 2
                P.emit(P.act, lambda e, pk=pk, sb=sb: e.activation(out=sqb[sb], in_=pk, func=AF.Square),
                       [tb[bi]], [t_sqb[sb]])
                held.add(bi)
                if pend is not None:
                    k_tail(*pend)
                    held.discard(pend[1])
                pend = (h, bi, pk)
            k_tail(*pend)
            held.discard(pend[1])
            P.dma(P.pool, kt_d[:, :, c * 512:(c + 1) * 512].rearrange("h p t -> p h t"), kst[b], [t_kst[b]], [t_kt],
                  dq_kst[b])
            if c + 2 < nkv:
                x1_load(c + 2)
            for blk in range(2):
                for kh in range(2):
                    for hf in range(2):
                        bi = nbank()
                        pv_ = banks[bi][:, :]
                        for kc in range(8):
                            P.emit(P.pe, lambda e, kc=kc, blk=blk, kh=kh, hf=hf, pv_=pv_, b=b: e.matmul(
                                out=pv_, lhsT=xnT1[b][:, kc, blk * 256 + kh:blk * 256 + 256:2],
                                rhs=wv[:, kc, hf * 512:(hf + 1) * 512], start=(kc == 0), stop=(kc == 7)),
                                [t_wv, t_xnT1[b]], [tb[bi]], ms=(kc == 7))
                        dst = vst[b][:, blk, hf * 4:(hf + 1) * 4, kh, 0:128]
                        src = pv_.rearrange("p (a d) -> p a d", d=128)
                        if hf == 0:
                            P.emit(P.act, lambda e, dst=dst, src=src: e.copy(out=dst, in_=src), [tb[bi]], [t_vst[b]])
                        else:
                            P.emit(P.dve, lambda e, dst=dst, src=src: e.tensor_copy(out=dst, in_=src), [tb[bi]],
                                   [t_vst[b]])
                if blk == 0 and c + 1 < nkv:
                    p1_transpose(c + 1)
            P.dma(P.pool, vv_d[:, 2 * c:2 * c + 2].rearrange("p a b c d -> p (a b c d)"),
                  vst[b].rearrange("p a b c d -> p (a b c d)"), [t_vst[b]], [t_vv], dq_vst[b])
        while conv_next[0] < len(conv_order):
            oi = conv_next[0]
            conv_cast_store(oi, act_only=False)
            if oi + 2 < len(conv_order):
                conv_load(oi + 2)
            conv_next[0] += 1
        P.emit(P.dve, lambda e: e.tensor_copy(out=kmean_b.rearrange("p a b -> p (a b)"),
                                              in_=kmean.rearrange("p a b -> p (a b)")), [t_kmean], [t_kmean])
        ph1 = t_xn4 + t_xin1 + t_ssq + t_rinv + t_xn + t_sqb + t_rkf + t_kst + t_vst + t_xnT1 + [t_junk, t_wk, t_wv, t_kt, t_vv,
                                                                                         t_kmean] + tb + t_stg + t_wbf
        P.barrier(ph1)
        AR.off = phase_mark

        NSLOT = 3
        wr = [AR.alloc([128, 8, 512], BF16) for _ in range(NSLOT)]
        t_wr = [T("wr%d" % i) for i in range(NSLOT)]
        dq_wr = [P.dmaq("wr%d" % i) for i in range(NSLOT)]
        seq_all = []
        for g in range(nch):
            seq_all += CHUNK_SEQ
        wstate = {"issued": 0, "used": 0}

        def w_issue():
            i = wstate["issued"]
            if i >= len(seq_all):
                return
            nm = seq_all[i]
            s_ = i % NSLOT
            ui = UIDX[nm]
            nk = UNITS[nm][0][2]
            P.dma(P.sp, wr[s_][:, 0:nk, :].rearrange("p a b -> p (a b)"), wsc[ui, :, 0:nk * 512], [t_unit[ui]],
                  [t_wr[s_]], dq_wr[s_])
            wstate["issued"] += 1

        def w_next(expect, ahead=NSLOT - 1):
            i = wstate["used"]
            assert seq_all[i] == expect, (seq_all[i], expect)
            while wstate["issued"] < min(i + ahead + 1, len(seq_all)):
                w_issue()
            wstate["used"] += 1
            s_ = i % NSLOT
            return wr[s_], t_wr[s_]

        xin = AR.alloc([128, 4, 1024], F32)
        t_xin = T("xin")
        dq_xin = P.dmaq("xin")
        xh = AR.alloc([32, 2, 1024], F32)
        t_xh = T("xh")
        dq_xh = P.dmaq("xh")
        a2t = AR.alloc([128, 2, 8, 64], F32)
        b2t = AR.alloc([128, 2, 64], F32)
        gbt = AR.alloc([128, 2, 64], F32)
        t_tab = T("tab")
        dq_tab = P.dmaq("tab")
        junk_cur[0] = AR.alloc([128, 1024], BF16)
        ssq2 = AR.alloc([128, 8], F32)
        rinv2 = AR.alloc([128, 8], F32)
        t_ssq2 = [T("ssq2_%d" % i) for i in range(6)]
        t_rinv2 = [T("rinv2_%d" % i) for i in range(6)]
        xn = [AR.alloc([128, 1024], BF16) for _ in range(2)]
        xnT_raw = AR.alloc([128, 8 * 576], BF16)
        xnT = xnT_raw.rearrange("p (a b) -> p a b", b=576)
        t_xnT = T("xnT")
        R1 = AR.alloc([128, 8, 576 + 512], F32)
        a_t = R1[:, :, 0:576]
        y_t = R1[:, :, 576:1088]
        act_t = R1.rearrange("p a b -> p (a b)").bitcast(BF16)[:, 0:FC * 512].rearrange("p (a b) -> p a b", b=512)
        t_a = T("a")
        t_y = T("y")
        t_actt = T("act")
        ybf = [AR.alloc([128, 512], BF16) for _ in range(2)]
        t_ybf = [T("ybf0"), T("ybf1")]
        ysq = [AR.alloc([128, 512], BF16) for _ in range(2)]
        t_ysq = [T("ysq0"), T("ysq1")]
        mean_t = AR.alloc([128, 512], F32)
        rstd_t = AR.alloc([128, 512], F32)
        t_mean = T("mean")
        t_rstd = T("rstd")
        xc = [AR.alloc([128, 512], F32) for _ in range(2)]
        t_xc = [T("xc0"), T("xc1")]
        sig_t = [x_[:, 0:288].rearrange("p (a b) -> p a b", a=1) for x_ in xc]
        t_sig = t_xc
        ysl = AR.alloc([128, 8, 512], BF16)
        t_ysl = T("ysl")
        mT = ysl
        t_mT = t_ysl
        t1 = AR.alloc([128, 8, 512], BF16)
        t_t1 = T("t1")
        qT = AR.alloc([128, 8, 512], BF16)
        t_qT = T("qT")
        qf = [AR.alloc([128, 512], F32) for _ in range(2)]
        t_qf = [T("qf0"), T("qf1")]
        off_g = AR.off
        gcs = AR.alloc([128, 8, 512], BF16)
        gas = AR.alloc([128, 8, 512], BF16)
        assert AR.off - off_g == 4096
        outbuf = AR.ap[:, off_g:off_g + 4096].rearrange("p (t d) -> p t d", d=1024)
        t_gcs = T("gcs")
        t_gas = T("gas")
        attT = AR.alloc([128, 8, 512], BF16)
        t_attT = T("attT")
        hnT = xnT_raw[:, 0:8 * 512].rearrange("p (a b) -> p a b", b=512)
        t_hnT = t_xnT
        NKV = 2
        ktp = [AR.alloc([128, 2048], BF16) for _ in range(NKV)]
        vp = [AR.alloc([128, 8, 2, 129], BF16) for _ in range(NKV)]
        t_ktp = [T("ktp%d" % i) for i in range(NKV)]
        t_vp = [T("vp%d" % i) for i in range(NKV)]
        dq_ktp = [P.dmaq("ktp%d" % i) for i in range(NKV)]
        dq_vp = [P.dmaq("vp%d" % i) for i in range(NKV)]
        NPT = 4
        PT = [AR.alloc([128, 512], BF16) for _ in range(NPT)]
        t_PT = [T("PT%d" % i) for i in range(NPT)]
        gs_t = [AR.alloc([128, 64], F32) for _ in range(4)]
        m8_t = [AR.alloc([128, 8], F32) for _ in range(4)]
        tmp_t = [AR.alloc([128, 64], F32) for _ in range(4)]
        hiv_t = [AR.alloc([128, 64], BF16) for _ in range(4)]
        t_gs = [T("gs%d" % i) for i in range(4)]
        t_m8 = [T("m8%d" % i) for i in range(4)]
        t_tmp = [T("tmp%d" % i) for i in range(4)]
        t_hiv = [T("hiv%d" % i) for i in range(4)]
        rl_t = [AR.alloc([128, 1], F32) for _ in range(2)]
        t_rl = [T("rl0"), T("rl1")]
        atok = [AR.alloc([128, 128], BF16) for _ in range(2)]
        t_atok = [T("atok0"), T("atok1")]
        sg_t = xc
        t_sg = t_xc
        tm2 = xc
        t_tm2 = t_xc
        dq_out = P.dmaq("out")
        t_outd = T("out_dram")
        kvstate = {"n": 0}
        ptstate = {"n": 0}

        def mm_group(out_ap, bank_i, pairs, extra_reads, first=True, last=True):
            n_ = len(pairs)
            for i_, (l_, r_) in enumerate(pairs):
                P.emit(P.pe, lambda e, l_=l_, r_=r_, i_=i_: e.matmul(out=out_ap, lhsT=l_, rhs=r_,
                                                                     start=(first and i_ == 0),
                                                                     stop=(last and i_ == n_ - 1)),
                       extra_reads, [tb[bank_i]], ms=(i_ == n_ - 1))

        for g in range(nch):
            P.dma(P.pool, xin, x_own[g * 512:(g + 1) * 512, :].rearrange("(t p) d -> p t d", p=128), [], [t_xin], dq_xin)
            P.dma(P.pool, xh, x_halo[g * 64:(g + 1) * 64, :].rearrange("(o p) d -> p o d", p=32), [], [t_xh], dq_xh)
            P.dma(P.pool, a2t, a2_d[2 * g:2 * g + 2].partition_broadcast(128), [], [t_tab], dq_tab)
            P.dma(P.pool, b2t, b2_d[2 * g:2 * g + 2].partition_broadcast(128), [], [t_tab], dq_tab)
            P.dma(P.pool, gbt, gate_bias[2 * g:2 * g + 2].partition_broadcast(128), [], [t_tab], dq_tab)
            nbuf = [(xn[0], t_xn[0]), (xn[1], t_xn[1]), (qf[0].bitcast(BF16), t_qf[0]), (qf[1].bitcast(BF16), t_qf[1])]
            for o in range(2):
                nb_ap, nb_t = nbuf[o]
                norm_tile(xh[0:32, o, :], 32, t_xh, ssq2[0:32, 4 + o:5 + o], rinv2[0:32, 4 + o:5 + o], t_ssq2[4 + o],
                          t_rinv2[4 + o], nb_ap[0:32, :], nb_t)
            for t in range(4):
                nb_ap, nb_t = nbuf[(t + 2) % 4]
                norm_tile(xin[:, t, :], 128, t_xin, ssq2[:, t:t + 1], rinv2[:, t:t + 1], t_ssq2[t], t_rinv2[t], nb_ap, nb_t)
                if t == 1:
                    for o in range(2):
                        nb_ap2, nb_t2 = nbuf[o]
                        transpose_tile(nb_ap2[0:32, :], 32, nb_t2, xnT[:, :, o * 288:o * 288 + 32], t_xnT,
                                       use_act=(o == 0))
            for t in range(4):
                nb_ap, nb_t = nbuf[(t + 2) % 4]
                c0 = (t // 2) * 288 + 32 + (t % 2) * 128
                transpose_tile(nb_ap, 128, nb_t, xnT[:, :, c0:c0 + 128], t_xnT, use_act=(t % 2 == 0))
            for u in range(4):
                w_, tw = w_next("cv%d" % u)
                for ci in range(2):
                    cc = 2 * u + ci
                    bu = nbank()
                    bg = nbank()
                    bu2 = nbank()
                    bg2 = nbank()
                    for ob, (bcu, bcg) in enumerate(((bu, bg), (bu2, bg2))):
                        mm_group(banks[bcu][:, 0:288],
                                 bcu, [(w_[:, kc, ci * 128:(ci + 1) * 128], xnT[:, kc, ob * 288:(ob + 1) * 288])
                                       for kc in range(8)], [tw, t_xnT])
                        mm_group(banks[bcg][:, 0:288],
                                 bcg, [(w_[:, kc, 256 + ci * 128:256 + (ci + 1) * 128], xnT[:, kc, ob * 288:(ob + 1) * 288])
                                       for kc in range(8)], [tw, t_xnT])
                        sb = ob
                        P.emit(P.act, lambda e, bcg=bcg, sb=sb, ob=ob: e.activation(out=sig_t[sb][:, 0, :],
                                                                                   in_=banks[bcg][:, 0:288],
                                                                                   func=AF.Sigmoid),
                               [tb[bcg]], [t_sig[sb]])
                        P.emit(P.dve, lambda e, bcu=bcu, sb=sb, ob=ob, cc=cc: e.tensor_tensor(
                            out=a_t[:, cc, ob * 288:(ob + 1) * 288], in0=banks[bcu][:, 0:288], in1=sig_t[sb][:, 0, :],
                            op=ALU.mult), [tb[bcu], t_sig[sb]], [t_a])
            def q_main(h, w_, tw, hi_):
                bi = nbank()
                pq_ = banks[bi][:, :]
                for ob in range(2):
                    mm_group(pq_[:, ob * 256:(ob + 1) * 256], bi,
                             [(w_[:, kc, hi_ * 128:(hi_ + 1) * 128], xnT[:, kc, ob * 288 + 32:ob * 288 + 288])
                              for kc in range(8)], [tw, t_xnT])
                sb = h % 2
                P.emit(P.act, lambda e, pq_=pq_, sb=sb: e.activation(out=ysq[sb], in_=pq_, func=AF.Square),
                       [tb[bi]], [t_ysq[sb]])
                held.add(bi)
                return (h, bi, pq_)

            def q_stage_a(stt_):
                h, bi, pq_ = stt_
                sb = h % 2
                b2i = nbank()
                p2 = banks[b2i][:, :]
                mm_group(p2, b2i, [(onesb, ysq[sb])], [t_ysq[sb]])
                P.emit(P.act, lambda e, p2=p2, sb=sb: e.activation(out=xc[sb], in_=p2, func=AF.Ln, scale=1.0 / DH,
                                                                   bias=epsc), [tb[b2i]], [t_xc[sb]])
                P.emit(P.act, lambda e, sb=sb: e.activation(out=xc[sb], in_=xc[sb], func=AF.Exp, scale=-0.5),
                       [t_xc[sb]], [t_xc[sb]])
                P.emit(P.dve, lambda e, pq_=pq_, sb=sb: e.scalar_tensor_tensor(
                    out=qf[sb], in0=pq_, scalar=qg[:, 0:1], in1=xc[sb], op0=ALU.mult, op1=ALU.mult),
                    [tb[bi], t_xc[sb]], [t_qf[sb]])
                P.emit(P.act, lambda e, sb=sb, h=h: e.copy(out=qT[:, h, :], in_=qf[sb]), [t_qf[sb]], [t_qT])
                held.discard(bi)
                return h

            def q_stage_b(h):
                sb = h % 2
                for t in range(4):
                    ob = t // 2
                    bgi = nbank()
                    pg = banks[bgi][:, 0:64]
                    mm_group(pg, bgi, [(qT[:, h, t * 128:(t + 1) * 128], kmean_b[:, h, :])], [t_qT, t_kmean])
                    P.emit(P.dve, lambda e, pg=pg, t=t, ob=ob: e.tensor_tensor(out=gs_t[t], in0=pg,
                                                                              in1=gbt[:, ob, :], op=ALU.add),
                           [tb[bgi], t_tab], [t_gs[t]])
                    P.emit(P.dve, lambda e, t=t: e.max(out=m8_t[t], in_=gs_t[t]), [t_gs[t]], [t_m8[t]])
                    P.emit(P.dve, lambda e, t=t, ob=ob, h=h: e.scalar_tensor_tensor(
                        out=tmp_t[t], in0=gs_t[t], scalar=m8_t[t][:, 2:3], in1=a2t[:, ob, h, :], op0=ALU.is_ge,
                        op1=ALU.mult), [t_gs[t], t_m8[t], t_tab], [t_tmp[t]])
                    P.emit(P.dve, lambda e, t=t, ob=ob: e.tensor_tensor(out=hiv_t[t], in0=tmp_t[t],
                                                                       in1=b2t[:, ob, :], op=ALU.add),
                           [t_tmp[t], t_tab], [t_hiv[t]])
                return h

            def q_stage_c(h):
                for t in range(4):
                    bti = nbank()
                    ptv = bf_view(bti)[0:64, 0:128]
                    P.emit(P.pe, lambda e, ptv=ptv, t=t: e.transpose(out=ptv, in_=hiv_t[t], identity=identb),
                           [t_hiv[t]], [tb[bti]])
                    P.emit(P.act, lambda e, ptv=ptv, h=h, t=t: e.copy(out=VT[0:64, h, t * 128:(t + 1) * 128], in_=ptv),
                           [tb[bti]], [t_vt64])

            def gate_mm(w_, tw, ci, cc, dst, tdst):
                bi = nbank()
                for ob in range(2):
                    mm_group(banks[bi][:, ob * 256:(ob + 1) * 256], bi,
                             [(w_[:, kc, ci * 128:(ci + 1) * 128], xnT[:, kc, ob * 288 + 32:ob * 288 + 288])
                              for kc in range(8)], [tw, t_xnT])
                P.emit(P.act, lambda e, bi=bi, dst=dst, cc=cc: e.activation(out=dst[:, cc, :], in_=banks[bi][:, :],
                                                                           func=AF.Sigmoid), [tb[bi]], [tdst])

            pa = pb = pc = None
            for u in range(2):
                wq, twq = w_next("q%d" % u, 2)
                wg, twg = w_next("gc%d" % u, 1)
                for hi_ in range(4):
                    h = 4 * u + hi_
                    cur = q_main(h, wq, twq, hi_)
                    na = q_stage_a(pa) if pa is not None else None
                    gate_mm(wg, twg, hi_, h, gcs, t_gcs)
                    if pc is not None:
                        q_stage_c(pc)
                    nb_ = q_stage_b(pb) if pb is not None else None
                    pa, pb, pc = cur, na, nb_
            for u in range(2):
                wg, twg = w_next("ga%d" % u, 2)
                for ci in range(4):
                    gate_mm(wg, twg, ci, 4 * u + ci, gas, t_gas)
                    na = q_stage_a(pa) if pa is not None else None
                    if pc is not None:
                        q_stage_c(pc)
                    nb_ = q_stage_b(pb) if pb is not None else None
                    pa, pb, pc = None, na, nb_
            assert pa is None and pb is None and pc is None
            def conv_cc(cc):
                yv = y_t[:, cc, :].rearrange("p (o l) -> p o l", l=256)

                def av(j, cc=cc):
                    return a_t[:, cc, :].rearrange("p (o l) -> p o l", l=288)[:, :, 2 + j:2 + j + 256]
                P.emit(P.dve, lambda e, yv=yv, av=av, cc=cc: e.tensor_scalar(out=yv, in0=av(0), scalar1=cw[:, cc, 0:1],
                                                                            scalar2=cb[:, cc:cc + 1], op0=ALU.mult,
                                                                            op1=ALU.add), [t_a], [t_y])
                for j in range(1, 31):
                    P.emit(P.dve, lambda e, yv=yv, av=av, cc=cc, j=j: e.scalar_tensor_tensor(
                        out=yv, in0=av(j), scalar=cw[:, cc, j:j + 1], in1=yv, op0=ALU.mult, op1=ALU.add), [t_a, t_y], [t_y])

            def normalize_head(h):
                for t in range(4):
                    ob_i = 4 + t
                    o_ap = banks[ob_i][:, 0:129]
                    mb = t % 2
                    P.emit(P.dve, lambda e, o_ap=o_ap, mb=mb: e.reciprocal(out=rl_t[mb], in_=o_ap[:, 128:129]),
                           [tb[ob_i]], [t_rl[mb]])
                    P.emit(P.dve, lambda e, o_ap=o_ap, mb=mb: e.tensor_scalar(out=atok[mb], in0=o_ap[:, 0:128],
                                                                              scalar1=rl_t[mb][:, 0:1], scalar2=None,
                                                                              op0=ALU.mult),
                           [tb[ob_i], t_rl[mb]], [t_atok[mb]])
                    bti = (srot[0] - 3) % 4 if t % 2 == 0 else srot[0] % 4
                    ptv = bf_view(bti)[:, 0:128]
                    P.emit(P.pe, lambda e, ptv=ptv, mb=mb: e.transpose(out=ptv, in_=atok[mb], identity=identb),
                           [t_atok[mb]], [tb[bti]])
                    P.emit(P.act, lambda e, ptv=ptv, h=h, t=t: e.copy(out=attT[:, h, t * 128:(t + 1) * 128], in_=ptv),
                           [tb[bti]], [t_attT])

            nblk = 8 * g + 8
            npp = nblk // 8
            units = [(h, n, kh) for h in range(NH) for n in range(nblk) for kh in range(2)]
            piece_list = [(h, n0) for h in range(NH) for n0 in range(0, nblk, 8)]
            piece_slot = {}
            piece_issued = [0]

            def ensure_piece(k):
                while piece_issued[0] <= k and piece_issued[0] < len(piece_list):
                    h_, n0_ = piece_list[piece_issued[0]]
                    s_ = kvstate["n"] % NKV
                    kvstate["n"] += 1
                    P.dma(P.pool, ktp[s_], kt_d[h_, :, n0_ * 256:(n0_ + 8) * 256], [t_kt], [t_ktp[s_]], dq_ktp[s_])
                    P.dma(P.pool, vp[s_], vv_d[:, n0_:n0_ + 8, h_, :, :], [t_vv], [t_vp[s_]], dq_vp[s_])
                    piece_slot[piece_issued[0]] = s_
                    piece_issued[0] += 1

            LA = 2
            srot = [0]
            ensure_piece(1)
            st = {}
            for idx in range(len(units) + LA):
                if idx < len(units):
                    h, n, kh = units[idx]
                    s_ = piece_slot[h * npp + n // 8]
                    nl = n % 8
                    bi = srot[0]
                    srot[0] = (srot[0] + 1) % 4
                    S_ = banks[bi][:, :]
                    cand = n >= 8 * g
                    c0 = 256 if n >= 8 * g + 4 else 0
                    P.emit(P.pe, lambda e, S_=S_, s_=s_, nl=nl, kh=kh, h=h, c0=c0: e.matmul(
                        out=S_[:, c0:512], lhsT=ktp[s_][:, nl * 256 + kh:nl * 256 + 256:2], rhs=qT[:, h, c0:512],
                        start=True, stop=False), [t_ktp[s_], t_qT], [tb[bi]], ms=False)
                    P.emit(P.pe, lambda e, S_=S_, n=n, h=h, cand=cand, c0=c0: e.matmul(
                        out=S_[:, c0:512], lhsT=esel_lhsT(n), rhs=VT[0:65, h, c0:512], start=False, stop=(not cand)),
                        [t_vt64], [tb[bi]], ms=(not cand))
                    if cand:
                        ob = (n - 8 * g) // 4
                        cnd = (n - 8 * g) % 4
                        P.emit(P.pe, lambda e, S_=S_, ob=ob, cnd=cnd, kh=kh: e.matmul(
                            out=S_[:, ob * 256:(ob + 1) * 256], lhsT=identb, rhs=cms[:, cnd, kh, :], start=False,
                            stop=True), [], [tb[bi]], ms=True)
                    st[idx] = (bi, s_)
                j = idx - LA
                if j >= 0:
                    h, n, kh = units[j]
                    bi, s_ = st.pop(j)
                    S_ = banks[bi][:, :]
                    nl = n % 8
                    if n % 8 == 0 and kh == 0:
                        ensure_piece(h * npp + n // 8 + 1)
                    first = (n == 0 and kh == 0)
                    lastblk = (n == nblk - 1 and kh == 1)
                    r_ = ptstate["n"] % NPT
                    ptstate["n"] += 1
                    c0 = 256 if n >= 8 * g + 4 else 0
                    lastfull = (n == 8 * g + 3 and kh == 1)
                    P.emit(P.act, lambda e, S_=S_, r_=r_, h=h, kh=kh, c0=c0: e.activation(
                        out=PT[r_][:, c0:512], in_=S_[:, c0:512], func=AF.Exp, bias=kb[:, 2 * h + kh:2 * h + kh + 1]),
                        [tb[bi]], [t_PT[r_]])
                    for t in range(c0 // 128, 4):
                        ob_i = 4 + t
                        o_ap = banks[ob_i][:, 0:129]
                        stp = lastblk if t >= 2 else lastfull
                        P.emit(P.pe, lambda e, o_ap=o_ap, r_=r_, t=t, s_=s_, nl=nl, kh=kh, first=first,
                               stp=stp: e.matmul(out=o_ap, lhsT=PT[r_][:, t * 128:(t + 1) * 128],
                                                 rhs=vp[s_][:, nl, kh, :], start=first, stop=stp),
                               [t_PT[r_], t_vp[s_]], [tb[ob_i]], ms=(t == 3))
                    if lastblk:
                        normalize_head(h)
                        conv_cc(h)
            for u in range(2):
                w_, tw = w_next("ao%d" % u)
                for ci in range(4):
                    cc = 4 * u + ci
                    bi = nbank()
                    mm_group(banks[bi][:, :], bi, [(w_[:, kc, ci * 128:(ci + 1) * 128], attT[:, kc, :]) for kc in range(8)],
                             [tw, t_attT])
                    P.emit(P.dve, lambda e, bi=bi, cc=cc: e.tensor_tensor(out=t1[:, cc, :], in0=banks[bi][:, :],
                                                                          in1=gas[:, cc, :], op=ALU.mult),
                           [tb[bi], t_gas], [t_t1])
            bs1 = nbank()
            bs2 = nbank()
            for cc in range(8):
                sb = cc % 2
                P.emit(P.act, lambda e, sb=sb, cc=cc: e.copy(out=ybf[sb], in_=y_t[:, cc, :]), [t_y], [t_ybf[sb]])
                P.emit(P.act, lambda e, sb=sb, cc=cc: e.activation(out=ysq[sb], in_=y_t[:, cc, :], func=AF.Square),
                       [t_y], [t_ysq[sb]])
                P.emit(P.pe, lambda e, sb=sb, cc=cc, bs1=bs1: e.matmul(out=banks[bs1][:, :], lhsT=onesb, rhs=ybf[sb],
                                                              start=(cc == 0), stop=(cc == 7)),
                       [t_ybf[sb]], [tb[bs1]], ms=True)
                P.emit(P.pe, lambda e, sb=sb, cc=cc, bs2=bs2: e.matmul(out=banks[bs2][:, :], lhsT=onesb, rhs=ysq[sb],
                                                              start=(cc == 0), stop=(cc == 7)),
                       [t_ysq[sb]], [tb[bs2]], ms=True)
            P.emit(P.dve, lambda e, bs1=bs1: e.tensor_scalar(out=mean_t, in0=banks[bs1][:, :], scalar1=1.0 / D, scalar2=None,
                                                    op0=ALU.mult), [tb[bs1]], [t_mean])
            P.emit(P.dve, lambda e: e.tensor_tensor(out=rstd_t, in0=mean_t, in1=mean_t, op=ALU.mult), [t_mean], [t_rstd])
            P.emit(P.dve, lambda e, bs2=bs2: e.scalar_tensor_tensor(out=rstd_t, in0=banks[bs2][:, :], scalar=1.0 / D, in1=rstd_t,
                                                           op0=ALU.mult, op1=ALU.subtract), [tb[bs2], t_rstd], [t_rstd])
            P.emit(P.act, lambda e: e.activation(out=rstd_t, in_=rstd_t, func=AF.Ln, bias=epsc), [t_rstd], [t_rstd])
            P.emit(P.act, lambda e: e.activation(out=rstd_t, in_=rstd_t, func=AF.Exp, scale=-0.5), [t_rstd], [t_rstd])
            for cc in range(8):
                sb = cc % 2
                P.emit(P.dve, lambda e, sb=sb, cc=cc: e.tensor_tensor(out=xc[sb], in0=y_t[:, cc, :], in1=mean_t,
                                                                      op=ALU.subtract), [t_y, t_mean], [t_xc[sb]])
                P.emit(P.dve, lambda e, sb=sb: e.tensor_tensor(out=xc[sb], in0=xc[sb], in1=rstd_t, op=ALU.mult),
                       [t_xc[sb], t_rstd], [t_xc[sb]])
                P.emit(P.act, lambda e, sb=sb, cc=cc: e.activation(out=ysl[:, cc, :], in_=xc[sb], func=AF.Silu,
                                                                   scale=lg[:, cc:cc + 1], bias=lb[:, cc:cc + 1]),
                       [t_xc[sb]], [t_ysl])
            for u in range(2):
                w_, tw = w_next("co%d" % u)
                for ci in range(4):
                    cc = 4 * u + ci
                    bi = nbank()
                    sb = cc % 2
                    mm_group(banks[bi][:, :], bi, [(w_[:, kc, ci * 128:(ci + 1) * 128], ysl[:, kc, :]) for kc in range(8)],
                             [tw, t_ysl])
                    P.emit(P.dve, lambda e, bi=bi, cc=cc, sb=sb: e.tensor_tensor(out=tm2[sb], in0=banks[bi][:, :],
                                                                                 in1=gcs[:, cc, :], op=ALU.mult),
                           [tb[bi], t_gcs], [t_tm2[sb]])
                    P.emit(P.dve, lambda e, cc=cc, sb=sb: e.tensor_tensor(out=t1[:, cc, :], in0=tm2[sb], in1=t1[:, cc, :],
                                                                          op=ALU.add), [t_tm2[sb], t_t1], [t_t1])
            for hf in range(2):
                w_, tw = w_next("wo%d" % hf)
                for t in range(4):
                    bi = nbank()
                    mm_group(banks[bi][:, :], bi, [(t1[:, kc, t * 128:(t + 1) * 128], w_[:, kc, :]) for kc in range(8)],
                             [tw, t_t1])
                    P.emit(P.dve, lambda e, bi=bi, t=t, hf=hf: e.tensor_tensor(
                        out=xin[:, t, hf * 512:(hf + 1) * 512], in0=banks[bi][:, :], in1=xin[:, t, hf * 512:(hf + 1) * 512],
                        op=ALU.add), [tb[bi], t_xin], [t_xin])
            for t in range(4):
                nb_ap, nb_t = nbuf[t]
                norm_tile(xin[:, t, :], 128, t_xin, ssq2[:, t:t + 1], rinv2[:, t:t + 1], t_ssq2[t], t_rinv2[t], nb_ap, nb_t)
            for t in range(4):
                nb_ap, nb_t = nbuf[t]
                transpose_tile(nb_ap, 128, nb_t, hnT[:, :, t * 128:(t + 1) * 128], t_hnT, use_act=(t % 2 == 0))
            for u in range(11):
                w_, tw = w_next("gu%d" % u)
                for ci in range(2):
                    fc = 2 * u + ci
                    sb = fc % 2
                    bgt = nbank()
                    mm_group(banks[bgt][:, :], bgt, [(w_[:, kc, ci * 128:(ci + 1) * 128], hnT[:, kc, :]) for kc in range(8)],
                             [tw, t_hnT])
                    but = nbank()
                    mm_group(banks[but][:, :], but,
                             [(w_[:, kc, 256 + ci * 128:256 + (ci + 1) * 128], hnT[:, kc, :]) for kc in range(8)],
                             [tw, t_hnT])
                    P.emit(P.act, lambda e, bgt=bgt, sb=sb: e.activation(out=sg_t[sb], in_=banks[bgt][:, :], func=AF.Silu),
                           [tb[bgt]], [t_sg[sb]])
                    P.emit(P.dve, lambda e, but=but, sb=sb, fc=fc: e.tensor_tensor(out=act_t[:, fc, :], in0=banks[but][:, :],
                                                                                   in1=sg_t[sb], op=ALU.mult),
                           [tb[but], t_sg[sb]], [t_actt, t_a, t_y])
            for hf in range(2):
                for gi, (f0, nf) in enumerate(((0, 8), (8, 8), (16, 6))):
                    w_, tw = w_next("dn%d_%d" % (hf, gi))
                    for t in range(4):
                        bi = 4 + t
                        for fl in range(nf):
                            fc = f0 + fl
                            P.emit(P.pe, lambda e, bi=bi, t=t, fc=fc, fl=fl, w_=w_: e.matmul(
                                out=banks[bi][:, :], lhsT=act_t[:, fc, t * 128:(t + 1) * 128], rhs=w_[:, fl, :],
                                start=(fc == 0), stop=(fc == FC - 1)), [tw, t_actt], [tb[bi]], ms=(fl == nf - 1))
                for t in range(4):
                    bi = 4 + t
                    P.emit(P.dve, lambda e, bi=bi, t=t, hf=hf: e.tensor_tensor(
                        out=outbuf[:, t, hf * 512:(hf + 1) * 512], in0=banks[bi][:, :],
                        in1=xin[:, t, hf * 512:(hf + 1) * 512], op=ALU.add), [tb[bi], t_xin], [t_gcs, t_gas])
            P.dma(P.pool, out_d[g * 512:(g + 1) * 512, :].rearrange("(t p) d -> p t d", p=128), outbuf, [t_gcs, t_gas],
                  [t_outd], dq_out)
            t_a.r += t_actt.r
            t_y.r += t_actt.r
            if t_actt.w is not None:
                t_a.r.append(t_actt.w)
                t_y.r.append(t_actt.w)
        P.wait_all(P.pool, [t_outd])
        P.wait_all(P.sp, [t_outd])
        P.run()
    return nc


def host_tables(j):
    slopes = 2.0 ** (-8.0 * np.arange(1, NH + 1) / NH)
    gate_bias = np.zeros((16, 64), np.float32)
    a2 = np.zeros((16, 8, 64), np.float32)
    b2 = np.full((16, 64), NEG, np.float32)
    for i in range(16):
        cur = 4 * i + j
        gate_bias[i, cur:] = -1e30
        b2[i, cur] = 0.0
        for n in range(cur):
            a2[i, :, n] = -NEG - slopes * 256.0 * (cur - n)
    cmsel = np.zeros((128, 4, 2, 256), np.float32)
    p = np.arange(128)[:, None]
    rq = np.arange(256)[None, :]
    for kh in range(2):
        rk = 2 * p + kh
        cmsel[:, j, kh, :] = np.where(rq >= rk, 0.0, NEG)
    lo = np.zeros((1, 8, 512), np.float32)
    for h in range(8):
        lo[0, h, :] = -slopes[h] * (np.arange(512) % 256)
    kbias = np.zeros((128, 16), np.float32)
    for h in range(8):
        for kh in range(2):
            kbias[:, 2 * h + kh] = slopes[h] * (2 * np.arange(128) + kh)
    return {"gate_bias": gate_bias, "a2": a2, "b2": b2, "cmsel": cmsel.reshape(128, -1),
            "lo": lo.reshape(1, -1), "kbias": kbias, "ident": np.eye(128, dtype=np.float32)}


def make_in_maps(inputs):
    x = np.asarray(inputs["x"], np.float32)
    shared = {
        "w_in": np.ascontiguousarray(inputs["w_in"][0]), "w_conv_out": np.ascontiguousarray(inputs["w_conv_out"][0]),
        "w_attn_out": np.ascontiguousarray(inputs["w_attn_out"][0]), "w_out": np.ascontiguousarray(inputs["w_out"][0]),
        "w_ffn_gate": np.ascontiguousarray(inputs["w_ffn_gate"][0]), "w_ffn_up": np.ascontiguousarray(inputs["w_ffn_up"][0]),
        "w_ffn_down": np.ascontiguousarray(inputs["w_ffn_down"][0]),
        "norm1_g": np.asarray(inputs["norm1_g"], np.float32).reshape(1, D),
        "norm2_g": np.asarray(inputs["norm2_g"], np.float32).reshape(1, D),
        "dw_w": np.ascontiguousarray(inputs["dw_w"][0]), "dw_b": np.asarray(inputs["dw_b"], np.float32).reshape(1, D),
        "conv_ln_g": np.asarray(inputs["conv_ln_g"], np.float32).reshape(1, D),
        "conv_ln_b": np.asarray(inputs["conv_ln_b"], np.float32).reshape(1, D),
        "q_norm_g": np.asarray(inputs["q_norm_g"], np.float32).reshape(1, DH),
        "k_norm_g": np.asarray(inputs["k_norm_g"], np.float32).reshape(1, DH),
    }
    shared = {k: np.asarray(v, np.float32) for k, v in shared.items()}
    maps = []
    for c in range(8):
        b, j = c // 4, c % 4
        xb = x[b]
        xblk = xb.reshape(NBK, L, D)
        own = [4 * i + j for i in range(16)]
        x_own = np.ascontiguousarray(xblk[own].reshape(4096, D))
        halo = np.zeros((16, 32, D), np.float32)
        for i, n in enumerate(own):
            if n > 0:
                halo[i] = xb[n * L - 32:n * L]
        m = dict(shared)
        m.update(host_tables(j))
        m["x_all"] = np.ascontiguousarray(xb)
        m["x_own"] = x_own
        m["x_halo"] = halo.reshape(512, D)
        maps.append(m)
    return maps


_NC_CACHE = {}


def kernel(**inputs):
    if "nc" not in _NC_CACHE:
        _NC_CACHE["nc"] = build_nc()
    nc = _NC_CACHE["nc"]
    maps = make_in_maps(inputs)
    res = run_bass_kernel_spmd(nc, maps, core_ids=list(range(8)))
    out = np.zeros((2, S, D), np.float32)
    ov = out.reshape(2, NBK, L, D)
    for c in range(8):
        b, j = c // 4, c % 4
        o = np.asarray(res.results[c]["out_own"], np.float32).reshape(16, L, D)
        for i in range(16):
            ov[b, 4 * i + j] = o[i]
    return out
```

```python
import numpy as np
from contextlib import ExitStack
import concourse.bass as bass
import concourse.mybir as mybir
from concourse.bass_utils import run_bass_kernel_spmd

F32 = mybir.dt.float32
BF16 = mybir.dt.bfloat16
AF = mybir.ActivationFunctionType
ALU = mybir.AluOpType
AX = mybir.AxisListType

D = 1024
KC = 8
NH = 8
DH = 128
FF = 2816
FC = 22
S = 16384
L = 256
NBK = 64
EPS = 1e-6
NEG = -30000.0


class T:
    __slots__ = ("name", "w", "r")

    def __init__(self, name):
        self.name = name
        self.w = None
        self.r = []


class Q:
    def __init__(self, name, sem, scale=1):
        self.name = name
        self.sem = sem
        self.scale = scale
        self.count = 0
        self.ops = []
        self.seen = {}


class Prog:
    def __init__(self, nc, es):
        self.nc = nc
        self.es = es
        self.pe = Q("pe", self.newsem("s_pe"))
        self.act = Q("act", self.newsem("s_act"))
        self.dve = Q("dve", self.newsem("s_dve"))
        self.pool = Q("pool", self.newsem("s_pool"))
        self.sp = Q("sp", self.newsem("s_sp"))
        self.engines = [self.pe, self.act, self.dve, self.pool, self.sp]

    def newsem(self, name):
        return self.es.enter_context(self.nc.semaphore(name))

    def dmaq(self, name):
        return Q(name, self.newsem("d_" + name), 16)

    def _wait(self, q, tok):
        sq, cnt = tok
        if q.seen.get(sq, 0) >= cnt:
            return
        q.seen[sq] = cnt
        q.ops.append(lambda e, s=sq.sem, v=cnt * sq.scale: e.wait_ge(s, v))

    def emit(self, q, fn, reads=(), writes=(), ms=True, sig=None):
        sig = sig or q
        for t in reads:
            if t.w is not None:
                self._wait(q, t.w)
        for t in writes:
            if t.w is not None and t.w[0] is not q:
                self._wait(q, t.w)
            for tok in t.r:
                if tok[0] is not q:
                    self._wait(q, tok)
        if ms:
            sig.count += 1
            tok = (sig, sig.count)
            q.ops.append(lambda e, s=sig.sem, v=sig.scale: fn(e).then_inc(s, v))
        else:
            tok = (sig, sig.count + 1)
            q.ops.append(lambda e: fn(e))
        for t in writes:
            t.w = tok
            t.r = []
        for t in reads:
            t.r.append(tok)
            if len(t.r) > 16:
                best = {}
                for sq, c in t.r:
                    if sq not in best or best[sq] < c:
                        best[sq] = c
                t.r = list(best.items())
        return tok

    def dma(self, q, out, in_, reads, writes, sig, **kw):
        return self.emit(q, lambda e: e.dma_start(out=out, in_=in_, **kw), reads, writes, sig=sig)

    def wait_all(self, q, ts):
        for t in ts:
            if t.w is not None:
                self._wait(q, t.w)
            for tok in t.r:
                self._wait(q, tok)

    def barrier(self, ts):
        for q in self.engines:
            self.wait_all(q, ts)

    def run(self):
        nc = self.nc
        with nc.Block() as block:
            @block.tensor
            def _(e):
                for op in self.pe.ops:
                    op(e)

            @block.scalar
            def _(e):
                for op in self.act.ops:
                    op(e)

            @block.vector
            def _(e):
                for op in self.dve.ops:
                    op(e)

            @block.gpsimd
            def _(e):
                for op in self.pool.ops:
                    op(e)

            @block.sync
            def _(e):
                for op in self.sp.ops:
                    op(e)


class Arena:
    def __init__(self, ap, nwords):
        self.ap = ap
        self.n = nwords
        self.off = 0

    def alloc(self, shape, dtype):
        shape = list(shape)
        np_ = shape[0]
        free = 1
        for s_ in shape[1:]:
            free *= s_
        esz = 4 if dtype == F32 else 2
        words = (free * esz + 3) // 4
        words = (words + 7) // 8 * 8
        assert self.off + words <= self.n, ("arena overflow", self.off, words, self.n)
        v = self.ap[0:np_, self.off:self.off + words]
        self.off += words
        if dtype != F32:
            v = v.bitcast(dtype)
        v = v[:, 0:free]
        if len(shape) == 2:
            return v
        names = " ".join("d%d" % i for i in range(len(shape) - 1))
        kw = {"d%d" % i: shape[i + 1] for i in range(len(shape) - 1)}
        return v.rearrange("p (%s) -> p %s" % (names, names), **kw)


def unit_table():
    u = {}
    for i in range(4):
        u["cv%d" % i] = [("w_in", 0, 8, (2 * i) * 128, 128, 0, "g1"), ("w_in", 0, 8, (2 * i + 1) * 128, 128, 128, "g1"),
                         ("w_in", 0, 8, 1024 + (2 * i) * 128, 128, 256, "g1"),
                         ("w_in", 0, 8, 1024 + (2 * i + 1) * 128, 128, 384, "g1")]
    for nm, c0 in (("q", 2048), ("k", 3072), ("v", 4096), ("gc", 5120), ("ga", 6144)):
        for i in range(2):
            u["%s%d" % (nm, i)] = [("w_in", 0, 8, c0 + i * 512, 512, 0, "g1")]
    for nm, src in (("co", "w_conv_out"), ("ao", "w_attn_out"), ("wo", "w_out")):
        for i in range(2):
            u["%s%d" % (nm, i)] = [(src, 0, 8, i * 512, 512, 0, None)]
    for i in range(11):
        u["gu%d" % i] = [("w_ffn_gate", 0, 8, (2 * i) * 128, 128, 0, "g2"),
                         ("w_ffn_gate", 0, 8, (2 * i + 1) * 128, 128, 128, "g2"),
                         ("w_ffn_up", 0, 8, (2 * i) * 128, 128, 256, "g2"),
                         ("w_ffn_up", 0, 8, (2 * i + 1) * 128, 128, 384, "g2")]
    for hf in range(2):
        for gi, (f0, nf) in enumerate(((0, 8), (8, 8), (16, 6))):
            u["dn%d_%d" % (hf, gi)] = [("w_ffn_down", f0 * 128, nf, hf * 512, 512, 0, None)]
    return u


UNITS = unit_table()
UNAMES = list(UNITS.keys())
UIDX = {n: i for i, n in enumerate(UNAMES)}
NU = len(UNAMES)
CHUNK_SEQ = (["cv%d" % i for i in range(4)] + ["q0", "gc0", "q1", "gc1", "ga0", "ga1", "ao0", "ao1", "co0", "co1",
                                               "wo0", "wo1"] + ["gu%d" % i for i in range(11)] +
             ["dn0_0", "dn0_1", "dn0_2", "dn1_0", "dn1_1", "dn1_2"])


def build_nc(nch=8, nkv=32):
    nc = bass.Bass("TRN2", target_bir_lowering=False)

    def din(name, shape):
        return nc.dram_tensor(name, list(shape), F32, kind="ExternalInput").ap()

    x_all = din("x_all", [S, D])
    x_own = din("x_own", [4096, D])
    x_halo = din("x_halo", [512, D])
    gate_bias = din("gate_bias", [16, 64])
    a2_d = din("a2", [16, 8, 64])
    b2_d = din("b2", [16, 64])
    cms_d = din("cmsel", [128, 4 * 2 * 256])
    lo_d = din("lo", [1, 8 * 512])
    kb_d = din("kbias", [128, 16])
    id_d = din("ident", [128, 128])
    wsrc = {
        "w_in": din("w_in", [D, 7168]), "w_conv_out": din("w_conv_out", [D, D]), "w_attn_out": din("w_attn_out", [D, D]),
        "w_out": din("w_out", [D, D]), "w_ffn_gate": din("w_ffn_gate", [D, FF]), "w_ffn_up": din("w_ffn_up", [D, FF]),
        "w_ffn_down": din("w_ffn_down", [FF, D]),
    }
    norm1_g = din("norm1_g", [1, D])
    norm2_g = din("norm2_g", [1, D])
    dw_w = din("dw_w", [31, D])
    dw_b = din("dw_b", [1, D])
    ln_g = din("conv_ln_g", [1, D])
    ln_b = din("conv_ln_b", [1, D])
    qg_d = din("q_norm_g", [1, DH])
    kg_d = din("k_norm_g", [1, DH])
    out_d = nc.dram_tensor("out_own", [4096, D], F32, kind="ExternalOutput").ap()
    wsc = nc.dram_tensor("wsc", [NU, 128, 4096], BF16).ap()
    kt_d = nc.dram_tensor("kt_s", [NH, 128, S], BF16).ap()
    vv_d = nc.dram_tensor("vv_s", [128, NBK, NH, 2, 129], BF16).ap()

    with ExitStack() as es:
        P = Prog(nc, es)
        NW = 53200
        arena_t = es.enter_context(nc.sbuf_tensor("arena", [128, NW], F32))
        AR = Arena(arena_t[:, :], NW)
        banks = [es.enter_context(nc.psum_tensor("pb%d" % i, [128, 512], F32)) for i in range(8)]
        tb = [T("pb%d" % i) for i in range(8)]
        rot = [0]

        held = set()

        def nbank():
            while rot[0] in held:
                rot[0] = (rot[0] + 1) % 8
            i = rot[0]
            rot[0] = (rot[0] + 1) % 8
            return i

        def bf_view(i):
            return banks[i][:, :].bitcast(BF16)

        identf = AR.alloc([128, 128], F32)
        identb = AR.alloc([128, 128], BF16)
        onesb = AR.alloc([128, 128], BF16)
        esel = AR.alloc([65, 64], BF16)
        cms = AR.alloc([128, 4, 2, 256], BF16)
        kb = AR.alloc([128, 16], F32)
        kmean = AR.alloc([128, 8, 64], F32)
        kmean_b = AR.alloc([128, 8, 64], BF16)
        VT = AR.alloc([65, 8, 512], BF16)
        g1 = AR.alloc([128, 8], F32)
        g2 = AR.alloc([128, 8], F32)
        cw = AR.alloc([128, 8, 31], F32)
        cb = AR.alloc([128, 8], F32)
        lg = AR.alloc([128, 8], F32)
        lb = AR.alloc([128, 8], F32)
        qg = AR.alloc([128, 1], F32)
        kg = AR.alloc([128, 1], F32)
        epsc = AR.alloc([128, 1], F32)
        t_const = T("const")
        t_kmean = T("kmean")
        t_vt64 = T("vt64")
        dq_c = P.dmaq("const")
        const_mark = AR.off

        cms_f = AR.alloc([128, 2048], F32)
        lo_f = AR.alloc([65, 4096], F32)
        t_cst = T("cst")
        pq = P.pool
        P.dma(pq, identf, id_d, [], [t_cst], dq_c)
        P.dma(pq, cms_f, cms_d, [], [t_cst], dq_c)
        P.dma(pq, lo_f[64:65, :], lo_d, [], [t_cst], dq_c)
        P.dma(pq, kb, kb_d, [], [t_cst], dq_c)
        P.dma(pq, g1, norm1_g[0].rearrange("(kc p) -> p kc", p=128), [], [t_cst], dq_c, allow_slow_non_contiguous=True)
        P.dma(pq, g2, norm2_g[0].rearrange("(kc p) -> p kc", p=128), [], [t_cst], dq_c, allow_slow_non_contiguous=True)
        P.dma(pq, cb, dw_b[0].rearrange("(kc p) -> p kc", p=128), [], [t_cst], dq_c, allow_slow_non_contiguous=True)
        P.dma(pq, lg, ln_g[0].rearrange("(kc p) -> p kc", p=128), [], [t_cst], dq_c, allow_slow_non_contiguous=True)
        P.dma(pq, lb, ln_b[0].rearrange("(kc p) -> p kc", p=128), [], [t_cst], dq_c, allow_slow_non_contiguous=True)
        for kc_ in range(8):
            P.dma(pq, cw[:, kc_, :], dw_w[:, kc_ * 128:(kc_ + 1) * 128].rearrange("j p -> p j"), [], [t_cst], dq_c,
                  allow_slow_non_contiguous=True)
        P.dma(pq, qg, qg_d.rearrange("o p -> p o"), [], [t_cst], dq_c, allow_slow_non_contiguous=True)
        P.dma(pq, kg, kg_d.rearrange("o p -> p o"), [], [t_cst], dq_c, allow_slow_non_contiguous=True)
        dv = P.dve
        P.emit(dv, lambda e: e.tensor_copy(out=identb, in_=identf), [t_cst], [t_const])
        P.emit(dv, lambda e: e.memset(onesb, 1.0), [], [t_const])
        P.emit(dv, lambda e: e.memset(kmean.rearrange("p a b -> p (a b)"), 0.0), [], [t_const])
        P.emit(dv, lambda e: e.memset(epsc, EPS), [], [t_const])
        P.emit(dv, lambda e: e.tensor_copy(out=esel[0:64, :], in_=identf[0:64, 0:64]), [t_cst], [t_const])
        P.emit(dv, lambda e: e.memset(esel[64:65, :], 1.0), [], [t_const])
        P.emit(dv, lambda e: e.tensor_copy(out=cms.rearrange("p a b c -> p (a b c)"), in_=cms_f), [t_cst], [t_const])
        P.emit(dv, lambda e: e.tensor_copy(out=VT[64:65, :, :].rearrange("p a b -> p (a b)"), in_=lo_f[64:65, :]),
               [t_cst], [t_const])
        P.emit(dv, lambda e: e.tensor_scalar(out=qg, in0=qg, scalar1=float(DH) ** -0.5, scalar2=None, op0=ALU.mult),
               [t_cst], [t_const])
        P.barrier([t_cst, t_const])
        AR.off = const_mark
        phase_mark = AR.off

        def esel_lhsT(n):
            a = esel[0:65, n:n + 1]
            return bass.AP(a.tensor, a.offset, [[a.ap[0][0], 65], [0, 128]])

        stg = [AR.alloc([128, 8, 512], F32) for _ in range(2)]
        wbf = [AR.alloc([128, 8, 512], BF16) for _ in range(2)]
        t_stg = [T("stg0"), T("stg1")]
        t_wbf = [T("wbf0"), T("wbf1")]
        dq_stg = [P.dmaq("stg0"), P.dmaq("stg1")]
        dq_wbf = [P.dmaq("wbf0"), P.dmaq("wbf1")]
        t_unit = [T("unit%d" % i) for i in range(NU)]
        gains = {"g1": g1, "g2": g2}
        kv_units = ["k0", "k1", "v0", "v1"]
        conv_order = kv_units + [n_ for n_ in UNAMES if n_ not in kv_units]

        def conv_load(oi):
            nm = conv_order[oi]
            b = oi % 2
            for (src, row0, nk_, c0, ncol, dst, _g) in UNITS[nm]:
                sap = wsrc[src][row0:row0 + nk_ * 128, c0:c0 + ncol].rearrange("(kc p) n -> p kc n", p=128)
                P.dma(P.sp, stg[b][:, 0:nk_, dst:dst + ncol], sap, [], [t_stg[b]], dq_stg[b])

        def conv_cast_store(oi, act_only):
            nm = conv_order[oi]
            ui = UIDX[nm]
            b = oi % 2
            segs = UNITS[nm]
            nk = segs[0][2]
            gname = segs[0][6]
            for kc in range(nk):
                use_dve = (kc % 2 == 0) and not act_only
                if gname is None:
                    if use_dve:
                        P.emit(P.dve, lambda e, b=b, kc=kc: e.tensor_copy(out=wbf[b][:, kc, :], in_=stg[b][:, kc, :]),
                               [t_stg[b]], [t_wbf[b]])
                    else:
                        P.emit(P.act, lambda e, b=b, kc=kc: e.copy(out=wbf[b][:, kc, :], in_=stg[b][:, kc, :]),
                               [t_stg[b]], [t_wbf[b]])
                else:
                    gt = gains[gname]
                    if use_dve:
                        P.emit(P.dve, lambda e, b=b, kc=kc, gt=gt: e.tensor_scalar(
                            out=wbf[b][:, kc, :], in0=stg[b][:, kc, :], scalar1=gt[:, kc:kc + 1], scalar2=None,
                            op0=ALU.mult), [t_stg[b]], [t_wbf[b]])
                    else:
                        P.emit(P.act, lambda e, b=b, kc=kc, gt=gt: e.activation(
                            out=wbf[b][:, kc, :], in_=stg[b][:, kc, :], func=AF.Copy, scale=gt[:, kc:kc + 1]),
                            [t_stg[b]], [t_wbf[b]])
            P.dma(P.sp, wsc[ui, :, 0:nk * 512], wbf[b][:, 0:nk, :].rearrange("p a b -> p (a b)"), [t_wbf[b]],
                  [t_unit[ui]], dq_wbf[b])

        conv_load(0)
        conv_load(1)
        for oi in range(4):
            conv_cast_store(oi, act_only=False)
            conv_load(oi + 2)
        conv_next = [4]

        wk = AR.alloc([128, 8, 1024], BF16)
        wv = AR.alloc([128, 8, 1024], BF16)
        t_wk = T("wk")
        t_wv = T("wv")
        dq_wk = P.dmaq("wk")
        dq_wv = P.dmaq("wv")
        for i in range(2):
            P.dma(P.pool, wk[:, :, i * 512:(i + 1) * 512], wsc[UIDX["k%d" % i]].rearrange("p (a b) -> p a b", b=512),
                  [t_unit[UIDX["k%d" % i]]], [t_wk], dq_wk)
            P.dma(P.pool, wv[:, :, i * 512:(i + 1) * 512], wsc[UIDX["v%d" % i]].rearrange("p (a b) -> p a b", b=512),
                  [t_unit[UIDX["v%d" % i]]], [t_wv], dq_wv)
        xin1 = [AR.alloc([128, 4, 1024], F32) for _ in range(2)]
        t_xin1 = [T("xin1_0"), T("xin1_1")]
        dq_xin1 = [P.dmaq("xin1_0"), P.dmaq("xin1_1")]
        junk_cur = [AR.alloc([128, 1024], BF16)]
        t_junk = T("junk")
        ssq = [AR.alloc([128, 4], F32) for _ in range(2)]
        t_ssq = [T("ssq0"), T("ssq1")]
        rinv = [AR.alloc([128, 4], F32) for _ in range(2)]
        t_rinv = [T("rinv0"), T("rinv1")]
        xn = [AR.alloc([128, 1024], BF16) for _ in range(2)]
        t_xn = [T("xn0"), T("xn1")]
        xnT1 = [AR.alloc([128, 8, 512], BF16) for _ in range(2)]
        t_xnT1 = [T("xnT1_0"), T("xnT1_1")]
        sqb = [AR.alloc([128, 512], BF16) for _ in range(2)]
        t_sqb = [T("sqb0"), T("sqb1")]
        rkf = [AR.alloc([128, 512], F32) for _ in range(2)]
        t_rkf = [T("rkf0"), T("rkf1")]
        kst = [AR.alloc([128, 8, 512], BF16) for _ in range(2)]
        t_kst = [T("kst0"), T("kst1")]
        dq_kst = [P.dmaq("kst0"), P.dmaq("kst1")]
        vst = [AR.alloc([128, 2, 8, 2, 129], BF16) for _ in range(2)]
        t_vst = [T("vst0"), T("vst1")]
        dq_vst = [P.dmaq("vst0"), P.dmaq("vst1")]
        t_kt = T("kt_dram")
        t_vv = T("vv_dram")
        for b in range(2):
            P.emit(P.dve, lambda e, b=b: e.memset(vst[b].rearrange("p a b c d -> p (a b c d)"), 1.0), [], [t_vst[b]])

        def norm_tile(src, np_, t_src, ssq_ap, rinv_ap, t_s, t_r, xn_ap, t_x):
            jv = junk_cur[0][0:np_, :]
            P.emit(P.dve, lambda e: e.memset(ssq_ap, 0.0), [], [t_s])
            P.emit(P.act, lambda e: e.activation(out=jv, in_=src, func=AF.Square, accum_out=ssq_ap),
                   [t_src], [t_junk, t_s])
            P.emit(P.act, lambda e: e.activation(out=rinv_ap, in_=ssq_ap, func=AF.Ln, scale=1.0 / D,
                                                 bias=epsc[0:np_, :]), [t_s], [t_r])
            P.emit(P.act, lambda e: e.activation(out=rinv_ap, in_=rinv_ap, func=AF.Exp, scale=-0.5), [t_r], [t_r])
            P.emit(P.dve, lambda e: e.tensor_scalar(out=xn_ap, in0=src, scalar1=rinv_ap, scalar2=None, op0=ALU.mult),
                   [t_src, t_r], [t_x])

        def transpose_tile(xn_ap, np_, t_x, dst, t_dst, use_act):
            bi = nbank()
            pv = bf_view(bi)[:, 0:8 * np_].rearrange("p (a b) -> p a b", b=np_)
            for kc in range(8):
                P.emit(P.pe, lambda e, kc=kc: e.transpose(out=pv[:, kc, :], in_=xn_ap[:, kc * 128:(kc + 1) * 128],
                                                          identity=identb[0:np_, 0:np_]),
                       [t_x], [tb[bi]], ms=(kc == 7))
            if use_act:
                P.emit(P.act, lambda e: e.copy(out=dst, in_=pv), [tb[bi]], [t_dst])
            else:
                P.emit(P.dve, lambda e: e.tensor_copy(out=dst, in_=pv), [tb[bi]], [t_dst])

        def x1_load(c):
            b = c % 2
            P.dma(P.pool, xin1[b], x_all[c * 512:(c + 1) * 512, :].rearrange("(t p) d -> p t d", p=128), [],
                  [t_xin1[b]], dq_xin1[b])

        xn4 = [AR.alloc([128, 1024], BF16) for _ in range(4)]
        t_xn4 = [T("xn4_%d" % i) for i in range(4)]

        def p1_norm(c):
            b = c % 2
            for t in range(4):
                norm_tile(xin1[b][:, t, :], 128, t_xin1[b], ssq[b][:, t:t + 1], rinv[b][:, t:t + 1], t_ssq[b], t_rinv[b],
                          xn4[t], t_xn4[t])

        def p1_transpose(c):
            b = c % 2
            for t in range(4):
                transpose_tile(xn4[t], 128, t_xn4[t], xnT1[b][:, :, t * 128:(t + 1) * 128], t_xnT1[b],
                               use_act=(t % 2 == 0))

        def p1_norm_transpose(c):
            p1_norm(c)
            p1_transpose(c)

        x1_load(0)
        if nkv > 1:
            x1_load(1)
        p1_norm_transpose(0)
        for c in range(nkv):
            b = c % 2
            if conv_next[0] < len(conv_order):
                oi = conv_next[0]
                conv_cast_store(oi, act_only=True)
                if oi + 2 < len(conv_order):
                    conv_load(oi + 2)
                conv_next[0] += 1
            if c + 1 < nkv:
                p1_norm(c + 1)
            pend = None

            def k_tail(h, bi, pk, b=b, c=c):
                sb = h % 2
                b2i = nbank()
                p2 = banks[b2i][:, :]
                P.emit(P.pe, lambda e, p2=p2, sb=sb: e.matmul(out=p2, lhsT=onesb, rhs=sqb[sb], start=True, stop=True),
                       [t_sqb[sb]], [tb[b2i]])
                P.emit(P.act, lambda e, p2=p2, sb=sb: e.activation(out=rkf[sb], in_=p2, func=AF.Ln, scale=1.0 / DH,
                                                                   bias=epsc), [tb[b2i]], [t_rkf[sb]])
                P.emit(P.act, lambda e, sb=sb: e.activation(out=rkf[sb], in_=rkf[sb], func=AF.Exp, scale=-0.5),
                       [t_rkf[sb]], [t_rkf[sb]])
                P.emit(P.dve, lambda e, pk=pk, sb=sb, h=h, b=b: e.scalar_tensor_tensor(
                    out=kst[b][:, h, :], in0=pk, scalar=kg[:, 0:1], in1=rkf[sb], op0=ALU.mult, op1=ALU.mult),
                    [tb[bi], t_rkf[sb]], [t_kst[b]])
                P.emit(P.dve, lambda e, h=h, b=b, c=c: e.tensor_reduce(
                    out=kmean[:, h, 2 * c:2 * c + 2], in_=kst[b][:, h, :].rearrange("p (a l) -> p a l", l=256),
                    axis=AX.X, op=ALU.add), [t_kst[b]], [t_kmean])

            for h in range(NH):
                bi = nbank()
                pk = banks[bi][:, :]
                for kc in range(8):
                    P.emit(P.pe, lambda e, kc=kc, h=h, pk=pk, b=b: e.matmul(out=pk, lhsT=wk[:, kc, h * 128:(h + 1) * 128],
                                                                           rhs=xnT1[b][:, kc, :], start=(kc == 0),
                                                                           stop=(kc == 7)),
                           [t_wk, t_xnT1[b]], [tb[bi]], ms=(kc == 7))
                sb = h % 2
                P.emit(P.act, lambda e, pk=pk, sb=sb: e.activation(out=sqb[sb], in_=pk, func=AF.Square),
                       [tb[bi]], [t_sqb[sb]])
                held.add(bi)
                if pend is not None:
                    k_tail(*pend)
                    held.discard(pend[1])
                pend = (h, bi, pk)
            k_tail(*pend)
            held.discard(pend[1])
            P.dma(P.pool, kt_d[:, :, c * 512:(c + 1) * 512].rearrange("h p t -> p h t"), kst[b], [t_kst[b]], [t_kt],
                  dq_kst[b])
            if c + 2 < nkv:
                x1_load(c + 2)
            for blk in range(2):
                for kh in range(2):
                    for hf in range(2):
                        bi = nbank()
                        pv_ = banks[bi][:, :]
                        for kc in range(8):
                            P.emit(P.pe, lambda e, kc=kc, blk=blk, kh=kh, hf=hf, pv_=pv_, b=b: e.matmul(
                                out=pv_, lhsT=xnT1[b][:, kc, blk * 256 + kh:blk * 256 + 256:2],
                                rhs=wv[:, kc, hf * 512:(hf + 1) * 512], start=(kc == 0), stop=(kc == 7)),
                                [t_wv, t_xnT1[b]], [tb[bi]], ms=(kc == 7))
                        dst = vst[b][:, blk, hf * 4:(hf + 1) * 4, kh, 0:128]
                        src = pv_.rearrange("p (a d) -> p a d", d=128)
                        if hf == 0:
                            P.emit(P.act, lambda e, dst=dst, src=src: e.copy(out=dst, in_=src), [tb[bi]], [t_vst[b]])
                        else:
                            P.emit(P.dve, lambda e, dst=dst, src=src: e.tensor_copy(out=dst, in_=src), [tb[bi]],
                                   [t_vst[b]])
                if blk == 0 and c + 1 < nkv:
                    p1_transpose(c + 1)
            P.dma(P.pool, vv_d[:, 2 * c:2 * c + 2].rearrange("p a b c d -> p (a b c d)"),
                  vst[b].rearrange("p a b c d -> p (a b c d)"), [t_vst[b]], [t_vv], dq_vst[b])
        while conv_next[0] < len(conv_order):
            oi = conv_next[0]
            conv_cast_store(oi, act_only=False)
            if oi + 2 < len(conv_order):
                conv_load(oi + 2)
            conv_next[0] += 1
        P.emit(P.dve, lambda e: e.tensor_copy(out=kmean_b.rearrange("p a b -> p (a b)"),
                                              in_=kmean.rearrange("p a b -> p (a b)")), [t_kmean], [t_kmean])
        ph1 = t_xn4 + t_xin1 + t_ssq + t_rinv + t_xn + t_sqb + t_rkf + t_kst + t_vst + t_xnT1 + [t_junk, t_wk, t_wv, t_kt, t_vv,
                                                                                         t_kmean] + tb + t_stg + t_wbf
        P.barrier(ph1)
        AR.off = phase_mark

        NSLOT = 3
        wr = [AR.alloc([128, 8, 512], BF16) for _ in range(NSLOT)]
        t_wr = [T("wr%d" % i) for i in range(NSLOT)]
        dq_wr = [P.dmaq("wr%d" % i) for i in range(NSLOT)]
        seq_all = []
        for g in range(nch):
            seq_all += CHUNK_SEQ
        wstate = {"issued": 0, "used": 0}

        def w_issue():
            i = wstate["issued"]
            if i >= len(seq_all):
                return
            nm = seq_all[i]
            s_ = i % NSLOT
            ui = UIDX[nm]
            nk = UNITS[nm][0][2]
            P.dma(P.sp, wr[s_][:, 0:nk, :].rearrange("p a b -> p (a b)"), wsc[ui, :, 0:nk * 512], [t_unit[ui]],
                  [t_wr[s_]], dq_wr[s_])
            wstate["issued"] += 1

        def w_next(expect, ahead=NSLOT - 1):
            i = wstate["used"]
            assert seq_all[i] == expect, (seq_all[i], expect)
            while wstate["issued"] < min(i + ahead + 1, len(seq_all)):
                w_issue()
            wstate["used"] += 1
            s_ = i % NSLOT
            return wr[s_], t_wr[s_]

        xin = AR.alloc([128, 4, 1024], F32)
        t_xin = T("xin")
        dq_xin = P.dmaq("xin")
        xh = AR.alloc([32, 2, 1024], F32)
        t_xh = T("xh")
        dq_xh = P.dmaq("xh")
        a2t = AR.alloc([128, 2, 8, 64], F32)
        b2t = AR.alloc([128, 2, 64], F32)
        gbt = AR.alloc([128, 2, 64], F32)
        t_tab = T("tab")
        dq_tab = P.dmaq("tab")
        junk_cur[0] = AR.alloc([128, 1024], BF16)
        ssq2 = AR.alloc([128, 8], F32)
        rinv2 = AR.alloc([128, 8], F32)
        t_ssq2 = [T("ssq2_%d" % i) for i in range(6)]
        t_rinv2 = [T("rinv2_%d" % i) for i in range(6)]
        xn = [AR.alloc([128, 1024], BF16) for _ in range(2)]
        xnT_raw = AR.alloc([128, 8 * 576], BF16)
        xnT = xnT_raw.rearrange("p (a b) -> p a b", b=576)
        t_xnT = T("xnT")
        R1 = AR.alloc([128, 8, 576 + 512], F32)
        a_t = R1[:, :, 0:576]
        y_t = R1[:, :, 576:1088]
        act_t = R1.rearrange("p a b -> p (a b)").bitcast(BF16)[:, 0:FC * 512].rearrange("p (a b) -> p a b", b=512)
        t_a = T("a")
        t_y = T("y")
        t_actt = T("act")
        ybf = [AR.alloc([128, 512], BF16) for _ in range(2)]
        t_ybf = [T("ybf0"), T("ybf1")]
        ysq = [AR.alloc([128, 512], BF16) for _ in range(2)]
        t_ysq = [T("ysq0"), T("ysq1")]
        mean_t = AR.alloc([128, 512], F32)
        rstd_t = AR.alloc([128, 512], F32)
        t_mean = T("mean")
        t_rstd = T("rstd")
        xc = [AR.alloc([128, 512], F32) for _ in range(2)]
        t_xc = [T("xc0"), T("xc1")]
        sig_t = [x_[:, 0:288].rearrange("p (a b) -> p a b", a=1) for x_ in xc]
        t_sig = t_xc
        ysl = AR.alloc([128, 8, 512], BF16)
        t_ysl = T("ysl")
        mT = ysl
        t_mT = t_ysl
        t1 = AR.alloc([128, 8, 512], BF16)
        t_t1 = T("t1")
        qT = AR.alloc([128, 8, 512], BF16)
        t_qT = T("qT")
        qf = [AR.alloc([128, 512], F32) for _ in range(2)]
        t_qf = [T("qf0"), T("qf1")]
        off_g = AR.off
        gcs = AR.alloc([128, 8, 512], BF16)
        gas = AR.alloc([128, 8, 512], BF16)
        assert AR.off - off_g == 4096
        outbuf = AR.ap[:, off_g:off_g + 4096].rearrange("p (t d) -> p t d", d=1024)
        t_gcs = T("gcs")
        t_gas = T("gas")
        attT = AR.alloc([128, 8, 512], BF16)
        t_attT = T("attT")
        hnT = xnT_raw[:, 0:8 * 512].rearrange("p (a b) -> p a b", b=512)
        t_hnT = t_xnT
        NKV = 2
        ktp = [AR.alloc([128, 2048], BF16) for _ in range(NKV)]
        vp = [AR.alloc([128, 8, 2, 129], BF16) for _ in range(NKV)]
        t_ktp = [T("ktp%d" % i) for i in range(NKV)]
        t_vp = [T("vp%d" % i) for i in range(NKV)]
        dq_ktp = [P.dmaq("ktp%d" % i) for i in range(NKV)]
        dq_vp = [P.dmaq("vp%d" % i) for i in range(NKV)]
        NPT = 4
        PT = [AR.alloc([128, 512], BF16) for _ in range(NPT)]
        t_PT = [T("PT%d" % i) for i in range(NPT)]
        gs_t = [AR.alloc([128, 64], F32) for _ in range(4)]
        m8_t = [AR.alloc([128, 8], F32) for _ in range(4)]
        tmp_t = [AR.alloc([128, 64], F32) for _ in range(4)]
        hiv_t = [AR.alloc([128, 64], BF16) for _ in range(4)]
        t_gs = [T("gs%d" % i) for i in range(4)]
        t_m8 = [T("m8%d" % i) for i in range(4)]
        t_tmp = [T("tmp%d" % i) for i in range(4)]
        t_hiv = [T("hiv%d" % i) for i in range(4)]
        rl_t = [AR.alloc([128, 1], F32) for _ in range(2)]
        t_rl = [T("rl0"), T("rl1")]
        atok = [AR.alloc([128, 128], BF16) for _ in range(2)]
        t_atok = [T("atok0"), T("atok1")]
        sg_t = xc
        t_sg = t_xc
        tm2 = xc
        t_tm2 = t_xc
        dq_out = P.dmaq("out")
        t_outd = T("out_dram")
        kvstate = {"n": 0}
        ptstate = {"n": 0}

        def mm_group(out_ap, bank_i, pairs, extra_reads, first=True, last=True):
            n_ = len(pairs)
            for i_, (l_, r_) in enumerate(pairs):
                P.emit(P.pe, lambda e, l_=l_, r_=r_, i_=i_: e.matmul(out=out_ap, lhsT=l_, rhs=r_,
                                                                     start=(first and i_ == 0),
                                                                     stop=(last and i_ == n_ - 1)),
                       extra_reads, [tb[bank_i]], ms=(i_ == n_ - 1))

        for g in range(nch):
            P.dma(P.pool, xin, x_own[g * 512:(g + 1) * 512, :].rearrange("(t p) d -> p t d", p=128), [], [t_xin], dq_xin)
            P.dma(P.pool, xh, x_halo[g * 64:(g + 1) * 64, :].rearrange("(o p) d -> p o d", p=32), [], [t_xh], dq_xh)
            P.dma(P.pool, a2t, a2_d[2 * g:2 * g + 2].partition_broadcast(128), [], [t_tab], dq_tab)
            P.dma(P.pool, b2t, b2_d[2 * g:2 * g + 2].partition_broadcast(128), [], [t_tab], dq_tab)
            P.dma(P.pool, gbt, gate_bias[2 * g:2 * g + 2].partition_broadcast(128), [], [t_tab], dq_tab)
            nbuf = [(xn[0], t_xn[0]), (xn[1], t_xn[1]), (qf[0].bitcast(BF16), t_qf[0]), (qf[1].bitcast(BF16), t_qf[1])]
            for o in range(2):
                nb_ap, nb_t = nbuf[o]
                norm_tile(xh[0:32, o, :], 32, t_xh, ssq2[0:32, 4 + o:5 + o], rinv2[0:32, 4 + o:5 + o], t_ssq2[4 + o],
                          t_rinv2[4 + o], nb_ap[0:32, :], nb_t)
            for t in range(4):
                nb_ap, nb_t = nbuf[(t + 2) % 4]
                norm_tile(xin[:, t, :], 128, t_xin, ssq2[:, t:t + 1], rinv2[:, t:t + 1], t_ssq2[t], t_rinv2[t], nb_ap, nb_t)
                if t == 1:
                    for o in range(2):
                        nb_ap2, nb_t2 = nbuf[o]
                        transpose_tile(nb_ap2[0:32, :], 32, nb_t2, xnT[:, :, o * 288:o * 288 + 32], t_xnT,
                                       use_act=(o == 0))
            for t in range(4):
                nb_ap, nb_t = nbuf[(t + 2) % 4]
                c0 = (t // 2) * 288 + 32 + (t % 2) * 128
                transpose_tile(nb_ap, 128, nb_t, xnT[:, :, c0:c0 + 128], t_xnT, use_act=(t % 2 == 0))
            for u in range(4):
                w_, tw = w_next("cv%d" % u)
                for ci in range(2):
                    cc = 2 * u + ci
                    bu = nbank()
                    bg = nbank()
                    bu2 = nbank()
                    bg2 = nbank()
                    for ob, (bcu, bcg) in enumerate(((bu, bg), (bu2, bg2))):
                        mm_group(banks[bcu][:, 0:288],
                                 bcu, [(w_[:, kc, ci * 128:(ci + 1) * 128], xnT[:, kc, ob * 288:(ob + 1) * 288])
                                       for kc in range(8)], [tw, t_xnT])
                        mm_group(banks[bcg][:, 0:288],
                                 bcg, [(w_[:, kc, 256 + ci * 128:256 + (ci + 1) * 128], xnT[:, kc, ob * 288:(ob + 1) * 288])
                                       for kc in range(8)], [tw, t_xnT])
                        sb = ob
                        P.emit(P.act, lambda e, bcg=bcg, sb=sb, ob=ob: e.activation(out=sig_t[sb][:, 0, :],
                                                                                   in_=banks[bcg][:, 0:288],
                                                                                   func=AF.Sigmoid),
                               [tb[bcg]], [t_sig[sb]])
                        P.emit(P.dve, lambda e, bcu=bcu, sb=sb, ob=ob, cc=cc: e.tensor_tensor(
                            out=a_t[:, cc, ob * 288:(ob + 1) * 288], in0=banks[bcu][:, 0:288], in1=sig_t[sb][:, 0, :],
                            op=ALU.mult), [tb[bcu], t_sig[sb]], [t_a])
            def q_main(h, w_, tw, hi_):
                bi = nbank()
                pq_ = banks[bi][:, :]
                for ob in range(2):
                    mm_group(pq_[:, ob * 256:(ob + 1) * 256], bi,
                             [(w_[:, kc, hi_ * 128:(hi_ + 1) * 128], xnT[:, kc, ob * 288 + 32:ob * 288 + 288])
                              for kc in range(8)], [tw, t_xnT])
                sb = h % 2
                P.emit(P.act, lambda e, pq_=pq_, sb=sb: e.activation(out=ysq[sb], in_=pq_, func=AF.Square),
                       [tb[bi]], [t_ysq[sb]])
                held.add(bi)
                return (h, bi, pq_)

            def q_stage_a(stt_):
                h, bi, pq_ = stt_
                sb = h % 2
                b2i = nbank()
                p2 = banks[b2i][:, :]
                mm_group(p2, b2i, [(onesb, ysq[sb])], [t_ysq[sb]])
                P.emit(P.act, lambda e, p2=p2, sb=sb: e.activation(out=xc[sb], in_=p2, func=AF.Ln, scale=1.0 / DH,
                                                                   bias=epsc), [tb[b2i]], [t_xc[sb]])
                P.emit(P.act, lambda e, sb=sb: e.activation(out=xc[sb], in_=xc[sb], func=AF.Exp, scale=-0.5),
                       [t_xc[sb]], [t_xc[sb]])
                P.emit(P.dve, lambda e, pq_=pq_, sb=sb: e.scalar_tensor_tensor(
                    out=qf[sb], in0=pq_, scalar=qg[:, 0:1], in1=xc[sb], op0=ALU.mult, op1=ALU.mult),
                    [tb[bi], t_xc[sb]], [t_qf[sb]])
                P.emit(P.act, lambda e, sb=sb, h=h: e.copy(out=qT[:, h, :], in_=qf[sb]), [t_qf[sb]], [t_qT])
                held.discard(bi)
                return h

            def q_stage_b(h):
                sb = h % 2
                for t in range(4):
                    ob = t // 2
                    bgi = nbank()
                    pg = banks[bgi][:, 0:64]
                    mm_group(pg, bgi, [(qT[:, h, t * 128:(t + 1) * 128], kmean_b[:, h, :])], [t_qT, t_kmean])
                    P.emit(P.dve, lambda e, pg=pg, t=t, ob=ob: e.tensor_tensor(out=gs_t[t], in0=pg,
                                                                              in1=gbt[:, ob, :], op=ALU.add),
                           [tb[bgi], t_tab], [t_gs[t]])
                    P.emit(P.dve, lambda e, t=t: e.max(out=m8_t[t], in_=gs_t[t]), [t_gs[t]], [t_m8[t]])
                    P.emit(P.dve, lambda e, t=t, ob=ob, h=h: e.scalar_tensor_tensor(
                        out=tmp_t[t], in0=gs_t[t], scalar=m8_t[t][:, 2:3], in1=a2t[:, ob, h, :], op0=ALU.is_ge,
                        op1=ALU.mult), [t_gs[t], t_m8[t], t_tab], [t_tmp[t]])
                    P.emit(P.dve, lambda e, t=t, ob=ob: e.tensor_tensor(out=hiv_t[t], in0=tmp_t[t],
                                                                       in1=b2t[:, ob, :], op=ALU.add),
                           [t_tmp[t], t_tab], [t_hiv[t]])
                return h

            def q_stage_c(h):
                for t in range(4):
                    bti = nbank()
                    ptv = bf_view(bti)[0:64, 0:128]
                    P.emit(P.pe, lambda e, ptv=ptv, t=t: e.transpose(out=ptv, in_=hiv_t[t], identity=identb),
                           [t_hiv[t]], [tb[bti]])
                    P.emit(P.act, lambda e, ptv=ptv, h=h, t=t: e.copy(out=VT[0:64, h, t * 128:(t + 1) * 128], in_=ptv),
                           [tb[bti]], [t_vt64])

            def gate_mm(w_, tw, ci, cc, dst, tdst):
                bi = nbank()
                for ob in range(2):
                    mm_group(banks[bi][:, ob * 256:(ob + 1) * 256], bi,
                             [(w_[:, kc, ci * 128:(ci + 1) * 128], xnT[:, kc, ob * 288 + 32:ob * 288 + 288])
                              for kc in range(8)], [tw, t_xnT])
                P.emit(P.act, lambda e, bi=bi, dst=dst, cc=cc: e.activation(out=dst[:, cc, :], in_=banks[bi][:, :],
                                                                           func=AF.Sigmoid), [tb[bi]], [tdst])

            pa = pb = pc = None
            for u in range(2):
                wq, twq = w_next("q%d" % u, 2)
                wg, twg = w_next("gc%d" % u, 1)
                for hi_ in range(4):
                    h = 4 * u + hi_
                    cur = q_main(h, wq, twq, hi_)
                    na = q_stage_a(pa) if pa is not None else None
                    gate_mm(wg, twg, hi_, h, gcs, t_gcs)
                    if pc is not None:
                        q_stage_c(pc)
                    nb_ = q_stage_b(pb) if pb is not None else None
                    pa, pb, pc = cur, na, nb_
            for u in range(2):
                wg, twg = w_next("ga%d" % u, 2)
                for ci in range(4):
                    gate_mm(wg, twg, ci, 4 * u + ci, gas, t_gas)
                    na = q_stage_a(pa) if pa is not None else None
                    if pc is not None:
                        q_stage_c(pc)
                    nb_ = q_stage_b(pb) if pb is not None else None
                    pa, pb, pc = None, na, nb_
            assert pa is None and pb is None and pc is None
            def conv_cc(cc):
                yv = y_t[:, cc, :].rearrange("p (o l) -> p o l", l=256)

                def av(j, cc=cc):
                    return a_t[:, cc, :].rearrange("p (o l) -> p o l", l=288)[:, :, 2 + j:2 + j + 256]
                P.emit(P.dve, lambda e, yv=yv, av=av, cc=cc: e.tensor_scalar(out=yv, in0=av(0), scalar1=cw[:, cc, 0:1],
                                                                            scalar2=cb[:, cc:cc + 1], op0=ALU.mult,
                                                                            op1=ALU.add), [t_a], [t_y])
                for j in range(1, 31):
                    P.emit(P.dve, lambda e, yv=yv, av=av, cc=cc, j=j: e.scalar_tensor_tensor(
                        out=yv, in0=av(j), scalar=cw[:, cc, j:j + 1], in1=yv, op0=ALU.mult, op1=ALU.add), [t_a, t_y], [t_y])

            def normalize_head(h):
                for t in range(4):
                    ob_i = 4 + t
                    o_ap = banks[ob_i][:, 0:129]
                    mb = t % 2
                    P.emit(P.dve, lambda e, o_ap=o_ap, mb=mb: e.reciprocal(out=rl_t[mb], in_=o_ap[:, 128:129]),
                           [tb[ob_i]], [t_rl[mb]])
                    P.emit(P.dve, lambda e, o_ap=o_ap, mb=mb: e.tensor_scalar(out=atok[mb], in0=o_ap[:, 0:128],
                                                                              scalar1=rl_t[mb][:, 0:1], scalar2=None,
                                                                              op0=ALU.mult),
                           [tb[ob_i], t_rl[mb]], [t_atok[mb]])
                    bti = (srot[0] - 3) % 4 if t % 2 == 0 else srot[0] % 4
                    ptv = bf_view(bti)[:, 0:128]
                    P.emit(P.pe, lambda e, ptv=ptv, mb=mb: e.transpose(out=ptv, in_=atok[mb], identity=identb),
                           [t_atok[mb]], [tb[bti]])
                    P.emit(P.act, lambda e, ptv=ptv, h=h, t=t: e.copy(out=attT[:, h, t * 128:(t + 1) * 128], in_=ptv),
                           [tb[bti]], [t_attT])

            nblk = 8 * g + 8
            npp = nblk // 8
            units = [(h, n, kh) for h in range(NH) for n in range(nblk) for kh in range(2)]
            piece_list = [(h, n0) for h in range(NH) for n0 in range(0, nblk, 8)]
            piece_slot = {}
            piece_issued = [0]

            def ensure_piece(k):
                while piece_issued[0] <= k and piece_issued[0] < len(piece_list):
                    h_, n0_ = piece_list[piece_issued[0]]
                    s_ = kvstate["n"] % NKV
                    kvstate["n"] += 1
                    P.dma(P.pool, ktp[s_], kt_d[h_, :, n0_ * 256:(n0_ + 8) * 256], [t_kt], [t_ktp[s_]], dq_ktp[s_])
                    P.dma(P.pool, vp[s_], vv_d[:, n0_:n0_ + 8, h_, :, :], [t_vv], [t_vp[s_]], dq_vp[s_])
                    piece_slot[piece_issued[0]] = s_
                    piece_issued[0] += 1

            LA = 2
            srot = [0]
            ensure_piece(1)
            conv_cc(0)
            st = {}
            for idx in range(len(units) + LA):
                if idx < len(units):
                    h, n, kh = units[idx]
                    s_ = piece_slot[h * npp + n // 8]
                    nl = n % 8
                    bi = srot[0]
                    srot[0] = (srot[0] + 1) % 4
                    S_ = banks[bi][:, :]
                    cand = n >= 8 * g
                    c0 = 256 if n >= 8 * g + 4 else 0
                    P.emit(P.pe, lambda e, S_=S_, s_=s_, nl=nl, kh=kh, h=h, c0=c0: e.matmul(
                        out=S_[:, c0:512], lhsT=ktp[s_][:, nl * 256 + kh:nl * 256 + 256:2], rhs=qT[:, h, c0:512],
                        start=True, stop=False), [t_ktp[s_], t_qT], [tb[bi]], ms=False)
                    P.emit(P.pe, lambda e, S_=S_, n=n, h=h, cand=cand, c0=c0: e.matmul(
                        out=S_[:, c0:512], lhsT=esel_lhsT(n), rhs=VT[0:65, h, c0:512], start=False, stop=(not cand)),
                        [t_vt64], [tb[bi]], ms=(not cand))
                    if cand:
                        ob = (n - 8 * g) // 4
                        cnd = (n - 8 * g) % 4
                        P.emit(P.pe, lambda e, S_=S_, ob=ob, cnd=cnd, kh=kh: e.matmul(
                            out=S_[:, ob * 256:(ob + 1) * 256], lhsT=identb, rhs=cms[:, cnd, kh, :], start=False,
                            stop=True), [], [tb[bi]], ms=True)
                    st[idx] = (bi, s_)
                j = idx - LA
                if j >= 0:
                    h, n, kh = units[j]
                    bi, s_ = st.pop(j)
                    S_ = banks[bi][:, :]
                    nl = n % 8
                    if n % 8 == 0 and kh == 0:
                        ensure_piece(h * npp + n // 8 + 1)
                    first = (n == 0 and kh == 0)
                    lastblk = (n == nblk - 1 and kh == 1)
                    r_ = ptstate["n"] % NPT
                    ptstate["n"] += 1
                    c0 = 256 if n >= 8 * g + 4 else 0
                    lastfull = (n == 8 * g + 3 and kh == 1)
                    P.emit(P.act, lambda e, S_=S_, r_=r_, h=h, kh=kh, c0=c0: e.activation(
                        out=PT[r_][:, c0:512], in_=S_[:, c0:512], func=AF.Exp, bias=kb[:, 2 * h + kh:2 * h + kh + 1]),
                        [tb[bi]], [t_PT[r_]])
                    for t in range(c0 // 128, 4):
                        ob_i = 4 + t
                        o_ap = banks[ob_i][:, 0:129]
                        stp = lastblk if t >= 2 else lastfull
                        P.emit(P.pe, lambda e, o_ap=o_ap, r_=r_, t=t, s_=s_, nl=nl, kh=kh, first=first,
                               stp=stp: e.matmul(out=o_ap, lhsT=PT[r_][:, t * 128:(t + 1) * 128],
                                                 rhs=vp[s_][:, nl, kh, :], start=first, stop=stp),
                               [t_PT[r_], t_vp[s_]], [tb[ob_i]], ms=(t == 3))
                    if lastblk:
                        normalize_head(h)
                        if h + 1 < NH:
                            conv_cc(h + 1)
            for u in range(2):
                w_, tw = w_next("ao%d" % u)
                for ci in range(4):
                    cc = 4 * u + ci
                    bi = nbank()
                    mm_group(banks[bi][:, :], bi, [(w_[:, kc, ci * 128:(ci + 1) * 128], attT[:, kc, :]) for kc in range(8)],
                             [tw, t_attT])
                    P.emit(P.dve, lambda e, bi=bi, cc=cc: e.tensor_tensor(out=t1[:, cc, :], in0=banks[bi][:, :],
                                                                          in1=gas[:, cc, :], op=ALU.mult),
                           [tb[bi], t_gas], [t_t1])
            bs1 = nbank()
            bs2 = nbank()
            for cc in range(8):
                sb = cc % 2
                P.emit(P.act, lambda e, sb=sb, cc=cc: e.copy(out=ybf[sb], in_=y_t[:, cc, :]), [t_y], [t_ybf[sb]])
                P.emit(P.act, lambda e, sb=sb, cc=cc: e.activation(out=ysq[sb], in_=y_t[:, cc, :], func=AF.Square),
                       [t_y], [t_ysq[sb]])
                P.emit(P.pe, lambda e, sb=sb, cc=cc, bs1=bs1: e.matmul(out=banks[bs1][:, :], lhsT=onesb, rhs=ybf[sb],
                                                              start=(cc == 0), stop=(cc == 7)),
                       [t_ybf[sb]], [tb[bs1]], ms=True)
                P.emit(P.pe, lambda e, sb=sb, cc=cc, bs2=bs2: e.matmul(out=banks[bs2][:, :], lhsT=onesb, rhs=ysq[sb],
                                                              start=(cc == 0), stop=(cc == 7)),
                       [t_ysq[sb]], [tb[bs2]], ms=True)
            P.emit(P.dve, lambda e, bs1=bs1: e.tensor_scalar(out=mean_t, in0=banks[bs1][:, :], scalar1=1.0 / D, scalar2=None,
                                                    op0=ALU.mult), [tb[bs1]], [t_mean])
            P.emit(P.dve, lambda e: e.tensor_tensor(out=rstd_t, in0=mean_t, in1=mean_t, op=ALU.mult), [t_mean], [t_rstd])
            P.emit(P.dve, lambda e, bs2=bs2: e.scalar_tensor_tensor(out=rstd_t, in0=banks[bs2][:, :], scalar=1.0 / D, in1=rstd_t,
                                                           op0=ALU.mult, op1=ALU.subtract), [tb[bs2], t_rstd], [t_rstd])
            P.emit(P.act, lambda e: e.activation(out=rstd_t, in_=rstd_t, func=AF.Ln, bias=epsc), [t_rstd], [t_rstd])
            P.emit(P.act, lambda e: e.activation(out=rstd_t, in_=rstd_t, func=AF.Exp, scale=-0.5), [t_rstd], [t_rstd])
            for cc in range(8):
                sb = cc % 2
                P.emit(P.dve, lambda e, sb=sb, cc=cc: e.tensor_tensor(out=xc[sb], in0=y_t[:, cc, :], in1=mean_t,
                                                                      op=ALU.subtract), [t_y, t_mean], [t_xc[sb]])
                P.emit(P.dve, lambda e, sb=sb: e.tensor_tensor(out=xc[sb], in0=xc[sb], in1=rstd_t, op=ALU.mult),
                       [t_xc[sb], t_rstd], [t_xc[sb]])
                P.emit(P.act, lambda e, sb=sb, cc=cc: e.activation(out=ysl[:, cc, :], in_=xc[sb], func=AF.Silu,
                                                                   scale=lg[:, cc:cc + 1], bias=lb[:, cc:cc + 1]),
                       [t_xc[sb]], [t_ysl])
            for u in range(2):
                w_, tw = w_next("co%d" % u)
                for ci in range(4):
                    cc = 4 * u + ci
                    bi = nbank()
                    sb = cc % 2
                    mm_group(banks[bi][:, :], bi, [(w_[:, kc, ci * 128:(ci + 1) * 128], ysl[:, kc, :]) for kc in range(8)],
                             [tw, t_ysl])
                    P.emit(P.dve, lambda e, bi=bi, cc=cc, sb=sb: e.tensor_tensor(out=tm2[sb], in0=banks[bi][:, :],
                                                                                 in1=gcs[:, cc, :], op=ALU.mult),
                           [tb[bi], t_gcs], [t_tm2[sb]])
                    P.emit(P.dve, lambda e, cc=cc, sb=sb: e.tensor_tensor(out=t1[:, cc, :], in0=tm2[sb], in1=t1[:, cc, :],
                                                                          op=ALU.add), [t_tm2[sb], t_t1], [t_t1])
            for hf in range(2):
                w_, tw = w_next("wo%d" % hf)
                for t in range(4):
                    bi = nbank()
                    mm_group(banks[bi][:, :], bi, [(t1[:, kc, t * 128:(t + 1) * 128], w_[:, kc, :]) for kc in range(8)],
                             [tw, t_t1])
                    P.emit(P.dve, lambda e, bi=bi, t=t, hf=hf: e.tensor_tensor(
                        out=xin[:, t, hf * 512:(hf + 1) * 512], in0=banks[bi][:, :], in1=xin[:, t, hf * 512:(hf + 1) * 512],
                        op=ALU.add), [tb[bi], t_xin], [t_xin])
            for t in range(4):
                nb_ap, nb_t = nbuf[t]
                norm_tile(xin[:, t, :], 128, t_xin, ssq2[:, t:t + 1], rinv2[:, t:t + 1], t_ssq2[t], t_rinv2[t], nb_ap, nb_t)
            for t in range(4):
                nb_ap, nb_t = nbuf[t]
                transpose_tile(nb_ap, 128, nb_t, hnT[:, :, t * 128:(t + 1) * 128], t_hnT, use_act=(t % 2 == 0))
            for u in range(11):
                w_, tw = w_next("gu%d" % u)
                for ci in range(2):
                    fc = 2 * u + ci
                    sb = fc % 2
                    bgt = nbank()
                    mm_group(banks[bgt][:, :], bgt, [(w_[:, kc, ci * 128:(ci + 1) * 128], hnT[:, kc, :]) for kc in range(8)],
                             [tw, t_hnT])
                    but = nbank()
                    mm_group(banks[but][:, :], but,
                             [(w_[:, kc, 256 + ci * 128:256 + (ci + 1) * 128], hnT[:, kc, :]) for kc in range(8)],
                             [tw, t_hnT])
                    P.emit(P.act, lambda e, bgt=bgt, sb=sb: e.activation(out=sg_t[sb], in_=banks[bgt][:, :], func=AF.Silu),
                           [tb[bgt]], [t_sg[sb]])
                    P.emit(P.dve, lambda e, but=but, sb=sb, fc=fc: e.tensor_tensor(out=act_t[:, fc, :], in0=banks[but][:, :],
                                                                                   in1=sg_t[sb], op=ALU.mult),
                           [tb[but], t_sg[sb]], [t_actt, t_a, t_y])
            for hf in range(2):
                for gi, (f0, nf) in enumerate(((0, 8), (8, 8), (16, 6))):
                    w_, tw = w_next("dn%d_%d" % (hf, gi))
                    for t in range(4):
                        bi = 4 + t
                        for fl in range(nf):
                            fc = f0 + fl
                            P.emit(P.pe, lambda e, bi=bi, t=t, fc=fc, fl=fl, w_=w_: e.matmul(
                                out=banks[bi][:, :], lhsT=act_t[:, fc, t * 128:(t + 1) * 128], rhs=w_[:, fl, :],
                                start=(fc == 0), stop=(fc == FC - 1)), [tw, t_actt], [tb[bi]], ms=(fl == nf - 1))
                for t in range(4):
                    bi = 4 + t
                    P.emit(P.dve, lambda e, bi=bi, t=t, hf=hf: e.tensor_tensor(
                        out=outbuf[:, t, hf * 512:(hf + 1) * 512], in0=banks[bi][:, :],
                        in1=xin[:, t, hf * 512:(hf + 1) * 512], op=ALU.add), [tb[bi], t_xin], [t_gcs, t_gas])
            P.dma(P.pool, out_d[g * 512:(g + 1) * 512, :].rearrange("(t p) d -> p t d", p=128), outbuf, [t_gcs, t_gas],
                  [t_outd], dq_out)
            t_a.r += t_actt.r
            t_y.r += t_actt.r
            if t_actt.w is not None:
                t_a.r.append(t_actt.w)
                t_y.r.append(t_actt.w)
        P.wait_all(P.pool, [t_outd])
        P.wait_all(P.sp, [t_outd])
        P.run()
    return nc


def host_tables(j):
    slopes = 2.0 ** (-8.0 * np.arange(1, NH + 1) / NH)
    gate_bias = np.zeros((16, 64), np.float32)
    a2 = np.zeros((16, 8, 64), np.float32)
    b2 = np.full((16, 64), NEG, np.float32)
    for i in range(16):
        cur = 4 * i + j
        gate_bias[i, cur:] = -1e30
        b2[i, cur] = 0.0
        for n in range(cur):
            a2[i, :, n] = -NEG - slopes * 256.0 * (cur - n)
    cmsel = np.zeros((128, 4, 2, 256), np.float32)
    p = np.arange(128)[:, None]
    rq = np.arange(256)[None, :]
    for kh in range(2):
        rk = 2 * p + kh
        cmsel[:, j, kh, :] = np.where(rq >= rk, 0.0, NEG)
    lo = np.zeros((1, 8, 512), np.float32)
    for h in range(8):
        lo[0, h, :] = -slopes[h] * (np.arange(512) % 256)
    kbias = np.zeros((128, 16), np.float32)
    for h in range(8):
        for kh in range(2):
            kbias[:, 2 * h + kh] = slopes[h] * (2 * np.arange(128) + kh)
    return {"gate_bias": gate_bias, "a2": a2, "b2": b2, "cmsel": cmsel.reshape(128, -1),
            "lo": lo.reshape(1, -1), "kbias": kbias, "ident": np.eye(128, dtype=np.float32)}


def make_in_maps(inputs):
    x = np.asarray(inputs["x"], np.float32)
    shared = {
        "w_in": np.ascontiguousarray(inputs["w_in"][0]), "w_conv_out": np.ascontiguousarray(inputs["w_conv_out"][0]),
        "w_attn_out": np.ascontiguousarray(inputs["w_attn_out"][0]), "w_out": np.ascontiguousarray(inputs["w_out"][0]),
        "w_ffn_gate": np.ascontiguousarray(inputs["w_ffn_gate"][0]), "w_ffn_up": np.ascontiguousarray(inputs["w_ffn_up"][0]),
        "w_ffn_down": np.ascontiguousarray(inputs["w_ffn_down"][0]),
        "norm1_g": np.asarray(inputs["norm1_g"], np.float32).reshape(1, D),
        "norm2_g": np.asarray(inputs["norm2_g"], np.float32).reshape(1, D),
        "dw_w": np.ascontiguousarray(inputs["dw_w"][0]), "dw_b": np.asarray(inputs["dw_b"], np.float32).reshape(1, D),
        "conv_ln_g": np.asarray(inputs["conv_ln_g"], np.float32).reshape(1, D),
        "conv_ln_b": np.asarray(inputs["conv_ln_b"], np.float32).reshape(1, D),
        "q_norm_g": np.asarray(inputs["q_norm_g"], np.float32).reshape(1, DH),
        "k_norm_g": np.asarray(inputs["k_norm_g"], np.float32).reshape(1, DH),
    }
    shared = {k: np.asarray(v, np.float32) for k, v in shared.items()}
    maps = []
    for c in range(8):
        b, j = c // 4, c % 4
        xb = x[b]
        xblk = xb.reshape(NBK, L, D)
        own = [4 * i + j for i in range(16)]
        x_own = np.ascontiguousarray(xblk[own].reshape(4096, D))
        halo = np.zeros((16, 32, D), np.float32)
        for i, n in enumerate(own):
            if n > 0:
                halo[i] = xb[n * L - 32:n * L]
        m = dict(shared)
        m.update(host_tables(j))
        m["x_all"] = np.ascontiguousarray(xb)
        m["x_own"] = x_own
        m["x_halo"] = halo.reshape(512, D)
        maps.append(m)
    return maps


_NC_CACHE = {}


def kernel(**inputs):
    if "nc" not in _NC_CACHE:
        _NC_CACHE["nc"] = build_nc()
    nc = _NC_CACHE["nc"]
    maps = make_in_maps(inputs)
    res = run_bass_kernel_spmd(nc, maps, core_ids=list(range(8)))
    out = np.zeros((2, S, D), np.float32)
    ov = out.reshape(2, NBK, L, D)
    for c in range(8):
        b, j = c // 4, c % 4
        o = np.asarray(res.results[c]["out_own"], np.float32).reshape(16, L, D)
        for i in range(16):
            ov[b, 4 * i + j] = o[i]
    return out
```
